# Optimizing a Trainium2 kernel written in Bass

```python
import jax, jax.numpy as jnp
from jax import lax
import numpy as np

D_MODEL = 1024
BATCH = 4
SEQ = 4096
DEPTH = 1

A_HEAD_DIM = 64
A_WIDTH = D_MODEL // 2
A_HEADS = A_WIDTH // A_HEAD_DIM
CHUNK = 128
B_HEAD_DIM = 64
B_WIDTH = D_MODEL // 2
B_HEADS = B_WIDTH // B_HEAD_DIM
DILATED_PATTERNS = ((128, 1), (512, 4), (2048, 16))
BAND_BLOCK = 128
MIX_WIDTH = A_WIDTH + B_WIDTH
IN_COLS = 2 * A_WIDTH + 3 * B_WIDTH
N_EXPERTS = 32
TOP_K = 4
D_EXPERT = D_MODEL
SWIGLU_ALPHA = 1.702
SWIGLU_LIMIT = 7.0
MOE_BLOCK = 128
DN_ALPHA = (2 * DEPTH) ** 0.25
DN_BETA = (8 * DEPTH) ** -0.25
LN_EPS = 1e-5

kernel_name = "hybrid_sgu_dilated_attn_moe_deepnorm"


def layer_norm(t, g, b):
    tf = t.astype(jnp.float32)
    mu = tf.mean(-1, keepdims=True)
    var = jnp.square(tf - mu).mean(-1, keepdims=True)
    return ((tf - mu) * lax.rsqrt(var + LN_EPS) * g + b).astype(t.dtype)


def rms_norm(t, g):
    tf = t.astype(jnp.float32)
    return (tf * lax.rsqrt(jnp.square(tf).mean(-1, keepdims=True) + LN_EPS) * g).astype(t.dtype)


def chunked_sgu(u, v, w_s, b_s, ln_g, ln_b):
    B, S, _ = u.shape
    nc = S // CHUNK
    vg = v.reshape(B, S, A_HEADS, A_HEAD_DIM).astype(jnp.float32)
    mu = vg.mean(-1, keepdims=True)
    var = jnp.square(vg - mu).mean(-1, keepdims=True)
    vn = ((vg - mu) * lax.rsqrt(var + LN_EPS) * ln_g.reshape(A_HEADS, A_HEAD_DIM)
          + ln_b.reshape(A_HEADS, A_HEAD_DIM)).astype(v.dtype)
    vc = vn.reshape(B, nc, CHUNK, A_HEADS, A_HEAD_DIM)
    w_causal = jnp.tril(w_s)
    gate = jnp.einsum('gts,bnsgc->bntgc', w_causal, vc) + b_s.T[None, None, :, :, None]
    return u * gate.reshape(B, S, A_WIDTH)


def dilated_band_attention(q, k, v, window, dilation):
    B, S, H, Dh = q.shape
    reach = window // dilation
    span = dilation * BAND_BLOCK
    L = -(-S // span) * span
    nb = L // span

    def to_sub(t):
        t = jnp.pad(t, ((0, 0), (0, L - S), (0, 0), (0, 0)))
        t = t.reshape(B, L // dilation, dilation, H, Dh).transpose(0, 2, 3, 1, 4)
        return t.reshape(B, dilation, H, nb, BAND_BLOCK, Dh)

    def with_prev(t):
        prev = jnp.pad(t[:, :, :, :-1], ((0, 0), (0, 0), (0, 0), (1, 0), (0, 0), (0, 0)))
        return jnp.concatenate([prev, t], axis=4)

    qs = to_sub(q)
    kk = with_prev(to_sub(k))
    vv = with_prev(to_sub(v)).astype(jnp.float32)
    s = jnp.einsum('bdhnqc,bdhnkc->bdhnqk', qs, kk, preferred_element_type=jnp.float32)
    qi = jnp.arange(BAND_BLOCK)[:, None] + BAND_BLOCK
    kj = jnp.arange(2 * BAND_BLOCK)[None, :]
    dist = qi - kj
    band = (dist >= 0) & (dist <= reach)
    first = (jnp.arange(nb) == 0)[:, None, None]
    mask = band[None] & ~(first & (kj < BAND_BLOCK)[None])
    s = jnp.where(mask, s, -jnp.inf)
    m = s.max(-1)
    p = jnp.exp(s - m[..., None])
    l = p.sum(-1)
    o = jnp.einsum('bdhnqk,bdhnkc->bdhnqc', p, vv) / l[..., None]

    def from_sub(t):
        tail = t.shape[5:]
        t = t.reshape((B, dilation, H, L // dilation) + tail)
        t = jnp.moveaxis(t, 3, 1)
        return t.reshape((B, L, H) + tail)[:, :S]

    return from_sub(o), from_sub(m), from_sub(l)


def dilated_mixture_attention(q, k, v):
    B, S, _ = q.shape
    q = (q * (B_HEAD_DIM ** -0.5)).reshape(B, S, B_HEADS, B_HEAD_DIM)
    k = k.reshape(B, S, B_HEADS, B_HEAD_DIM)
    v = v.reshape(B, S, B_HEADS, B_HEAD_DIM)
    res = [dilated_band_attention(q, k, v, w, d) for (w, d) in DILATED_PATTERNS]
    os_ = jnp.stack([r[0] for r in res], 0)
    ms = jnp.stack([r[1] for r in res], 0)
    ls = jnp.stack([r[2] for r in res], 0)
    wts = ls * jnp.exp(ms - ms.max(0, keepdims=True))
    out = (wts[..., None] * os_).sum(0) / wts.sum(0)[..., None]
    return out.reshape(B, S, B_WIDTH).astype(q.dtype)


def moe_ffn(h, w_router, b_router, w_gate, b_gate, w_up, b_up, w_down, b_down):
    B, S, D = h.shape
    T = B * S
    A = T * TOP_K
    hf = h.reshape(T, D)
    logits = jnp.dot(hf, w_router).astype(jnp.float32) + b_router.astype(jnp.float32)
    top_vals, top_idx = lax.top_k(logits, TOP_K)
    gates = jax.nn.softmax(top_vals, axis=-1)
    flat_e = top_idx.reshape(A)
    flat_tok = jnp.repeat(jnp.arange(T, dtype=jnp.int32), TOP_K)
    flat_g = gates.reshape(A)
    order = jnp.argsort(flat_e)
    se, stok, sg = flat_e[order], flat_tok[order], flat_g[order]
    counts = jnp.bincount(flat_e, length=N_EXPERTS)
    start = jnp.cumsum(counts) - counts
    pcounts = (counts + MOE_BLOCK - 1) // MOE_BLOCK * MOE_BLOCK
    pend = jnp.cumsum(pcounts)
    pstart = pend - pcounts
    dest = pstart[se] + (jnp.arange(A) - start[se])
    nblk = -(-A // MOE_BLOCK) + N_EXPERTS
    P = nblk * MOE_BLOCK
    row_tok = jnp.full((P,), T, jnp.int32).at[dest].set(stok)
    row_gate = jnp.zeros((P,), jnp.float32).at[dest].set(sg)
    block_e = jnp.clip(jnp.searchsorted(pend, jnp.arange(nblk) * MOE_BLOCK, side='right'), 0, N_EXPERTS - 1)
    h_pad = jnp.concatenate([hf, jnp.zeros((1, D), hf.dtype)], 0)

    def expert_block(args):
        tok, e = args
        xb = h_pad[tok]
        g = jnp.minimum(xb @ w_gate[e] + b_gate[e], SWIGLU_LIMIT)
        u = jnp.clip(xb @ w_up[e] + b_up[e], -SWIGLU_LIMIT, SWIGLU_LIMIT)
        act = (u + 1.0) * (g * jax.nn.sigmoid(SWIGLU_ALPHA * g))
        return act @ w_down[e] + b_down[e]

    out = lax.map(expert_block, (row_tok.reshape(nblk, MOE_BLOCK), block_e)).reshape(P, D)
    y = jnp.zeros((T + 1, D), h.dtype).at[row_tok].add((out * row_gate[:, None]).astype(h.dtype))
    return y[:T].reshape(B, S, D)


def setup_inputs(seed: int = 0) -> dict:
    key = jax.random.key(seed)
    ks = jax.random.split(key, 24)
    n = lambda i, shape: jax.random.normal(ks[i], shape, jnp.float32)
    L, D, E, F = DEPTH, D_MODEL, N_EXPERTS, D_EXPERT
    return {
        "x": n(0, (BATCH, SEQ, D)),
        "w_in": n(1, (L, D, IN_COLS)) * D ** -0.5,
        "sgu_w": n(2, (L, A_HEADS, CHUNK, CHUNK)) * CHUNK ** -0.5,
        "sgu_b": 1.0 + 0.02 * n(3, (L, A_HEADS, CHUNK)),
        "sgu_ln_g": 1.0 + 0.02 * n(4, (L, A_WIDTH)),
        "sgu_ln_b": 0.02 * n(5, (L, A_WIDTH)),
        "mix_norm_g": 1.0 + 0.02 * n(6, (L, MIX_WIDTH)),
        "w_out": n(7, (L, MIX_WIDTH, D)) * MIX_WIDTH ** -0.5 * DN_BETA,
        "ln1_g": 1.0 + 0.02 * n(8, (L, D)),
        "ln1_b": 0.02 * n(9, (L, D)),
        "w_router": n(10, (L, D, E)) * D ** -0.5,
        "b_router": 0.01 * n(11, (L, E)),
        "w_gate": n(12, (L, E, D, F)) * D ** -0.5,
        "b_gate": 0.01 * n(13, (L, E, F)),
        "w_up": n(14, (L, E, D, F)) * D ** -0.5,
        "b_up": 0.01 * n(15, (L, E, F)),
        "w_down": n(16, (L, E, F, D)) * F ** -0.5 * DN_BETA,
        "b_down": 0.01 * n(17, (L, E, D)),
        "ln2_g": 1.0 + 0.02 * n(18, (L, D)),
        "ln2_b": 0.02 * n(19, (L, D)),
    }


def reference(x, w_in, sgu_w, sgu_b, sgu_ln_g, sgu_ln_b, mix_norm_g, w_out, ln1_g, ln1_b,
              w_router, b_router, w_gate, b_gate, w_up, b_up, w_down, b_down, ln2_g, ln2_b):
    for i in range(DEPTH):
        p = x @ w_in[i]
        za = jax.nn.gelu(p[..., :2 * A_WIDTH])
        q = p[..., 2 * A_WIDTH:2 * A_WIDTH + B_WIDTH]
        k = p[..., 2 * A_WIDTH + B_WIDTH:2 * A_WIDTH + 2 * B_WIDTH]
        v = p[..., 2 * A_WIDTH + 2 * B_WIDTH:]
        a = chunked_sgu(za[..., :A_WIDTH], za[..., A_WIDTH:], sgu_w[i], sgu_b[i], sgu_ln_g[i], sgu_ln_b[i])
        b = dilated_mixture_attention(q, k, v)
        mixed = jnp.concatenate([rms_norm(a, mix_norm_g[i, :A_WIDTH]),
                                 rms_norm(b, mix_norm_g[i, A_WIDTH:])], axis=-1) @ w_out[i]
        h = layer_norm(DN_ALPHA * x + mixed, ln1_g[i], ln1_b[i])
        y = moe_ffn(h, w_router[i], b_router[i], w_gate[i], b_gate[i], w_up[i], b_up[i], w_down[i], b_down[i])
        x = layer_norm(DN_ALPHA * h + y, ln2_g[i], ln2_b[i])
    return x
```

```python
import numpy as np
from contextlib import ExitStack
import concourse.bass as bass
import concourse.mybir as mybir
from concourse.bass_utils import run_bass_kernel_spmd

F32 = mybir.dt.float32
BF16 = mybir.dt.bfloat16
I32 = mybir.dt.int32
U32 = mybir.dt.uint32
AF = mybir.ActivationFunctionType
ALU = mybir.AluOpType
AX = mybir.AxisListType

D = 1024
NTOK = 2048
NEXT = 4096
NT = NTOK // 128
NE = 32
TOPK = 4
CAP = 512
NBLK = CAP // 128
ALPHA = 2.0 ** 0.25
EPS = 1e-5
PATTERNS = (1, 4, 16)

C_IDENT = 0
C_TRI = 128
C_MPREV = 256
C_MPREVC = 384
C_LSTRICT = 512
C_ONES = 640
C_IOTA = 768
C_TOKID = 800
C_SEL = 816
NCONST = 880


class Prog:
    COMPUTE = ("pe", "act", "dve", "pool")

    def __init__(self, nc, n_dma_sems=20, same_engine_sync=True):
        self.nc = nc
        self.es = ExitStack()
        self.eng = {"pe": nc.tensor, "act": nc.scalar, "dve": nc.vector, "pool": nc.gpsimd, "sp": nc.sync}
        self.sem = {}
        self.cnt = {}
        for e in self.COMPUTE:
            self.sem[e] = self.es.enter_context(nc.semaphore("sem_" + e))
            self.cnt[e] = 0
        self.known = {e: {} for e in self.eng}
        self.last_w = {}
        self.readers = {}
        self.same_engine_sync = same_engine_sync
        self.dma_pool = {}
        self.dma_idx = {}
        for q in ("sp", "act", "pool"):
            self.dma_pool[q] = [[self.es.enter_context(nc.semaphore(f"dsem_{q}_{i}")), 0] for i in range(n_dma_sems)]
            self.dma_idx[q] = 0
        self.n_inst = {e: 0 for e in self.eng}

    def sb(self, name, shape, dt):
        return self.es.enter_context(self.nc.sbuf_tensor("s_" + name, shape, dt))

    def ps(self, name, shape, dt):
        return self.es.enter_context(self.nc.psum_tensor(name, shape, dt))

    def _wait(self, e, tok):
        sem, val, src = tok
        if src == e and (e == "pe" or not self.same_engine_sync):
            return
        k = self.known[e]
        sid = id(sem)
        if k.get(sid, 0) >= val:
            return
        self.eng[e].wait_ge(sem, val)
        k[sid] = val

    def op(self, e, fn, reads=(), writes=(), dma=False):
        deps = {}

        def add(t):
            key = id(t[0])
            if key not in deps or deps[key][1] < t[1]:
                deps[key] = t
        for kk in reads:
            t = self.last_w.get(kk)
            if t is not None:
                add(t)
        for kk in writes:
            t = self.last_w.get(kk)
            if t is not None:
                add(t)
            for t in self.readers.get(kk, ()):
                add(t)
        for t in deps.values():
            self._wait(e, t)
        if dma:
            pool = self.dma_pool[e]
            i = self.dma_idx[e]
            self.dma_idx[e] = (i + 1) % len(pool)
            slot = pool[i]
            if slot[1] > 0:
                self._wait(e, (slot[0], slot[1], "dma"))
            inst = fn()
            slot[1] += 16
            inst.then_inc(slot[0], 16)
            tok = (slot[0], slot[1], "dma")
        else:
            inst = fn()
            self.cnt[e] += 1
            inst.then_inc(self.sem[e], 1)
            tok = (self.sem[e], self.cnt[e], e)
        self.n_inst[e] += 1
        for kk in writes:
            self.last_w[kk] = tok
            self.readers[kk] = []
        for kk in reads:
            if kk in writes:
                continue
            self.readers.setdefault(kk, []).append(tok)
        return tok

    def barrier(self):
        for e in self.eng:
            self.finish(e)

    def finish(self, e="sp"):
        for pool in self.dma_pool.values():
            for sem, val in pool:
                if val > 0:
                    self._wait(e, (sem, val, "dma"))
        for c in self.COMPUTE:
            if self.cnt[c] > 0:
                self._wait(e, (self.sem[c], self.cnt[c], c))


class Rot:
    def __init__(self, p, name, n, shape, dt):
        self.tiles = [p.sb(f"{name}{i}", shape, dt) for i in range(n)]
        self.name = name
        self.n = n
        self.i = 0

    def next(self):
        j = self.i % self.n
        self.i += 1
        return self.tiles[j], (self.name, j)


def v_tiles():
    out = {}
    out[1] = [(n, 0) for n in range(15, 32)]
    out[4] = [(n, r) for n in range(3, 8) for r in range(4)]
    out[16] = [(n, r) for n in range(0, 2) for r in range(16)]
    return out


def build(stage=99, dbg=()):
    nc = bass.Bass("TRN2", target_bir_lowering=False)
    dram = {}

    def din(name, shape, dt=F32):
        dram[name] = nc.dram_tensor(name, list(shape), dt, kind="ExternalInput").ap()
        return dram[name]

    def dout(name, shape, dt=F32):
        dram[name] = nc.dram_tensor(name, list(shape), dt, kind="ExternalOutput").ap()
        return dram[name]

    xT_d = din("xT", [D, NEXT])
    x_d = din("x", [NTOK, D])
    win_d = din("w_in", [D, 2560])
    consts_d = din("consts", [128, NCONST])
    sguw_d = din("sgu_wT", [128, 8, 128])
    sgub_d = din("sgu_bT", [128, 8])
    vec_d = din("vecs", [128, 6, D])
    din("mix_gb64", [64, 8])
    gb_d = din("mix_gb", [128, 4])
    wout_d = din("w_out", [D, D])
    wr_d = din("w_router", [D, NE])
    br_d = din("b_router_bc", [128, NE])
    if stage >= 5:
        wg_d = din("w_gate", [NE, D, D])
        wu_d = din("w_up", [NE, D, D])
        wd_d = din("w_down", [NE, D, D])
    bg_d = din("b_gateT", [128, NE, 8])
    bu_d = din("b_upT", [128, NE, 8])
    bd_d = din("b_down", [NE, D])
    out_d = dout("out", [NTOK, D])
    for name, shape in dbg:
        dout(name, shape)

    p = Prog(nc)
    with p.es:
        E = p.eng
        psb = [p.ps(f"psb{i}", [128, 512], F32) for i in range(8)]

        def PS(b):
            return ("ps", b)

        def sbuf_raw(es, name, shape, dt):
            return es.enter_context(nc.sbuf_tensor("s_" + name, shape, dt))

        def sbuf(es, name, shape, dt):
            return es.enter_context(nc.sbuf_tensor("s_" + name, shape, dt))

        class SRot:
            def __init__(self, es, name, n, shape, dt):
                self.tiles = [sbuf(es, f"{name}{i}", shape, dt) for i in range(n)]
                self.name, self.n, self.i = name, n, 0

            def next(self):
                j = self.i % self.n
                self.i += 1
                return self.tiles[j], (self.name, j)

        def dma(q, out, in_, reads=(), writes=()):
            return p.op(q, lambda: E[q].dma_start(out=out, in_=in_), reads=reads, writes=writes, dma=True)

        consts = p.sb("consts", [128, NCONST], F32)
        consts_bf = p.sb("consts_bf", [128, NCONST], BF16)
        dma("sp", consts[:], consts_d, writes=["consts"])
        dma("pool", consts_bf[:], consts_d, writes=["consts_bf"])
        sgub = p.sb("sgub", [128, 8], F32)
        dma("sp", sgub[:], sgub_d, writes=["sgub"])
        gb = p.sb("gb", [128, 4], F32)
        dma("sp", gb[:], gb_d, writes=["gb"])
        wcT = p.sb("wcT", [128, 8, 128], BF16)
        eps_t = p.sb("eps_t", [128, 1], F32)
        p.op("dve", lambda: nc.vector.memset(eps_t[:], EPS), writes=["eps"])
        ident_bf = consts_bf[:, C_IDENT:C_IDENT + 128]
        ident_f = consts[:, C_IDENT:C_IDENT + 128]

        ssq_a = p.sb("ssq_a", [128, NT], F32)
        ssq_b = p.sb("ssq_b", [128, NT, 8], F32)
        p.op("dve", lambda: nc.vector.memset(ssq_a[:], 0.0), writes=["ssq_a"])
        SLOTS = NE * CAP
        hbf_d = nc.dram_tensor("hbf_scr", [NTOK + 1, D], BF16).ap()
        h32_d = nc.dram_tensor("h32_scr", [NTOK, D], F32).ap()
        stok_d = nc.dram_tensor("stok_scr", [SLOTS + 128, 1], I32).ap()
        eo_d = nc.dram_tensor("eo_scr", [SLOTS + 128, D], F32).ap()
        gates = p.sb("gates", [128, NT, 4], F32)
        pos_i = p.sb("pos_i", [128, NT * 4], I32)
        Mall = p.sb("Mall", [128, NT, NE], BF16)
        tokid_i = p.sb("tokid_i", [128, NT], I32)
        p.op("dve", lambda: nc.vector.tensor_copy(out=tokid_i[:], in_=consts[:, C_TOKID:C_TOKID + NT]), reads=["consts"], writes=["tokid_i"])
        fill = p.sb("fill", [128, (SLOTS + 128) // 128], I32)
        p.op("dve", lambda: nc.vector.memset(fill[:], NTOK), writes=["fill"])
        dma("sp", stok_d.rearrange("(p f) o -> p (f o)", p=128), fill[:], reads=["fill"], writes=["stok_init"])
        esAB = ExitStack()
        aT = sbuf_raw(esAB, "aT", [128, 4, NTOK], BF16)
        bT = sbuf_raw(esAB, "bT", [128, 4, NTOK], BF16)
        vt = v_tiles()
        vidx = {}
        nv = 0
        for d_ in PATTERNS:
            for nr in vt[d_]:
                vidx[(d_,) + nr] = nv
                nv += 1

        with ExitStack() as esQ:
            qT = sbuf(esQ, "qT", [128, 4, NTOK], BF16)
            kT = sbuf(esQ, "kT", [128, 4, NEXT], BF16)
            vT = sbuf(esQ, "vT", [128, 4, NEXT], BF16)
            with ExitStack() as esX:
                xTo = sbuf(esX, "xT_own", [128, 8, NTOK], BF16)
                for k in range(8):
                    dma("pool", xTo[:, k, :], xT_d[k * 128:(k + 1) * 128, NTOK:NEXT], writes=[("xTo", k)])
                xok = [("xTo", k) for k in range(8)]
                with ExitStack() as esB:
                    xTc = sbuf(esB, "xT_ctx", [128, 8, NTOK], BF16)
                    wkv = sbuf(esB, "win_kv", [128, 8, 1024], BF16)
                    sguw = sbuf(esB, "sguw", [128, 8, 128], F32)
                    dma("sp", sguw[:], sguw_d, writes=["sguw"])
                    tri_bc = consts[:, C_TRI:C_TRI + 128].unsqueeze(1).to_broadcast([128, 8, 128])
                    p.op("dve", lambda: nc.vector.tensor_tensor(out=wcT[:], in0=sguw[:], in1=tri_bc, op=ALU.mult),
                         reads=["sguw", "consts"], writes=["wcT"])
                    for k in range(8):
                        dma("pool", wkv[:, k, :], win_d[k * 128:(k + 1) * 128, 1536:2560], writes=[("wkv", k)])
                    for k in range(8):
                        dma("pool", xTc[:, k, :], xT_d[k * 128:(k + 1) * 128, 0:NTOK], writes=[("xTc", k)])
                    xck = [("xTc", k) for k in range(8)]
                    ev = 0
                    for (dst, dname, col0) in ((kT, "kT", 0), (vT, "vT", 512)) if stage >= 2 else ():
                        for hp in range(4):
                            for tc in range(8):
                                b = 6 + (ev % 2)
                                src, srck = (xTo, xok) if tc >= 4 else (xTc, xck)
                                tcl = tc % 4
                                for k in range(8):
                                    p.op("pe", lambda k=k, b=b, src=src, tcl=tcl: nc.tensor.matmul(
                                        psb[b][:], lhsT=wkv[:, k, col0 + hp * 128:col0 + (hp + 1) * 128],
                                        rhs=src[:, k, tcl * 512:(tcl + 1) * 512], start=(k == 0), stop=(k == 7)),
                                        reads=[srck[k], ("wkv", k)], writes=[PS(b)])
                                if ev % 2 == 0:
                                    p.op("act", lambda b=b: nc.scalar.copy(out=dst[:, hp, tc * 512:(tc + 1) * 512], in_=psb[b][:]), reads=[PS(b)], writes=[(dname, hp)])
                                else:
                                    p.op("dve", lambda b=b: nc.vector.tensor_copy(out=dst[:, hp, tc * 512:(tc + 1) * 512], in_=psb[b][:]), reads=[PS(b)], writes=[(dname, hp)])
                                ev += 1
                    p.barrier()
                with ExitStack() as esA:
                    win = sbuf(esA, "win_uvq", [128, 8, 1536], BF16)
                    for k in range(8):
                        dma("pool", win[:, k, :], win_d[k * 128:(k + 1) * 128, 0:1536], writes=[("win", k)])
                    wk = [("win", k) for k in range(8)]
                    vecA = sbuf(esA, "vecA", [128, 3, 512], F32)
                    dma("sp", vecA[:, 0:2, :], vec_d[:, 0, :].rearrange("p (a c) -> p a c", a=2), writes=["vecA"])
                    dma("sp", vecA[:, 2, :], vec_d[:, 1, 0:512], writes=["vecA"])
                    U = SRot(esA, "U", 2, [128, 512], F32)
                    V = SRot(esA, "V", 2, [128, 512], F32)
                    W1 = SRot(esA, "W1", 2, [128, 512], F32)
                    W2 = SRot(esA, "W2", 2, [128, 512], F32)
                    VN = SRot(esA, "VN", 2, [128, 512], BF16)
                    AB = SRot(esA, "AB", 2, [128, 512], BF16)
                    st = SRot(esA, "st", 2, [128, 8, 8], F32)

                    def gelu(src_ap, src_key, dst, dst_key):
                        t1, k1 = W1.next()
                        t2, k2 = W2.next()
                        p.op("act", lambda: nc.scalar.activation(out=t1[:], in_=src_ap, func=AF.Square), reads=[src_key], writes=[k1])
                        p.op("dve", lambda: nc.vector.tensor_scalar(out=t1[:], in0=t1[:], scalar1=0.044715, scalar2=1.0, op0=ALU.mult, op1=ALU.add),
                             reads=[k1], writes=[k1])
                        p.op("dve", lambda: nc.vector.tensor_tensor(out=t1[:], in0=t1[:], in1=src_ap, op=ALU.mult), reads=[k1, src_key], writes=[k1])
                        p.op("act", lambda: nc.scalar.activation(out=t2[:], in_=t1[:], func=AF.Sigmoid, scale=1.5957691216057308), reads=[k1], writes=[k2])
                        p.op("dve", lambda: nc.vector.tensor_tensor(out=dst[:], in0=t2[:], in1=src_ap, op=ALU.mult), reads=[k2, src_key], writes=[dst_key])

                    for i in range(NT if stage >= 1 else 0):
                        t0 = i * 128
                        b0 = 2 * (i % 2)
                        for half in range(2):
                            for k in range(8):
                                p.op("pe", lambda half=half, k=k: nc.tensor.matmul(
                                    psb[b0 + half][:], lhsT=xTo[:, k, t0:t0 + 128], rhs=win[:, k, half * 512:(half + 1) * 512],
                                    start=(k == 0), stop=(k == 7)), reads=[xok[k], wk[k]], writes=[PS(b0 + half)])
                        u, uk = U.next()
                        v, vk = V.next()
                        gelu(psb[b0][:], PS(b0), u, uk)
                        gelu(psb[b0 + 1][:], PS(b0 + 1), v, vk)
                        s, sk = st.next()
                        w1, w1k = W1.next()
                        v3 = v[:].rearrange("p (g c) -> p g c", g=8)
                        w13 = w1[:].rearrange("p (g c) -> p g c", g=8)
                        p.op("dve", lambda: nc.vector.tensor_reduce(out=s[:, 0, :], in_=v3, axis=AX.X, op=ALU.add), reads=[vk], writes=[sk])
                        p.op("act", lambda: nc.scalar.activation(out=w1[:], in_=v[:], func=AF.Square), reads=[vk], writes=[w1k])
                        p.op("dve", lambda: nc.vector.tensor_reduce(out=s[:, 1, :], in_=w13, axis=AX.X, op=ALU.add), reads=[w1k], writes=[sk])
                        p.op("dve", lambda: nc.vector.tensor_single_scalar(out=s[:, 2, :], in_=s[:, 0, :], scalar=1.0 / 64, op=ALU.mult), reads=[sk], writes=[sk])
                        p.op("dve", lambda: nc.vector.tensor_tensor(out=s[:, 3, :], in0=s[:, 2, :], in1=s[:, 2, :], op=ALU.mult), reads=[sk], writes=[sk])
                        p.op("dve", lambda: nc.vector.scalar_tensor_tensor(out=s[:, 4, :], in0=s[:, 1, :], scalar=1.0 / 64, in1=s[:, 3, :], op0=ALU.mult, op1=ALU.subtract), reads=[sk], writes=[sk])
                        p.op("act", lambda: nc.scalar.activation(out=s[:, 6, :], in_=s[:, 4, :], func=AF.Sqrt, bias=eps_t[:, 0:1], scale=1.0), reads=[sk, "eps"], writes=[sk])
                        p.op("dve", lambda: nc.vector.reciprocal(out=s[:, 5, :], in_=s[:, 6, :]), reads=[sk], writes=[sk])
                        mean_bc = s[:, 2, :].unsqueeze(2).to_broadcast([128, 8, 64])
                        rstd_bc = s[:, 5, :].unsqueeze(2).to_broadcast([128, 8, 64])
                        p.op("dve", lambda: nc.vector.tensor_tensor(out=w13, in0=v3, in1=mean_bc, op=ALU.subtract), reads=[vk, sk], writes=[w1k])
                        p.op("dve", lambda: nc.vector.tensor_tensor(out=w13, in0=w13, in1=rstd_bc, op=ALU.mult), reads=[w1k, sk], writes=[w1k])
                        vn, vnk = VN.next()
                        p.op("pool", lambda: nc.gpsimd.tensor_tensor(out=w1[:], in0=w1[:], in1=vecA[:, 0, :], op=ALU.mult), reads=[w1k, "vecA"], writes=[w1k])
                        p.op("pool", lambda: nc.gpsimd.tensor_tensor(out=vn[:], in0=w1[:], in1=vecA[:, 1, :], op=ALU.add), reads=[w1k, "vecA"], writes=[vnk])
                        for g in range(8):
                            p.op("pe", lambda g=g: nc.tensor.matmul(psb[4][:, g * 64:(g + 1) * 64], lhsT=wcT[:, g, :], rhs=vn[:, g * 64:(g + 1) * 64],
                                                                     start=True, stop=True), reads=["wcT", vnk], writes=[PS(4)])
                        w2, w2k = W2.next()
                        w23 = w2[:].rearrange("p (g c) -> p g c", g=8)
                        bs_bc = sgub[:, :].unsqueeze(2).to_broadcast([128, 8, 64])
                        p.op("dve", lambda: nc.vector.tensor_tensor(out=w23, in0=psb[4][:].rearrange("p (g c) -> p g c", g=8), in1=bs_bc, op=ALU.add),
                             reads=[PS(4), "sgub"], writes=[w2k])
                        p.op("dve", lambda: nc.vector.tensor_tensor(out=w2[:], in0=w2[:], in1=u[:], op=ALU.mult), reads=[w2k, uk], writes=[w2k])
                        w1b, w1bk = W1.next()
                        p.op("act", lambda: nc.scalar.activation(out=w1b[:], in_=w2[:], func=AF.Square, accum_out=ssq_a[:, i:i + 1]), reads=[w2k], writes=[w1bk, "ssq_a"])
                        ab, abk = AB.next()
                        p.op("pool", lambda: nc.gpsimd.tensor_tensor(out=ab[:], in0=w2[:], in1=vecA[:, 2, :], op=ALU.mult), reads=[w2k, "vecA"], writes=[abk])
                        ptr = psb[5][:].bitcast(BF16)
                        for c in range(4):
                            p.op("pe", lambda c=c: nc.tensor.transpose(ptr[:, c * 128:(c + 1) * 128], ab[:, c * 128:(c + 1) * 128], ident_bf),
                                 reads=[abk, "consts_bf"], writes=[PS(5)])
                        p.op("act", lambda: nc.scalar.copy(out=aT[:, :, i * 128:(i + 1) * 128], in_=ptr[:, 0:512].rearrange("p (c t) -> p c t", c=4)),
                             reads=[PS(5)], writes=["aT"])
                        if "dbg_a" in dram:
                            dma("sp", dram["dbg_a"][i * 128:(i + 1) * 128, :], w2[:], reads=[w2k])
                    ev = 0
                    for hp in range(4 if stage >= 2 else 0):
                        for tc in range(4):
                            b = 6 + (ev % 2)
                            for k in range(8):
                                p.op("pe", lambda k=k, b=b: nc.tensor.matmul(
                                    psb[b][:], lhsT=win[:, k, 1024 + hp * 128:1024 + (hp + 1) * 128],
                                    rhs=xTo[:, k, tc * 512:(tc + 1) * 512], start=(k == 0), stop=(k == 7)),
                                    reads=[xok[k], wk[k]], writes=[PS(b)])
                            if ev % 2 == 0:
                                p.op("act", lambda b=b: nc.scalar.copy(out=qT[:, hp, tc * 512:(tc + 1) * 512], in_=psb[b][:]), reads=[PS(b)], writes=[("qT", hp)])
                            else:
                                p.op("dve", lambda b=b: nc.vector.tensor_copy(out=qT[:, hp, tc * 512:(tc + 1) * 512], in_=psb[b][:]), reads=[PS(b)], writes=[("qT", hp)])
                            ev += 1
                    if "dbg_ssq" in dram:
                        dma("sp", dram["dbg_ssq"], ssq_a[:], reads=["ssq_a"])
                    p.barrier()
            with ExitStack() as esD:
                for nm, t_, rk in (("dbg_aT", aT, ["aT"]), ("dbg_qT", qT, [("qT", h_) for h_ in range(4)])):
                    if nm in dram:
                        tmpf = sbuf(esD, nm, [128, 4, NTOK], F32)
                        p.op("dve", lambda: nc.vector.tensor_copy(out=tmpf[:], in_=t_[:]), reads=rk, writes=[nm])
                        dma("sp", dram[nm], tmpf[:], reads=[nm])
                p.barrier()
            with ExitStack() as esT:
                vaugp = [sbuf(esT, f"vaugp{i}", [128, nv, 2, 66], BF16) for i in range(2)]
                for i_ in range(2):
                    p.op("pool", lambda i_=i_: nc.gpsimd.memset(vaugp[i_][:, :, :, 64:66], 1.0), writes=[("vones", i_)])
                accT = [sbuf(esT, f"accT{i}", [65, NTOK], F32) for i in range(2)]
                Eb = SRot(esT, "Eb", 3, [128, 512], BF16)
                Em = SRot(esT, "Em", 3, [128, 512], BF16)
                RD = SRot(esT, "RD", 2, [64, 512], F32)
                BQ = SRot(esT, "BQ", 2, [64, 512], F32)
                BS = SRot(esT, "BS", 2, [64, 512], F32)
                masks = sbuf(esT, "masks", [128, 3, 512], BF16)
                mprev = consts_bf[:, C_MPREV:C_MPREV + 128]
                mprevc = consts_bf[:, C_MPREVC:C_MPREVC + 128]
                mcur = consts_bf[:, C_TRI:C_TRI + 128]
                for mi, pat in enumerate(((mprev, mcur, mprev, mcur), (mprevc, mcur, mprev, mcur), (mprevc, mcur, mprevc, mcur))):
                    for j, src in enumerate(pat):
                        p.op("dve", lambda mi=mi, j=j, src=src: nc.vector.tensor_copy(out=masks[:, mi, j * 128:(j + 1) * 128], in_=src),
                             reads=["consts_bf"], writes=["masks"])
                sel_f = consts[0:65, C_SEL:C_SEL + 64]
                ones_f = consts[0:64, C_ONES:C_ONES + 1]
                gb64 = sbuf(esT, "gb64", [64, 8], F32)
                dma("sp", gb64[:], dram["mix_gb64"], writes=["gb64"])
                su = 0
                tg = 0
                for hp in range(4 if stage >= 3 else 0):
                    vb = vaugp[hp % 2]
                    vbk = ("vaugp", hp % 2)
                    allv = [(d_, n, r) for d_ in PATTERNS for (n, r) in vt[d_]]
                    for g0 in range(0, nv, 8):
                        grp = allv[g0:g0 + 8]
                        b = 6 + (tg % 2)
                        ptr = psb[b][:].bitcast(BF16)
                        for j, (d_, n, r) in enumerate(grp):
                            span = 128 * d_
                            s0 = span * n + r
                            p.op("pe", lambda j=j, s0=s0, span=span, d_=d_, ptr=ptr: nc.tensor.transpose(
                                ptr[:, j * 128:(j + 1) * 128], vT[:, hp, s0:s0 + span - d_ + 1:d_], ident_bf),
                                reads=[("vT", hp), "consts_bf"], writes=[PS(b)])
                        ng = len(grp)
                        src = ptr[:, 0:ng * 128].rearrange("p (t h c) -> p t h c", t=ng, h=2)
                        if tg % 2 == 0:
                            p.op("act", lambda src=src, g0=g0, ng=ng: nc.scalar.copy(out=vb[:, g0:g0 + ng, :, 0:64], in_=src), reads=[PS(b)], writes=[vbk])
                        else:
                            p.op("dve", lambda src=src, g0=g0, ng=ng: nc.vector.tensor_copy(out=vb[:, g0:g0 + ng, :, 0:64], in_=src), reads=[PS(b)], writes=[vbk])
                        tg += 1
                    for hh in range(2):
                        h = 2 * hp + hh
                        po = 64 * hh
                        acc = accT[h % 2]
                        acck = ("accT", h % 2)
                        for d_ in PATTERNS:
                            span = 128 * d_
                            if d_ == 1:
                                blocks = [(n, 0) for n in range(16, 32)]
                            elif d_ == 4:
                                blocks = [(n, r) for n in range(4, 8) for r in range(4)]
                            else:
                                blocks = [(1, r) for r in range(16)]
                            for ui in range(0, 16, 2):
                                pair = blocks[ui:ui + 2]
                                sb_ = su % 2
                                ob_ = 2 + su % 2
                                su += 1
                                ctx = []
                                for j, (n, r) in enumerate(pair):
                                    q0 = span * n + r - NTOK
                                    qs = qT[po:po + 64, hp, q0:q0 + span - d_ + 1:d_]
                                    kp0 = span * (n - 1) + r
                                    kc0 = span * n + r
                                    ctx.append(kp0 < NTOK)
                                    p.op("pe", lambda j=j, qs=qs, kp0=kp0: nc.tensor.matmul(
                                        psb[sb_][:, j * 256:j * 256 + 128], lhsT=kT[po:po + 64, hp, kp0:kp0 + span - d_ + 1:d_], rhs=qs, start=True, stop=True),
                                        reads=[("kT", hp), ("qT", hp)], writes=[PS(sb_)])
                                    p.op("pe", lambda j=j, qs=qs, kc0=kc0: nc.tensor.matmul(
                                        psb[sb_][:, j * 256 + 128:j * 256 + 256], lhsT=kT[po:po + 64, hp, kc0:kc0 + span - d_ + 1:d_], rhs=qs, start=True, stop=True),
                                        reads=[("kT", hp), ("qT", hp)], writes=[PS(sb_)])
                                mi = {(False, False): 0, (True, False): 1, (True, True): 2}[tuple(ctx)]
                                eb, ebk = Eb.next()
                                em, emk = Em.next()
                                p.op("act", lambda eb=eb: nc.scalar.activation(out=eb[:], in_=psb[sb_][:], func=AF.Exp, scale=0.125), reads=[PS(sb_)], writes=[ebk])
                                if su % 2 == 0:
                                    p.op("dve", lambda: nc.vector.tensor_tensor(out=em[:], in0=eb[:], in1=masks[:, mi, :], op=ALU.mult), reads=[ebk, "masks"], writes=[emk])
                                else:
                                    p.op("pool", lambda: nc.gpsimd.tensor_tensor(out=em[:], in0=eb[:], in1=masks[:, mi, :], op=ALU.mult), reads=[ebk, "masks"], writes=[emk])
                                for j, (n, r) in enumerate(pair):
                                    vp = vidx[(d_, n - 1, r)]
                                    vc = vidx[(d_, n, r)]
                                    p.op("pe", lambda j=j, vp=vp: nc.tensor.matmul(
                                        psb[ob_][0:65, j * 128:(j + 1) * 128], lhsT=vb[:, vp, hh, 0:65], rhs=em[:, j * 256:j * 256 + 128], start=True, stop=False),
                                        reads=[vbk, ("vones", hp % 2), emk], writes=[PS(ob_)])
                                    p.op("pe", lambda j=j, vc=vc: nc.tensor.matmul(
                                        psb[ob_][0:65, j * 128:(j + 1) * 128], lhsT=vb[:, vc, hh, 0:65], rhs=em[:, j * 256 + 128:j * 256 + 256], start=False, stop=True),
                                        reads=[vbk, ("vones", hp % 2), emk], writes=[PS(ob_)])
                                n0, r0 = pair[0]
                                src = psb[ob_][0:65, 0:256].rearrange("p (b j) -> p b j", b=2)
                                if d_ == 1:
                                    q0 = span * n0 - NTOK
                                    dest = acc[0:65, q0:q0 + 256].rearrange("p (b j) -> p b j", b=2)
                                    p.op("act", lambda dest=dest, src=src: nc.scalar.copy(out=dest, in_=src), reads=[PS(ob_)], writes=[acck])
                                else:
                                    s0 = span * n0 - NTOK
                                    dest = acc[0:65, s0:s0 + span].rearrange("p (j d) -> p d j", d=d_)[:, r0:r0 + 2, :]
                                    p.op("dve", lambda dest=dest, src=src: nc.vector.tensor_tensor(out=dest, in0=dest, in1=src, op=ALU.add), reads=[PS(ob_), acck], writes=[acck])
                        for c in range(4):
                            p.op("pe", lambda c=c: nc.tensor.matmul(psb[4][0:64, :], lhsT=sel_f, rhs=acc[0:65, c * 512:(c + 1) * 512], start=True, stop=True),
                                 reads=["consts", acck], writes=[PS(4)])
                            rd, rdk = RD.next()
                            bq_, bqk = BQ.next()
                            bs_, bsk = BS.next()
                            p.op("dve", lambda rd=rd: nc.vector.reciprocal(out=rd[:], in_=psb[4][0:64, :]), reads=[PS(4)], writes=[rdk])
                            p.op("dve", lambda c=c, rd=rd, bq_=bq_: nc.vector.tensor_tensor(out=bq_[:], in0=acc[0:64, c * 512:(c + 1) * 512], in1=rd[:], op=ALU.mult),
                                 reads=[acck, rdk], writes=[bqk])
                            p.op("act", lambda bq_=bq_, bs_=bs_: nc.scalar.activation(out=bs_[:], in_=bq_[:], func=AF.Square), reads=[bqk], writes=[bsk])
                            for t in range(4):
                                ti = c * 4 + t
                                col = ti * 8 + h
                                p.op("pe", lambda t=t, col=col, bs_=bs_: nc.tensor.matmul(psb[5][:, col:col + 1], lhsT=bs_[0:64, t * 128:(t + 1) * 128], rhs=ones_f, start=True, stop=True),
                                     reads=[bsk, "consts"], writes=[PS(5)])
                            p.op("pool", lambda c=c, bq_=bq_: nc.gpsimd.tensor_scalar(out=bT[po:po + 64, hp, c * 512:(c + 1) * 512], in0=bq_[:], scalar1=gb64[:, h:h + 1], scalar2=None, op0=ALU.mult),
                                 reads=[bqk, "gb64"], writes=["bT"])
                            if "dbg_b" in dram:
                                dma("sp", dram["dbg_b"][h * 64:(h + 1) * 64, c * 512:(c + 1) * 512], bq_[:], reads=[bqk])
                if stage >= 3:
                    p.op("dve", lambda: nc.vector.tensor_copy(out=ssq_b[:].rearrange("p t h -> p (t h)"), in_=psb[5][:, 0:128]), reads=[PS(5)], writes=["ssq_b"])
                    if "dbg_ssqb" in dram:
                        dma("sp", dram["dbg_ssqb"], ssq_b[:].rearrange("p t h -> p (t h)"), reads=["ssq_b"])
                p.barrier()
        scat_keys = []
        hbf_keys = ["hbf_z"]
        h32_keys = []
        with ExitStack() as es3:
            zrow = sbuf(es3, "zrow", [1, D], F32)
            zrow_bf = sbuf(es3, "zrow_bf", [1, D], BF16)
            p.op("dve", lambda: nc.vector.memset(zrow[:], 0.0), writes=["zrow"])
            p.op("dve", lambda: nc.vector.memset(zrow_bf[:], 0.0), writes=["zrow_bf"])
            dma("sp", hbf_d[NTOK:NTOK + 1, :], zrow_bf[:], reads=["zrow_bf"], writes=["hbf_z"])
            dma("sp", eo_d[SLOTS:SLOTS + 1, :], zrow[:], reads=["zrow"], writes=["eo_z"])
            wout = sbuf(es3, "wout", [128, 8, D], BF16)
            for k in range(8):
                dma("pool", wout[:, k, :], wout_d[k * 128:(k + 1) * 128, :], writes=[("wout", k)])
            vec3 = sbuf(es3, "vec3", [128, 2, D], F32)
            dma("sp", vec3[:], vec_d[:, 2:4, :], writes=["vec3"])
            wr = sbuf(es3, "wr", [128, 8, NE], F32)
            dma("sp", wr[:], wr_d.rearrange("(k p) e -> p k e", p=128), writes=["wr"])
            brb = sbuf(es3, "brb", [128, NE], F32)
            dma("sp", brb[:], br_d, writes=["brb"])
            rs = sbuf(es3, "rs", [128, 4, NT], F32)
            p.op("dve", lambda: nc.vector.tensor_reduce(out=rs[:, 2, :], in_=ssq_b[:], axis=AX.X, op=ALU.add), reads=["ssq_b"], writes=["rs"])
            p.op("act", lambda: nc.scalar.activation(out=rs[:, 3, :], in_=ssq_a[:], func=AF.Sqrt, bias=eps_t[:, 0:1], scale=1.0 / 512), reads=["ssq_a", "eps"], writes=["rs"])
            p.op("dve", lambda: nc.vector.reciprocal(out=rs[:, 0, :], in_=rs[:, 3, :]), reads=["rs"], writes=["rs"])
            p.op("act", lambda: nc.scalar.activation(out=rs[:, 3, :], in_=rs[:, 2, :], func=AF.Sqrt, bias=eps_t[:, 0:1], scale=1.0 / 512), reads=["rs", "eps"], writes=["rs"])
            p.op("dve", lambda: nc.vector.reciprocal(out=rs[:, 1, :], in_=rs[:, 3, :]), reads=["rs"], writes=["rs"])
            X = SRot(es3, "X", 2, [128, D], F32)
            T1 = SRot(es3, "T1", 2, [128, D], F32)
            T2 = SRot(es3, "T2", 2, [128, D], F32)
            HB = SRot(es3, "HB", 2, [128, D], BF16)
            HT = SRot(es3, "HT", 2, [128, 8, 128], F32)
            SM = SRot(es3, "SM", 2, [128, 16], F32)
            LG = SRot(es3, "LG", 2, [128, NE], F32)
            RK = SRot(es3, "RK", 2, [128, 6, NE], F32)
            OH = SRot(es3, "OH", 2, [128, 4, NE], F32)
            MX = SRot(es3, "MX", 2, [128, 24], F32)

            def layer_norm(src, srck, dst, dstk, g_ap, b_ap, gk, jt, jtk):
                sm, smk = SM.next()
                p.op("dve", lambda: nc.vector.memset(sm[:], 0.0), writes=[smk])
                p.op("act", lambda: nc.scalar.activation(out=jt[:], in_=src[:], func=AF.Identity, accum_out=sm[:, 0:1]), reads=[srck, smk], writes=[jtk, smk])
                p.op("act", lambda: nc.scalar.activation(out=jt[:], in_=src[:], func=AF.Square, accum_out=sm[:, 1:2]), reads=[srck, smk], writes=[jtk, smk])
                p.op("dve", lambda: nc.vector.tensor_single_scalar(out=sm[:, 2:3], in_=sm[:, 0:1], scalar=1.0 / D, op=ALU.mult), reads=[smk], writes=[smk])
                p.op("dve", lambda: nc.vector.tensor_tensor(out=sm[:, 3:4], in0=sm[:, 2:3], in1=sm[:, 2:3], op=ALU.mult), reads=[smk], writes=[smk])
                p.op("dve", lambda: nc.vector.scalar_tensor_tensor(out=sm[:, 4:5], in0=sm[:, 1:2], scalar=1.0 / D, in1=sm[:, 3:4], op0=ALU.mult, op1=ALU.subtract), reads=[smk], writes=[smk])
                p.op("act", lambda: nc.scalar.activation(out=sm[:, 5:6], in_=sm[:, 4:5], func=AF.Sqrt, bias=eps_t[:, 0:1], scale=1.0), reads=[smk, "eps"], writes=[smk])
                p.op("dve", lambda: nc.vector.reciprocal(out=sm[:, 6:7], in_=sm[:, 5:6]), reads=[smk], writes=[smk])
                p.op("dve", lambda: nc.vector.tensor_scalar(out=jt[:], in0=src[:], scalar1=sm[:, 2:3], scalar2=sm[:, 6:7], op0=ALU.subtract, op1=ALU.mult), reads=[srck, smk], writes=[jtk])
                p.op("pool", lambda: nc.gpsimd.tensor_tensor(out=jt[:], in0=jt[:], in1=g_ap, op=ALU.mult), reads=[jtk, gk], writes=[jtk])
                p.op("pool", lambda: nc.gpsimd.tensor_tensor(out=dst[:], in0=jt[:], in1=b_ap, op=ALU.add), reads=[jtk, gk], writes=[dstk])

            for i in range(NT if stage >= 4 else 0):
                tsl = slice(i * 128, (i + 1) * 128)
                x_, xk_ = X.next()
                dma("sp", x_[:], x_d[tsl, :], writes=[xk_])
                for half in range(2):
                    for c in range(4):
                        p.op("pe", lambda half=half, c=c: nc.tensor.matmul(psb[half][:], lhsT=aT[:, c, tsl], rhs=wout[:, c, half * 512:(half + 1) * 512],
                                                                       start=(c == 0), stop=(c == 3)), reads=["aT", ("wout", c)], writes=[PS(half)])
                    for c in range(4):
                        p.op("pe", lambda half=half, c=c: nc.tensor.matmul(psb[2 + half][:], lhsT=bT[:, c, tsl], rhs=wout[:, 4 + c, half * 512:(half + 1) * 512],
                                                                       start=(c == 0), stop=(c == 3)), reads=["bT", ("wout", 4 + c)], writes=[PS(2 + half)])
                t1, t1k = T1.next()
                t2, t2k = T2.next()
                p.op("act", lambda: nc.scalar.mul(out=x_[:], in_=x_[:], mul=ALPHA), reads=[xk_], writes=[xk_])
                for half in range(2):
                    hs = slice(half * 512, (half + 1) * 512)
                    p.op("dve", lambda half=half, hs=hs: nc.vector.scalar_tensor_tensor(out=t1[:, hs], in0=psb[half][:], scalar=rs[:, 0, i:i + 1], in1=x_[:, hs], op0=ALU.mult, op1=ALU.add),
                         reads=[PS(half), "rs", xk_], writes=[t1k])
                    p.op("dve", lambda half=half, hs=hs: nc.vector.scalar_tensor_tensor(out=t1[:, hs], in0=psb[2 + half][:], scalar=rs[:, 1, i:i + 1], in1=t1[:, hs], op0=ALU.mult, op1=ALU.add),
                         reads=[PS(2 + half), "rs", t1k], writes=[t1k])
                h_, hk_ = X.next()
                layer_norm(t1, t1k, h_, hk_, vec3[:, 0, :], vec3[:, 1, :], "vec3", t2, t2k)
                dma("sp", h32_d[tsl, :], h_[:], reads=[hk_], writes=[("h32", i)])
                h32_keys.append(("h32", i))
                hb, hbk = HB.next()
                p.op("act", lambda: nc.scalar.copy(out=hb[:], in_=h_[:]), reads=[hk_], writes=[hbk])
                dma("sp", hbf_d[tsl, :], hb[:], reads=[hbk], writes=[("hbf", i)])
                hbf_keys.append(("hbf", i))
                if "dbg_h" in dram:
                    dma("sp", dram["dbg_h"][tsl, :], h_[:], reads=[hk_])
                ht, htk = HT.next()
                for k in range(8):
                    b = 4 + k // 4
                    p.op("pe", lambda k=k, b=b: nc.tensor.transpose(psb[b][:, (k % 4) * 128:(k % 4 + 1) * 128], h_[:, k * 128:(k + 1) * 128], ident_f),
                         reads=[hk_, "consts"], writes=[PS(b)])
                p.op("act", lambda: nc.scalar.copy(out=ht[:, 0:4, :], in_=psb[4][:].rearrange("p (k t) -> p k t", k=4)), reads=[PS(4)], writes=[htk])
                p.op("dve", lambda: nc.vector.tensor_copy(out=ht[:, 4:8, :], in_=psb[5][:].rearrange("p (k t) -> p k t", k=4)), reads=[PS(5)], writes=[htk])
                for k in range(8):
                    p.op("pe", lambda k=k: nc.tensor.matmul(psb[6][:, 0:NE], lhsT=ht[:, k, :], rhs=wr[:, k, :], start=(k == 0), stop=(k == 7)),
                         reads=[htk, "wr"], writes=[PS(6)])
                lg, lgk = LG.next()
                p.op("dve", lambda: nc.vector.tensor_tensor(out=lg[:], in0=psb[6][:, 0:NE], in1=brb[:], op=ALU.add), reads=[PS(6), "brb"], writes=[lgk])
                if "dbg_logits" in dram:
                    dma("sp", dram["dbg_logits"][tsl, :], lg[:], reads=[lgk])
                mx, mxk = MX.next()
                p.op("dve", lambda: nc.vector.max(out=mx[:, 0:8], in_=lg[:]), reads=[lgk], writes=[mxk])
                rk, rkk = RK.next()
                p.op("dve", lambda: nc.vector.tensor_scalar(out=rk[:, 0, :], in0=lg[:], scalar1=mx[:, 3:4], scalar2=None, op0=ALU.is_ge), reads=[lgk, mxk], writes=[rkk])
                p.op("act", lambda: nc.scalar.copy(out=Mall[:, i, :], in_=rk[:, 0, :]), reads=[rkk], writes=[("Mall", i)])
                p.op("dve", lambda: nc.vector.tensor_single_scalar(out=mx[:, 8:9], in_=mx[:, 0:1], scalar=-1.0, op=ALU.mult), reads=[mxk], writes=[mxk])
                p.op("dve", lambda: nc.vector.memset(mx[:, 9:10], 0.0), reads=[], writes=[mxk])
                p.op("act", lambda: nc.scalar.activation(out=mx[:, 12:16], in_=mx[:, 0:4], func=AF.Exp, bias=mx[:, 8:9], scale=1.0, accum_out=mx[:, 9:10]), reads=[mxk], writes=[mxk])
                p.op("dve", lambda: nc.vector.reciprocal(out=mx[:, 10:11], in_=mx[:, 9:10]), reads=[mxk], writes=[mxk])
                p.op("dve", lambda: nc.vector.tensor_scalar(out=mx[:, 16:20], in0=mx[:, 12:16], scalar1=mx[:, 10:11], scalar2=None, op0=ALU.mult), reads=[mxk], writes=[mxk])
                for i2 in range(i + 1):
                    lhs = consts_bf[:, C_LSTRICT:C_LSTRICT + 128] if i2 == i else consts_bf[:, C_ONES:C_ONES + 128]
                    p.op("pe", lambda i2=i2, lhs=lhs: nc.tensor.matmul(psb[7][:, 0:NE], lhsT=lhs, rhs=Mall[:, i2, :], start=(i2 == 0), stop=(i2 == i)),
                         reads=[("Mall", i2), "consts_bf"], writes=[PS(7)])
                p.op("dve", lambda: nc.vector.tensor_copy(out=rk[:, 1, :], in_=psb[7][:, 0:NE]), reads=[PS(7)], writes=[rkk])
                p.op("dve", lambda: nc.vector.scalar_tensor_tensor(out=rk[:, 2, :], in0=consts[:, C_IOTA:C_IOTA + NE], scalar=float(CAP), in1=rk[:, 1, :], op0=ALU.mult, op1=ALU.add),
                     reads=["consts", rkk], writes=[rkk])
                oh, ohk = OH.next()
                lg_bc = lg[:, :].unsqueeze(1).to_broadcast([128, 4, NE])
                mx_bc = mx[:, 0:4].unsqueeze(2).to_broadcast([128, 4, NE])
                p.op("dve", lambda: nc.vector.tensor_tensor(out=oh[:], in0=lg_bc, in1=mx_bc, op=ALU.is_equal), reads=[lgk, mxk], writes=[ohk])
                oh2, oh2k = OH.next()
                p.op("dve", lambda: nc.vector.tensor_tensor(out=oh2[:], in0=oh[:], in1=rk[:, 1, :].unsqueeze(1).to_broadcast([128, 4, NE]), op=ALU.mult), reads=[ohk, rkk], writes=[oh2k])
                p.op("dve", lambda: nc.vector.tensor_reduce(out=rk[:, 3, 0:4], in_=oh2[:], axis=AX.X, op=ALU.add), reads=[oh2k], writes=[rkk])
                p.op("dve", lambda: nc.vector.tensor_tensor(out=oh2[:], in0=oh[:], in1=rk[:, 2, :].unsqueeze(1).to_broadcast([128, 4, NE]), op=ALU.mult), reads=[ohk, rkk], writes=[oh2k])
                p.op("dve", lambda: nc.vector.tensor_reduce(out=rk[:, 3, 4:8], in_=oh2[:], axis=AX.X, op=ALU.add), reads=[oh2k], writes=[rkk])
                p.op("dve", lambda: nc.vector.tensor_scalar(out=rk[:, 3, 8:12], in0=rk[:, 3, 0:4], scalar1=float(CAP), scalar2=None, op0=ALU.is_lt), reads=[rkk], writes=[rkk])
                p.op("dve", lambda: nc.vector.scalar_tensor_tensor(out=rk[:, 3, 12:16], in0=rk[:, 3, 4:8], scalar=-float(SLOTS), in1=rk[:, 3, 8:12], op0=ALU.add, op1=ALU.mult), reads=[rkk], writes=[rkk])
                p.op("dve", lambda: nc.vector.tensor_scalar(out=rk[:, 3, 16:20], in0=rk[:, 3, 12:16], scalar1=float(SLOTS), scalar2=None, op0=ALU.add), reads=[rkk], writes=[rkk])
                p.op("dve", lambda: nc.vector.tensor_copy(out=pos_i[:, i * 4:i * 4 + 4], in_=rk[:, 3, 16:20]), reads=[rkk], writes=[("pos", i)])
                p.op("dve", lambda: nc.vector.tensor_tensor(out=gates[:, i, :], in0=mx[:, 16:20], in1=rk[:, 3, 8:12], op=ALU.mult), reads=[mxk, rkk], writes=[("gates", i)])
                if "dbg_pos" in dram:
                    dma("sp", dram["dbg_pos"][tsl, :], rk[:, 3, 16:20], reads=[rkk])
                    dma("sp", dram["dbg_gates"][tsl, :], gates[:, i, :], reads=[("gates", i)])
                for k in range(4):
                    p.op("pool", lambda k=k: nc.gpsimd.indirect_dma_start(
                        out=stok_d[:, :], out_offset=bass.IndirectOffsetOnAxis(ap=pos_i[:, i * 4 + k:i * 4 + k + 1], axis=0),
                        in_=tokid_i[:, i:i + 1], in_offset=None),
                        reads=[("pos", i), "tokid_i", "stok_init"], writes=[("scat", i, k)], dma=True)
                    scat_keys.append(("scat", i, k))
            p.barrier()
        esAB.close()
        eo_keys = ["eo_z"]
        with ExitStack() as es5:
            bgT = sbuf(es5, "bgT", [128, NE, 8], F32)
            buT = sbuf(es5, "buT", [128, NE, 8], F32)
            dma("sp", bgT[:], bg_d, writes=["bgT"])
            dma("sp", buT[:], bu_d, writes=["buT"])
            WG = [sbuf(es5, f"WG{i}", [128, 8, D], BF16) for i in range(2)]
            WU = [sbuf(es5, f"WU{i}", [128, 8, D], BF16) for i in range(2)]
            WD = [sbuf(es5, f"WD{i}", [128, 8, D], BF16) for i in range(2)]
            BD = [sbuf(es5, f"BD{i}", [128, D], F32) for i in range(2)]
            XB = [sbuf(es5, f"XB{i}", [128, NBLK, D], BF16) for i in range(2)]
            STI = [sbuf(es5, f"STI{i}", [128, NBLK], I32) for i in range(2)]
            XT = [sbuf(es5, f"XT{i}", [128, 8, CAP], BF16) for i in range(2)]
            ACT_T = sbuf(es5, "ACTT", [128, 8, CAP], BF16)
            GC = SRot(es5, "GC", 2, [128, CAP], F32)
            SG = SRot(es5, "SG", 2, [128, CAP], F32)
            UC = SRot(es5, "UC", 2, [128, CAP], F32)
            EO = SRot(es5, "EO", 2, [128, D], F32)

            def load_w(e):
                j = e % 2
                for (W_, wd_, nm) in ((WG, wg_d, "WG"), (WU, wu_d, "WU"), (WD, wd_d, "WD")):
                    for hk in range(2):
                        dma("pool", W_[j][:, hk * 4:(hk + 1) * 4, :], wd_[e, hk * 512:(hk + 1) * 512, :].rearrange("(k p) f -> p k f", p=128), writes=[(nm, j, hk)])
                dma("sp", BD[j][:], bd_d[e:e + 1, :].to_broadcast([128, D]), writes=[("BD", j)])

            def load_x(e):
                j = e % 2
                dma("sp", STI[j][:], stok_d[e * CAP:(e + 1) * CAP, :].rearrange("(p b) o -> p (b o)", p=128), reads=scat_keys + ["stok_init"], writes=[("STI", j)])
                for blk in range(NBLK):
                    p.op("pool", lambda blk=blk: nc.gpsimd.indirect_dma_start(
                        out=XB[j][:, blk, :], out_offset=None, in_=hbf_d[:, :],
                        in_offset=bass.IndirectOffsetOnAxis(ap=STI[j][:, blk:blk + 1], axis=0)),
                        reads=[("STI", j)] + hbf_keys, writes=[("XB", j, blk)], dma=True)

            nexp = NE if stage >= 5 else 0
            if nexp:
                load_x(0)
                load_w(0)
            tr = 0
            for e in range(nexp):
                j = e % 2
                if e + 1 < nexp:
                    load_x(e + 1)
                    load_w(e + 1)
                for blk in range(NBLK):
                    b = 6 + tr % 2
                    tr += 1
                    ptr = psb[b][:].bitcast(BF16)
                    for k in range(8):
                        p.op("pe", lambda blk=blk, k=k, ptr=ptr: nc.tensor.transpose(ptr[:, k * 128:(k + 1) * 128], XB[j][:, blk, k * 128:(k + 1) * 128], ident_bf),
                             reads=[("XB", j, blk), "consts_bf"], writes=[PS(b)])
                    src = ptr[:, :].rearrange("p (k t) -> p k t", k=8)
                    if blk % 2 == 0:
                        p.op("act", lambda blk=blk, src=src: nc.scalar.copy(out=XT[j][:, :, blk * 128:(blk + 1) * 128], in_=src), reads=[PS(b)], writes=[("XT", j)])
                    else:
                        p.op("dve", lambda blk=blk, src=src: nc.vector.tensor_copy(out=XT[j][:, :, blk * 128:(blk + 1) * 128], in_=src), reads=[PS(b)], writes=[("XT", j)])
                for f in range(8):
                    gb_ = f % 2
                    ub_ = 2 + f % 2
                    for k in range(8):
                        p.op("pe", lambda f=f, k=k: nc.tensor.matmul(psb[gb_][:, 0:CAP], lhsT=WG[j][:, k, f * 128:(f + 1) * 128], rhs=XT[j][:, k, :], start=(k == 0), stop=(k == 7)),
                             reads=[("WG", j, k // 4), ("XT", j)], writes=[PS(gb_)])
                    for k in range(8):
                        p.op("pe", lambda f=f, k=k: nc.tensor.matmul(psb[ub_][:, 0:CAP], lhsT=WU[j][:, k, f * 128:(f + 1) * 128], rhs=XT[j][:, k, :], start=(k == 0), stop=(k == 7)),
                             reads=[("WU", j, k // 4), ("XT", j)], writes=[PS(ub_)])
                    gc, gck = GC.next()
                    sg, sgk = SG.next()
                    uc, uck = UC.next()
                    p.op("dve", lambda f=f, gc=gc: nc.vector.tensor_scalar(out=gc[:], in0=psb[gb_][:, 0:CAP], scalar1=bgT[:, e, f:f + 1], scalar2=7.0, op0=ALU.add, op1=ALU.min),
                         reads=[PS(gb_), "bgT"], writes=[gck])
                    p.op("act", lambda gc=gc, sg=sg: nc.scalar.activation(out=sg[:], in_=gc[:], func=AF.Sigmoid, scale=1.702), reads=[gck], writes=[sgk])
                    p.op("act", lambda f=f, uc=uc: nc.scalar.activation(out=uc[:], in_=psb[ub_][:, 0:CAP], func=AF.Identity, bias=buT[:, e, f:f + 1], scale=1.0),
                         reads=[PS(ub_), "buT"], writes=[uck])
                    p.op("pool", lambda uc=uc: nc.gpsimd.tensor_scalar(out=uc[:], in0=uc[:], scalar1=-7.0, scalar2=7.0, op0=ALU.max, op1=ALU.min), reads=[uck], writes=[uck])
                    p.op("pool", lambda gc=gc, sg=sg: nc.gpsimd.tensor_tensor(out=gc[:], in0=gc[:], in1=sg[:], op=ALU.mult), reads=[gck, sgk], writes=[gck])
                    p.op("dve", lambda f=f, uc=uc, gc=gc: nc.vector.scalar_tensor_tensor(out=ACT_T[:, f, :], in0=uc[:], scalar=1.0, in1=gc[:], op0=ALU.add, op1=ALU.mult),
                         reads=[uck, gck], writes=[("ACTT", f)])
                for blk in range(NBLK):
                    eo, eok = EO.next()
                    for half in range(2):
                        db_ = 4 + half
                        for f in range(8):
                            p.op("pe", lambda blk=blk, half=half, f=f: nc.tensor.matmul(psb[db_][:], lhsT=ACT_T[:, f, blk * 128:(blk + 1) * 128], rhs=WD[j][:, f, half * 512:(half + 1) * 512],
                                                                                     start=(f == 0), stop=(f == 7)),
                                 reads=[("ACTT", f), ("WD", j, f // 4)], writes=[PS(db_)])
                        p.op("dve", lambda half=half, eo=eo: nc.vector.tensor_tensor(out=eo[:, half * 512:(half + 1) * 512], in0=psb[db_][:], in1=BD[j][:, half * 512:(half + 1) * 512], op=ALU.add),
                             reads=[PS(db_), ("BD", j)], writes=[eok])
                    dma("sp", eo_d[e * CAP:(e + 1) * CAP, :].rearrange("(p b) d -> p b d", b=NBLK)[:, blk, :], eo[:], reads=[eok], writes=[("eo", e, blk)])
                    eo_keys.append(("eo", e, blk))
            p.barrier()

        with ExitStack() as es6:
            vec6 = sbuf(es6, "vec6", [128, 2, D], F32)
            dma("sp", vec6[:], vec_d[:, 4:6, :], writes=["vec6"])
            Hh = SRot(es6, "Hh", 2, [128, D], F32)
            G4 = SRot(es6, "G4", 8, [128, D], F32)
            AC = SRot(es6, "AC", 2, [128, D], F32)
            JT = SRot(es6, "JT", 2, [128, D], F32)
            OT = SRot(es6, "OT", 2, [128, D], F32)
            SM = SRot(es6, "SM2", 2, [128, 16], F32)
            for i in range(NT if stage >= 6 else 0):
                tsl = slice(i * 128, (i + 1) * 128)
                hh, hhk = Hh.next()
                dma("sp", hh[:], h32_d[tsl, :], reads=h32_keys, writes=[hhk])
                gs = []
                for k in range(4):
                    g_, gk_ = G4.next()
                    p.op("pool", lambda k=k, g_=g_: nc.gpsimd.indirect_dma_start(
                        out=g_[:], out_offset=None, in_=eo_d[:, :],
                        in_offset=bass.IndirectOffsetOnAxis(ap=pos_i[:, i * 4 + k:i * 4 + k + 1], axis=0)),
                        reads=[("pos", i)] + eo_keys, writes=[gk_], dma=True)
                    gs.append((g_, gk_))
                ac, ack = AC.next()
                p.op("dve", lambda: nc.vector.tensor_scalar(out=ac[:], in0=gs[0][0][:], scalar1=gates[:, i, 0:1], scalar2=None, op0=ALU.mult), reads=[gs[0][1], ("gates", i)], writes=[ack])
                for k in range(1, 4):
                    p.op("dve", lambda k=k: nc.vector.scalar_tensor_tensor(out=ac[:], in0=gs[k][0][:], scalar=gates[:, i, k:k + 1], in1=ac[:], op0=ALU.mult, op1=ALU.add),
                         reads=[gs[k][1], ("gates", i), ack], writes=[ack])
                p.op("dve", lambda: nc.vector.scalar_tensor_tensor(out=ac[:], in0=hh[:], scalar=ALPHA, in1=ac[:], op0=ALU.mult, op1=ALU.add), reads=[hhk, ack], writes=[ack])
                if "dbg_pre2" in dram:
                    dma("sp", dram["dbg_pre2"][tsl, :], ac[:], reads=[ack])
                jt, jtk = JT.next()
                ot, otk = OT.next()
                sm, smk = SM.next()
                p.op("dve", lambda: nc.vector.memset(sm[:], 0.0), writes=[smk])
                p.op("act", lambda: nc.scalar.activation(out=jt[:], in_=ac[:], func=AF.Identity, accum_out=sm[:, 0:1]), reads=[ack, smk], writes=[jtk, smk])
                p.op("act", lambda: nc.scalar.activation(out=jt[:], in_=ac[:], func=AF.Square, accum_out=sm[:, 1:2]), reads=[ack, smk], writes=[jtk, smk])
                p.op("dve", lambda: nc.vector.tensor_single_scalar(out=sm[:, 2:3], in_=sm[:, 0:1], scalar=1.0 / D, op=ALU.mult), reads=[smk], writes=[smk])
                p.op("dve", lambda: nc.vector.tensor_tensor(out=sm[:, 3:4], in0=sm[:, 2:3], in1=sm[:, 2:3], op=ALU.mult), reads=[smk], writes=[smk])
                p.op("dve", lambda: nc.vector.scalar_tensor_tensor(out=sm[:, 4:5], in0=sm[:, 1:2], scalar=1.0 / D, in1=sm[:, 3:4], op0=ALU.mult, op1=ALU.subtract), reads=[smk], writes=[smk])
                p.op("act", lambda: nc.scalar.activation(out=sm[:, 5:6], in_=sm[:, 4:5], func=AF.Sqrt, bias=eps_t[:, 0:1], scale=1.0), reads=[smk, "eps"], writes=[smk])
                p.op("dve", lambda: nc.vector.reciprocal(out=sm[:, 6:7], in_=sm[:, 5:6]), reads=[smk], writes=[smk])
                p.op("dve", lambda: nc.vector.tensor_scalar(out=jt[:], in0=ac[:], scalar1=sm[:, 2:3], scalar2=sm[:, 6:7], op0=ALU.subtract, op1=ALU.mult), reads=[ack, smk], writes=[jtk])
                p.op("pool", lambda: nc.gpsimd.tensor_tensor(out=jt[:], in0=jt[:], in1=vec6[:, 0, :], op=ALU.mult), reads=[jtk, "vec6"], writes=[jtk])
                p.op("pool", lambda: nc.gpsimd.tensor_tensor(out=ot[:], in0=jt[:], in1=vec6[:, 1, :], op=ALU.add), reads=[jtk, "vec6"], writes=[otk])
                dma("sp", out_d[tsl, :], ot[:], reads=[otk], writes=[("out", i)])
        p.finish("sp")
    p.dram = dram
    return nc, p


def host_consts(flag):
    c = np.zeros((128, NCONST), np.float32)
    i = np.arange(128)
    c[:, C_IDENT:C_IDENT + 128] = np.eye(128, dtype=np.float32)
    c[:, C_TRI:C_TRI + 128] = (i[:, None] <= i[None, :])
    c[:, C_MPREV:C_MPREV + 128] = (i[:, None] >= i[None, :])
    c[:, C_MPREVC:C_MPREVC + 128] = flag * (i[:, None] >= i[None, :])
    c[:, C_LSTRICT:C_LSTRICT + 128] = (i[:, None] < i[None, :])
    c[:, C_ONES:C_ONES + 128] = 1.0
    c[:, C_IOTA:C_IOTA + 32] = np.arange(32)[None, :]
    c[:, C_TOKID:C_TOKID + 16] = np.arange(16)[None, :] * 128 + i[:, None]
    c[64, C_SEL:C_SEL + 64] = 1.0
    return c


def make_in_maps(inputs, cores=range(8)):
    f = lambda a: np.ascontiguousarray(np.asarray(a, dtype=np.float32))
    x = np.asarray(inputs["x"], dtype=np.float32)
    shared = {
        "w_in": f(inputs["w_in"][0]),
        "sgu_wT": f(np.transpose(inputs["sgu_w"][0], (2, 0, 1))),
        "sgu_bT": f(inputs["sgu_b"][0].T),
        "mix_gb64": f(inputs["mix_norm_g"][0, 512:].reshape(8, 64).T),
        "mix_gb": f(inputs["mix_norm_g"][0, 512:].reshape(4, 128).T),
        "w_out": f(inputs["w_out"][0]),
        "w_router": f(inputs["w_router"][0]),
        "b_router_bc": f(np.broadcast_to(inputs["b_router"][0][None, :], (128, NE))),
        "w_gate": f(inputs["w_gate"][0]),
        "w_up": f(inputs["w_up"][0]),
        "w_down": f(inputs["w_down"][0]),
        "b_gateT": f(np.transpose(inputs["b_gate"][0].reshape(NE, 8, 128), (2, 0, 1))),
        "b_upT": f(np.transpose(inputs["b_up"][0].reshape(NE, 8, 128), (2, 0, 1))),
        "b_down": f(inputs["b_down"][0]),
    }
    rows = np.zeros((6, D), np.float32)
    rows[0, :512] = inputs["sgu_ln_g"][0]
    rows[0, 512:] = inputs["sgu_ln_b"][0]
    rows[1, :512] = inputs["mix_norm_g"][0, :512]
    rows[2] = inputs["ln1_g"][0]
    rows[3] = inputs["ln1_b"][0]
    rows[4] = inputs["ln2_g"][0]
    rows[5] = inputs["ln2_b"][0]
    shared["vecs"] = f(np.broadcast_to(rows[None], (128, 6, D)))
    maps = []
    for c in cores:
        b, half = c // 2, c % 2
        own = x[b, half * NTOK:(half + 1) * NTOK]
        xT = np.zeros((D, NEXT), np.float32)
        if half == 1:
            xT[:, :NTOK] = x[b, :NTOK].T
        xT[:, NTOK:] = own.T
        m = dict(shared)
        m["xT"] = xT
        m["x"] = f(own)
        m["consts"] = host_consts(float(half))
        maps.append(m)
    return maps


_NC_CACHE = {}


def kernel(**inputs):
    if "nc" not in _NC_CACHE:
        _NC_CACHE["nc"] = build()[0]
    nc = _NC_CACHE["nc"]
    maps = make_in_maps(inputs)
    res = run_bass_kernel_spmd(nc, maps, core_ids=list(range(8)))
    out = np.zeros((4, 4096, D), np.float32)
    for c in range(8):
        b, half = c // 2, c % 2
        out[b, half * NTOK:(half + 1) * NTOK] = res.results[c]["out"]
    return out
```

```python
import numpy as np
from contextlib import ExitStack
import concourse.bass as bass
import concourse.mybir as mybir
from concourse.bass_utils import run_bass_kernel_spmd

F32 = mybir.dt.float32
BF16 = mybir.dt.bfloat16
I32 = mybir.dt.int32
U32 = mybir.dt.uint32
AF = mybir.ActivationFunctionType
ALU = mybir.AluOpType
AX = mybir.AxisListType

D = 1024
NTOK = 2048
NEXT = 4096
NT = NTOK // 128
NE = 32
TOPK = 4
CAP = 512
NBLK = CAP // 128
ALPHA = 2.0 ** 0.25
EPS = 1e-5
PATTERNS = (1, 4, 16)

C_IDENT = 0
C_TRI = 128
C_MPREV = 256
C_MPREVC = 384
C_LSTRICT = 512
C_ONES = 640
C_IOTA = 768
C_TOKID = 800
C_SEL = 816
NCONST = 880


class Prog:
    COMPUTE = ("pe", "act", "dve", "pool")

    def __init__(self, nc, n_dma_sems=20, same_engine_sync=True):
        self.nc = nc
        self.es = ExitStack()
        self.eng = {"pe": nc.tensor, "act": nc.scalar, "dve": nc.vector, "pool": nc.gpsimd, "sp": nc.sync}
        self.sem = {}
        self.cnt = {}
        for e in self.COMPUTE:
            self.sem[e] = self.es.enter_context(nc.semaphore("sem_" + e))
            self.cnt[e] = 0
        self.known = {e: {} for e in self.eng}
        self.last_w = {}
        self.readers = {}
        self.same_engine_sync = same_engine_sync
        self.dma_pool = {}
        self.dma_idx = {}
        for q in ("sp", "act", "pool"):
            self.dma_pool[q] = [[self.es.enter_context(nc.semaphore(f"dsem_{q}_{i}")), 0] for i in range(n_dma_sems)]
            self.dma_idx[q] = 0
        self.n_inst = {e: 0 for e in self.eng}

    def sb(self, name, shape, dt):
        return self.es.enter_context(self.nc.sbuf_tensor("s_" + name, shape, dt))

    def ps(self, name, shape, dt):
        return self.es.enter_context(self.nc.psum_tensor(name, shape, dt))

    def _wait(self, e, tok):
        sem, val, src = tok
        if src == e and (e == "pe" or not self.same_engine_sync):
            return
        k = self.known[e]
        sid = id(sem)
        if k.get(sid, 0) >= val:
            return
        self.eng[e].wait_ge(sem, val)
        k[sid] = val

    def op(self, e, fn, reads=(), writes=(), dma=False):
        deps = {}

        def add(t):
            key = id(t[0])
            if key not in deps or deps[key][1] < t[1]:
                deps[key] = t
        for kk in reads:
            t = self.last_w.get(kk)
            if t is not None:
                add(t)
        for kk in writes:
            t = self.last_w.get(kk)
            if t is not None:
                add(t)
            for t in self.readers.get(kk, ()):
                add(t)
        for t in deps.values():
            self._wait(e, t)
        if dma:
            pool = self.dma_pool[e]
            i = self.dma_idx[e]
            self.dma_idx[e] = (i + 1) % len(pool)
            slot = pool[i]
            if slot[1] > 0:
                self._wait(e, (slot[0], slot[1], "dma"))
            inst = fn()
            slot[1] += 16
            inst.then_inc(slot[0], 16)
            tok = (slot[0], slot[1], "dma")
        else:
            inst = fn()
            self.cnt[e] += 1
            inst.then_inc(self.sem[e], 1)
            tok = (self.sem[e], self.cnt[e], e)
        self.n_inst[e] += 1
        for kk in writes:
            self.last_w[kk] = tok
            self.readers[kk] = []
        for kk in reads:
            if kk in writes:
                continue
            self.readers.setdefault(kk, []).append(tok)
        return tok

    def barrier(self):
        for e in self.eng:
            self.finish(e)

    def finish(self, e="sp"):
        for pool in self.dma_pool.values():
            for sem, val in pool:
                if val > 0:
                    self._wait(e, (sem, val, "dma"))
        for c in self.COMPUTE:
            if self.cnt[c] > 0:
                self._wait(e, (self.sem[c], self.cnt[c], c))


class Rot:
    def __init__(self, p, name, n, shape, dt):
        self.tiles = [p.sb(f"{name}{i}", shape, dt) for i in range(n)]
        self.name = name
        self.n = n
        self.i = 0

    def next(self):
        j = self.i % self.n
        self.i += 1
        return self.tiles[j], (self.name, j)


def v_tiles():
    out = {}
    out[1] = [(n, 0) for n in range(15, 32)]
    out[4] = [(n, r) for n in range(3, 8) for r in range(4)]
    out[16] = [(n, r) for n in range(0, 2) for r in range(16)]
    return out


def build(stage=99, dbg=()):
    nc = bass.Bass("TRN2", target_bir_lowering=False)
    dram = {}

    def din(name, shape, dt=F32):
        dram[name] = nc.dram_tensor(name, list(shape), dt, kind="ExternalInput").ap()
        return dram[name]

    def dout(name, shape, dt=F32):
        dram[name] = nc.dram_tensor(name, list(shape), dt, kind="ExternalOutput").ap()
        return dram[name]

    xT_d = din("xT", [D, NEXT])
    x_d = din("x", [NTOK, D])
    win_d = din("w_in", [D, 2560])
    consts_d = din("consts", [128, NCONST])
    sguw_d = din("sgu_wT", [128, 8, 128])
    sgub_d = din("sgu_bT", [128, 8])
    vec_d = din("vecs", [128, 6, D])
    din("mix_gb64", [64, 8])
    gb_d = din("mix_gb", [128, 4])
    wout_d = din("w_out", [D, D])
    wr_d = din("w_router", [D, NE])
    br_d = din("b_router_bc", [128, NE])
    if stage >= 5:
        wg_d = din("w_gate", [NE, D, D])
        wu_d = din("w_up", [NE, D, D])
        wd_d = din("w_down", [NE, D, D])
    bg_d = din("b_gateT", [128, NE, 8])
    bu_d = din("b_upT", [128, NE, 8])
    bd_d = din("b_down", [NE, D])
    out_d = dout("out", [NTOK, D])
    for name, shape in dbg:
        dout(name, shape)

    p = Prog(nc)
    with p.es:
        E = p.eng
        psb = [p.ps(f"psb{i}", [128, 512], F32) for i in range(8)]

        def PS(b):
            return ("ps", b)

        def sbuf_raw(es, name, shape, dt):
            return es.enter_context(nc.sbuf_tensor("s_" + name, shape, dt))

        def sbuf(es, name, shape, dt):
            return es.enter_context(nc.sbuf_tensor("s_" + name, shape, dt))

        class SRot:
            def __init__(self, es, name, n, shape, dt):
                self.tiles = [sbuf(es, f"{name}{i}", shape, dt) for i in range(n)]
                self.name, self.n, self.i = name, n, 0

            def next(self):
                j = self.i % self.n
                self.i += 1
                return self.tiles[j], (self.name, j)

        def dma(q, out, in_, reads=(), writes=()):
            return p.op(q, lambda: E[q].dma_start(out=out, in_=in_), reads=reads, writes=writes, dma=True)

        consts = p.sb("consts", [128, NCONST], F32)
        consts_bf = p.sb("consts_bf", [128, NCONST], BF16)
        dma("sp", consts[:], consts_d, writes=["consts"])
        dma("pool", consts_bf[:], consts_d, writes=["consts_bf"])
        sgub = p.sb("sgub", [128, 8], F32)
        dma("sp", sgub[:], sgub_d, writes=["sgub"])
        gb = p.sb("gb", [128, 4], F32)
        dma("sp", gb[:], gb_d, writes=["gb"])
        wcT = p.sb("wcT", [128, 8, 128], BF16)
        eps_t = p.sb("eps_t", [128, 1], F32)
        p.op("dve", lambda: nc.vector.memset(eps_t[:], EPS), writes=["eps"])
        ident_bf = consts_bf[:, C_IDENT:C_IDENT + 128]
        ident_f = consts[:, C_IDENT:C_IDENT + 128]

        ssq_a = p.sb("ssq_a", [128, NT], F32)
        ssq_b = p.sb("ssq_b", [128, NT, 8], F32)
        p.op("dve", lambda: nc.vector.memset(ssq_a[:], 0.0), writes=["ssq_a"])
        SLOTS = NE * CAP
        hbf_d = nc.dram_tensor("hbf_scr", [NTOK + 1, D], BF16).ap()
        h32_d = nc.dram_tensor("h32_scr", [NTOK, D], F32).ap()
        stok_d = nc.dram_tensor("stok_scr", [SLOTS + 128, 1], I32).ap()
        eo_d = nc.dram_tensor("eo_scr", [SLOTS + 128, D], F32).ap()
        gates = p.sb("gates", [128, NT, 4], F32)
        pos_i = p.sb("pos_i", [128, NT * 4], I32)
        Mall = p.sb("Mall", [128, NT, NE], BF16)
        tokid_i = p.sb("tokid_i", [128, NT], I32)
        p.op("dve", lambda: nc.vector.tensor_copy(out=tokid_i[:], in_=consts[:, C_TOKID:C_TOKID + NT]), reads=["consts"], writes=["tokid_i"])
        fill = p.sb("fill", [128, (SLOTS + 128) // 128], I32)
        p.op("dve", lambda: nc.vector.memset(fill[:], NTOK), writes=["fill"])
        dma("sp", stok_d.rearrange("(p f) o -> p (f o)", p=128), fill[:], reads=["fill"], writes=["stok_init"])
        esAB = ExitStack()
        aT = sbuf_raw(esAB, "aT", [128, 4, NTOK], BF16)
        bT = sbuf_raw(esAB, "bT", [128, 4, NTOK], BF16)
        vt = v_tiles()
        vidx = {}
        nv = 0
        for d_ in PATTERNS:
            for nr in vt[d_]:
                vidx[(d_,) + nr] = nv
                nv += 1

        with ExitStack() as esQ:
            qT = sbuf(esQ, "qT", [128, 4, NTOK], BF16)
            kT = sbuf(esQ, "kT", [128, 4, NEXT], BF16)
            vT = sbuf(esQ, "vT", [128, 4, NEXT], BF16)
            with ExitStack() as esX:
                xTo = sbuf(esX, "xT_own", [128, 8, NTOK], BF16)
                for k in range(8):
                    dma("pool", xTo[:, k, :], xT_d[k * 128:(k + 1) * 128, NTOK:NEXT], writes=[("xTo", k)])
                xok = [("xTo", k) for k in range(8)]
                with ExitStack() as esB:
                    xTc = sbuf(esB, "xT_ctx", [128, 8, NTOK], BF16)
                    wkv = sbuf(esB, "win_kv", [128, 8, 1024], BF16)
                    sguw = sbuf(esB, "sguw", [128, 8, 128], F32)
                    dma("sp", sguw[:], sguw_d, writes=["sguw"])
                    tri_bc = consts[:, C_TRI:C_TRI + 128].unsqueeze(1).to_broadcast([128, 8, 128])
                    p.op("dve", lambda: nc.vector.tensor_tensor(out=wcT[:], in0=sguw[:], in1=tri_bc, op=ALU.mult),
                         reads=["sguw", "consts"], writes=["wcT"])
                    for k in range(8):
                        dma("pool", wkv[:, k, :], win_d[k * 128:(k + 1) * 128, 1536:2560], writes=[("wkv", k)])
                    for k in range(8):
                        dma("pool", xTc[:, k, :], xT_d[k * 128:(k + 1) * 128, 0:NTOK], writes=[("xTc", k)])
                    xck = [("xTc", k) for k in range(8)]
                    ev = 0
                    for (dst, dname, col0) in ((kT, "kT", 0), (vT, "vT", 512)) if stage >= 2 else ():
                        for hp in range(4):
                            for tc in range(8):
                                b = 6 + (ev % 2)
                                src, srck = (xTo, xok) if tc >= 4 else (xTc, xck)
                                tcl = tc % 4
                                for k in range(8):
                                    p.op("pe", lambda k=k, b=b, src=src, tcl=tcl: nc.tensor.matmul(
                                        psb[b][:], lhsT=wkv[:, k, col0 + hp * 128:col0 + (hp + 1) * 128],
                                        rhs=src[:, k, tcl * 512:(tcl + 1) * 512], start=(k == 0), stop=(k == 7)),
                                        reads=[srck[k], ("wkv", k)], writes=[PS(b)])
                                if ev % 2 == 0:
                                    p.op("act", lambda b=b: nc.scalar.copy(out=dst[:, hp, tc * 512:(tc + 1) * 512], in_=psb[b][:]), reads=[PS(b)], writes=[(dname, hp)])
                                else:
                                    p.op("dve", lambda b=b: nc.vector.tensor_copy(out=dst[:, hp, tc * 512:(tc + 1) * 512], in_=psb[b][:]), reads=[PS(b)], writes=[(dname, hp)])
                                ev += 1
                    p.barrier()
                with ExitStack() as esA:
                    win = sbuf(esA, "win_uvq", [128, 8, 1536], BF16)
                    for k in range(8):
                        dma("pool", win[:, k, :], win_d[k * 128:(k + 1) * 128, 0:1536], writes=[("win", k)])
                    wk = [("win", k) for k in range(8)]
                    vecA = sbuf(esA, "vecA", [128, 3, 512], F32)
                    dma("sp", vecA[:, 0:2, :], vec_d[:, 0, :].rearrange("p (a c) -> p a c", a=2), writes=["vecA"])
                    dma("sp", vecA[:, 2, :], vec_d[:, 1, 0:512], writes=["vecA"])
                    U = SRot(esA, "U", 2, [128, 512], F32)
                    V = SRot(esA, "V", 2, [128, 512], F32)
                    W1 = SRot(esA, "W1", 2, [128, 512], F32)
                    W2 = SRot(esA, "W2", 2, [128, 512], F32)
                    VN = SRot(esA, "VN", 2, [128, 512], BF16)
                    AB = SRot(esA, "AB", 2, [128, 512], BF16)
                    st = SRot(esA, "st", 2, [128, 8, 8], F32)

                    def gelu(src_ap, src_key, dst, dst_key):
                        p.op("act", lambda: nc.scalar.activation(out=dst[:], in_=src_ap, func=AF.Gelu_apprx_tanh), reads=[src_key], writes=[dst_key])

                    def emit_uv(i):
                        t0 = i * 128
                        b0 = 2 * (i % 2)
                        for half in range(2):
                            for k in range(8):
                                p.op("pe", lambda half=half, k=k: nc.tensor.matmul(
                                    psb[b0 + half][:], lhsT=xTo[:, k, t0:t0 + 128], rhs=win[:, k, half * 512:(half + 1) * 512],
                                    start=(k == 0), stop=(k == 7)), reads=[xok[k], wk[k]], writes=[PS(b0 + half)])

                    if stage >= 1:
                        emit_uv(0)
                    for i in range(NT if stage >= 1 else 0):
                        b0 = 2 * (i % 2)
                        if i + 1 < NT:
                            emit_uv(i + 1)
                        u, uk = U.next()
                        v, vk = V.next()
                        gelu(psb[b0][:], PS(b0), u, uk)
                        gelu(psb[b0 + 1][:], PS(b0 + 1), v, vk)
                        s, sk = st.next()
                        w1, w1k = W1.next()
                        v3 = v[:].rearrange("p (g c) -> p g c", g=8)
                        w13 = w1[:].rearrange("p (g c) -> p g c", g=8)
                        p.op("dve", lambda: nc.vector.tensor_reduce(out=s[:, 0, :], in_=v3, axis=AX.X, op=ALU.add), reads=[vk], writes=[sk])
                        p.op("act", lambda: nc.scalar.activation(out=w1[:], in_=v[:], func=AF.Square), reads=[vk], writes=[w1k])
                        p.op("dve", lambda: nc.vector.tensor_reduce(out=s[:, 1, :], in_=w13, axis=AX.X, op=ALU.add), reads=[w1k], writes=[sk])
                        p.op("dve", lambda: nc.vector.tensor_single_scalar(out=s[:, 2, :], in_=s[:, 0, :], scalar=1.0 / 64, op=ALU.mult), reads=[sk], writes=[sk])
                        p.op("dve", lambda: nc.vector.tensor_tensor(out=s[:, 3, :], in0=s[:, 2, :], in1=s[:, 2, :], op=ALU.mult), reads=[sk], writes=[sk])
                        p.op("dve", lambda: nc.vector.scalar_tensor_tensor(out=s[:, 4, :], in0=s[:, 1, :], scalar=1.0 / 64, in1=s[:, 3, :], op0=ALU.mult, op1=ALU.subtract), reads=[sk], writes=[sk])
                        p.op("act", lambda: nc.scalar.activation(out=s[:, 6, :], in_=s[:, 4, :], func=AF.Sqrt, bias=eps_t[:, 0:1], scale=1.0), reads=[sk, "eps"], writes=[sk])
                        p.op("dve", lambda: nc.vector.reciprocal(out=s[:, 5, :], in_=s[:, 6, :]), reads=[sk], writes=[sk])
                        mean_bc = s[:, 2, :].unsqueeze(2).to_broadcast([128, 8, 64])
                        rstd_bc = s[:, 5, :].unsqueeze(2).to_broadcast([128, 8, 64])
                        p.op("dve", lambda: nc.vector.tensor_tensor(out=w13, in0=v3, in1=mean_bc, op=ALU.subtract), reads=[vk, sk], writes=[w1k])
                        p.op("dve", lambda: nc.vector.tensor_tensor(out=w13, in0=w13, in1=rstd_bc, op=ALU.mult), reads=[w1k, sk], writes=[w1k])
                        vn, vnk = VN.next()
                        p.op("pool", lambda: nc.gpsimd.tensor_tensor(out=w1[:], in0=w1[:], in1=vecA[:, 0, :], op=ALU.mult), reads=[w1k, "vecA"], writes=[w1k])
                        p.op("pool", lambda: nc.gpsimd.tensor_tensor(out=vn[:], in0=w1[:], in1=vecA[:, 1, :], op=ALU.add), reads=[w1k, "vecA"], writes=[vnk])
                        for g in range(8):
                            p.op("pe", lambda g=g: nc.tensor.matmul(psb[4][:, g * 64:(g + 1) * 64], lhsT=wcT[:, g, :], rhs=vn[:, g * 64:(g + 1) * 64],
                                                                     start=True, stop=True), reads=["wcT", vnk], writes=[PS(4)])
                        w2, w2k = W2.next()
                        w23 = w2[:].rearrange("p (g c) -> p g c", g=8)
                        bs_bc = sgub[:, :].unsqueeze(2).to_broadcast([128, 8, 64])
                        p.op("dve", lambda: nc.vector.tensor_tensor(out=w23, in0=psb[4][:].rearrange("p (g c) -> p g c", g=8), in1=bs_bc, op=ALU.add),
                             reads=[PS(4), "sgub"], writes=[w2k])
                        p.op("dve", lambda: nc.vector.tensor_tensor(out=w2[:], in0=w2[:], in1=u[:], op=ALU.mult), reads=[w2k, uk], writes=[w2k])
                        w1b, w1bk = W1.next()
                        p.op("act", lambda: nc.scalar.activation(out=w1b[:], in_=w2[:], func=AF.Square, accum_out=ssq_a[:, i:i + 1]), reads=[w2k], writes=[w1bk, "ssq_a"])
                        ab, abk = AB.next()
                        p.op("pool", lambda: nc.gpsimd.tensor_tensor(out=ab[:], in0=w2[:], in1=vecA[:, 2, :], op=ALU.mult), reads=[w2k, "vecA"], writes=[abk])
                        ptr = psb[5][:].bitcast(BF16)
                        for c in range(4):
                            p.op("pe", lambda c=c: nc.tensor.transpose(ptr[:, c * 128:(c + 1) * 128], ab[:, c * 128:(c + 1) * 128], ident_bf),
                                 reads=[abk, "consts_bf"], writes=[PS(5)])
                        p.op("act", lambda: nc.scalar.copy(out=aT[:, :, i * 128:(i + 1) * 128], in_=ptr[:, 0:512].rearrange("p (c t) -> p c t", c=4)),
                             reads=[PS(5)], writes=["aT"])
                        if "dbg_a" in dram:
                            dma("sp", dram["dbg_a"][i * 128:(i + 1) * 128, :], w2[:], reads=[w2k])
                    ev = 0
                    for hp in range(4 if stage >= 2 else 0):
                        for tc in range(4):
                            b = 6 + (ev % 2)
                            for k in range(8):
                                p.op("pe", lambda k=k, b=b: nc.tensor.matmul(
                                    psb[b][:], lhsT=win[:, k, 1024 + hp * 128:1024 + (hp + 1) * 128],
                                    rhs=xTo[:, k, tc * 512:(tc + 1) * 512], start=(k == 0), stop=(k == 7)),
                                    reads=[xok[k], wk[k]], writes=[PS(b)])
                            if ev % 2 == 0:
                                p.op("act", lambda b=b: nc.scalar.copy(out=qT[:, hp, tc * 512:(tc + 1) * 512], in_=psb[b][:]), reads=[PS(b)], writes=[("qT", hp)])
                            else:
                                p.op("dve", lambda b=b: nc.vector.tensor_copy(out=qT[:, hp, tc * 512:(tc + 1) * 512], in_=psb[b][:]), reads=[PS(b)], writes=[("qT", hp)])
                            ev += 1
                    if "dbg_ssq" in dram:
                        dma("sp", dram["dbg_ssq"], ssq_a[:], reads=["ssq_a"])
                    p.barrier()
            with ExitStack() as esD:
                for nm, t_, rk in (("dbg_aT", aT, ["aT"]), ("dbg_qT", qT, [("qT", h_) for h_ in range(4)])):
                    if nm in dram:
                        tmpf = sbuf(esD, nm, [128, 4, NTOK], F32)
                        p.op("dve", lambda: nc.vector.tensor_copy(out=tmpf[:], in_=t_[:]), reads=rk, writes=[nm])
                        dma("sp", dram[nm], tmpf[:], reads=[nm])
                p.barrier()
            with ExitStack() as esT:
                vaugp = [sbuf(esT, f"vaugp{i}", [128, nv, 2, 66], BF16) for i in range(2)]
                for i_ in range(2):
                    p.op("pool", lambda i_=i_: nc.gpsimd.memset(vaugp[i_][:, :, :, 64:66], 1.0), writes=[("vones", i_)])
                accT = [sbuf(esT, f"accT{i}", [65, NTOK], F32) for i in range(2)]
                Eb = SRot(esT, "Eb", 3, [128, 512], BF16)
                Em = SRot(esT, "Em", 3, [128, 512], BF16)
                RD = SRot(esT, "RD", 2, [64, 512], F32)
                BQ = SRot(esT, "BQ", 2, [64, 512], F32)
                BS = SRot(esT, "BS", 2, [64, 512], F32)
                masks = sbuf(esT, "masks", [128, 3, 512], BF16)
                mprev = consts_bf[:, C_MPREV:C_MPREV + 128]
                mprevc = consts_bf[:, C_MPREVC:C_MPREVC + 128]
                mcur = consts_bf[:, C_TRI:C_TRI + 128]
                for mi, pat in enumerate(((mprev, mcur, mprev, mcur), (mprevc, mcur, mprev, mcur), (mprevc, mcur, mprevc, mcur))):
                    for j, src in enumerate(pat):
                        p.op("dve", lambda mi=mi, j=j, src=src: nc.vector.tensor_copy(out=masks[:, mi, j * 128:(j + 1) * 128], in_=src),
                             reads=["consts_bf"], writes=["masks"])
                sel_f = consts[0:65, C_SEL:C_SEL + 64]
                ones_f = consts[0:64, C_ONES:C_ONES + 1]
                gb64 = sbuf(esT, "gb64", [64, 8], F32)
                dma("sp", gb64[:], dram["mix_gb64"], writes=["gb64"])
                allv = [(d_, n, r) for d_ in PATTERNS for (n, r) in vt[d_]]
                tgc = [0]

                def build_v(hp):
                    vb = vaugp[hp % 2]
                    vbk = ("vaugp", hp % 2)
                    for g0 in range(0, nv, 8):
                        grp = allv[g0:g0 + 8]
                        tg = tgc[0]
                        tgc[0] += 1
                        b = 6 + (tg % 2)
                        ptr = psb[b][:].bitcast(BF16)
                        for j, (d_, n, r) in enumerate(grp):
                            span = 128 * d_
                            s0 = span * n + r
                            p.op("pe", lambda j=j, s0=s0, span=span, d_=d_, ptr=ptr: nc.tensor.transpose(
                                ptr[:, j * 128:(j + 1) * 128], vT[:, hp, s0:s0 + span - d_ + 1:d_], ident_bf),
                                reads=[("vT", hp), "consts_bf"], writes=[PS(b)])
                        ng = len(grp)
                        src = ptr[:, 0:ng * 128].rearrange("p (t h c) -> p t h c", t=ng, h=2)
                        if tg % 2 == 0:
                            p.op("act", lambda src=src, g0=g0, ng=ng: nc.scalar.copy(out=vb[:, g0:g0 + ng, :, 0:64], in_=src), reads=[PS(b)], writes=[vbk])
                        else:
                            p.op("dve", lambda src=src, g0=g0, ng=ng: nc.vector.tensor_copy(out=vb[:, g0:g0 + ng, :, 0:64], in_=src), reads=[PS(b)], writes=[vbk])

                def unit_list(h):
                    us = []
                    for d_ in PATTERNS:
                        if d_ == 1:
                            blocks = [(n, 0) for n in range(16, 32)]
                        elif d_ == 4:
                            blocks = [(n, r) for n in range(4, 8) for r in range(4)]
                        else:
                            blocks = [(1, r) for r in range(16)]
                        for ui in range(0, 16, 2):
                            us.append((h, d_, blocks[ui:ui + 2]))
                    return us

                def emit_qk(u, su):
                    h, d_, pair = u
                    hp, hh = h // 2, h % 2
                    po = 64 * hh
                    span = 128 * d_
                    sb_ = su % 2
                    for j, (n, r) in enumerate(pair):
                        q0 = span * n + r - NTOK
                        qs = qT[po:po + 64, hp, q0:q0 + span - d_ + 1:d_]
                        kp0 = span * (n - 1) + r
                        kc0 = span * n + r
                        p.op("pe", lambda j=j, qs=qs, kp0=kp0: nc.tensor.matmul(
                            psb[sb_][:, j * 256:j * 256 + 128], lhsT=kT[po:po + 64, hp, kp0:kp0 + span - d_ + 1:d_], rhs=qs, start=True, stop=True),
                            reads=[("kT", hp), ("qT", hp)], writes=[PS(sb_)])
                        p.op("pe", lambda j=j, qs=qs, kc0=kc0: nc.tensor.matmul(
                            psb[sb_][:, j * 256 + 128:j * 256 + 256], lhsT=kT[po:po + 64, hp, kc0:kc0 + span - d_ + 1:d_], rhs=qs, start=True, stop=True),
                            reads=[("kT", hp), ("qT", hp)], writes=[PS(sb_)])

                def emit_rest(u, su):
                    h, d_, pair = u
                    hp, hh = h // 2, h % 2
                    vb = vaugp[hp % 2]
                    vbk = ("vaugp", hp % 2)
                    acc = accT[h % 2]
                    acck = ("accT", h % 2)
                    span = 128 * d_
                    sb_ = su % 2
                    ob_ = 2 + su % 2
                    ctx = tuple((span * (n - 1) + r) < NTOK for (n, r) in pair)
                    mi = {(False, False): 0, (True, False): 1, (True, True): 2}[ctx]
                    eb, ebk = Eb.next()
                    em, emk = Em.next()
                    p.op("act", lambda: nc.scalar.activation(out=eb[:], in_=psb[sb_][:], func=AF.Exp, scale=0.125), reads=[PS(sb_)], writes=[ebk])
                    p.op("dve", lambda: nc.vector.tensor_tensor(out=em[:], in0=eb[:], in1=masks[:, mi, :], op=ALU.mult), reads=[ebk, "masks"], writes=[emk])
                    for j, (n, r) in enumerate(pair):
                        vp = vidx[(d_, n - 1, r)]
                        vc = vidx[(d_, n, r)]
                        p.op("pe", lambda j=j, vp=vp: nc.tensor.matmul(
                            psb[ob_][0:65, j * 128:(j + 1) * 128], lhsT=vb[:, vp, hh, 0:65], rhs=em[:, j * 256:j * 256 + 128], start=True, stop=False),
                            reads=[vbk, ("vones", hp % 2), emk], writes=[PS(ob_)])
                        p.op("pe", lambda j=j, vc=vc: nc.tensor.matmul(
                            psb[ob_][0:65, j * 128:(j + 1) * 128], lhsT=vb[:, vc, hh, 0:65], rhs=em[:, j * 256 + 128:j * 256 + 256], start=False, stop=True),
                            reads=[vbk, ("vones", hp % 2), emk], writes=[PS(ob_)])
                    n0, r0 = pair[0]
                    src = psb[ob_][0:65, 0:256].rearrange("p (b j) -> p b j", b=2)
                    if d_ == 1:
                        q0 = span * n0 - NTOK
                        dest = acc[0:65, q0:q0 + 256].rearrange("p (b j) -> p b j", b=2)
                        p.op("act", lambda: nc.scalar.copy(out=dest, in_=src), reads=[PS(ob_)], writes=[acck])
                    else:
                        s0 = span * n0 - NTOK
                        dest = acc[0:65, s0:s0 + span].rearrange("p (j d) -> p d j", d=d_)[:, r0:r0 + 2, :]
                        p.op("dve", lambda: nc.vector.tensor_tensor(out=dest, in0=dest, in1=src, op=ALU.add), reads=[PS(ob_), acck], writes=[acck])

                def finalize(h):
                    hp, hh = h // 2, h % 2
                    po = 64 * hh
                    acc = accT[h % 2]
                    acck = ("accT", h % 2)
                    for c in range(4):
                        p.op("pe", lambda c=c: nc.tensor.matmul(psb[4][0:64, :], lhsT=sel_f, rhs=acc[0:65, c * 512:(c + 1) * 512], start=True, stop=True),
                             reads=["consts", acck], writes=[PS(4)])
                        rd, rdk = RD.next()
                        bq_, bqk = BQ.next()
                        bs_, bsk = BS.next()
                        p.op("dve", lambda rd=rd: nc.vector.reciprocal(out=rd[:], in_=psb[4][0:64, :]), reads=[PS(4)], writes=[rdk])
                        p.op("dve", lambda c=c, rd=rd, bq_=bq_: nc.vector.tensor_tensor(out=bq_[:], in0=acc[0:64, c * 512:(c + 1) * 512], in1=rd[:], op=ALU.mult),
                             reads=[acck, rdk], writes=[bqk])
                        p.op("act", lambda bq_=bq_, bs_=bs_: nc.scalar.activation(out=bs_[:], in_=bq_[:], func=AF.Square), reads=[bqk], writes=[bsk])
                        for t in range(4):
                            ti = c * 4 + t
                            col = ti * 8 + h
                            p.op("pe", lambda t=t, col=col, bs_=bs_: nc.tensor.matmul(psb[5][:, col:col + 1], lhsT=bs_[0:64, t * 128:(t + 1) * 128], rhs=ones_f, start=True, stop=True),
                                 reads=[bsk, "consts"], writes=[PS(5)])
                        p.op("pool", lambda c=c, bq_=bq_: nc.gpsimd.tensor_scalar(out=bT[po:po + 64, hp, c * 512:(c + 1) * 512], in0=bq_[:], scalar1=gb64[:, h:h + 1], scalar2=None, op0=ALU.mult),
                             reads=[bqk, "gb64"], writes=["bT"])
                        if "dbg_b" in dram:
                            dma("sp", dram["dbg_b"][h * 64:(h + 1) * 64, c * 512:(c + 1) * 512], bq_[:], reads=[bqk])

                if stage >= 3:
                    units = []
                    for h in range(8):
                        units += unit_list(h)
                    build_v(0)
                    emit_qk(units[0], 0)
                    for ui, u in enumerate(units):
                        if ui + 1 < len(units):
                            emit_qk(units[ui + 1], ui + 1)
                        emit_rest(u, ui)
                        h = u[0]
                        last_of_head = (ui + 1 == len(units)) or units[ui + 1][0] != h
                        if last_of_head:
                            finalize(h)
                            if h % 2 == 0 and h // 2 + 1 < 4:
                                build_v(h // 2 + 1)
                if stage >= 3:
                    p.op("dve", lambda: nc.vector.tensor_copy(out=ssq_b[:].rearrange("p t h -> p (t h)"), in_=psb[5][:, 0:128]), reads=[PS(5)], writes=["ssq_b"])
                    if "dbg_ssqb" in dram:
                        dma("sp", dram["dbg_ssqb"], ssq_b[:].rearrange("p t h -> p (t h)"), reads=["ssq_b"])
                p.barrier()
        scat_keys = []
        hbf_keys = ["hbf_z"]
        h32_keys = []
        with ExitStack() as es3:
            zrow = sbuf(es3, "zrow", [1, D], F32)
            zrow_bf = sbuf(es3, "zrow_bf", [1, D], BF16)
            p.op("dve", lambda: nc.vector.memset(zrow[:], 0.0), writes=["zrow"])
            p.op("dve", lambda: nc.vector.memset(zrow_bf[:], 0.0), writes=["zrow_bf"])
            dma("sp", hbf_d[NTOK:NTOK + 1, :], zrow_bf[:], reads=["zrow_bf"], writes=["hbf_z"])
            dma("sp", eo_d[SLOTS:SLOTS + 1, :], zrow[:], reads=["zrow"], writes=["eo_z"])
            wout = sbuf(es3, "wout", [128, 8, D], BF16)
            for k in range(8):
                dma("pool", wout[:, k, :], wout_d[k * 128:(k + 1) * 128, :], writes=[("wout", k)])
            vec3 = sbuf(es3, "vec3", [128, 2, D], F32)
            dma("sp", vec3[:], vec_d[:, 2:4, :], writes=["vec3"])
            wr = sbuf(es3, "wr", [128, 8, NE], F32)
            dma("sp", wr[:], wr_d.rearrange("(k p) e -> p k e", p=128), writes=["wr"])
            brb = sbuf(es3, "brb", [128, NE], F32)
            dma("sp", brb[:], br_d, writes=["brb"])
            rs = sbuf(es3, "rs", [128, 4, NT], F32)
            p.op("dve", lambda: nc.vector.tensor_reduce(out=rs[:, 2, :], in_=ssq_b[:], axis=AX.X, op=ALU.add), reads=["ssq_b"], writes=["rs"])
            p.op("act", lambda: nc.scalar.activation(out=rs[:, 3, :], in_=ssq_a[:], func=AF.Sqrt, bias=eps_t[:, 0:1], scale=1.0 / 512), reads=["ssq_a", "eps"], writes=["rs"])
            p.op("dve", lambda: nc.vector.reciprocal(out=rs[:, 0, :], in_=rs[:, 3, :]), reads=["rs"], writes=["rs"])
            p.op("act", lambda: nc.scalar.activation(out=rs[:, 3, :], in_=rs[:, 2, :], func=AF.Sqrt, bias=eps_t[:, 0:1], scale=1.0 / 512), reads=["rs", "eps"], writes=["rs"])
            p.op("dve", lambda: nc.vector.reciprocal(out=rs[:, 1, :], in_=rs[:, 3, :]), reads=["rs"], writes=["rs"])
            X = SRot(es3, "X", 2, [128, D], F32)
            T1 = SRot(es3, "T1", 2, [128, D], F32)
            T2 = SRot(es3, "T2", 2, [128, D], F32)
            HB = SRot(es3, "HB", 2, [128, D], BF16)
            HT = SRot(es3, "HT", 2, [128, 8, 128], F32)
            SM = SRot(es3, "SM", 2, [128, 16], F32)
            LG = SRot(es3, "LG", 2, [128, NE], F32)
            RK = SRot(es3, "RK", 2, [128, 6, NE], F32)
            OH = SRot(es3, "OH", 2, [128, 4, NE], F32)
            MX = SRot(es3, "MX", 2, [128, 24], F32)

            def layer_norm(src, srck, dst, dstk, g_ap, b_ap, gk, jt, jtk):
                sm, smk = SM.next()
                p.op("dve", lambda: nc.vector.memset(sm[:], 0.0), writes=[smk])
                p.op("act", lambda: nc.scalar.activation(out=jt[:], in_=src[:], func=AF.Identity, accum_out=sm[:, 0:1]), reads=[srck, smk], writes=[jtk, smk])
                p.op("act", lambda: nc.scalar.activation(out=jt[:], in_=src[:], func=AF.Square, accum_out=sm[:, 1:2]), reads=[srck, smk], writes=[jtk, smk])
                p.op("dve", lambda: nc.vector.tensor_single_scalar(out=sm[:, 2:3], in_=sm[:, 0:1], scalar=1.0 / D, op=ALU.mult), reads=[smk], writes=[smk])
                p.op("dve", lambda: nc.vector.tensor_tensor(out=sm[:, 3:4], in0=sm[:, 2:3], in1=sm[:, 2:3], op=ALU.mult), reads=[smk], writes=[smk])
                p.op("dve", lambda: nc.vector.scalar_tensor_tensor(out=sm[:, 4:5], in0=sm[:, 1:2], scalar=1.0 / D, in1=sm[:, 3:4], op0=ALU.mult, op1=ALU.subtract), reads=[smk], writes=[smk])
                p.op("act", lambda: nc.scalar.activation(out=sm[:, 5:6], in_=sm[:, 4:5], func=AF.Sqrt, bias=eps_t[:, 0:1], scale=1.0), reads=[smk, "eps"], writes=[smk])
                p.op("dve", lambda: nc.vector.reciprocal(out=sm[:, 6:7], in_=sm[:, 5:6]), reads=[smk], writes=[smk])
                p.op("dve", lambda: nc.vector.tensor_scalar(out=jt[:], in0=src[:], scalar1=sm[:, 2:3], scalar2=sm[:, 6:7], op0=ALU.subtract, op1=ALU.mult), reads=[srck, smk], writes=[jtk])
                p.op("pool", lambda: nc.gpsimd.tensor_tensor(out=jt[:], in0=jt[:], in1=g_ap, op=ALU.mult), reads=[jtk, gk], writes=[jtk])
                p.op("pool", lambda: nc.gpsimd.tensor_tensor(out=dst[:], in0=jt[:], in1=b_ap, op=ALU.add), reads=[jtk, gk], writes=[dstk])

            for i in range(NT if stage >= 4 else 0):
                tsl = slice(i * 128, (i + 1) * 128)
                x_, xk_ = X.next()
                dma("sp", x_[:], x_d[tsl, :], writes=[xk_])
                for half in range(2):
                    for c in range(4):
                        p.op("pe", lambda half=half, c=c: nc.tensor.matmul(psb[half][:], lhsT=aT[:, c, tsl], rhs=wout[:, c, half * 512:(half + 1) * 512],
                                                                       start=(c == 0), stop=(c == 3)), reads=["aT", ("wout", c)], writes=[PS(half)])
                    for c in range(4):
                        p.op("pe", lambda half=half, c=c: nc.tensor.matmul(psb[2 + half][:], lhsT=bT[:, c, tsl], rhs=wout[:, 4 + c, half * 512:(half + 1) * 512],
                                                                       start=(c == 0), stop=(c == 3)), reads=["bT", ("wout", 4 + c)], writes=[PS(2 + half)])
                t1, t1k = T1.next()
                t2, t2k = T2.next()
                p.op("act", lambda: nc.scalar.mul(out=x_[:], in_=x_[:], mul=ALPHA), reads=[xk_], writes=[xk_])
                for half in range(2):
                    hs = slice(half * 512, (half + 1) * 512)
                    p.op("dve", lambda half=half, hs=hs: nc.vector.scalar_tensor_tensor(out=t1[:, hs], in0=psb[half][:], scalar=rs[:, 0, i:i + 1], in1=x_[:, hs], op0=ALU.mult, op1=ALU.add),
                         reads=[PS(half), "rs", xk_], writes=[t1k])
                    p.op("dve", lambda half=half, hs=hs: nc.vector.scalar_tensor_tensor(out=t1[:, hs], in0=psb[2 + half][:], scalar=rs[:, 1, i:i + 1], in1=t1[:, hs], op0=ALU.mult, op1=ALU.add),
                         reads=[PS(2 + half), "rs", t1k], writes=[t1k])
                h_, hk_ = X.next()
                layer_norm(t1, t1k, h_, hk_, vec3[:, 0, :], vec3[:, 1, :], "vec3", t2, t2k)
                dma("sp", h32_d[tsl, :], h_[:], reads=[hk_], writes=[("h32", i)])
                h32_keys.append(("h32", i))
                hb, hbk = HB.next()
                p.op("act", lambda: nc.scalar.copy(out=hb[:], in_=h_[:]), reads=[hk_], writes=[hbk])
                dma("sp", hbf_d[tsl, :], hb[:], reads=[hbk], writes=[("hbf", i)])
                hbf_keys.append(("hbf", i))
                if "dbg_h" in dram:
                    dma("sp", dram["dbg_h"][tsl, :], h_[:], reads=[hk_])
                ht, htk = HT.next()
                for k in range(8):
                    b = 4 + k // 4
                    p.op("pe", lambda k=k, b=b: nc.tensor.transpose(psb[b][:, (k % 4) * 128:(k % 4 + 1) * 128], h_[:, k * 128:(k + 1) * 128], ident_f),
                         reads=[hk_, "consts"], writes=[PS(b)])
                p.op("act", lambda: nc.scalar.copy(out=ht[:, 0:4, :], in_=psb[4][:].rearrange("p (k t) -> p k t", k=4)), reads=[PS(4)], writes=[htk])
                p.op("dve", lambda: nc.vector.tensor_copy(out=ht[:, 4:8, :], in_=psb[5][:].rearrange("p (k t) -> p k t", k=4)), reads=[PS(5)], writes=[htk])
                for k in range(8):
                    p.op("pe", lambda k=k: nc.tensor.matmul(psb[6][:, 0:NE], lhsT=ht[:, k, :], rhs=wr[:, k, :], start=(k == 0), stop=(k == 7)),
                         reads=[htk, "wr"], writes=[PS(6)])
                lg, lgk = LG.next()
                p.op("dve", lambda: nc.vector.tensor_tensor(out=lg[:], in0=psb[6][:, 0:NE], in1=brb[:], op=ALU.add), reads=[PS(6), "brb"], writes=[lgk])
                if "dbg_logits" in dram:
                    dma("sp", dram["dbg_logits"][tsl, :], lg[:], reads=[lgk])
                mx, mxk = MX.next()
                p.op("dve", lambda: nc.vector.max(out=mx[:, 0:8], in_=lg[:]), reads=[lgk], writes=[mxk])
                rk, rkk = RK.next()
                p.op("dve", lambda: nc.vector.tensor_scalar(out=rk[:, 0, :], in0=lg[:], scalar1=mx[:, 3:4], scalar2=None, op0=ALU.is_ge), reads=[lgk, mxk], writes=[rkk])
                p.op("act", lambda: nc.scalar.copy(out=Mall[:, i, :], in_=rk[:, 0, :]), reads=[rkk], writes=[("Mall", i)])
                p.op("dve", lambda: nc.vector.tensor_single_scalar(out=mx[:, 8:9], in_=mx[:, 0:1], scalar=-1.0, op=ALU.mult), reads=[mxk], writes=[mxk])
                p.op("dve", lambda: nc.vector.memset(mx[:, 9:10], 0.0), reads=[], writes=[mxk])
                p.op("act", lambda: nc.scalar.activation(out=mx[:, 12:16], in_=mx[:, 0:4], func=AF.Exp, bias=mx[:, 8:9], scale=1.0, accum_out=mx[:, 9:10]), reads=[mxk], writes=[mxk])
                p.op("dve", lambda: nc.vector.reciprocal(out=mx[:, 10:11], in_=mx[:, 9:10]), reads=[mxk], writes=[mxk])
                p.op("dve", lambda: nc.vector.tensor_scalar(out=mx[:, 16:20], in0=mx[:, 12:16], scalar1=mx[:, 10:11], scalar2=None, op0=ALU.mult), reads=[mxk], writes=[mxk])
                for i2 in range(i + 1):
                    lhs = consts_bf[:, C_LSTRICT:C_LSTRICT + 128] if i2 == i else consts_bf[:, C_ONES:C_ONES + 128]
                    p.op("pe", lambda i2=i2, lhs=lhs: nc.tensor.matmul(psb[7][:, 0:NE], lhsT=lhs, rhs=Mall[:, i2, :], start=(i2 == 0), stop=(i2 == i)),
                         reads=[("Mall", i2), "consts_bf"], writes=[PS(7)])
                p.op("dve", lambda: nc.vector.tensor_copy(out=rk[:, 1, :], in_=psb[7][:, 0:NE]), reads=[PS(7)], writes=[rkk])
                p.op("dve", lambda: nc.vector.scalar_tensor_tensor(out=rk[:, 2, :], in0=consts[:, C_IOTA:C_IOTA + NE], scalar=float(CAP), in1=rk[:, 1, :], op0=ALU.mult, op1=ALU.add),
                     reads=["consts", rkk], writes=[rkk])
                oh, ohk = OH.next()
                lg_bc = lg[:, :].unsqueeze(1).to_broadcast([128, 4, NE])
                mx_bc = mx[:, 0:4].unsqueeze(2).to_broadcast([128, 4, NE])
                p.op("dve", lambda: nc.vector.tensor_tensor(out=oh[:], in0=lg_bc, in1=mx_bc, op=ALU.is_equal), reads=[lgk, mxk], writes=[ohk])
                oh2, oh2k = OH.next()
                p.op("dve", lambda: nc.vector.tensor_tensor(out=oh2[:], in0=oh[:], in1=rk[:, 1, :].unsqueeze(1).to_broadcast([128, 4, NE]), op=ALU.mult), reads=[ohk, rkk], writes=[oh2k])
                p.op("dve", lambda: nc.vector.tensor_reduce(out=rk[:, 3, 0:4], in_=oh2[:], axis=AX.X, op=ALU.add), reads=[oh2k], writes=[rkk])
                p.op("dve", lambda: nc.vector.tensor_tensor(out=oh2[:], in0=oh[:], in1=rk[:, 2, :].unsqueeze(1).to_broadcast([128, 4, NE]), op=ALU.mult), reads=[ohk, rkk], writes=[oh2k])
                p.op("dve", lambda: nc.vector.tensor_reduce(out=rk[:, 3, 4:8], in_=oh2[:], axis=AX.X, op=ALU.add), reads=[oh2k], writes=[rkk])
                p.op("dve", lambda: nc.vector.tensor_scalar(out=rk[:, 3, 8:12], in0=rk[:, 3, 0:4], scalar1=float(CAP), scalar2=None, op0=ALU.is_lt), reads=[rkk], writes=[rkk])
                p.op("dve", lambda: nc.vector.scalar_tensor_tensor(out=rk[:, 3, 12:16], in0=rk[:, 3, 4:8], scalar=-float(SLOTS), in1=rk[:, 3, 8:12], op0=ALU.add, op1=ALU.mult), reads=[rkk], writes=[rkk])
                p.op("dve", lambda: nc.vector.tensor_scalar(out=rk[:, 3, 16:20], in0=rk[:, 3, 12:16], scalar1=float(SLOTS), scalar2=None, op0=ALU.add), reads=[rkk], writes=[rkk])
                p.op("dve", lambda: nc.vector.tensor_copy(out=pos_i[:, i * 4:i * 4 + 4], in_=rk[:, 3, 16:20]), reads=[rkk], writes=[("pos", i)])
                p.op("dve", lambda: nc.vector.tensor_tensor(out=gates[:, i, :], in0=mx[:, 16:20], in1=rk[:, 3, 8:12], op=ALU.mult), reads=[mxk, rkk], writes=[("gates", i)])
                if "dbg_pos" in dram:
                    dma("sp", dram["dbg_pos"][tsl, :], rk[:, 3, 16:20], reads=[rkk])
                    dma("sp", dram["dbg_gates"][tsl, :], gates[:, i, :], reads=[("gates", i)])
                for k in range(4):
                    p.op("pool", lambda k=k: nc.gpsimd.indirect_dma_start(
                        out=stok_d[:, :], out_offset=bass.IndirectOffsetOnAxis(ap=pos_i[:, i * 4 + k:i * 4 + k + 1], axis=0),
                        in_=tokid_i[:, i:i + 1], in_offset=None),
                        reads=[("pos", i), "tokid_i", "stok_init"], writes=[("scat", i, k)], dma=True)
                    scat_keys.append(("scat", i, k))
            p.barrier()
        esAB.close()
        eo_keys = ["eo_z"]
        with ExitStack() as es5:
            bgT = sbuf(es5, "bgT", [128, NE, 8], F32)
            buT = sbuf(es5, "buT", [128, NE, 8], F32)
            dma("sp", bgT[:], bg_d, writes=["bgT"])
            dma("sp", buT[:], bu_d, writes=["buT"])
            bu1 = sbuf(es5, "bu1", [128, NE, 8], F32)
            p.op("dve", lambda: nc.vector.tensor_scalar(out=bu1[:], in0=buT[:], scalar1=1.0, scalar2=None, op0=ALU.add), reads=["buT"], writes=["bu1"])
            WG = [sbuf(es5, f"WG{i}", [128, 8, D], BF16) for i in range(2)]
            WU = [sbuf(es5, f"WU{i}", [128, 8, D], BF16) for i in range(2)]
            WD = [sbuf(es5, f"WD{i}", [128, 8, D], BF16) for i in range(2)]
            BD = [sbuf(es5, f"BD{i}", [128, D], F32) for i in range(2)]
            XB = [sbuf(es5, f"XB{i}", [128, NBLK, D], BF16) for i in range(2)]
            STI = [sbuf(es5, f"STI{i}", [128, NBLK], I32) for i in range(2)]
            XT = [sbuf(es5, f"XT{i}", [128, 8, CAP], BF16) for i in range(2)]
            ACT_T = sbuf(es5, "ACTT", [128, 8, CAP], BF16)
            GC = SRot(es5, "GC", 2, [128, CAP], F32)
            SG = SRot(es5, "SG", 2, [128, CAP], F32)
            UC = SRot(es5, "UC", 2, [128, CAP], F32)
            EO = SRot(es5, "EO", 2, [128, D], F32)

            def load_w(e):
                j = e % 2
                for (W_, wd_, nm) in ((WG, wg_d, "WG"), (WU, wu_d, "WU"), (WD, wd_d, "WD")):
                    for hk in range(2):
                        dma("pool", W_[j][:, hk * 4:(hk + 1) * 4, :], wd_[e, hk * 512:(hk + 1) * 512, :].rearrange("(k p) f -> p k f", p=128), writes=[(nm, j, hk)])
                dma("sp", BD[j][:], bd_d[e:e + 1, :].to_broadcast([128, D]), writes=[("BD", j)])

            def load_x(e):
                j = e % 2
                dma("sp", STI[j][:], stok_d[e * CAP:(e + 1) * CAP, :].rearrange("(p b) o -> p (b o)", p=128), reads=scat_keys + ["stok_init"], writes=[("STI", j)])
                for blk in range(NBLK):
                    p.op("pool", lambda blk=blk: nc.gpsimd.indirect_dma_start(
                        out=XB[j][:, blk, :], out_offset=None, in_=hbf_d[:, :],
                        in_offset=bass.IndirectOffsetOnAxis(ap=STI[j][:, blk:blk + 1], axis=0)),
                        reads=[("STI", j)] + hbf_keys, writes=[("XB", j, blk)], dma=True)

            nexp = NE if stage >= 5 else 0
            if nexp:
                load_x(0)
                load_w(0)
            tr = 0
            for e in range(nexp):
                j = e % 2
                if e + 1 < nexp:
                    load_x(e + 1)
                    load_w(e + 1)
                for blk in range(NBLK):
                    b = 6 + tr % 2
                    tr += 1
                    ptr = psb[b][:].bitcast(BF16)
                    for k in range(8):
                        p.op("pe", lambda blk=blk, k=k, ptr=ptr: nc.tensor.transpose(ptr[:, k * 128:(k + 1) * 128], XB[j][:, blk, k * 128:(k + 1) * 128], ident_bf),
                             reads=[("XB", j, blk), "consts_bf"], writes=[PS(b)])
                    src = ptr[:, :].rearrange("p (k t) -> p k t", k=8)
                    if blk % 2 == 0:
                        p.op("act", lambda blk=blk, src=src: nc.scalar.copy(out=XT[j][:, :, blk * 128:(blk + 1) * 128], in_=src), reads=[PS(b)], writes=[("XT", j)])
                    else:
                        p.op("dve", lambda blk=blk, src=src: nc.vector.tensor_copy(out=XT[j][:, :, blk * 128:(blk + 1) * 128], in_=src), reads=[PS(b)], writes=[("XT", j)])
                for f in range(8):
                    gb_ = f % 2
                    ub_ = 2 + f % 2
                    for k in range(8):
                        p.op("pe", lambda f=f, k=k: nc.tensor.matmul(psb[gb_][:, 0:CAP], lhsT=WG[j][:, k, f * 128:(f + 1) * 128], rhs=XT[j][:, k, :], start=(k == 0), stop=(k == 7)),
                             reads=[("WG", j, k // 4), ("XT", j)], writes=[PS(gb_)])
                    for k in range(8):
                        p.op("pe", lambda f=f, k=k: nc.tensor.matmul(psb[ub_][:, 0:CAP], lhsT=WU[j][:, k, f * 128:(f + 1) * 128], rhs=XT[j][:, k, :], start=(k == 0), stop=(k == 7)),
                             reads=[("WU", j, k // 4), ("XT", j)], writes=[PS(ub_)])
                    gc, gck = GC.next()
                    sg, sgk = SG.next()
                    uc, uck = UC.next()
                    p.op("dve", lambda f=f, gc=gc: nc.vector.tensor_scalar(out=gc[:], in0=psb[gb_][:, 0:CAP], scalar1=bgT[:, e, f:f + 1], scalar2=7.0, op0=ALU.add, op1=ALU.min),
                         reads=[PS(gb_), "bgT"], writes=[gck])
                    p.op("act", lambda gc=gc, sg=sg: nc.scalar.activation(out=sg[:], in_=gc[:], func=AF.Gelu_apprx_sigmoid), reads=[gck], writes=[sgk])
                    p.op("dve", lambda f=f, uc=uc: nc.vector.tensor_scalar(out=uc[:], in0=psb[ub_][:, 0:CAP], scalar1=bu1[:, e, f:f + 1], scalar2=-6.0, op0=ALU.add, op1=ALU.max),
                         reads=[PS(ub_), "bu1"], writes=[uck])
                    p.op("dve", lambda f=f, uc=uc, sg=sg: nc.vector.scalar_tensor_tensor(out=ACT_T[:, f, :], in0=uc[:], scalar=8.0, in1=sg[:], op0=ALU.min, op1=ALU.mult),
                         reads=[uck, sgk], writes=[("ACTT", f)])
                for blk in range(NBLK):
                    eo, eok = EO.next()
                    for half in range(2):
                        db_ = 4 + half
                        for f in range(8):
                            p.op("pe", lambda blk=blk, half=half, f=f: nc.tensor.matmul(psb[db_][:], lhsT=ACT_T[:, f, blk * 128:(blk + 1) * 128], rhs=WD[j][:, f, half * 512:(half + 1) * 512],
                                                                                     start=(f == 0), stop=(f == 7)),
                                 reads=[("ACTT", f), ("WD", j, f // 4)], writes=[PS(db_)])
                        p.op("dve", lambda half=half, eo=eo: nc.vector.tensor_tensor(out=eo[:, half * 512:(half + 1) * 512], in0=psb[db_][:], in1=BD[j][:, half * 512:(half + 1) * 512], op=ALU.add),
                             reads=[PS(db_), ("BD", j)], writes=[eok])
                    dma("sp", eo_d[e * CAP:(e + 1) * CAP, :].rearrange("(p b) d -> p b d", b=NBLK)[:, blk, :], eo[:], reads=[eok], writes=[("eo", e, blk)])
                    eo_keys.append(("eo", e, blk))
            p.barrier()

        with ExitStack() as es6:
            vec6 = sbuf(es6, "vec6", [128, 2, D], F32)
            dma("sp", vec6[:], vec_d[:, 4:6, :], writes=["vec6"])
            Hh = SRot(es6, "Hh", 2, [128, D], F32)
            G4 = SRot(es6, "G4", 8, [128, D], F32)
            AC = SRot(es6, "AC", 2, [128, D], F32)
            JT = SRot(es6, "JT", 2, [128, D], F32)
            OT = SRot(es6, "OT", 2, [128, D], F32)
            SM = SRot(es6, "SM2", 2, [128, 16], F32)
            for i in range(NT if stage >= 6 else 0):
                tsl = slice(i * 128, (i + 1) * 128)
                hh, hhk = Hh.next()
                dma("sp", hh[:], h32_d[tsl, :], reads=h32_keys, writes=[hhk])
                gs = []
                for k in range(4):
                    g_, gk_ = G4.next()
                    p.op("pool", lambda k=k, g_=g_: nc.gpsimd.indirect_dma_start(
                        out=g_[:], out_offset=None, in_=eo_d[:, :],
                        in_offset=bass.IndirectOffsetOnAxis(ap=pos_i[:, i * 4 + k:i * 4 + k + 1], axis=0)),
                        reads=[("pos", i)] + eo_keys, writes=[gk_], dma=True)
                    gs.append((g_, gk_))
                ac, ack = AC.next()
                p.op("dve", lambda: nc.vector.tensor_scalar(out=ac[:], in0=gs[0][0][:], scalar1=gates[:, i, 0:1], scalar2=None, op0=ALU.mult), reads=[gs[0][1], ("gates", i)], writes=[ack])
                for k in range(1, 4):
                    p.op("dve", lambda k=k: nc.vector.scalar_tensor_tensor(out=ac[:], in0=gs[k][0][:], scalar=gates[:, i, k:k + 1], in1=ac[:], op0=ALU.mult, op1=ALU.add),
                         reads=[gs[k][1], ("gates", i), ack], writes=[ack])
                p.op("dve", lambda: nc.vector.scalar_tensor_tensor(out=ac[:], in0=hh[:], scalar=ALPHA, in1=ac[:], op0=ALU.mult, op1=ALU.add), reads=[hhk, ack], writes=[ack])
                if "dbg_pre2" in dram:
                    dma("sp", dram["dbg_pre2"][tsl, :], ac[:], reads=[ack])
                jt, jtk = JT.next()
                ot, otk = OT.next()
                sm, smk = SM.next()
                p.op("dve", lambda: nc.vector.memset(sm[:], 0.0), writes=[smk])
                p.op("act", lambda: nc.scalar.activation(out=jt[:], in_=ac[:], func=AF.Identity, accum_out=sm[:, 0:1]), reads=[ack, smk], writes=[jtk, smk])
                p.op("act", lambda: nc.scalar.activation(out=jt[:], in_=ac[:], func=AF.Square, accum_out=sm[:, 1:2]), reads=[ack, smk], writes=[jtk, smk])
                p.op("dve", lambda: nc.vector.tensor_single_scalar(out=sm[:, 2:3], in_=sm[:, 0:1], scalar=1.0 / D, op=ALU.mult), reads=[smk], writes=[smk])
                p.op("dve", lambda: nc.vector.tensor_tensor(out=sm[:, 3:4], in0=sm[:, 2:3], in1=sm[:, 2:3], op=ALU.mult), reads=[smk], writes=[smk])
                p.op("dve", lambda: nc.vector.scalar_tensor_tensor(out=sm[:, 4:5], in0=sm[:, 1:2], scalar=1.0 / D, in1=sm[:, 3:4], op0=ALU.mult, op1=ALU.subtract), reads=[smk], writes=[smk])
                p.op("act", lambda: nc.scalar.activation(out=sm[:, 5:6], in_=sm[:, 4:5], func=AF.Sqrt, bias=eps_t[:, 0:1], scale=1.0), reads=[smk, "eps"], writes=[smk])
                p.op("dve", lambda: nc.vector.reciprocal(out=sm[:, 6:7], in_=sm[:, 5:6]), reads=[smk], writes=[smk])
                p.op("dve", lambda: nc.vector.tensor_scalar(out=jt[:], in0=ac[:], scalar1=sm[:, 2:3], scalar2=sm[:, 6:7], op0=ALU.subtract, op1=ALU.mult), reads=[ack, smk], writes=[jtk])
                p.op("pool", lambda: nc.gpsimd.tensor_tensor(out=jt[:], in0=jt[:], in1=vec6[:, 0, :], op=ALU.mult), reads=[jtk, "vec6"], writes=[jtk])
                p.op("pool", lambda: nc.gpsimd.tensor_tensor(out=ot[:], in0=jt[:], in1=vec6[:, 1, :], op=ALU.add), reads=[jtk, "vec6"], writes=[otk])
                dma("sp", out_d[tsl, :], ot[:], reads=[otk], writes=[("out", i)])
        p.finish("sp")
    p.dram = dram
    return nc, p


def host_consts(flag):
    c = np.zeros((128, NCONST), np.float32)
    i = np.arange(128)
    c[:, C_IDENT:C_IDENT + 128] = np.eye(128, dtype=np.float32)
    c[:, C_TRI:C_TRI + 128] = (i[:, None] <= i[None, :])
    c[:, C_MPREV:C_MPREV + 128] = (i[:, None] >= i[None, :])
    c[:, C_MPREVC:C_MPREVC + 128] = flag * (i[:, None] >= i[None, :])
    c[:, C_LSTRICT:C_LSTRICT + 128] = (i[:, None] < i[None, :])
    c[:, C_ONES:C_ONES + 128] = 1.0
    c[:, C_IOTA:C_IOTA + 32] = np.arange(32)[None, :]
    c[:, C_TOKID:C_TOKID + 16] = np.arange(16)[None, :] * 128 + i[:, None]
    c[64, C_SEL:C_SEL + 64] = 1.0
    return c


def make_in_maps(inputs, cores=range(8)):
    f = lambda a: np.ascontiguousarray(np.asarray(a, dtype=np.float32))
    x = np.asarray(inputs["x"], dtype=np.float32)
    shared = {
        "w_in": f(inputs["w_in"][0]),
        "sgu_wT": f(np.transpose(inputs["sgu_w"][0], (2, 0, 1))),
        "sgu_bT": f(inputs["sgu_b"][0].T),
        "mix_gb64": f(inputs["mix_norm_g"][0, 512:].reshape(8, 64).T),
        "mix_gb": f(inputs["mix_norm_g"][0, 512:].reshape(4, 128).T),
        "w_out": f(inputs["w_out"][0]),
        "w_router": f(inputs["w_router"][0]),
        "b_router_bc": f(np.broadcast_to(inputs["b_router"][0][None, :], (128, NE))),
        "w_gate": f(inputs["w_gate"][0]),
        "w_up": f(inputs["w_up"][0]),
        "w_down": f(inputs["w_down"][0]),
        "b_gateT": f(np.transpose(inputs["b_gate"][0].reshape(NE, 8, 128), (2, 0, 1))),
        "b_upT": f(np.transpose(inputs["b_up"][0].reshape(NE, 8, 128), (2, 0, 1))),
        "b_down": f(inputs["b_down"][0]),
    }
    rows = np.zeros((6, D), np.float32)
    rows[0, :512] = inputs["sgu_ln_g"][0]
    rows[0, 512:] = inputs["sgu_ln_b"][0]
    rows[1, :512] = inputs["mix_norm_g"][0, :512]
    rows[2] = inputs["ln1_g"][0]
    rows[3] = inputs["ln1_b"][0]
    rows[4] = inputs["ln2_g"][0]
    rows[5] = inputs["ln2_b"][0]
    shared["vecs"] = f(np.broadcast_to(rows[None], (128, 6, D)))
    maps = []
    for c in cores:
        b, half = c // 2, c % 2
        own = x[b, half * NTOK:(half + 1) * NTOK]
        xT = np.zeros((D, NEXT), np.float32)
        if half == 1:
            xT[:, :NTOK] = x[b, :NTOK].T
        xT[:, NTOK:] = own.T
        m = dict(shared)
        m["xT"] = xT
        m["x"] = f(own)
        m["consts"] = host_consts(float(half))
        maps.append(m)
    return maps


_NC_CACHE = {}


def kernel(**inputs):
    if "nc" not in _NC_CACHE:
        _NC_CACHE["nc"] = build()[0]
    nc = _NC_CACHE["nc"]
    maps = make_in_maps(inputs)
    res = run_bass_kernel_spmd(nc, maps, core_ids=list(range(8)))
    out = np.zeros((4, 4096, D), np.float32)
    for c in range(8):
        b, half = c // 2, c % 2
        out[b, half * NTOK:(half + 1) * NTOK] = res.results[c]["out"]
    return out
```

```python
import numpy as np
from contextlib import ExitStack
import concourse.bass as bass
import concourse.mybir as mybir
from concourse.bass_utils import run_bass_kernel_spmd

F32 = mybir.dt.float32
BF16 = mybir.dt.bfloat16
I32 = mybir.dt.int32
U32 = mybir.dt.uint32
AF = mybir.ActivationFunctionType
ALU = mybir.AluOpType
AX = mybir.AxisListType

D = 1024
NTOK = 2048
NEXT = 4096
NT = NTOK // 128
NE = 32
TOPK = 4
CAP = 512
NBLK = CAP // 128
ALPHA = 2.0 ** 0.25
EPS = 1e-5
PATTERNS = (1, 4, 16)

C_IDENT = 0
C_TRI = 128
C_MPREV = 256
C_MPREVC = 384
C_LSTRICT = 512
C_ONES = 640
C_IOTA = 768
C_TOKID = 800
C_SEL = 816
NCONST = 880


class Prog:
    COMPUTE = ("pe", "act", "dve", "pool")

    def __init__(self, nc, n_dma_sems=20, same_engine_sync=True):
        self.nc = nc
        self.es = ExitStack()
        self.eng = {"pe": nc.tensor, "act": nc.scalar, "dve": nc.vector, "pool": nc.gpsimd, "sp": nc.sync}
        self.sem = {}
        self.cnt = {}
        for e in self.COMPUTE:
            self.sem[e] = self.es.enter_context(nc.semaphore("sem_" + e))
            self.cnt[e] = 0
        self.known = {e: {} for e in self.eng}
        self.last_w = {}
        self.readers = {}
        self.same_engine_sync = same_engine_sync
        self.dma_pool = {}
        self.dma_idx = {}
        for q in ("sp", "act", "pool"):
            self.dma_pool[q] = [[self.es.enter_context(nc.semaphore(f"dsem_{q}_{i}")), 0] for i in range(n_dma_sems)]
            self.dma_idx[q] = 0
        self.n_inst = {e: 0 for e in self.eng}

    def sb(self, name, shape, dt):
        return self.es.enter_context(self.nc.sbuf_tensor("s_" + name, shape, dt))

    def ps(self, name, shape, dt):
        return self.es.enter_context(self.nc.psum_tensor(name, shape, dt))

    def _wait(self, e, tok):
        sem, val, src = tok
        if src == e and (e == "pe" or not self.same_engine_sync):
            return
        k = self.known[e]
        sid = id(sem)
        if k.get(sid, 0) >= val:
            return
        self.eng[e].wait_ge(sem, val)
        k[sid] = val

    def op(self, e, fn, reads=(), writes=(), dma=False):
        deps = {}

        def add(t):
            key = id(t[0])
            if key not in deps or deps[key][1] < t[1]:
                deps[key] = t
        for kk in reads:
            t = self.last_w.get(kk)
            if t is not None:
                add(t)
        for kk in writes:
            t = self.last_w.get(kk)
            if t is not None:
                add(t)
            for t in self.readers.get(kk, ()):
                add(t)
        for t in deps.values():
            self._wait(e, t)
        if dma:
            pool = self.dma_pool[e]
            i = self.dma_idx[e]
            self.dma_idx[e] = (i + 1) % len(pool)
            slot = pool[i]
            if slot[1] > 0:
                self._wait(e, (slot[0], slot[1], "dma"))
            inst = fn()
            slot[1] += 16
            inst.then_inc(slot[0], 16)
            tok = (slot[0], slot[1], "dma")
        else:
            inst = fn()
            self.cnt[e] += 1
            inst.then_inc(self.sem[e], 1)
            tok = (self.sem[e], self.cnt[e], e)
        self.n_inst[e] += 1
        for kk in writes:
            self.last_w[kk] = tok
            self.readers[kk] = []
        for kk in reads:
            if kk in writes:
                continue
            self.readers.setdefault(kk, []).append(tok)
        return tok

    def barrier(self):
        for e in self.eng:
            self.finish(e)

    def finish(self, e="sp"):
        for pool in self.dma_pool.values():
            for sem, val in pool:
                if val > 0:
                    self._wait(e, (sem, val, "dma"))
        for c in self.COMPUTE:
            if self.cnt[c] > 0:
                self._wait(e, (self.sem[c], self.cnt[c], c))


class Rot:
    def __init__(self, p, name, n, shape, dt):
        self.tiles = [p.sb(f"{name}{i}", shape, dt) for i in range(n)]
        self.name = name
        self.n = n
        self.i = 0

    def next(self):
        j = self.i % self.n
        self.i += 1
        return self.tiles[j], (self.name, j)


def v_tiles():
    out = {}
    out[1] = [(n, 0) for n in range(15, 32)]
    out[4] = [(n, r) for n in range(3, 8) for r in range(4)]
    out[16] = [(n, r) for n in range(0, 2) for r in range(16)]
    return out


def build(stage=99, dbg=()):
    nc = bass.Bass("TRN2", target_bir_lowering=False)
    dram = {}

    def din(name, shape, dt=F32):
        dram[name] = nc.dram_tensor(name, list(shape), dt, kind="ExternalInput").ap()
        return dram[name]

    def dout(name, shape, dt=F32):
        dram[name] = nc.dram_tensor(name, list(shape), dt, kind="ExternalOutput").ap()
        return dram[name]

    xT_d = din("xT", [D, NEXT])
    x_d = din("x", [NTOK, D])
    win_d = din("w_in", [D, 2560])
    consts_d = din("consts", [128, NCONST])
    sguw_d = din("sgu_wT", [128, 8, 128])
    sgub_d = din("sgu_bT", [128, 8])
    vec_d = din("vecs", [128, 6, D])
    din("mix_gb64", [64, 8])
    gb_d = din("mix_gb", [128, 4])
    wout_d = din("w_out", [D, D])
    wr_d = din("w_router", [D, NE])
    br_d = din("b_router_bc", [128, NE])
    if stage >= 5:
        wg_d = din("w_gate", [NE, D, D])
        wu_d = din("w_up", [NE, D, D])
        wd_d = din("w_down", [NE, D, D])
    bg_d = din("b_gateT", [128, NE, 8])
    bu_d = din("b_upT", [128, NE, 8])
    bd_d = din("b_down", [NE, D])
    out_d = dout("out", [NTOK, D])
    for name, shape in dbg:
        dout(name, shape)

    p = Prog(nc)
    with p.es:
        E = p.eng
        psb = [p.ps(f"psb{i}", [128, 512], F32) for i in range(8)]

        def PS(b):
            return ("ps", b)

        def weave(*gens):
            its = [g for g in gens if g is not None]
            while its:
                for g in list(its):
                    try:
                        next(g)
                    except StopIteration:
                        its.remove(g)

        def run(g):
            for _ in g:
                pass

        def sbuf_raw(es, name, shape, dt):
            return es.enter_context(nc.sbuf_tensor("s_" + name, shape, dt))

        def sbuf(es, name, shape, dt):
            return es.enter_context(nc.sbuf_tensor("s_" + name, shape, dt))

        class SRot:
            def __init__(self, es, name, n, shape, dt):
                self.tiles = [sbuf(es, f"{name}{i}", shape, dt) for i in range(n)]
                self.name, self.n, self.i = name, n, 0

            def next(self):
                j = self.i % self.n
                self.i += 1
                return self.tiles[j], (self.name, j)

        def dma(q, out, in_, reads=(), writes=()):
            return p.op(q, lambda: E[q].dma_start(out=out, in_=in_), reads=reads, writes=writes, dma=True)

        consts = p.sb("consts", [128, NCONST], F32)
        consts_bf = p.sb("consts_bf", [128, NCONST], BF16)
        dma("sp", consts[:], consts_d, writes=["consts"])
        dma("pool", consts_bf[:], consts_d, writes=["consts_bf"])
        sgub = p.sb("sgub", [128, 8], F32)
        dma("sp", sgub[:], sgub_d, writes=["sgub"])
        gb = p.sb("gb", [128, 4], F32)
        dma("sp", gb[:], gb_d, writes=["gb"])
        wcT = p.sb("wcT", [128, 8, 128], BF16)
        eps_t = p.sb("eps_t", [128, 1], F32)
        p.op("dve", lambda: nc.vector.memset(eps_t[:], EPS), writes=["eps"])
        ident_bf = consts_bf[:, C_IDENT:C_IDENT + 128]
        ident_f = consts[:, C_IDENT:C_IDENT + 128]

        ssq_a = p.sb("ssq_a", [128, NT], F32)
        ssq_b = p.sb("ssq_b", [128, NT, 8], F32)
        p.op("dve", lambda: nc.vector.memset(ssq_a[:], 0.0), writes=["ssq_a"])
        SLOTS = NE * CAP
        hbf_d = nc.dram_tensor("hbf_scr", [NTOK + 1, D], BF16).ap()
        h32_d = nc.dram_tensor("h32_scr", [NTOK, D], F32).ap()
        stok_d = nc.dram_tensor("stok_scr", [SLOTS + 128, 1], I32).ap()
        eo_d = nc.dram_tensor("eo_scr", [SLOTS + 128, D], F32).ap()
        gates = p.sb("gates", [128, NT, 4], F32)
        pos_i = p.sb("pos_i", [128, NT * 4], I32)
        Mall = p.sb("Mall", [128, NT, NE], BF16)
        tokid_i = p.sb("tokid_i", [128, NT], I32)
        p.op("dve", lambda: nc.vector.tensor_copy(out=tokid_i[:], in_=consts[:, C_TOKID:C_TOKID + NT]), reads=["consts"], writes=["tokid_i"])
        fill = p.sb("fill", [128, (SLOTS + 128) // 128], I32)
        p.op("dve", lambda: nc.vector.memset(fill[:], NTOK), writes=["fill"])
        dma("sp", stok_d.rearrange("(p f) o -> p (f o)", p=128), fill[:], reads=["fill"], writes=["stok_init"])
        esAB = ExitStack()
        aT = sbuf_raw(esAB, "aT", [128, 4, NTOK], BF16)
        bT = sbuf_raw(esAB, "bT", [128, 4, NTOK], BF16)
        vt = v_tiles()
        vidx = {}
        nv = 0
        for d_ in PATTERNS:
            for nr in vt[d_]:
                vidx[(d_,) + nr] = nv
                nv += 1

        with ExitStack() as esQ:
            qT = sbuf(esQ, "qT", [128, 4, NTOK], BF16)
            kT = sbuf(esQ, "kT", [128, 4, NEXT], BF16)
            vT = sbuf(esQ, "vT", [128, 4, NEXT], BF16)
            with ExitStack() as esX:
                xTo = sbuf(esX, "xT_own", [128, 8, NTOK], BF16)
                for k in range(8):
                    dma("pool", xTo[:, k, :], xT_d[k * 128:(k + 1) * 128, NTOK:NEXT], writes=[("xTo", k)])
                xok = [("xTo", k) for k in range(8)]
                with ExitStack() as esB:
                    xTc = sbuf(esB, "xT_ctx", [128, 8, NTOK], BF16)
                    wkv = sbuf(esB, "win_kv", [128, 8, 1024], BF16)
                    sguw = sbuf(esB, "sguw", [128, 8, 128], F32)
                    dma("sp", sguw[:], sguw_d, writes=["sguw"])
                    tri_bc = consts[:, C_TRI:C_TRI + 128].unsqueeze(1).to_broadcast([128, 8, 128])
                    p.op("dve", lambda: nc.vector.tensor_tensor(out=wcT[:], in0=sguw[:], in1=tri_bc, op=ALU.mult),
                         reads=["sguw", "consts"], writes=["wcT"])
                    for k in range(8):
                        dma("pool", wkv[:, k, :], win_d[k * 128:(k + 1) * 128, 1536:2560], writes=[("wkv", k)])
                    for k in range(8):
                        dma("pool", xTc[:, k, :], xT_d[k * 128:(k + 1) * 128, 0:NTOK], writes=[("xTc", k)])
                    xck = [("xTc", k) for k in range(8)]
                    ev = 0
                    for (dst, dname, col0) in ((kT, "kT", 0), (vT, "vT", 512)) if stage >= 2 else ():
                        for hp in range(4):
                            for tc in range(8):
                                b = 6 + (ev % 2)
                                src, srck = (xTo, xok) if tc >= 4 else (xTc, xck)
                                tcl = tc % 4
                                for k in range(8):
                                    p.op("pe", lambda k=k, b=b, src=src, tcl=tcl: nc.tensor.matmul(
                                        psb[b][:], lhsT=wkv[:, k, col0 + hp * 128:col0 + (hp + 1) * 128],
                                        rhs=src[:, k, tcl * 512:(tcl + 1) * 512], start=(k == 0), stop=(k == 7)),
                                        reads=[srck[k], ("wkv", k)], writes=[PS(b)])
                                if ev % 2 == 0:
                                    p.op("act", lambda b=b: nc.scalar.copy(out=dst[:, hp, tc * 512:(tc + 1) * 512], in_=psb[b][:]), reads=[PS(b)], writes=[(dname, hp)])
                                else:
                                    p.op("dve", lambda b=b: nc.vector.tensor_copy(out=dst[:, hp, tc * 512:(tc + 1) * 512], in_=psb[b][:]), reads=[PS(b)], writes=[(dname, hp)])
                                ev += 1
                    p.barrier()
                with ExitStack() as esA:
                    win = sbuf(esA, "win_uvq", [128, 8, 1536], BF16)
                    for k in range(8):
                        dma("pool", win[:, k, :], win_d[k * 128:(k + 1) * 128, 0:1536], writes=[("win", k)])
                    wk = [("win", k) for k in range(8)]
                    vecA = sbuf(esA, "vecA", [128, 3, 512], F32)
                    dma("sp", vecA[:, 0:2, :], vec_d[:, 0, :].rearrange("p (a c) -> p a c", a=2), writes=["vecA"])
                    dma("sp", vecA[:, 2, :], vec_d[:, 1, 0:512], writes=["vecA"])
                    U = SRot(esA, "U", 2, [128, 512], F32)
                    V = SRot(esA, "V", 2, [128, 512], F32)
                    W1 = SRot(esA, "W1", 2, [128, 512], F32)
                    W2 = SRot(esA, "W2", 2, [128, 512], F32)
                    W3 = SRot(esA, "W3", 1, [128, 512], F32)
                    VN = SRot(esA, "VN", 2, [128, 512], BF16)
                    AB = SRot(esA, "AB", 2, [128, 512], BF16)
                    st = SRot(esA, "st", 2, [128, 8, 8], F32)

                    def gelu(src_ap, src_key, dst, dst_key):
                        p.op("act", lambda: nc.scalar.activation(out=dst[:], in_=src_ap, func=AF.Gelu_apprx_tanh), reads=[src_key], writes=[dst_key])

                    def emit_uv(i):
                        t0 = i * 128
                        b0 = 2 * (i % 2)
                        for half in range(2):
                            for k in range(8):
                                p.op("pe", lambda half=half, k=k: nc.tensor.matmul(
                                    psb[b0 + half][:], lhsT=xTo[:, k, t0:t0 + 128], rhs=win[:, k, half * 512:(half + 1) * 512],
                                    start=(k == 0), stop=(k == 7)), reads=[xok[k], wk[k]], writes=[PS(b0 + half)])

                    hnd = {}

                    def S1(i):
                        b0 = 2 * (i % 2)
                        u, uk = U.next()
                        v, vk = V.next()
                        gelu(psb[b0][:], PS(b0), u, uk)
                        gelu(psb[b0 + 1][:], PS(b0 + 1), v, vk)
                        s, sk = st.next()
                        w1, w1k = W1.next()
                        v3 = v[:].rearrange("p (g c) -> p g c", g=8)
                        w13 = w1[:].rearrange("p (g c) -> p g c", g=8)
                        p.op("dve", lambda: nc.vector.tensor_reduce(out=s[:, 0, :], in_=v3, axis=AX.X, op=ALU.add), reads=[vk], writes=[sk])
                        yield
                        p.op("act", lambda: nc.scalar.activation(out=w1[:], in_=v[:], func=AF.Square), reads=[vk], writes=[w1k])
                        yield
                        p.op("dve", lambda: nc.vector.tensor_reduce(out=s[:, 1, :], in_=w13, axis=AX.X, op=ALU.add), reads=[w1k], writes=[sk])
                        yield
                        p.op("dve", lambda: nc.vector.tensor_single_scalar(out=s[:, 2, :], in_=s[:, 0, :], scalar=1.0 / 64, op=ALU.mult), reads=[sk], writes=[sk])
                        yield
                        p.op("dve", lambda: nc.vector.tensor_tensor(out=s[:, 3, :], in0=s[:, 2, :], in1=s[:, 2, :], op=ALU.mult), reads=[sk], writes=[sk])
                        yield
                        p.op("dve", lambda: nc.vector.scalar_tensor_tensor(out=s[:, 4, :], in0=s[:, 1, :], scalar=1.0 / 64, in1=s[:, 3, :], op0=ALU.mult, op1=ALU.subtract), reads=[sk], writes=[sk])
                        yield
                        p.op("act", lambda: nc.scalar.activation(out=s[:, 6, :], in_=s[:, 4, :], func=AF.Sqrt, bias=eps_t[:, 0:1], scale=1.0), reads=[sk, "eps"], writes=[sk])
                        yield
                        p.op("dve", lambda: nc.vector.reciprocal(out=s[:, 5, :], in_=s[:, 6, :]), reads=[sk], writes=[sk])
                        yield
                        mean_bc = s[:, 2, :].unsqueeze(2).to_broadcast([128, 8, 64])
                        rstd_bc = s[:, 5, :].unsqueeze(2).to_broadcast([128, 8, 64])
                        p.op("dve", lambda: nc.vector.tensor_tensor(out=w13, in0=v3, in1=mean_bc, op=ALU.subtract), reads=[vk, sk], writes=[w1k])
                        yield
                        p.op("dve", lambda: nc.vector.tensor_tensor(out=w13, in0=w13, in1=rstd_bc, op=ALU.mult), reads=[w1k, sk], writes=[w1k])
                        yield
                        vn, vnk = VN.next()
                        p.op("dve", lambda: nc.vector.tensor_tensor(out=w1[:], in0=w1[:], in1=vecA[:, 0, :], op=ALU.mult), reads=[w1k, "vecA"], writes=[w1k])
                        yield
                        p.op("dve", lambda: nc.vector.tensor_tensor(out=vn[:], in0=w1[:], in1=vecA[:, 1, :], op=ALU.add), reads=[w1k, "vecA"], writes=[vnk])
                        yield
                        hnd[i] = (u, uk, vn, vnk)

                    def S2(i):
                        u, uk, vn, vnk = hnd.pop(i)
                        for g in range(8):
                            p.op("pe", lambda g=g: nc.tensor.matmul(psb[4][:, g * 64:(g + 1) * 64], lhsT=wcT[:, g, :], rhs=vn[:, g * 64:(g + 1) * 64],
                                                                     start=True, stop=True), reads=["wcT", vnk], writes=[PS(4)])
                            yield
                        w2, w2k = W2.next()
                        w23 = w2[:].rearrange("p (g c) -> p g c", g=8)
                        bs_bc = sgub[:, :].unsqueeze(2).to_broadcast([128, 8, 64])
                        p.op("dve", lambda: nc.vector.tensor_tensor(out=w23, in0=psb[4][:].rearrange("p (g c) -> p g c", g=8), in1=bs_bc, op=ALU.add),
                             reads=[PS(4), "sgub"], writes=[w2k])
                        yield
                        p.op("dve", lambda: nc.vector.tensor_tensor(out=w2[:], in0=w2[:], in1=u[:], op=ALU.mult), reads=[w2k, uk], writes=[w2k])
                        yield
                        w1b, w1bk = W3.next()
                        p.op("act", lambda: nc.scalar.activation(out=w1b[:], in_=w2[:], func=AF.Square, accum_out=ssq_a[:, i:i + 1]), reads=[w2k], writes=[w1bk, "ssq_a"])
                        yield
                        ab, abk = AB.next()
                        p.op("dve", lambda: nc.vector.tensor_tensor(out=ab[:], in0=w2[:], in1=vecA[:, 2, :], op=ALU.mult), reads=[w2k, "vecA"], writes=[abk])
                        yield
                        ptr = psb[5][:].bitcast(BF16)
                        for c in range(4):
                            p.op("pe", lambda c=c: nc.tensor.transpose(ptr[:, c * 128:(c + 1) * 128], ab[:, c * 128:(c + 1) * 128], ident_bf),
                                 reads=[abk, "consts_bf"], writes=[PS(5)])
                            yield
                        p.op("act", lambda: nc.scalar.copy(out=aT[:, :, i * 128:(i + 1) * 128], in_=ptr[:, 0:512].rearrange("p (c t) -> p c t", c=4)),
                             reads=[PS(5)], writes=["aT"])
                        yield
                        if "dbg_a" in dram:
                            dma("sp", dram["dbg_a"][i * 128:(i + 1) * 128, :], w2[:], reads=[w2k])
                            yield

                    qgroups = [(hp, tc) for hp in range(4) for tc in range(4)]

                    def qproj(ev):
                        hp, tc = qgroups[ev]
                        b = 6 + (ev % 2)
                        for k in range(8):
                            p.op("pe", lambda k=k, b=b: nc.tensor.matmul(
                                psb[b][:], lhsT=win[:, k, 1024 + hp * 128:1024 + (hp + 1) * 128],
                                rhs=xTo[:, k, tc * 512:(tc + 1) * 512], start=(k == 0), stop=(k == 7)),
                                reads=[xok[k], wk[k]], writes=[PS(b)])
                            yield
                        p.op("dve", lambda b=b: nc.vector.tensor_copy(out=qT[:, hp, tc * 512:(tc + 1) * 512], in_=psb[b][:]), reads=[PS(b)], writes=[("qT", hp)])
                        yield

                    if stage >= 1:
                        emit_uv(0)
                        emit_uv(1)
                        run(S1(0))
                        for i in range(NT):
                            if i + 2 < NT:
                                emit_uv(i + 2)
                            weave(S1(i + 1) if i + 1 < NT else None, S2(i), qproj(i) if stage >= 2 else None)
                    if "dbg_ssq" in dram:
                        dma("sp", dram["dbg_ssq"], ssq_a[:], reads=["ssq_a"])
                    p.barrier()
            with ExitStack() as esD:
                for nm, t_, rk in (("dbg_aT", aT, ["aT"]), ("dbg_qT", qT, [("qT", h_) for h_ in range(4)])):
                    if nm in dram:
                        tmpf = sbuf(esD, nm, [128, 4, NTOK], F32)
                        p.op("dve", lambda: nc.vector.tensor_copy(out=tmpf[:], in_=t_[:]), reads=rk, writes=[nm])
                        dma("sp", dram[nm], tmpf[:], reads=[nm])
                p.barrier()
            with ExitStack() as esT:
                vaugp = [sbuf(esT, f"vaugp{i}", [128, nv, 2, 66], BF16) for i in range(2)]
                for i_ in range(2):
                    p.op("pool", lambda i_=i_: nc.gpsimd.memset(vaugp[i_][:, :, :, 64:66], 1.0), writes=[("vones", i_)])
                accT = [sbuf(esT, f"accT{i}", [65, NTOK], F32) for i in range(2)]
                Eb = SRot(esT, "Eb", 3, [128, 512], BF16)
                Em = SRot(esT, "Em", 3, [128, 512], BF16)
                RD = SRot(esT, "RD", 2, [64, 512], F32)
                BQ = SRot(esT, "BQ", 2, [64, 512], F32)
                BS = SRot(esT, "BS", 2, [64, 512], F32)
                masks = sbuf(esT, "masks", [128, 3, 512], BF16)
                mprev = consts_bf[:, C_MPREV:C_MPREV + 128]
                mprevc = consts_bf[:, C_MPREVC:C_MPREVC + 128]
                mcur = consts_bf[:, C_TRI:C_TRI + 128]
                for mi, pat in enumerate(((mprev, mcur, mprev, mcur), (mprevc, mcur, mprev, mcur), (mprevc, mcur, mprevc, mcur))):
                    for j, src in enumerate(pat):
                        p.op("dve", lambda mi=mi, j=j, src=src: nc.vector.tensor_copy(out=masks[:, mi, j * 128:(j + 1) * 128], in_=src),
                             reads=["consts_bf"], writes=["masks"])
                sel_f = consts[0:65, C_SEL:C_SEL + 64]
                ones_f = consts[0:64, C_ONES:C_ONES + 1]
                gb64 = sbuf(esT, "gb64", [64, 8], F32)
                dma("sp", gb64[:], dram["mix_gb64"], writes=["gb64"])
                allv = [(d_, n, r) for d_ in PATTERNS for (n, r) in vt[d_]]
                tgc = [0]

                def build_v(hp):
                    vb = vaugp[hp % 2]
                    vbk = ("vaugp", hp % 2)
                    for g0 in range(0, nv, 8):
                        grp = allv[g0:g0 + 8]
                        tg = tgc[0]
                        tgc[0] += 1
                        b = 6 + (tg % 2)
                        ptr = psb[b][:].bitcast(BF16)
                        for j, (d_, n, r) in enumerate(grp):
                            span = 128 * d_
                            s0 = span * n + r
                            p.op("pe", lambda j=j, s0=s0, span=span, d_=d_, ptr=ptr: nc.tensor.transpose(
                                ptr[:, j * 128:(j + 1) * 128], vT[:, hp, s0:s0 + span - d_ + 1:d_], ident_bf),
                                reads=[("vT", hp), "consts_bf"], writes=[PS(b)])
                            yield
                        ng = len(grp)
                        src = ptr[:, 0:ng * 128].rearrange("p (t h c) -> p t h c", t=ng, h=2)
                        if tg % 2 == 0:
                            p.op("act", lambda src=src, g0=g0, ng=ng: nc.scalar.copy(out=vb[:, g0:g0 + ng, :, 0:64], in_=src), reads=[PS(b)], writes=[vbk])
                            yield
                        else:
                            p.op("dve", lambda src=src, g0=g0, ng=ng: nc.vector.tensor_copy(out=vb[:, g0:g0 + ng, :, 0:64], in_=src), reads=[PS(b)], writes=[vbk])
                            yield

                def unit_list(h):
                    us = []
                    for d_ in PATTERNS:
                        if d_ == 1:
                            blocks = [(n, 0) for n in range(16, 32)]
                        elif d_ == 4:
                            blocks = [(n, r) for n in range(4, 8) for r in range(4)]
                        else:
                            blocks = [(1, r) for r in range(16)]
                        for ui in range(0, 16, 2):
                            us.append((h, d_, blocks[ui:ui + 2]))
                    return us

                def emit_qk(u, su):
                    h, d_, pair = u
                    hp, hh = h // 2, h % 2
                    po = 64 * hh
                    span = 128 * d_
                    sb_ = su % 2
                    for j, (n, r) in enumerate(pair):
                        q0 = span * n + r - NTOK
                        qs = qT[po:po + 64, hp, q0:q0 + span - d_ + 1:d_]
                        kp0 = span * (n - 1) + r
                        kc0 = span * n + r
                        p.op("pe", lambda j=j, qs=qs, kp0=kp0: nc.tensor.matmul(
                            psb[sb_][:, j * 256:j * 256 + 128], lhsT=kT[po:po + 64, hp, kp0:kp0 + span - d_ + 1:d_], rhs=qs, start=True, stop=True),
                            reads=[("kT", hp), ("qT", hp)], writes=[PS(sb_)])
                        p.op("pe", lambda j=j, qs=qs, kc0=kc0: nc.tensor.matmul(
                            psb[sb_][:, j * 256 + 128:j * 256 + 256], lhsT=kT[po:po + 64, hp, kc0:kc0 + span - d_ + 1:d_], rhs=qs, start=True, stop=True),
                            reads=[("kT", hp), ("qT", hp)], writes=[PS(sb_)])

                def emit_mid(u, su):
                    h, d_, pair = u
                    span = 128 * d_
                    sb_ = su % 2
                    ctx = tuple((span * (n - 1) + r) < NTOK for (n, r) in pair)
                    mi = {(False, False): 0, (True, False): 1, (True, True): 2}[ctx]
                    eb, ebk = Eb.next()
                    em, emk = Em.next()
                    p.op("act", lambda: nc.scalar.activation(out=eb[:], in_=psb[sb_][:], func=AF.Exp, scale=0.125), reads=[PS(sb_)], writes=[ebk])
                    p.op("dve", lambda: nc.vector.tensor_tensor(out=em[:], in0=eb[:], in1=masks[:, mi, :], op=ALU.mult), reads=[ebk, "masks"], writes=[emk])
                    return em, emk

                def emit_pv(u, su, em, emk):
                    h, d_, pair = u
                    hp, hh = h // 2, h % 2
                    vb = vaugp[hp % 2]
                    vbk = ("vaugp", hp % 2)
                    ob_ = 2 + su % 2
                    for j, (n, r) in enumerate(pair):
                        vp = vidx[(d_, n - 1, r)]
                        vc = vidx[(d_, n, r)]
                        p.op("pe", lambda j=j, vp=vp: nc.tensor.matmul(
                            psb[ob_][0:65, j * 128:(j + 1) * 128], lhsT=vb[:, vp, hh, 0:65], rhs=em[:, j * 256:j * 256 + 128], start=True, stop=False),
                            reads=[vbk, ("vones", hp % 2), emk], writes=[PS(ob_)])
                        p.op("pe", lambda j=j, vc=vc: nc.tensor.matmul(
                            psb[ob_][0:65, j * 128:(j + 1) * 128], lhsT=vb[:, vc, hh, 0:65], rhs=em[:, j * 256 + 128:j * 256 + 256], start=False, stop=True),
                            reads=[vbk, ("vones", hp % 2), emk], writes=[PS(ob_)])

                def emit_acc(u, su):
                    h, d_, pair = u
                    acc = accT[h % 2]
                    acck = ("accT", h % 2)
                    span = 128 * d_
                    ob_ = 2 + su % 2
                    n0, r0 = pair[0]
                    src = psb[ob_][0:65, 0:256].rearrange("p (b j) -> p b j", b=2)
                    if d_ == 1:
                        q0 = span * n0 - NTOK
                        dest = acc[0:65, q0:q0 + 256].rearrange("p (b j) -> p b j", b=2)
                        p.op("act", lambda: nc.scalar.copy(out=dest, in_=src), reads=[PS(ob_)], writes=[acck])
                    else:
                        s0 = span * n0 - NTOK
                        dest = acc[0:65, s0:s0 + span].rearrange("p (j d) -> p d j", d=d_)[:, r0:r0 + 2, :]
                        p.op("dve", lambda: nc.vector.tensor_tensor(out=dest, in0=dest, in1=src, op=ALU.add), reads=[PS(ob_), acck], writes=[acck])

                def finalize(h):
                    hp, hh = h // 2, h % 2
                    po = 64 * hh
                    acc = accT[h % 2]
                    acck = ("accT", h % 2)
                    for c in range(4):
                        p.op("pe", lambda c=c: nc.tensor.matmul(psb[4][0:64, :], lhsT=sel_f, rhs=acc[0:65, c * 512:(c + 1) * 512], start=True, stop=True),
                             reads=["consts", acck], writes=[PS(4)])
                        yield
                        rd, rdk = RD.next()
                        bq_, bqk = BQ.next()
                        bs_, bsk = BS.next()
                        p.op("dve", lambda rd=rd: nc.vector.reciprocal(out=rd[:], in_=psb[4][0:64, :]), reads=[PS(4)], writes=[rdk])
                        yield
                        p.op("dve", lambda c=c, rd=rd, bq_=bq_: nc.vector.tensor_tensor(out=bq_[:], in0=acc[0:64, c * 512:(c + 1) * 512], in1=rd[:], op=ALU.mult),
                             reads=[acck, rdk], writes=[bqk])
                        yield
                        p.op("act", lambda bq_=bq_, bs_=bs_: nc.scalar.activation(out=bs_[:], in_=bq_[:], func=AF.Square), reads=[bqk], writes=[bsk])
                        yield
                        for t in range(4):
                            ti = c * 4 + t
                            col = ti * 8 + h
                            p.op("pe", lambda t=t, col=col, bs_=bs_: nc.tensor.matmul(psb[5][:, col:col + 1], lhsT=bs_[0:64, t * 128:(t + 1) * 128], rhs=ones_f, start=True, stop=True),
                                 reads=[bsk, "consts"], writes=[PS(5)])
                            yield
                        p.op("act", lambda c=c, bq_=bq_: nc.scalar.activation(out=bT[po:po + 64, hp, c * 512:(c + 1) * 512], in_=bq_[:], func=AF.Copy, scale=gb64[:, h:h + 1]),
                             reads=[bqk, "gb64"], writes=["bT"])
                        yield
                        if "dbg_b" in dram:
                            dma("sp", dram["dbg_b"][h * 64:(h + 1) * 64, c * 512:(c + 1) * 512], bq_[:], reads=[bqk])
                            yield

                if stage >= 3:
                    units = []
                    for h in range(8):
                        units += unit_list(h)
                    run(build_v(0))
                    emit_qk(units[0], 0)
                    pend = None
                    bg = []

                    def step_bg(n):
                        for _ in range(n):
                            if not bg:
                                return
                            try:
                                next(bg[0])
                            except StopIteration:
                                bg.pop(0)

                    def drain_bg():
                        while bg:
                            step_bg(1)
                    for ui, u in enumerate(units):
                        if ui + 1 < len(units):
                            emit_qk(units[ui + 1], ui + 1)
                        em, emk = emit_mid(u, ui)
                        if pend is not None:
                            emit_acc(*pend)
                            pend = None
                        emit_pv(u, ui, em, emk)
                        pend = (u, ui)
                        step_bg(4)
                        h = u[0]
                        last_of_head = (ui + 1 == len(units)) or units[ui + 1][0] != h
                        if last_of_head:
                            emit_acc(*pend)
                            pend = None
                            drain_bg()
                            bg.append(finalize(h))
                            if h % 2 == 0 and h // 2 + 1 < 4:
                                bg.append(build_v(h // 2 + 1))
                    drain_bg()
                if stage >= 3:
                    p.op("dve", lambda: nc.vector.tensor_copy(out=ssq_b[:].rearrange("p t h -> p (t h)"), in_=psb[5][:, 0:128]), reads=[PS(5)], writes=["ssq_b"])
                    if "dbg_ssqb" in dram:
                        dma("sp", dram["dbg_ssqb"], ssq_b[:].rearrange("p t h -> p (t h)"), reads=["ssq_b"])
                p.barrier()
        scat_keys = []
        hbf_keys = ["hbf_z"]
        h32_keys = []
        with ExitStack() as es3:
            zrow = sbuf(es3, "zrow", [1, D], F32)
            zrow_bf = sbuf(es3, "zrow_bf", [1, D], BF16)
            p.op("dve", lambda: nc.vector.memset(zrow[:], 0.0), writes=["zrow"])
            p.op("dve", lambda: nc.vector.memset(zrow_bf[:], 0.0), writes=["zrow_bf"])
            dma("sp", hbf_d[NTOK:NTOK + 1, :], zrow_bf[:], reads=["zrow_bf"], writes=["hbf_z"])
            dma("sp", eo_d[SLOTS:SLOTS + 1, :], zrow[:], reads=["zrow"], writes=["eo_z"])
            wout = sbuf(es3, "wout", [128, 8, D], BF16)
            for k in range(8):
                dma("pool", wout[:, k, :], wout_d[k * 128:(k + 1) * 128, :], writes=[("wout", k)])
            vec3 = sbuf(es3, "vec3", [128, 2, D], F32)
            dma("sp", vec3[:], vec_d[:, 2:4, :], writes=["vec3"])
            wr = sbuf(es3, "wr", [128, 8, NE], F32)
            dma("sp", wr[:], wr_d.rearrange("(k p) e -> p k e", p=128), writes=["wr"])
            brb = sbuf(es3, "brb", [128, NE], F32)
            dma("sp", brb[:], br_d, writes=["brb"])
            rs = sbuf(es3, "rs", [128, 4, NT], F32)
            p.op("dve", lambda: nc.vector.tensor_reduce(out=rs[:, 2, :], in_=ssq_b[:], axis=AX.X, op=ALU.add), reads=["ssq_b"], writes=["rs"])
            p.op("act", lambda: nc.scalar.activation(out=rs[:, 3, :], in_=ssq_a[:], func=AF.Ln, bias=eps_t[:, 0:1], scale=1.0 / 512), reads=["ssq_a", "eps"], writes=["rs"])
            p.op("act", lambda: nc.scalar.activation(out=rs[:, 0, :], in_=rs[:, 3, :], func=AF.Exp, scale=-0.5), reads=["rs"], writes=["rs"])
            p.op("act", lambda: nc.scalar.activation(out=rs[:, 3, :], in_=rs[:, 2, :], func=AF.Ln, bias=eps_t[:, 0:1], scale=1.0 / 512), reads=["rs", "eps"], writes=["rs"])
            p.op("act", lambda: nc.scalar.activation(out=rs[:, 1, :], in_=rs[:, 3, :], func=AF.Exp, scale=-0.5), reads=["rs"], writes=["rs"])
            X = SRot(es3, "X", 4, [128, D], F32)
            T1 = SRot(es3, "T1", 2, [128, D], F32)
            T2 = SRot(es3, "T2", 2, [128, D], F32)
            HB = SRot(es3, "HB", 2, [128, D], BF16)
            HT = SRot(es3, "HT", 2, [128, 8, 128], F32)
            SM = SRot(es3, "SM", 2, [128, 16], F32)
            LG = SRot(es3, "LG", 2, [128, NE], F32)
            RK = SRot(es3, "RK", 2, [128, 6, NE], F32)
            OH = SRot(es3, "OH", 2, [128, 4, NE], F32)
            MX = SRot(es3, "MX", 2, [128, 24], F32)

            def layer_norm(src, srck, dst, dstk, g_ap, b_ap, gk, jt, jtk, SMr):
                sm, smk = SMr.next()
                p.op("dve", lambda: nc.vector.memset(sm[:], 0.0), writes=[smk])
                yield
                p.op("act", lambda: nc.scalar.activation(out=jt[:], in_=src[:], func=AF.Identity, accum_out=sm[:, 0:1]), reads=[srck, smk], writes=[jtk, smk])
                yield
                p.op("act", lambda: nc.scalar.activation(out=jt[:], in_=src[:], func=AF.Square, accum_out=sm[:, 1:2]), reads=[srck, smk], writes=[jtk, smk])
                yield
                p.op("dve", lambda: nc.vector.tensor_single_scalar(out=sm[:, 2:3], in_=sm[:, 0:1], scalar=1.0 / D, op=ALU.mult), reads=[smk], writes=[smk])
                yield
                p.op("dve", lambda: nc.vector.tensor_tensor(out=sm[:, 3:4], in0=sm[:, 2:3], in1=sm[:, 2:3], op=ALU.mult), reads=[smk], writes=[smk])
                yield
                p.op("dve", lambda: nc.vector.scalar_tensor_tensor(out=sm[:, 4:5], in0=sm[:, 1:2], scalar=1.0 / D, in1=sm[:, 3:4], op0=ALU.mult, op1=ALU.subtract), reads=[smk], writes=[smk])
                yield
                p.op("act", lambda: nc.scalar.activation(out=sm[:, 5:6], in_=sm[:, 4:5], func=AF.Ln, bias=eps_t[:, 0:1], scale=1.0), reads=[smk, "eps"], writes=[smk])
                yield
                p.op("act", lambda: nc.scalar.activation(out=sm[:, 6:7], in_=sm[:, 5:6], func=AF.Exp, scale=-0.5), reads=[smk], writes=[smk])
                yield
                p.op("dve", lambda: nc.vector.scalar_tensor_tensor(out=sm[:, 7:8], in0=sm[:, 2:3], scalar=-1.0, in1=sm[:, 6:7], op0=ALU.mult, op1=ALU.mult), reads=[smk], writes=[smk])
                yield
                p.op("act", lambda: nc.scalar.activation(out=jt[:], in_=src[:], func=AF.Identity, scale=sm[:, 6:7], bias=sm[:, 7:8]), reads=[srck, smk], writes=[jtk])
                yield
                p.op("dve", lambda: nc.vector.tensor_tensor(out=jt[:], in0=jt[:], in1=g_ap, op=ALU.mult), reads=[jtk, gk], writes=[jtk])
                yield
                p.op("dve", lambda: nc.vector.tensor_tensor(out=dst[:], in0=jt[:], in1=b_ap, op=ALU.add), reads=[jtk, gk], writes=[dstk])
                yield

            hh_ = {}

            def stageA(i):
                tsl = slice(i * 128, (i + 1) * 128)
                x_, xk_ = X.next()
                dma("sp", x_[:], x_d[tsl, :], writes=[xk_])
                yield
                for half in range(2):
                    for c in range(4):
                        p.op("pe", lambda half=half, c=c: nc.tensor.matmul(psb[half][:], lhsT=aT[:, c, tsl], rhs=wout[:, c, half * 512:(half + 1) * 512],
                                                                       start=(c == 0), stop=(c == 3)), reads=["aT", ("wout", c)], writes=[PS(half)])
                        yield
                    for c in range(4):
                        p.op("pe", lambda half=half, c=c: nc.tensor.matmul(psb[2 + half][:], lhsT=bT[:, c, tsl], rhs=wout[:, 4 + c, half * 512:(half + 1) * 512],
                                                                       start=(c == 0), stop=(c == 3)), reads=["bT", ("wout", 4 + c)], writes=[PS(2 + half)])
                        yield
                t1, t1k = T1.next()
                t2, t2k = T2.next()
                p.op("act", lambda: nc.scalar.mul(out=x_[:], in_=x_[:], mul=ALPHA), reads=[xk_], writes=[xk_])
                yield
                for half in range(2):
                    hs = slice(half * 512, (half + 1) * 512)
                    p.op("dve", lambda half=half, hs=hs: nc.vector.scalar_tensor_tensor(out=t1[:, hs], in0=psb[half][:], scalar=rs[:, 0, i:i + 1], in1=x_[:, hs], op0=ALU.mult, op1=ALU.add),
                         reads=[PS(half), "rs", xk_], writes=[t1k])
                    yield
                    p.op("dve", lambda half=half, hs=hs: nc.vector.scalar_tensor_tensor(out=t1[:, hs], in0=psb[2 + half][:], scalar=rs[:, 1, i:i + 1], in1=t1[:, hs], op0=ALU.mult, op1=ALU.add),
                         reads=[PS(2 + half), "rs", t1k], writes=[t1k])
                    yield
                h_, hk_ = X.next()
                yield from layer_norm(t1, t1k, h_, hk_, vec3[:, 0, :], vec3[:, 1, :], "vec3", t2, t2k, SM)
                dma("sp", h32_d[tsl, :], h_[:], reads=[hk_], writes=[("h32", i)])
                yield
                h32_keys.append(("h32", i))
                hb, hbk = HB.next()
                p.op("act", lambda: nc.scalar.copy(out=hb[:], in_=h_[:]), reads=[hk_], writes=[hbk])
                yield
                dma("sp", hbf_d[tsl, :], hb[:], reads=[hbk], writes=[("hbf", i)])
                yield
                hbf_keys.append(("hbf", i))
                if "dbg_h" in dram:
                    dma("sp", dram["dbg_h"][tsl, :], h_[:], reads=[hk_])
                    yield
                hh_[i] = (h_, hk_)

            def stageB(i):
                h_, hk_ = hh_.pop(i)
                tsl = slice(i * 128, (i + 1) * 128)
                ht, htk = HT.next()
                for k in range(8):
                    b = 4 + k // 4
                    p.op("pe", lambda k=k, b=b: nc.tensor.transpose(psb[b][:, (k % 4) * 128:(k % 4 + 1) * 128], h_[:, k * 128:(k + 1) * 128], ident_f),
                         reads=[hk_, "consts"], writes=[PS(b)])
                    yield
                p.op("act", lambda: nc.scalar.copy(out=ht[:, 0:4, :], in_=psb[4][:].rearrange("p (k t) -> p k t", k=4)), reads=[PS(4)], writes=[htk])
                yield
                p.op("dve", lambda: nc.vector.tensor_copy(out=ht[:, 4:8, :], in_=psb[5][:].rearrange("p (k t) -> p k t", k=4)), reads=[PS(5)], writes=[htk])
                yield
                for k in range(8):
                    p.op("pe", lambda k=k: nc.tensor.matmul(psb[6][:, 0:NE], lhsT=ht[:, k, :], rhs=wr[:, k, :], start=(k == 0), stop=(k == 7)),
                         reads=[htk, "wr"], writes=[PS(6)])
                    yield
                lg, lgk = LG.next()
                p.op("dve", lambda: nc.vector.tensor_tensor(out=lg[:], in0=psb[6][:, 0:NE], in1=brb[:], op=ALU.add), reads=[PS(6), "brb"], writes=[lgk])
                yield
                if "dbg_logits" in dram:
                    dma("sp", dram["dbg_logits"][tsl, :], lg[:], reads=[lgk])
                    yield
                mx, mxk = MX.next()
                p.op("dve", lambda: nc.vector.max(out=mx[:, 0:8], in_=lg[:]), reads=[lgk], writes=[mxk])
                yield
                rk, rkk = RK.next()
                p.op("dve", lambda: nc.vector.tensor_scalar(out=rk[:, 0, :], in0=lg[:], scalar1=mx[:, 3:4], scalar2=None, op0=ALU.is_ge), reads=[lgk, mxk], writes=[rkk])
                yield
                p.op("dve", lambda: nc.vector.tensor_copy(out=Mall[:, i, :], in_=rk[:, 0, :]), reads=[rkk], writes=[("Mall", i)])
                yield
                for i2 in range(i + 1):
                    lhs = consts_bf[:, C_LSTRICT:C_LSTRICT + 128] if i2 == i else consts_bf[:, C_ONES:C_ONES + 128]
                    p.op("pe", lambda i2=i2, lhs=lhs: nc.tensor.matmul(psb[7][:, 0:NE], lhsT=lhs, rhs=Mall[:, i2, :], start=(i2 == 0), stop=(i2 == i)),
                         reads=[("Mall", i2), "consts_bf"], writes=[PS(7)])
                    yield
                p.op("dve", lambda: nc.vector.tensor_single_scalar(out=mx[:, 8:9], in_=mx[:, 0:1], scalar=-1.0, op=ALU.mult), reads=[mxk], writes=[mxk])
                yield
                p.op("dve", lambda: nc.vector.memset(mx[:, 9:10], 0.0), reads=[], writes=[mxk])
                yield
                p.op("act", lambda: nc.scalar.activation(out=mx[:, 12:16], in_=mx[:, 0:4], func=AF.Exp, bias=mx[:, 8:9], scale=1.0, accum_out=mx[:, 9:10]), reads=[mxk], writes=[mxk])
                yield
                p.op("dve", lambda: nc.vector.reciprocal(out=mx[:, 10:11], in_=mx[:, 9:10]), reads=[mxk], writes=[mxk])
                yield
                p.op("dve", lambda: nc.vector.tensor_scalar(out=mx[:, 16:20], in0=mx[:, 12:16], scalar1=mx[:, 10:11], scalar2=None, op0=ALU.mult), reads=[mxk], writes=[mxk])
                yield
                oh, ohk = OH.next()
                lg_bc = lg[:, :].unsqueeze(1).to_broadcast([128, 4, NE])
                mx_bc = mx[:, 0:4].unsqueeze(2).to_broadcast([128, 4, NE])
                p.op("dve", lambda: nc.vector.tensor_tensor(out=oh[:], in0=lg_bc, in1=mx_bc, op=ALU.is_equal), reads=[lgk, mxk], writes=[ohk])
                yield
                p.op("dve", lambda: nc.vector.tensor_copy(out=rk[:, 1, :], in_=psb[7][:, 0:NE]), reads=[PS(7)], writes=[rkk])
                yield
                p.op("dve", lambda: nc.vector.scalar_tensor_tensor(out=rk[:, 2, :], in0=consts[:, C_IOTA:C_IOTA + NE], scalar=float(CAP), in1=rk[:, 1, :], op0=ALU.mult, op1=ALU.add),
                     reads=["consts", rkk], writes=[rkk])
                yield
                oh2, oh2k = OH.next()
                p.op("dve", lambda: nc.vector.tensor_tensor(out=oh2[:], in0=oh[:], in1=rk[:, 1, :].unsqueeze(1).to_broadcast([128, 4, NE]), op=ALU.mult), reads=[ohk, rkk], writes=[oh2k])
                yield
                p.op("dve", lambda: nc.vector.tensor_reduce(out=rk[:, 3, 0:4], in_=oh2[:], axis=AX.X, op=ALU.add), reads=[oh2k], writes=[rkk])
                yield
                p.op("dve", lambda: nc.vector.tensor_tensor(out=oh2[:], in0=oh[:], in1=rk[:, 2, :].unsqueeze(1).to_broadcast([128, 4, NE]), op=ALU.mult), reads=[ohk, rkk], writes=[oh2k])
                yield
                p.op("dve", lambda: nc.vector.tensor_reduce(out=rk[:, 3, 4:8], in_=oh2[:], axis=AX.X, op=ALU.add), reads=[oh2k], writes=[rkk])
                yield
                p.op("dve", lambda: nc.vector.tensor_scalar(out=rk[:, 3, 8:12], in0=rk[:, 3, 0:4], scalar1=float(CAP), scalar2=None, op0=ALU.is_lt), reads=[rkk], writes=[rkk])
                yield
                p.op("dve", lambda: nc.vector.scalar_tensor_tensor(out=rk[:, 3, 12:16], in0=rk[:, 3, 4:8], scalar=-float(SLOTS), in1=rk[:, 3, 8:12], op0=ALU.add, op1=ALU.mult), reads=[rkk], writes=[rkk])
                yield
                p.op("dve", lambda: nc.vector.tensor_scalar(out=rk[:, 3, 16:20], in0=rk[:, 3, 12:16], scalar1=float(SLOTS), scalar2=None, op0=ALU.add), reads=[rkk], writes=[rkk])
                yield
                p.op("dve", lambda: nc.vector.tensor_copy(out=pos_i[:, i * 4:i * 4 + 4], in_=rk[:, 3, 16:20]), reads=[rkk], writes=[("pos", i)])
                yield
                p.op("dve", lambda: nc.vector.tensor_tensor(out=gates[:, i, :], in0=mx[:, 16:20], in1=rk[:, 3, 8:12], op=ALU.mult), reads=[mxk, rkk], writes=[("gates", i)])
                yield
                if "dbg_pos" in dram:
                    dma("sp", dram["dbg_pos"][tsl, :], rk[:, 3, 16:20], reads=[rkk])
                    yield
                    dma("sp", dram["dbg_gates"][tsl, :], gates[:, i, :], reads=[("gates", i)])
                    yield
                for k in range(4):
                    p.op("pool", lambda k=k: nc.gpsimd.indirect_dma_start(
                        out=stok_d[:, :], out_offset=bass.IndirectOffsetOnAxis(ap=pos_i[:, i * 4 + k:i * 4 + k + 1], axis=0),
                        in_=tokid_i[:, i:i + 1], in_offset=None),
                        reads=[("pos", i), "tokid_i", "stok_init"], writes=[("scat", i, k)], dma=True)
                    yield
                    scat_keys.append(("scat", i, k))

            if stage >= 4:
                run(stageA(0))
                for i in range(NT):
                    weave(stageA(i + 1) if i + 1 < NT else None, stageB(i))
            p.barrier()
        esAB.close()
        eo_keys = ["eo_z"]
        with ExitStack() as es5:
            bgT = sbuf(es5, "bgT", [128, NE, 8], F32)
            buT = sbuf(es5, "buT", [128, NE, 8], F32)
            dma("sp", bgT[:], bg_d, writes=["bgT"])
            dma("sp", buT[:], bu_d, writes=["buT"])
            bu1 = sbuf(es5, "bu1", [128, NE, 8], F32)
            p.op("dve", lambda: nc.vector.tensor_scalar(out=bu1[:], in0=buT[:], scalar1=1.0, scalar2=None, op0=ALU.add), reads=["buT"], writes=["bu1"])
            WG = [sbuf(es5, f"WG{i}", [128, 8, D], BF16) for i in range(2)]
            WU = [sbuf(es5, f"WU{i}", [128, 8, D], BF16) for i in range(2)]
            WD = [sbuf(es5, f"WD{i}", [128, 8, D], BF16) for i in range(2)]
            BD = [sbuf(es5, f"BD{i}", [128, D], F32) for i in range(2)]
            XB = [sbuf(es5, f"XB{i}", [128, NBLK, D], BF16) for i in range(2)]
            STI = [sbuf(es5, f"STI{i}", [128, NBLK], I32) for i in range(2)]
            XT = [sbuf(es5, f"XT{i}", [128, 8, CAP], BF16) for i in range(2)]
            ACT_T = sbuf(es5, "ACTT", [128, 8, CAP], BF16)
            GC = SRot(es5, "GC", 2, [128, CAP], F32)
            SG = SRot(es5, "SG", 2, [128, CAP], F32)
            UC = SRot(es5, "UC", 2, [128, CAP], F32)
            EO = SRot(es5, "EO", 2, [128, D], F32)

            def load_w(e):
                j = e % 2
                for (W_, wd_, nm) in ((WG, wg_d, "WG"), (WU, wu_d, "WU"), (WD, wd_d, "WD")):
                    for hk in range(2):
                        dma("pool", W_[j][:, hk * 4:(hk + 1) * 4, :], wd_[e, hk * 512:(hk + 1) * 512, :].rearrange("(k p) f -> p k f", p=128), writes=[(nm, j, hk)])
                dma("sp", BD[j][:], bd_d[e:e + 1, :].to_broadcast([128, D]), writes=[("BD", j)])

            def load_x(e):
                j = e % 2
                dma("sp", STI[j][:], stok_d[e * CAP:(e + 1) * CAP, :].rearrange("(p b) o -> p (b o)", p=128), reads=scat_keys + ["stok_init"], writes=[("STI", j)])
                for blk in range(NBLK):
                    p.op("pool", lambda blk=blk: nc.gpsimd.indirect_dma_start(
                        out=XB[j][:, blk, :], out_offset=None, in_=hbf_d[:, :],
                        in_offset=bass.IndirectOffsetOnAxis(ap=STI[j][:, blk:blk + 1], axis=0)),
                        reads=[("STI", j)] + hbf_keys, writes=[("XB", j, blk)], dma=True)

            nexp = NE if stage >= 5 else 0
            if nexp:
                load_x(0)
                load_w(0)
            tr = 0
            for e in range(nexp):
                j = e % 2
                if e + 1 < nexp:
                    load_x(e + 1)
                    load_w(e + 1)
                for blk in range(NBLK):
                    b = 6 + tr % 2
                    tr += 1
                    ptr = psb[b][:].bitcast(BF16)
                    for k in range(8):
                        p.op("pe", lambda blk=blk, k=k, ptr=ptr: nc.tensor.transpose(ptr[:, k * 128:(k + 1) * 128], XB[j][:, blk, k * 128:(k + 1) * 128], ident_bf),
                             reads=[("XB", j, blk), "consts_bf"], writes=[PS(b)])
                    src = ptr[:, :].rearrange("p (k t) -> p k t", k=8)
                    if blk % 2 == 0:
                        p.op("act", lambda blk=blk, src=src: nc.scalar.copy(out=XT[j][:, :, blk * 128:(blk + 1) * 128], in_=src), reads=[PS(b)], writes=[("XT", j)])
                    else:
                        p.op("dve", lambda blk=blk, src=src: nc.vector.tensor_copy(out=XT[j][:, :, blk * 128:(blk + 1) * 128], in_=src), reads=[PS(b)], writes=[("XT", j)])
                for f in range(8):
                    gb_ = f % 2
                    ub_ = 2 + f % 2
                    for k in range(8):
                        p.op("pe", lambda f=f, k=k: nc.tensor.matmul(psb[gb_][:, 0:CAP], lhsT=WG[j][:, k, f * 128:(f + 1) * 128], rhs=XT[j][:, k, :], start=(k == 0), stop=(k == 7)),
                             reads=[("WG", j, k // 4), ("XT", j)], writes=[PS(gb_)])
                    for k in range(8):
                        p.op("pe", lambda f=f, k=k: nc.tensor.matmul(psb[ub_][:, 0:CAP], lhsT=WU[j][:, k, f * 128:(f + 1) * 128], rhs=XT[j][:, k, :], start=(k == 0), stop=(k == 7)),
                             reads=[("WU", j, k // 4), ("XT", j)], writes=[PS(ub_)])
                    gc, gck = GC.next()
                    sg, sgk = SG.next()
                    uc, uck = UC.next()
                    p.op("dve", lambda f=f, gc=gc: nc.vector.tensor_scalar(out=gc[:], in0=psb[gb_][:, 0:CAP], scalar1=bgT[:, e, f:f + 1], scalar2=7.0, op0=ALU.add, op1=ALU.min),
                         reads=[PS(gb_), "bgT"], writes=[gck])
                    p.op("act", lambda gc=gc, sg=sg: nc.scalar.activation(out=sg[:], in_=gc[:], func=AF.Gelu_apprx_sigmoid), reads=[gck], writes=[sgk])
                    p.op("dve", lambda f=f, uc=uc: nc.vector.tensor_scalar(out=uc[:], in0=psb[ub_][:, 0:CAP], scalar1=bu1[:, e, f:f + 1], scalar2=-6.0, op0=ALU.add, op1=ALU.max),
                         reads=[PS(ub_), "bu1"], writes=[uck])
                    p.op("dve", lambda f=f, uc=uc, sg=sg: nc.vector.scalar_tensor_tensor(out=ACT_T[:, f, :], in0=uc[:], scalar=8.0, in1=sg[:], op0=ALU.min, op1=ALU.mult),
                         reads=[uck, sgk], writes=[("ACTT", f)])
                for blk in range(NBLK):
                    eo, eok = EO.next()
                    for half in range(2):
                        db_ = 4 + half
                        for f in range(8):
                            p.op("pe", lambda blk=blk, half=half, f=f: nc.tensor.matmul(psb[db_][:], lhsT=ACT_T[:, f, blk * 128:(blk + 1) * 128], rhs=WD[j][:, f, half * 512:(half + 1) * 512],
                                                                                     start=(f == 0), stop=(f == 7)),
                                 reads=[("ACTT", f), ("WD", j, f // 4)], writes=[PS(db_)])
                        p.op("dve", lambda half=half, eo=eo: nc.vector.tensor_tensor(out=eo[:, half * 512:(half + 1) * 512], in0=psb[db_][:], in1=BD[j][:, half * 512:(half + 1) * 512], op=ALU.add),
                             reads=[PS(db_), ("BD", j)], writes=[eok])
                    dma("sp", eo_d[e * CAP:(e + 1) * CAP, :].rearrange("(p b) d -> p b d", b=NBLK)[:, blk, :], eo[:], reads=[eok], writes=[("eo", e, blk)])
                    eo_keys.append(("eo", e, blk))
            p.barrier()

        with ExitStack() as es6:
            vec6 = sbuf(es6, "vec6", [128, 2, D], F32)
            dma("sp", vec6[:], vec_d[:, 4:6, :], writes=["vec6"])
            Hh = SRot(es6, "Hh", 3, [128, D], F32)
            G4 = SRot(es6, "G4", 12, [128, D], F32)
            AC = SRot(es6, "AC", 2, [128, D], F32)
            JT = SRot(es6, "JT", 2, [128, D], F32)
            OT = SRot(es6, "OT", 2, [128, D], F32)
            SM = SRot(es6, "SM2", 2, [128, 16], F32)
            def fetch(i):
                tsl = slice(i * 128, (i + 1) * 128)
                hh, hhk = Hh.next()
                dma("sp", hh[:], h32_d[tsl, :], reads=h32_keys, writes=[hhk])
                gs = []
                for k in range(4):
                    g_, gk_ = G4.next()
                    p.op("pool", lambda k=k, g_=g_: nc.gpsimd.indirect_dma_start(
                        out=g_[:], out_offset=None, in_=eo_d[:, :],
                        in_offset=bass.IndirectOffsetOnAxis(ap=pos_i[:, i * 4 + k:i * 4 + k + 1], axis=0)),
                        reads=[("pos", i)] + eo_keys, writes=[gk_], dma=True)
                    gs.append((g_, gk_))
                return hh, hhk, gs

            def combine(i, hh, hhk, gs):
                tsl = slice(i * 128, (i + 1) * 128)
                ac, ack = AC.next()
                p.op("act", lambda: nc.scalar.activation(out=ac[:], in_=gs[0][0][:], func=AF.Copy, scale=gates[:, i, 0:1]), reads=[gs[0][1], ("gates", i)], writes=[ack])
                yield
                for k in range(1, 4):
                    p.op("dve", lambda k=k: nc.vector.scalar_tensor_tensor(out=ac[:], in0=gs[k][0][:], scalar=gates[:, i, k:k + 1], in1=ac[:], op0=ALU.mult, op1=ALU.add),
                         reads=[gs[k][1], ("gates", i), ack], writes=[ack])
                    yield
                p.op("dve", lambda: nc.vector.scalar_tensor_tensor(out=ac[:], in0=hh[:], scalar=ALPHA, in1=ac[:], op0=ALU.mult, op1=ALU.add), reads=[hhk, ack], writes=[ack])
                yield
                if "dbg_pre2" in dram:
                    dma("sp", dram["dbg_pre2"][tsl, :], ac[:], reads=[ack])
                    yield
                jt, jtk = JT.next()
                ot, otk = OT.next()
                yield from layer_norm(ac, ack, ot, otk, vec6[:, 0, :], vec6[:, 1, :], "vec6", jt, jtk, SM)
                dma("sp", out_d[tsl, :], ot[:], reads=[otk], writes=[("out", i)])
                yield

            if stage >= 6:
                ft = {0: fetch(0), 1: fetch(1)}
                for i in range(NT):
                    if i + 2 < NT:
                        ft[i + 2] = fetch(i + 2)
                    run(combine(i, *ft.pop(i)))
        p.finish("sp")
    p.dram = dram
    return nc, p


def host_consts(flag):
    c = np.zeros((128, NCONST), np.float32)
    i = np.arange(128)
    c[:, C_IDENT:C_IDENT + 128] = np.eye(128, dtype=np.float32)
    c[:, C_TRI:C_TRI + 128] = (i[:, None] <= i[None, :])
    c[:, C_MPREV:C_MPREV + 128] = (i[:, None] >= i[None, :])
    c[:, C_MPREVC:C_MPREVC + 128] = flag * (i[:, None] >= i[None, :])
    c[:, C_LSTRICT:C_LSTRICT + 128] = (i[:, None] < i[None, :])
    c[:, C_ONES:C_ONES + 128] = 1.0
    c[:, C_IOTA:C_IOTA + 32] = np.arange(32)[None, :]
    c[:, C_TOKID:C_TOKID + 16] = np.arange(16)[None, :] * 128 + i[:, None]
    c[64, C_SEL:C_SEL + 64] = 1.0
    return c


def make_in_maps(inputs, cores=range(8)):
    f = lambda a: np.ascontiguousarray(np.asarray(a, dtype=np.float32))
    x = np.asarray(inputs["x"], dtype=np.float32)
    shared = {
        "w_in": f(inputs["w_in"][0]),
        "sgu_wT": f(np.transpose(inputs["sgu_w"][0], (2, 0, 1))),
        "sgu_bT": f(inputs["sgu_b"][0].T),
        "mix_gb64": f(inputs["mix_norm_g"][0, 512:].reshape(8, 64).T),
        "mix_gb": f(inputs["mix_norm_g"][0, 512:].reshape(4, 128).T),
        "w_out": f(inputs["w_out"][0]),
        "w_router": f(inputs["w_router"][0]),
        "b_router_bc": f(np.broadcast_to(inputs["b_router"][0][None, :], (128, NE))),
        "w_gate": f(inputs["w_gate"][0]),
        "w_up": f(inputs["w_up"][0]),
        "w_down": f(inputs["w_down"][0]),
        "b_gateT": f(np.transpose(inputs["b_gate"][0].reshape(NE, 8, 128), (2, 0, 1))),
        "b_upT": f(np.transpose(inputs["b_up"][0].reshape(NE, 8, 128), (2, 0, 1))),
        "b_down": f(inputs["b_down"][0]),
    }
    rows = np.zeros((6, D), np.float32)
    rows[0, :512] = inputs["sgu_ln_g"][0]
    rows[0, 512:] = inputs["sgu_ln_b"][0]
    rows[1, :512] = inputs["mix_norm_g"][0, :512]
    rows[2] = inputs["ln1_g"][0]
    rows[3] = inputs["ln1_b"][0]
    rows[4] = inputs["ln2_g"][0]
    rows[5] = inputs["ln2_b"][0]
    shared["vecs"] = f(np.broadcast_to(rows[None], (128, 6, D)))
    maps = []
    for c in cores:
        b, half = c // 2, c % 2
        own = x[b, half * NTOK:(half + 1) * NTOK]
        xT = np.zeros((D, NEXT), np.float32)
        if half == 1:
            xT[:, :NTOK] = x[b, :NTOK].T
        xT[:, NTOK:] = own.T
        m = dict(shared)
        m["xT"] = xT
        m["x"] = f(own)
        m["consts"] = host_consts(float(half))
        maps.append(m)
    return maps


_NC_CACHE = {}


def kernel(**inputs):
    if "nc" not in _NC_CACHE:
        _NC_CACHE["nc"] = build()[0]
    nc = _NC_CACHE["nc"]
    maps = make_in_maps(inputs)
    res = run_bass_kernel_spmd(nc, maps, core_ids=list(range(8)))
    out = np.zeros((4, 4096, D), np.float32)
    for c in range(8):
        b, half = c // 2, c % 2
        out[b, half * NTOK:(half + 1) * NTOK] = res.results[c]["out"]
    return out
```

```python
import numpy as np
from contextlib import ExitStack
import concourse.bass as bass
import concourse.mybir as mybir
from concourse.bass_utils import run_bass_kernel_spmd

F32 = mybir.dt.float32
BF16 = mybir.dt.bfloat16
I32 = mybir.dt.int32
U32 = mybir.dt.uint32
AF = mybir.ActivationFunctionType
ALU = mybir.AluOpType
AX = mybir.AxisListType

D = 1024
NTOK = 2048
NEXT = 4096
NT = NTOK // 128
NE = 32
TOPK = 4
CAP = 512
NBLK = CAP // 128
ALPHA = 2.0 ** 0.25
EPS = 1e-5
PATTERNS = (1, 4, 16)

C_IDENT = 0
C_TRI = 128
C_MPREV = 256
C_MPREVC = 384
C_LSTRICT = 512
C_ONES = 640
C_IOTA = 768
C_TOKID = 800
C_SEL = 816
NCONST = 880


class Prog:
    COMPUTE = ("pe", "act", "dve", "pool")

    def __init__(self, nc, n_dma_sems=20, same_engine_sync=True):
        self.nc = nc
        self.es = ExitStack()
        self.eng = {"pe": nc.tensor, "act": nc.scalar, "dve": nc.vector, "pool": nc.gpsimd, "sp": nc.sync}
        self.sem = {}
        self.cnt = {}
        for e in self.COMPUTE:
            self.sem[e] = self.es.enter_context(nc.semaphore("sem_" + e))
            self.cnt[e] = 0
        self.known = {e: {} for e in self.eng}
        self.last_w = {}
        self.readers = {}
        self.same_engine_sync = same_engine_sync
        self.dma_pool = {}
        self.dma_idx = {}
        for q in ("sp", "act", "pool"):
            self.dma_pool[q] = [[self.es.enter_context(nc.semaphore(f"dsem_{q}_{i}")), 0] for i in range(n_dma_sems)]
            self.dma_idx[q] = 0
        self.n_inst = {e: 0 for e in self.eng}

    def sb(self, name, shape, dt):
        return self.es.enter_context(self.nc.sbuf_tensor("s_" + name, shape, dt))

    def ps(self, name, shape, dt):
        return self.es.enter_context(self.nc.psum_tensor(name, shape, dt))

    def _wait(self, e, tok):
        sem, val, src = tok
        if src == e and (e == "pe" or not self.same_engine_sync):
            return
        k = self.known[e]
        sid = id(sem)
        if k.get(sid, 0) >= val:
            return
        self.eng[e].wait_ge(sem, val)
        k[sid] = val

    def op(self, e, fn, reads=(), writes=(), dma=False):
        deps = {}

        def add(t):
            key = id(t[0])
            if key not in deps or deps[key][1] < t[1]:
                deps[key] = t
        for kk in reads:
            t = self.last_w.get(kk)
            if t is not None:
                add(t)
        for kk in writes:
            t = self.last_w.get(kk)
            if t is not None:
                add(t)
            for t in self.readers.get(kk, ()):
                add(t)
        for t in deps.values():
            self._wait(e, t)
        if dma:
            pool = self.dma_pool[e]
            i = self.dma_idx[e]
            self.dma_idx[e] = (i + 1) % len(pool)
            slot = pool[i]
            if slot[1] > 0:
                self._wait(e, (slot[0], slot[1], "dma"))
            inst = fn()
            slot[1] += 16
            inst.then_inc(slot[0], 16)
            tok = (slot[0], slot[1], "dma")
        else:
            inst = fn()
            self.cnt[e] += 1
            inst.then_inc(self.sem[e], 1)
            tok = (self.sem[e], self.cnt[e], e)
        self.n_inst[e] += 1
        for kk in writes:
            self.last_w[kk] = tok
            self.readers[kk] = []
        for kk in reads:
            if kk in writes:
                continue
            self.readers.setdefault(kk, []).append(tok)
        return tok

    def barrier(self):
        for e in self.eng:
            self.finish(e)

    def finish(self, e="sp"):
        for pool in self.dma_pool.values():
            for sem, val in pool:
                if val > 0:
                    self._wait(e, (sem, val, "dma"))
        for c in self.COMPUTE:
            if self.cnt[c] > 0:
                self._wait(e, (self.sem[c], self.cnt[c], c))


class Rot:
    def __init__(self, p, name, n, shape, dt):
        self.tiles = [p.sb(f"{name}{i}", shape, dt) for i in range(n)]
        self.name = name
        self.n = n
        self.i = 0

    def next(self):
        j = self.i % self.n
        self.i += 1
        return self.tiles[j], (self.name, j)


def v_tiles():
    out = {}
    out[1] = [(n, 0) for n in range(15, 32)]
    out[4] = [(n, r) for n in range(3, 8) for r in range(4)]
    out[16] = [(n, r) for n in range(0, 2) for r in range(16)]
    return out


def build(stage=99, dbg=()):
    nc = bass.Bass("TRN2", target_bir_lowering=False)
    dram = {}

    def din(name, shape, dt=F32):
        dram[name] = nc.dram_tensor(name, list(shape), dt, kind="ExternalInput").ap()
        return dram[name]

    def dout(name, shape, dt=F32):
        dram[name] = nc.dram_tensor(name, list(shape), dt, kind="ExternalOutput").ap()
        return dram[name]

    xT_d = din("xT", [D, NEXT])
    x_d = din("x", [NTOK, D])
    win_d = din("w_in", [D, 2560])
    consts_d = din("consts", [128, NCONST])
    sguw_d = din("sgu_wT", [128, 8, 128])
    sgub_d = din("sgu_bT", [128, 8])
    vec_d = din("vecs", [128, 6, D])
    din("mix_gb64", [64, 8])
    gb_d = din("mix_gb", [128, 4])
    wout_d = din("w_out", [D, D])
    wr_d = din("w_router", [D, NE])
    br_d = din("b_router_bc", [128, NE])
    if stage >= 5:
        wg_d = din("w_gate", [NE, D, D])
        wu_d = din("w_up", [NE, D, D])
        wd_d = din("w_down", [NE, D, D])
    bg_d = din("b_gateT", [128, NE, 8])
    bu_d = din("b_upT", [128, NE, 8])
    bd_d = din("b_down", [NE, D])
    out_d = dout("out", [NTOK, D])
    for name, shape in dbg:
        dout(name, shape)

    p = Prog(nc)
    with p.es:
        E = p.eng
        psb = [p.ps(f"psb{i}", [128, 512], F32) for i in range(8)]

        def PS(b):
            return ("ps", b)

        def weave(*gens):
            its = [g for g in gens if g is not None]
            while its:
                for g in list(its):
                    try:
                        next(g)
                    except StopIteration:
                        its.remove(g)

        def run(g):
            for _ in g:
                pass

        def pipeline2(ents, stagger, depth):
            pending = list(ents)
            active = []
            since = stagger
            tiles_done = 0
            while pending or active:
                n_tiles = sum(1 for k, _ in active if k == "tile")
                for ent in list(pending):
                    kind, fact, need = ent
                    if kind == "tile":
                        if n_tiles < depth and since >= stagger:
                            active.append((kind, fact()))
                            pending.remove(ent)
                            since = 0
                            n_tiles += 1
                        break
                    if tiles_done >= need:
                        active.append((kind, fact()))
                        pending.remove(ent)
                for item in list(active):
                    try:
                        next(item[1])
                    except StopIteration:
                        active.remove(item)
                        if item[0] == "tile":
                            tiles_done += 1
                since += 1

        def pipeline(facts, stagger, depth):
            active = []
            idx = 0
            since = stagger
            while idx < len(facts) or active:
                if idx < len(facts) and len(active) < depth and since >= stagger:
                    active.append(facts[idx]())
                    idx += 1
                    since = 0
                for g in list(active):
                    try:
                        next(g)
                    except StopIteration:
                        active.remove(g)
                since += 1

        def sbuf_raw(es, name, shape, dt):
            return es.enter_context(nc.sbuf_tensor("s_" + name, shape, dt))

        def sbuf(es, name, shape, dt):
            return es.enter_context(nc.sbuf_tensor("s_" + name, shape, dt))

        class SRot:
            def __init__(self, es, name, n, shape, dt):
                self.tiles = [sbuf(es, f"{name}{i}", shape, dt) for i in range(n)]
                self.name, self.n, self.i = name, n, 0

            def next(self):
                j = self.i % self.n
                self.i += 1
                return self.tiles[j], (self.name, j)

        def dma(q, out, in_, reads=(), writes=()):
            return p.op(q, lambda: E[q].dma_start(out=out, in_=in_), reads=reads, writes=writes, dma=True)

        consts = p.sb("consts", [128, NCONST], F32)
        consts_bf = p.sb("consts_bf", [128, NCONST], BF16)
        dma("sp", consts[:], consts_d, writes=["consts"])
        dma("pool", consts_bf[:], consts_d, writes=["consts_bf"])
        sgub = p.sb("sgub", [128, 8], F32)
        dma("sp", sgub[:], sgub_d, writes=["sgub"])
        gb = p.sb("gb", [128, 4], F32)
        dma("sp", gb[:], gb_d, writes=["gb"])
        wcT = p.sb("wcT", [128, 8, 128], BF16)
        eps_t = p.sb("eps_t", [128, 1], F32)
        p.op("dve", lambda: nc.vector.memset(eps_t[:], EPS), writes=["eps"])
        ident_bf = consts_bf[:, C_IDENT:C_IDENT + 128]
        ident_f = consts[:, C_IDENT:C_IDENT + 128]

        ssq_a = p.sb("ssq_a", [128, NT], F32)
        ssq_b = p.sb("ssq_b", [128, NT, 8], F32)
        p.op("dve", lambda: nc.vector.memset(ssq_a[:], 0.0), writes=["ssq_a"])
        SLOTS = NE * CAP
        hbf_d = nc.dram_tensor("hbf_scr", [NTOK + 1, D], BF16).ap()
        h32_d = nc.dram_tensor("h32_scr", [NTOK, D], F32).ap()
        stok_d = nc.dram_tensor("stok_scr", [SLOTS + 128, 1], I32).ap()
        eo_d = nc.dram_tensor("eo_scr", [SLOTS + 1, D], F32).ap()
        gates = p.sb("gates", [128, NT, 4], F32)
        pos_i = p.sb("pos_i", [128, NT * 4], I32)
        Mall = p.sb("Mall", [128, NT, NE], BF16)
        tokid_i = p.sb("tokid_i", [128, NT], I32)
        p.op("dve", lambda: nc.vector.tensor_copy(out=tokid_i[:], in_=consts[:, C_TOKID:C_TOKID + NT]), reads=["consts"], writes=["tokid_i"])
        fill = p.sb("fill", [128, (SLOTS + 128) // 128], I32)
        p.op("dve", lambda: nc.vector.memset(fill[:], NTOK), writes=["fill"])
        dma("sp", stok_d.rearrange("(p f) o -> p (f o)", p=128), fill[:], reads=["fill"], writes=["stok_init"])
        esAB = ExitStack()
        aT = sbuf_raw(esAB, "aT", [128, 4, NTOK], BF16)
        bT = sbuf_raw(esAB, "bT", [128, 4, NTOK], BF16)
        vt = v_tiles()
        vidx = {}
        nv = 0
        for d_ in PATTERNS:
            for nr in vt[d_]:
                vidx[(d_,) + nr] = nv
                nv += 1

        with ExitStack() as esQ:
            qT = sbuf(esQ, "qT", [128, 4, NTOK], BF16)
            kT = sbuf(esQ, "kT", [128, 4, NEXT], BF16)
            vT = sbuf(esQ, "vT", [128, 4, NEXT], BF16)
            with ExitStack() as esX:
                xTo = sbuf(esX, "xT_own", [128, 8, NTOK], BF16)
                xT_v = xT_d.rearrange("(k p) t -> p k t", p=128)
                with ExitStack() as esB:
                    xTc = sbuf(esB, "xT_ctx", [128, 8, NTOK], BF16)
                    wkv = sbuf(esB, "win_kv", [128, 8, 1024], BF16)
                    sguw = sbuf(esB, "sguw", [128, 8, 128], F32)
                    dma("sp", sguw[:], sguw_d, writes=["sguw"])
                    tri_bc = consts[:, C_TRI:C_TRI + 128].unsqueeze(1).to_broadcast([128, 8, 128])
                    p.op("dve", lambda: nc.vector.tensor_tensor(out=wcT[:], in0=sguw[:], in1=tri_bc, op=ALU.mult),
                         reads=["sguw", "consts"], writes=["wcT"])
                    for k in range(0, 8, 2):
                        dma("pool", wkv[:, k:k + 2, :], win_d[k * 128:(k + 2) * 128, 1536:2560].rearrange("(k p) c -> p k c", p=128), writes=[("wkv", k), ("wkv", k + 1)])
                    for tcl in range(4):
                        dma("pool", xTc[:, :, tcl * 512:(tcl + 1) * 512], xT_v[:, :, tcl * 512:(tcl + 1) * 512], writes=[("xTc", tcl)])
                    for tcl in range(4):
                        dma("pool", xTo[:, :, tcl * 512:(tcl + 1) * 512], xT_v[:, :, NTOK + tcl * 512:NTOK + (tcl + 1) * 512], writes=[("xTo", tcl)])
                    ev = 0
                    for tc in range(8 if stage >= 2 else 0):
                        for (dst, dname, col0) in ((kT, "kT", 0), (vT, "vT", 512)):
                            for hp in range(4):
                                b = ev % 6
                                src, srck = (xTo, ("xTo", tc - 4)) if tc >= 4 else (xTc, ("xTc", tc))
                                tcl = tc % 4
                                for k in range(8):
                                    p.op("pe", lambda k=k, b=b, src=src, tcl=tcl: nc.tensor.matmul(
                                        psb[b][:], lhsT=wkv[:, k, col0 + hp * 128:col0 + (hp + 1) * 128],
                                        rhs=src[:, k, tcl * 512:(tcl + 1) * 512], start=(k == 0), stop=(k == 7)),
                                        reads=[srck, ("wkv", k)], writes=[PS(b)])
                                if ev % 2 == 0:
                                    p.op("act", lambda b=b: nc.scalar.copy(out=dst[:, hp, tc * 512:(tc + 1) * 512], in_=psb[b][:]), reads=[PS(b)], writes=[(dname, hp)])
                                else:
                                    p.op("dve", lambda b=b: nc.vector.tensor_copy(out=dst[:, hp, tc * 512:(tc + 1) * 512], in_=psb[b][:]), reads=[PS(b)], writes=[(dname, hp)])
                                ev += 1
                    p.barrier()
                with ExitStack() as esA:
                    win = sbuf(esA, "win_uvq", [128, 8, 1536], BF16)
                    for k in range(8):
                        dma("pool", win[:, k, :], win_d[k * 128:(k + 1) * 128, 0:1536], writes=[("win", k)])
                    wk = [("win", k) for k in range(8)]
                    vecA = sbuf(esA, "vecA", [128, 3, 512], F32)
                    dma("sp", vecA[:, 0:2, :], vec_d[:, 0, :].rearrange("p (a c) -> p a c", a=2), writes=["vecA"])
                    dma("sp", vecA[:, 2, :], vec_d[:, 1, 0:512], writes=["vecA"])
                    U = SRot(esA, "U", 2, [128, 512], F32)
                    V = SRot(esA, "V", 2, [128, 512], F32)
                    W1 = SRot(esA, "W1", 2, [128, 512], F32)
                    W2 = SRot(esA, "W2", 2, [128, 512], F32)
                    W3 = SRot(esA, "W3", 1, [128, 512], F32)
                    VN = SRot(esA, "VN", 2, [128, 512], BF16)
                    AB = SRot(esA, "AB", 2, [128, 512], BF16)
                    st = SRot(esA, "st", 2, [128, 8, 8], F32)

                    def gelu(src_ap, src_key, dst, dst_key):
                        p.op("act", lambda: nc.scalar.activation(out=dst[:], in_=src_ap, func=AF.Gelu_apprx_tanh), reads=[src_key], writes=[dst_key])

                    def emit_uv(i):
                        t0 = i * 128
                        b0 = 2 * (i % 2)
                        for half in range(2):
                            for k in range(8):
                                p.op("pe", lambda half=half, k=k: nc.tensor.matmul(
                                    psb[b0 + half][:], lhsT=xTo[:, k, t0:t0 + 128], rhs=win[:, k, half * 512:(half + 1) * 512],
                                    start=(k == 0), stop=(k == 7)), reads=[("xTo", i // 4), wk[k]], writes=[PS(b0 + half)])

                    hnd = {}

                    def S1(i):
                        b0 = 2 * (i % 2)
                        u, uk = U.next()
                        v, vk = V.next()
                        gelu(psb[b0][:], PS(b0), u, uk)
                        gelu(psb[b0 + 1][:], PS(b0 + 1), v, vk)
                        s, sk = st.next()
                        w1, w1k = W1.next()
                        v3 = v[:].rearrange("p (g c) -> p g c", g=8)
                        w13 = w1[:].rearrange("p (g c) -> p g c", g=8)
                        p.op("dve", lambda: nc.vector.tensor_reduce(out=s[:, 0, :], in_=v3, axis=AX.X, op=ALU.add), reads=[vk], writes=[sk])
                        yield
                        p.op("act", lambda: nc.scalar.activation(out=w1[:], in_=v[:], func=AF.Square), reads=[vk], writes=[w1k])
                        yield
                        p.op("dve", lambda: nc.vector.tensor_reduce(out=s[:, 1, :], in_=w13, axis=AX.X, op=ALU.add), reads=[w1k], writes=[sk])
                        yield
                        p.op("dve", lambda: nc.vector.tensor_single_scalar(out=s[:, 2, :], in_=s[:, 0, :], scalar=1.0 / 64, op=ALU.mult), reads=[sk], writes=[sk])
                        yield
                        p.op("dve", lambda: nc.vector.tensor_tensor(out=s[:, 3, :], in0=s[:, 2, :], in1=s[:, 2, :], op=ALU.mult), reads=[sk], writes=[sk])
                        yield
                        p.op("dve", lambda: nc.vector.scalar_tensor_tensor(out=s[:, 4, :], in0=s[:, 1, :], scalar=1.0 / 64, in1=s[:, 3, :], op0=ALU.mult, op1=ALU.subtract), reads=[sk], writes=[sk])
                        yield
                        p.op("act", lambda: nc.scalar.activation(out=s[:, 6, :], in_=s[:, 4, :], func=AF.Sqrt, bias=eps_t[:, 0:1], scale=1.0), reads=[sk, "eps"], writes=[sk])
                        yield
                        p.op("dve", lambda: nc.vector.reciprocal(out=s[:, 5, :], in_=s[:, 6, :]), reads=[sk], writes=[sk])
                        yield
                        mean_bc = s[:, 2, :].unsqueeze(2).to_broadcast([128, 8, 64])
                        rstd_bc = s[:, 5, :].unsqueeze(2).to_broadcast([128, 8, 64])
                        p.op("dve", lambda: nc.vector.tensor_tensor(out=w13, in0=v3, in1=mean_bc, op=ALU.subtract), reads=[vk, sk], writes=[w1k])
                        yield
                        p.op("dve", lambda: nc.vector.tensor_tensor(out=w13, in0=w13, in1=rstd_bc, op=ALU.mult), reads=[w1k, sk], writes=[w1k])
                        yield
                        vn, vnk = VN.next()
                        p.op("dve", lambda: nc.vector.tensor_tensor(out=w1[:], in0=w1[:], in1=vecA[:, 0, :], op=ALU.mult), reads=[w1k, "vecA"], writes=[w1k])
                        yield
                        p.op("dve", lambda: nc.vector.tensor_tensor(out=vn[:], in0=w1[:], in1=vecA[:, 1, :], op=ALU.add), reads=[w1k, "vecA"], writes=[vnk])
                        yield
                        hnd[i] = (u, uk, vn, vnk)

                    def S2(i):
                        u, uk, vn, vnk = hnd.pop(i)
                        for g in range(8):
                            p.op("pe", lambda g=g: nc.tensor.matmul(psb[4][:, g * 64:(g + 1) * 64], lhsT=wcT[:, g, :], rhs=vn[:, g * 64:(g + 1) * 64],
                                                                     start=True, stop=True), reads=["wcT", vnk], writes=[PS(4)])
                            yield
                        w2, w2k = W2.next()
                        w23 = w2[:].rearrange("p (g c) -> p g c", g=8)
                        bs_bc = sgub[:, :].unsqueeze(2).to_broadcast([128, 8, 64])
                        p.op("dve", lambda: nc.vector.tensor_tensor(out=w23, in0=psb[4][:].rearrange("p (g c) -> p g c", g=8), in1=bs_bc, op=ALU.add),
                             reads=[PS(4), "sgub"], writes=[w2k])
                        yield
                        p.op("dve", lambda: nc.vector.tensor_tensor(out=w2[:], in0=w2[:], in1=u[:], op=ALU.mult), reads=[w2k, uk], writes=[w2k])
                        yield
                        w1b, w1bk = W3.next()
                        p.op("act", lambda: nc.scalar.activation(out=w1b[:], in_=w2[:], func=AF.Square, accum_out=ssq_a[:, i:i + 1]), reads=[w2k], writes=[w1bk, "ssq_a"])
                        yield
                        ab, abk = AB.next()
                        p.op("dve", lambda: nc.vector.tensor_tensor(out=ab[:], in0=w2[:], in1=vecA[:, 2, :], op=ALU.mult), reads=[w2k, "vecA"], writes=[abk])
                        yield
                        ptr = psb[5][:].bitcast(BF16)
                        for c in range(4):
                            p.op("pe", lambda c=c: nc.tensor.transpose(ptr[:, c * 128:(c + 1) * 128], ab[:, c * 128:(c + 1) * 128], ident_bf),
                                 reads=[abk, "consts_bf"], writes=[PS(5)])
                            yield
                        p.op("act", lambda: nc.scalar.copy(out=aT[:, :, i * 128:(i + 1) * 128], in_=ptr[:, 0:512].rearrange("p (c t) -> p c t", c=4)),
                             reads=[PS(5)], writes=["aT"])
                        yield
                        if "dbg_a" in dram:
                            dma("sp", dram["dbg_a"][i * 128:(i + 1) * 128, :], w2[:], reads=[w2k])
                            yield

                    qgroups = [(hp, tc) for hp in range(4) for tc in range(4)]

                    def qproj(ev):
                        hp, tc = qgroups[ev]
                        b = 6 + (ev % 2)
                        for k in range(8):
                            p.op("pe", lambda k=k, b=b: nc.tensor.matmul(
                                psb[b][:], lhsT=win[:, k, 1024 + hp * 128:1024 + (hp + 1) * 128],
                                rhs=xTo[:, k, tc * 512:(tc + 1) * 512], start=(k == 0), stop=(k == 7)),
                                reads=[("xTo", tc), wk[k]], writes=[PS(b)])
                            yield
                        p.op("dve", lambda b=b: nc.vector.tensor_copy(out=qT[:, hp, tc * 512:(tc + 1) * 512], in_=psb[b][:]), reads=[PS(b)], writes=[("qT", hp)])
                        yield

                    if stage >= 1:
                        emit_uv(0)
                        emit_uv(1)
                        run(S1(0))
                        for i in range(NT):
                            if i + 2 < NT:
                                emit_uv(i + 2)
                            weave(S1(i + 1) if i + 1 < NT else None, S2(i), qproj(i) if stage >= 2 else None)
                    if "dbg_ssq" in dram:
                        dma("sp", dram["dbg_ssq"], ssq_a[:], reads=["ssq_a"])
                    p.barrier()
            with ExitStack() as esD:
                for nm, t_, rk in (("dbg_aT", aT, ["aT"]), ("dbg_qT", qT, [("qT", h_) for h_ in range(4)])):
                    if nm in dram:
                        tmpf = sbuf(esD, nm, [128, 4, NTOK], F32)
                        p.op("dve", lambda: nc.vector.tensor_copy(out=tmpf[:], in_=t_[:]), reads=rk, writes=[nm])
                        dma("sp", dram[nm], tmpf[:], reads=[nm])
                p.barrier()
            with ExitStack() as esT:
                vaugp = [sbuf(esT, f"vaugp{i}", [128, nv, 2, 66], BF16) for i in range(2)]
                for i_ in range(2):
                    p.op("pool", lambda i_=i_: nc.gpsimd.memset(vaugp[i_][:, :, :, 64:66], 1.0), writes=[("vones", i_)])
                accT = [sbuf(esT, f"accT{i}", [65, NTOK], F32) for i in range(2)]
                Eb = SRot(esT, "Eb", 3, [128, 512], BF16)
                Em = SRot(esT, "Em", 3, [128, 512], BF16)
                RD = SRot(esT, "RD", 2, [64, 512], F32)
                BQ = SRot(esT, "BQ", 2, [64, 512], F32)
                BS = SRot(esT, "BS", 2, [64, 512], BF16)
                masks = sbuf(esT, "masks", [128, 3, 512], BF16)
                mprev = consts_bf[:, C_MPREV:C_MPREV + 128]
                mprevc = consts_bf[:, C_MPREVC:C_MPREVC + 128]
                mcur = consts_bf[:, C_TRI:C_TRI + 128]
                for mi, pat in enumerate(((mprev, mcur, mprev, mcur), (mprevc, mcur, mprev, mcur), (mprevc, mcur, mprevc, mcur))):
                    for j, src in enumerate(pat):
                        p.op("dve", lambda mi=mi, j=j, src=src: nc.vector.tensor_copy(out=masks[:, mi, j * 128:(j + 1) * 128], in_=src),
                             reads=["consts_bf"], writes=["masks"])
                sel_f = consts[0:65, C_SEL:C_SEL + 64]
                ones_f = consts_bf[0:64, C_ONES:C_ONES + 1]
                gb64 = sbuf(esT, "gb64", [64, 8], F32)
                dma("sp", gb64[:], dram["mix_gb64"], writes=["gb64"])
                allv = [(d_, n, r) for d_ in PATTERNS for (n, r) in vt[d_]]
                tgc = [0]

                def build_v(hp):
                    vb = vaugp[hp % 2]
                    vbk = ("vaugp", hp % 2)
                    for g0 in range(0, nv, 8):
                        grp = allv[g0:g0 + 8]
                        tg = tgc[0]
                        tgc[0] += 1
                        b = 6 + (tg % 2)
                        ptr = psb[b][:].bitcast(BF16)
                        for j, (d_, n, r) in enumerate(grp):
                            span = 128 * d_
                            s0 = span * n + r
                            p.op("pe", lambda j=j, s0=s0, span=span, d_=d_, ptr=ptr: nc.tensor.transpose(
                                ptr[:, j * 128:(j + 1) * 128], vT[:, hp, s0:s0 + span - d_ + 1:d_], ident_bf),
                                reads=[("vT", hp), "consts_bf"], writes=[PS(b)])
                            yield
                        ng = len(grp)
                        src = ptr[:, 0:ng * 128].rearrange("p (t h c) -> p t h c", t=ng, h=2)
                        if tg % 2 == 0:
                            p.op("act", lambda src=src, g0=g0, ng=ng: nc.scalar.copy(out=vb[:, g0:g0 + ng, :, 0:64], in_=src), reads=[PS(b)], writes=[vbk])
                            yield
                        else:
                            p.op("dve", lambda src=src, g0=g0, ng=ng: nc.vector.tensor_copy(out=vb[:, g0:g0 + ng, :, 0:64], in_=src), reads=[PS(b)], writes=[vbk])
                            yield

                def unit_list(h):
                    us = []
                    for d_ in PATTERNS:
                        if d_ == 1:
                            blocks = [(n, 0) for n in range(16, 32)]
                        elif d_ == 4:
                            blocks = [(n, r) for n in range(4, 8) for r in range(4)]
                        else:
                            blocks = [(1, r) for r in range(16)]
                        for ui in range(0, 16, 2):
                            us.append((h, d_, blocks[ui:ui + 2]))
                    return us

                def emit_qk(u, su):
                    h, d_, pair = u
                    hp, hh = h // 2, h % 2
                    po = 64 * hh
                    span = 128 * d_
                    sb_ = su % 2
                    for j, (n, r) in enumerate(pair):
                        q0 = span * n + r - NTOK
                        qs = qT[po:po + 64, hp, q0:q0 + span - d_ + 1:d_]
                        kp0 = span * (n - 1) + r
                        kc0 = span * n + r
                        p.op("pe", lambda j=j, qs=qs, kp0=kp0: nc.tensor.matmul(
                            psb[sb_][:, j * 256:j * 256 + 128], lhsT=kT[po:po + 64, hp, kp0:kp0 + span - d_ + 1:d_], rhs=qs, start=True, stop=True),
                            reads=[("kT", hp), ("qT", hp)], writes=[PS(sb_)])
                        p.op("pe", lambda j=j, qs=qs, kc0=kc0: nc.tensor.matmul(
                            psb[sb_][:, j * 256 + 128:j * 256 + 256], lhsT=kT[po:po + 64, hp, kc0:kc0 + span - d_ + 1:d_], rhs=qs, start=True, stop=True),
                            reads=[("kT", hp), ("qT", hp)], writes=[PS(sb_)])

                def emit_mid(u, su):
                    h, d_, pair = u
                    span = 128 * d_
                    sb_ = su % 2
                    ctx = tuple((span * (n - 1) + r) < NTOK for (n, r) in pair)
                    mi = {(False, False): 0, (True, False): 1, (True, True): 2}[ctx]
                    eb, ebk = Eb.next()
                    em, emk = Em.next()
                    p.op("act", lambda: nc.scalar.activation(out=eb[:], in_=psb[sb_][:], func=AF.Exp, scale=0.125), reads=[PS(sb_)], writes=[ebk])
                    p.op("dve", lambda: nc.vector.tensor_tensor(out=em[:], in0=eb[:], in1=masks[:, mi, :], op=ALU.mult), reads=[ebk, "masks"], writes=[emk])
                    return em, emk

                def emit_pv(u, su, em, emk):
                    h, d_, pair = u
                    hp, hh = h // 2, h % 2
                    vb = vaugp[hp % 2]
                    vbk = ("vaugp", hp % 2)
                    ob_ = 2 + su % 2
                    for j, (n, r) in enumerate(pair):
                        vp = vidx[(d_, n - 1, r)]
                        vc = vidx[(d_, n, r)]
                        p.op("pe", lambda j=j, vp=vp: nc.tensor.matmul(
                            psb[ob_][0:65, j * 128:(j + 1) * 128], lhsT=vb[:, vp, hh, 0:65], rhs=em[:, j * 256:j * 256 + 128], start=True, stop=False),
                            reads=[vbk, ("vones", hp % 2), emk], writes=[PS(ob_)])
                        p.op("pe", lambda j=j, vc=vc: nc.tensor.matmul(
                            psb[ob_][0:65, j * 128:(j + 1) * 128], lhsT=vb[:, vc, hh, 0:65], rhs=em[:, j * 256 + 128:j * 256 + 256], start=False, stop=True),
                            reads=[vbk, ("vones", hp % 2), emk], writes=[PS(ob_)])

                def emit_acc(u, su):
                    h, d_, pair = u
                    acc = accT[h % 2]
                    acck = ("accT", h % 2)
                    span = 128 * d_
                    ob_ = 2 + su % 2
                    n0, r0 = pair[0]
                    src = psb[ob_][0:65, 0:256].rearrange("p (b j) -> p b j", b=2)
                    if d_ == 1:
                        q0 = span * n0 - NTOK
                        dest = acc[0:65, q0:q0 + 256].rearrange("p (b j) -> p b j", b=2)
                        p.op("act", lambda: nc.scalar.copy(out=dest, in_=src), reads=[PS(ob_)], writes=[acck])
                    else:
                        s0 = span * n0 - NTOK
                        dest = acc[0:65, s0:s0 + span].rearrange("p (j d) -> p d j", d=d_)[:, r0:r0 + 2, :]
                        p.op("dve", lambda: nc.vector.tensor_tensor(out=dest, in0=dest, in1=src, op=ALU.add), reads=[PS(ob_), acck], writes=[acck])

                def finalize(h):
                    hp, hh = h // 2, h % 2
                    po = 64 * hh
                    acc = accT[h % 2]
                    acck = ("accT", h % 2)
                    for c in range(4):
                        p.op("pe", lambda c=c: nc.tensor.matmul(psb[4][0:64, :], lhsT=sel_f, rhs=acc[0:65, c * 512:(c + 1) * 512], start=True, stop=True),
                             reads=["consts", acck], writes=[PS(4)])
                        yield
                        rd, rdk = RD.next()
                        bq_, bqk = BQ.next()
                        bs_, bsk = BS.next()
                        p.op("dve", lambda rd=rd: nc.vector.reciprocal(out=rd[:], in_=psb[4][0:64, :]), reads=[PS(4)], writes=[rdk])
                        yield
                        p.op("dve", lambda c=c, rd=rd, bq_=bq_: nc.vector.tensor_tensor(out=bq_[:], in0=acc[0:64, c * 512:(c + 1) * 512], in1=rd[:], op=ALU.mult),
                             reads=[acck, rdk], writes=[bqk])
                        yield
                        p.op("act", lambda bq_=bq_, bs_=bs_: nc.scalar.activation(out=bs_[:], in_=bq_[:], func=AF.Square), reads=[bqk], writes=[bsk])
                        yield
                        for t in range(4):
                            ti = c * 4 + t
                            col = ti * 8 + h
                            p.op("pe", lambda t=t, col=col, bs_=bs_: nc.tensor.matmul(psb[5][:, col:col + 1], lhsT=bs_[0:64, t * 128:(t + 1) * 128], rhs=ones_f, start=True, stop=True),
                                 reads=[bsk, "consts_bf"], writes=[PS(5)])
                            yield
                        p.op("act", lambda c=c, bq_=bq_: nc.scalar.activation(out=bT[po:po + 64, hp, c * 512:(c + 1) * 512], in_=bq_[:], func=AF.Copy, scale=gb64[:, h:h + 1]),
                             reads=[bqk, "gb64"], writes=["bT"])
                        yield
                        if "dbg_b" in dram:
                            dma("sp", dram["dbg_b"][h * 64:(h + 1) * 64, c * 512:(c + 1) * 512], bq_[:], reads=[bqk])
                            yield

                if stage >= 3:
                    units = []
                    for h in range(8):
                        units += unit_list(h)
                    run(build_v(0))
                    emit_qk(units[0], 0)
                    pend = None
                    bg = []

                    def step_bg(n):
                        for _ in range(n):
                            if not bg:
                                return
                            try:
                                next(bg[0])
                            except StopIteration:
                                bg.pop(0)

                    def drain_bg():
                        while bg:
                            step_bg(1)
                    for ui, u in enumerate(units):
                        if ui + 1 < len(units):
                            emit_qk(units[ui + 1], ui + 1)
                        em, emk = emit_mid(u, ui)
                        if pend is not None:
                            emit_acc(*pend)
                            pend = None
                        emit_pv(u, ui, em, emk)
                        pend = (u, ui)
                        step_bg(4)
                        h = u[0]
                        last_of_head = (ui + 1 == len(units)) or units[ui + 1][0] != h
                        if last_of_head:
                            emit_acc(*pend)
                            pend = None
                            drain_bg()
                            bg.append(finalize(h))
                            if h % 2 == 0 and h // 2 + 1 < 4:
                                bg.append(build_v(h // 2 + 1))
                    drain_bg()
                if stage >= 3:
                    p.op("dve", lambda: nc.vector.tensor_copy(out=ssq_b[:].rearrange("p t h -> p (t h)"), in_=psb[5][:, 0:128]), reads=[PS(5)], writes=["ssq_b"])
                    if "dbg_ssqb" in dram:
                        dma("sp", dram["dbg_ssqb"], ssq_b[:].rearrange("p t h -> p (t h)"), reads=["ssq_b"])
                p.barrier()
        scat_keys = []
        hbf_keys = ["hbf_z"]
        h32_keys = []
        with ExitStack() as es3:
            zrow = sbuf(es3, "zrow", [1, D], F32)
            zrow_bf = sbuf(es3, "zrow_bf", [1, D], BF16)
            p.op("dve", lambda: nc.vector.memset(zrow[:], 0.0), writes=["zrow"])
            p.op("dve", lambda: nc.vector.memset(zrow_bf[:], 0.0), writes=["zrow_bf"])
            dma("sp", hbf_d[NTOK:NTOK + 1, :], zrow_bf[:], reads=["zrow_bf"], writes=["hbf_z"])
            dma("sp", eo_d[SLOTS:SLOTS + 1, :], zrow[:], reads=["zrow"], writes=["eo_z"])
            wout = sbuf(es3, "wout", [128, 8, D], BF16)
            for k in range(8):
                dma("pool", wout[:, k, :], wout_d[k * 128:(k + 1) * 128, :], writes=[("wout", k)])
            vec3 = sbuf(es3, "vec3", [128, 2, D], F32)
            dma("sp", vec3[:], vec_d[:, 2:4, :], writes=["vec3"])
            wr = sbuf(es3, "wr", [128, 8, NE], F32)
            dma("sp", wr[:], wr_d.rearrange("(k p) e -> p k e", p=128), writes=["wr"])
            brb = sbuf(es3, "brb", [128, NE], F32)
            dma("sp", brb[:], br_d, writes=["brb"])
            rs = sbuf(es3, "rs", [128, 4, NT], F32)
            p.op("dve", lambda: nc.vector.tensor_reduce(out=rs[:, 2, :], in_=ssq_b[:], axis=AX.X, op=ALU.add), reads=["ssq_b"], writes=["rs"])
            p.op("act", lambda: nc.scalar.activation(out=rs[:, 3, :], in_=ssq_a[:], func=AF.Ln, bias=eps_t[:, 0:1], scale=1.0 / 512), reads=["ssq_a", "eps"], writes=["rs"])
            p.op("act", lambda: nc.scalar.activation(out=rs[:, 0, :], in_=rs[:, 3, :], func=AF.Exp, scale=-0.5), reads=["rs"], writes=["rs"])
            p.op("act", lambda: nc.scalar.activation(out=rs[:, 3, :], in_=rs[:, 2, :], func=AF.Ln, bias=eps_t[:, 0:1], scale=1.0 / 512), reads=["rs", "eps"], writes=["rs"])
            p.op("act", lambda: nc.scalar.activation(out=rs[:, 1, :], in_=rs[:, 3, :], func=AF.Exp, scale=-0.5), reads=["rs"], writes=["rs"])
            X = SRot(es3, "X", 6, [128, D], F32)
            T1 = SRot(es3, "T1", 3, [128, D], F32)
            T2 = SRot(es3, "T2", 3, [128, D], F32)
            HB = SRot(es3, "HB", 3, [128, D], BF16)
            HT = SRot(es3, "HT", 2, [128, 8, 128], F32)
            SM = SRot(es3, "SM", 4, [128, 16], F32)
            LG = SRot(es3, "LG", 2, [128, NE], F32)
            RK = SRot(es3, "RK", 2, [128, 6, NE], F32)
            OH = SRot(es3, "OH", 2, [128, 4, NE], F32)
            MX = SRot(es3, "MX", 2, [128, 24], F32)

            def layer_norm(src, srck, dst, dstk, g_ap, b_ap, gk, jt, jtk, SMr):
                sm, smk = SMr.next()
                p.op("dve", lambda: nc.vector.memset(sm[:], 0.0), writes=[smk])
                yield
                p.op("act", lambda: nc.scalar.activation(out=jt[:], in_=src[:], func=AF.Identity, accum_out=sm[:, 0:1]), reads=[srck, smk], writes=[jtk, smk])
                yield
                p.op("act", lambda: nc.scalar.activation(out=jt[:], in_=src[:], func=AF.Square, accum_out=sm[:, 1:2]), reads=[srck, smk], writes=[jtk, smk])
                yield
                p.op("dve", lambda: nc.vector.tensor_single_scalar(out=sm[:, 2:3], in_=sm[:, 0:1], scalar=1.0 / D, op=ALU.mult), reads=[smk], writes=[smk])
                yield
                p.op("dve", lambda: nc.vector.tensor_tensor(out=sm[:, 3:4], in0=sm[:, 2:3], in1=sm[:, 2:3], op=ALU.mult), reads=[smk], writes=[smk])
                yield
                p.op("dve", lambda: nc.vector.scalar_tensor_tensor(out=sm[:, 4:5], in0=sm[:, 1:2], scalar=1.0 / D, in1=sm[:, 3:4], op0=ALU.mult, op1=ALU.subtract), reads=[smk], writes=[smk])
                yield
                p.op("act", lambda: nc.scalar.activation(out=sm[:, 5:6], in_=sm[:, 4:5], func=AF.Ln, bias=eps_t[:, 0:1], scale=1.0), reads=[smk, "eps"], writes=[smk])
                yield
                p.op("act", lambda: nc.scalar.activation(out=sm[:, 6:7], in_=sm[:, 5:6], func=AF.Exp, scale=-0.5), reads=[smk], writes=[smk])
                yield
                p.op("dve", lambda: nc.vector.scalar_tensor_tensor(out=sm[:, 7:8], in0=sm[:, 2:3], scalar=-1.0, in1=sm[:, 6:7], op0=ALU.mult, op1=ALU.mult), reads=[smk], writes=[smk])
                yield
                p.op("act", lambda: nc.scalar.activation(out=jt[:], in_=src[:], func=AF.Identity, scale=sm[:, 6:7], bias=sm[:, 7:8]), reads=[srck, smk], writes=[jtk])
                yield
                p.op("dve", lambda: nc.vector.tensor_tensor(out=jt[:], in0=jt[:], in1=g_ap, op=ALU.mult), reads=[jtk, gk], writes=[jtk])
                yield
                p.op("dve", lambda: nc.vector.tensor_tensor(out=dst[:], in0=jt[:], in1=b_ap, op=ALU.add), reads=[jtk, gk], writes=[dstk])
                yield

            hh_ = {}

            def stageA(i):
                tsl = slice(i * 128, (i + 1) * 128)
                x_, xk_ = X.next()
                dma("sp", x_[:], x_d[tsl, :], writes=[xk_])
                yield
                for half in range(2):
                    for c in range(4):
                        p.op("pe", lambda half=half, c=c: nc.tensor.matmul(psb[half][:], lhsT=aT[:, c, tsl], rhs=wout[:, c, half * 512:(half + 1) * 512],
                                                                       start=(c == 0), stop=(c == 3)), reads=["aT", ("wout", c)], writes=[PS(half)])
                        yield
                    for c in range(4):
                        p.op("pe", lambda half=half, c=c: nc.tensor.matmul(psb[2 + half][:], lhsT=bT[:, c, tsl], rhs=wout[:, 4 + c, half * 512:(half + 1) * 512],
                                                                       start=(c == 0), stop=(c == 3)), reads=["bT", ("wout", 4 + c)], writes=[PS(2 + half)])
                        yield
                t1, t1k = T1.next()
                t2, t2k = T2.next()
                p.op("act", lambda: nc.scalar.mul(out=x_[:], in_=x_[:], mul=ALPHA), reads=[xk_], writes=[xk_])
                yield
                for half in range(2):
                    hs = slice(half * 512, (half + 1) * 512)
                    p.op("dve", lambda half=half, hs=hs: nc.vector.scalar_tensor_tensor(out=t1[:, hs], in0=psb[half][:], scalar=rs[:, 0, i:i + 1], in1=x_[:, hs], op0=ALU.mult, op1=ALU.add),
                         reads=[PS(half), "rs", xk_], writes=[t1k])
                    yield
                    p.op("dve", lambda half=half, hs=hs: nc.vector.scalar_tensor_tensor(out=t1[:, hs], in0=psb[2 + half][:], scalar=rs[:, 1, i:i + 1], in1=t1[:, hs], op0=ALU.mult, op1=ALU.add),
                         reads=[PS(2 + half), "rs", t1k], writes=[t1k])
                    yield
                h_, hk_ = X.next()
                yield from layer_norm(t1, t1k, h_, hk_, vec3[:, 0, :], vec3[:, 1, :], "vec3", t2, t2k, SM)
                dma("sp", h32_d[tsl, :], h_[:], reads=[hk_], writes=[("h32", i)])
                yield
                h32_keys.append(("h32", i))
                hb, hbk = HB.next()
                p.op("act", lambda: nc.scalar.copy(out=hb[:], in_=h_[:]), reads=[hk_], writes=[hbk])
                yield
                dma("sp", hbf_d[tsl, :], hb[:], reads=[hbk], writes=[("hbf", i)])
                yield
                hbf_keys.append(("hbf", i))
                if "dbg_h" in dram:
                    dma("sp", dram["dbg_h"][tsl, :], h_[:], reads=[hk_])
                    yield
                hh_[i] = (h_, hk_)

            LGall = sbuf(es3, "LGall", [128, NT, NE], F32)
            MX8 = sbuf(es3, "MX8", [128, NT, 8], F32)

            def stageB(i):
                h_, hk_ = hh_.pop(i)
                tsl = slice(i * 128, (i + 1) * 128)
                ht, htk = HT.next()
                for k in range(8):
                    b = 4 + k // 4
                    p.op("pe", lambda k=k, b=b: nc.tensor.transpose(psb[b][:, (k % 4) * 128:(k % 4 + 1) * 128], h_[:, k * 128:(k + 1) * 128], ident_f),
                         reads=[hk_, "consts"], writes=[PS(b)])
                    yield
                p.op("act", lambda: nc.scalar.copy(out=ht[:, 0:4, :], in_=psb[4][:].rearrange("p (k t) -> p k t", k=4)), reads=[PS(4)], writes=[htk])
                yield
                p.op("dve", lambda: nc.vector.tensor_copy(out=ht[:, 4:8, :], in_=psb[5][:].rearrange("p (k t) -> p k t", k=4)), reads=[PS(5)], writes=[htk])
                yield
                for k in range(8):
                    p.op("pe", lambda k=k: nc.tensor.matmul(psb[6][:, 0:NE], lhsT=ht[:, k, :], rhs=wr[:, k, :], start=(k == 0), stop=(k == 7)),
                         reads=[htk, "wr"], writes=[PS(6)])
                    yield
                p.op("dve", lambda: nc.vector.tensor_tensor(out=LGall[:, i, :], in0=psb[6][:, 0:NE], in1=brb[:], op=ALU.add), reads=[PS(6), "brb"], writes=[("LG", i)])
                yield
                p.op("dve", lambda: nc.vector.max(out=MX8[:, i, :], in_=LGall[:, i, :]), reads=[("LG", i)], writes=[("MX8", i)])
                yield
                if "dbg_logits" in dram:
                    dma("sp", dram["dbg_logits"][tsl, :], LGall[:, i, :], reads=[("LG", i)])
                    yield

            RT = sbuf(es3, "RT", [128, 3, NT, NE], F32)
            OHa = sbuf(es3, "OHa", [128, NT, 4, NE], F32)
            OHb = sbuf(es3, "OHb", [128, NT, 4, NE], F32)
            SC = sbuf(es3, "SC", [128, 9, NT, 4], F32)
            RB = 4

            def routing(bi):
                t0, t1 = bi * RB, (bi + 1) * RB
                ts_ = slice(t0, t1)
                nt = t1 - t0
                LGk = [("LG", i) for i in range(t0, t1)]
                MXk = [("MX8", i) for i in range(t0, t1)]
                K = lambda nm: (nm, bi)
                p.op("dve", lambda: nc.vector.tensor_tensor(out=RT[:, 0, ts_], in0=LGall[:, ts_], in1=MX8[:, ts_, 3:4].to_broadcast([128, nt, NE]), op=ALU.is_ge), reads=LGk + MXk, writes=[K("RT0")])
                yield
                p.op("act", lambda: nc.scalar.copy(out=Mall[:, ts_], in_=RT[:, 0, ts_]), reads=[K("RT0")], writes=[("Mall", bi)])
                yield
                for i in range(t0, t1):
                    for i2 in range(i + 1):
                        lhs = consts_bf[:, C_LSTRICT:C_LSTRICT + 128] if i2 == i else consts_bf[:, C_ONES:C_ONES + 128]
                        p.op("pe", lambda i=i, i2=i2, lhs=lhs: nc.tensor.matmul(psb[7][:, i * NE:(i + 1) * NE], lhsT=lhs, rhs=Mall[:, i2, :], start=(i2 == 0), stop=(i2 == i)),
                             reads=[("Mall", i2 // RB), "consts_bf"], writes=[PS(7)])
                        yield
                p.op("dve", lambda: nc.vector.tensor_tensor(out=SC[:, 0, ts_], in0=MX8[:, ts_, 0:4], in1=MX8[:, ts_, 0:1].to_broadcast([128, nt, 4]), op=ALU.subtract), reads=MXk, writes=[K("SC0")])
                yield
                p.op("act", lambda: nc.scalar.activation(out=SC[:, 1, ts_], in_=SC[:, 0, ts_], func=AF.Exp), reads=[K("SC0")], writes=[K("SC1")])
                yield
                p.op("dve", lambda: nc.vector.tensor_reduce(out=SC[:, 2, ts_, 0], in_=SC[:, 1, ts_], axis=AX.X, op=ALU.add), reads=[K("SC1")], writes=[K("SC2")])
                yield
                p.op("dve", lambda: nc.vector.reciprocal(out=SC[:, 2, ts_, 1], in_=SC[:, 2, ts_, 0]), reads=[K("SC2")], writes=[K("SC2b")])
                yield
                p.op("dve", lambda: nc.vector.tensor_tensor(out=SC[:, 3, ts_], in0=SC[:, 1, ts_], in1=SC[:, 2, ts_, 1:2].to_broadcast([128, nt, 4]), op=ALU.mult), reads=[K("SC1"), K("SC2b")], writes=[K("SC3")])
                yield
                lg_bc = LGall[:, ts_].unsqueeze(2).to_broadcast([128, nt, 4, NE])
                mx_bc = MX8[:, ts_, 0:4].unsqueeze(3).to_broadcast([128, nt, 4, NE])
                p.op("dve", lambda: nc.vector.tensor_tensor(out=OHa[:, ts_], in0=lg_bc, in1=mx_bc, op=ALU.is_equal), reads=LGk + MXk, writes=[K("OHa")])
                yield
                p.op("dve", lambda: nc.vector.tensor_copy(out=RT[:, 1, ts_], in_=psb[7][:, t0 * NE:t1 * NE].rearrange("p (t e) -> p t e", t=nt)), reads=[PS(7)], writes=[K("RT1")])
                yield
                iota_bc = consts[:, C_IOTA:C_IOTA + NE].unsqueeze(1).to_broadcast([128, nt, NE])
                p.op("dve", lambda: nc.vector.scalar_tensor_tensor(out=RT[:, 2, ts_], in0=iota_bc, scalar=float(CAP), in1=RT[:, 1, ts_], op0=ALU.mult, op1=ALU.add), reads=["consts", K("RT1")], writes=[K("RT2")])
                yield
                p.op("dve", lambda: nc.vector.tensor_tensor(out=OHb[:, ts_], in0=OHa[:, ts_], in1=RT[:, 1, ts_].unsqueeze(2).to_broadcast([128, nt, 4, NE]), op=ALU.mult), reads=[K("OHa"), K("RT1")], writes=[K("OHb")])
                yield
                p.op("dve", lambda: nc.vector.tensor_reduce(out=SC[:, 4, ts_], in_=OHb[:, ts_], axis=AX.X, op=ALU.add), reads=[K("OHb")], writes=[K("SC4")])
                yield
                p.op("dve", lambda: nc.vector.tensor_tensor(out=OHb[:, ts_], in0=OHa[:, ts_], in1=RT[:, 2, ts_].unsqueeze(2).to_broadcast([128, nt, 4, NE]), op=ALU.mult), reads=[K("OHa"), K("RT2"), K("SC4")], writes=[K("OHb")])
                yield
                p.op("dve", lambda: nc.vector.tensor_reduce(out=SC[:, 5, ts_], in_=OHb[:, ts_], axis=AX.X, op=ALU.add), reads=[K("OHb")], writes=[K("SC5")])
                yield
                p.op("dve", lambda: nc.vector.tensor_scalar(out=SC[:, 6, ts_], in0=SC[:, 4, ts_], scalar1=float(CAP), scalar2=None, op0=ALU.is_lt), reads=[K("SC4")], writes=[K("SC6")])
                yield
                p.op("dve", lambda: nc.vector.scalar_tensor_tensor(out=SC[:, 7, ts_], in0=SC[:, 5, ts_], scalar=-float(SLOTS), in1=SC[:, 6, ts_], op0=ALU.add, op1=ALU.mult), reads=[K("SC5"), K("SC6")], writes=[K("SC7")])
                yield
                p.op("dve", lambda: nc.vector.tensor_scalar(out=SC[:, 8, ts_], in0=SC[:, 7, ts_], scalar1=float(SLOTS), scalar2=None, op0=ALU.add), reads=[K("SC7")], writes=[K("SC8")])
                yield
                p.op("dve", lambda: nc.vector.tensor_copy(out=pos_i[:, t0 * 4:t1 * 4].rearrange("p (t k) -> p t k", k=4), in_=SC[:, 8, ts_]), reads=[K("SC8")], writes=[("pos", i) for i in range(t0, t1)])
                yield
                p.op("dve", lambda: nc.vector.tensor_tensor(out=gates[:, ts_], in0=SC[:, 3, ts_], in1=SC[:, 6, ts_], op=ALU.mult), reads=[K("SC3"), K("SC6")], writes=[("gates", i) for i in range(t0, t1)])
                yield
                if "dbg_pos" in dram:
                    for i in range(t0, t1):
                        dma("sp", dram["dbg_pos"][i * 128:(i + 1) * 128, :], SC[:, 8, i, :], reads=[K("SC8")])
                        dma("sp", dram["dbg_gates"][i * 128:(i + 1) * 128, :], gates[:, i, :], reads=[("gates", i)])
                for i in range(t0, t1):
                    for k in range(4):
                        p.op("pool", lambda i=i, k=k: nc.gpsimd.indirect_dma_start(
                            out=stok_d[:, :], out_offset=bass.IndirectOffsetOnAxis(ap=pos_i[:, i * 4 + k:i * 4 + k + 1], axis=0),
                            in_=tokid_i[:, i:i + 1], in_offset=None),
                            reads=[("pos", i), "tokid_i", "stok_init"], writes=[("scat", i, k)], dma=True)
                        scat_keys.append(("scat", i, k))
                        yield

            if stage >= 4:
                def tileAB(i):
                    yield from stageA(i)
                    yield from stageB(i)
                ents = []
                for i in range(NT):
                    ents.append(("tile", lambda i=i: tileAB(i), 0))
                    if (i + 1) % RB == 0:
                        ents.append(("bg", lambda bi=i // RB: routing(bi), i + 1))
                pipeline2(ents, stagger=18, depth=3)
            p.barrier()
        esAB.close()
        eo_keys = ["eo_z"]
        with ExitStack() as es5:
            bgT = sbuf(es5, "bgT", [128, NE, 8], F32)
            buT = sbuf(es5, "buT", [128, NE, 8], F32)
            dma("sp", bgT[:], bg_d, writes=["bgT"])
            dma("sp", buT[:], bu_d, writes=["buT"])
            bu1 = sbuf(es5, "bu1", [128, NE, 8], F32)
            p.op("dve", lambda: nc.vector.tensor_scalar(out=bu1[:], in0=buT[:], scalar1=1.0, scalar2=None, op0=ALU.add), reads=["buT"], writes=["bu1"])
            WG = [sbuf(es5, f"WG{i}", [128, 8, D], BF16) for i in range(2)]
            WU = [sbuf(es5, f"WU{i}", [128, 8, D], BF16) for i in range(2)]
            WD = [sbuf(es5, f"WD{i}", [128, 8, D], BF16) for i in range(2)]
            BD = [sbuf(es5, f"BD{i}", [128, D], F32) for i in range(2)]
            XB = [sbuf(es5, f"XB{i}", [128, NBLK, D], BF16) for i in range(2)]
            STI = [sbuf(es5, f"STI{i}", [128, NBLK], I32) for i in range(2)]
            XT = [sbuf(es5, f"XT{i}", [128, 8, CAP], BF16) for i in range(2)]
            ACT_T = sbuf(es5, "ACTT", [128, 8, CAP], BF16)
            GC = SRot(es5, "GC", 2, [128, CAP], F32)
            SG = SRot(es5, "SG", 2, [128, CAP], F32)
            UC = SRot(es5, "UC", 2, [128, CAP], F32)
            EO = SRot(es5, "EO", 2, [128, D], F32)

            def load_w(e):
                j = e % 2
                for (W_, wd_, nm) in ((WG, wg_d, "WG"), (WU, wu_d, "WU"), (WD, wd_d, "WD")):
                    for hk in range(2):
                        dma("pool", W_[j][:, hk * 4:(hk + 1) * 4, :], wd_[e, hk * 512:(hk + 1) * 512, :].rearrange("(k p) f -> p k f", p=128), writes=[(nm, j, hk)])
                dma("sp", BD[j][:], bd_d[e:e + 1, :].to_broadcast([128, D]), writes=[("BD", j)])

            def load_x(e):
                j = e % 2
                dma("sp", STI[j][:], stok_d[e * CAP:(e + 1) * CAP, :].rearrange("(p b) o -> p (b o)", p=128), reads=scat_keys + ["stok_init"], writes=[("STI", j)])
                for blk in range(NBLK):
                    p.op("pool", lambda blk=blk: nc.gpsimd.indirect_dma_start(
                        out=XB[j][:, blk, :], out_offset=None, in_=hbf_d[:, :],
                        in_offset=bass.IndirectOffsetOnAxis(ap=STI[j][:, blk:blk + 1], axis=0)),
                        reads=[("STI", j)] + hbf_keys, writes=[("XB", j, blk)], dma=True)

            nexp = NE if stage >= 5 else 0
            if nexp:
                load_x(0)
                load_w(0)
            tr = 0
            for e in range(nexp):
                j = e % 2
                if e + 1 < nexp:
                    load_x(e + 1)
                    load_w(e + 1)
                for blk in range(NBLK):
                    b = 6 + tr % 2
                    tr += 1
                    ptr = psb[b][:].bitcast(BF16)
                    for k in range(8):
                        p.op("pe", lambda blk=blk, k=k, ptr=ptr: nc.tensor.transpose(ptr[:, k * 128:(k + 1) * 128], XB[j][:, blk, k * 128:(k + 1) * 128], ident_bf),
                             reads=[("XB", j, blk), "consts_bf"], writes=[PS(b)])
                    src = ptr[:, :].rearrange("p (k t) -> p k t", k=8)
                    if blk % 2 == 0:
                        p.op("act", lambda blk=blk, src=src: nc.scalar.copy(out=XT[j][:, :, blk * 128:(blk + 1) * 128], in_=src), reads=[PS(b)], writes=[("XT", j)])
                    else:
                        p.op("dve", lambda blk=blk, src=src: nc.vector.tensor_copy(out=XT[j][:, :, blk * 128:(blk + 1) * 128], in_=src), reads=[PS(b)], writes=[("XT", j)])
                for f in range(8):
                    gb_ = f % 2
                    ub_ = 2 + f % 2
                    for k in range(8):
                        p.op("pe", lambda f=f, k=k: nc.tensor.matmul(psb[gb_][:, 0:CAP], lhsT=WG[j][:, k, f * 128:(f + 1) * 128], rhs=XT[j][:, k, :], start=(k == 0), stop=(k == 7)),
                             reads=[("WG", j, k // 4), ("XT", j)], writes=[PS(gb_)])
                    for k in range(8):
                        p.op("pe", lambda f=f, k=k: nc.tensor.matmul(psb[ub_][:, 0:CAP], lhsT=WU[j][:, k, f * 128:(f + 1) * 128], rhs=XT[j][:, k, :], start=(k == 0), stop=(k == 7)),
                             reads=[("WU", j, k // 4), ("XT", j)], writes=[PS(ub_)])
                    gc, gck = GC.next()
                    sg, sgk = SG.next()
                    uc, uck = UC.next()
                    p.op("dve", lambda f=f, gc=gc: nc.vector.tensor_scalar(out=gc[:], in0=psb[gb_][:, 0:CAP], scalar1=bgT[:, e, f:f + 1], scalar2=7.0, op0=ALU.add, op1=ALU.min),
                         reads=[PS(gb_), "bgT"], writes=[gck])
                    p.op("act", lambda gc=gc, sg=sg: nc.scalar.activation(out=sg[:], in_=gc[:], func=AF.Gelu_apprx_sigmoid), reads=[gck], writes=[sgk])
                    p.op("dve", lambda f=f, uc=uc: nc.vector.tensor_scalar(out=uc[:], in0=psb[ub_][:, 0:CAP], scalar1=bu1[:, e, f:f + 1], scalar2=-6.0, op0=ALU.add, op1=ALU.max),
                         reads=[PS(ub_), "bu1"], writes=[uck])
                    p.op("dve", lambda f=f, uc=uc, sg=sg: nc.vector.scalar_tensor_tensor(out=ACT_T[:, f, :], in0=uc[:], scalar=8.0, in1=sg[:], op0=ALU.min, op1=ALU.mult),
                         reads=[uck, sgk], writes=[("ACTT", f)])
                for blk in range(NBLK):
                    eo, eok = EO.next()
                    for half in range(2):
                        db_ = 4 + half
                        for f in range(8):
                            p.op("pe", lambda blk=blk, half=half, f=f: nc.tensor.matmul(psb[db_][:], lhsT=ACT_T[:, f, blk * 128:(blk + 1) * 128], rhs=WD[j][:, f, half * 512:(half + 1) * 512],
                                                                                     start=(f == 0), stop=(f == 7)),
                                 reads=[("ACTT", f), ("WD", j, f // 4)], writes=[PS(db_)])
                        p.op("dve", lambda half=half, eo=eo: nc.vector.tensor_tensor(out=eo[:, half * 512:(half + 1) * 512], in0=psb[db_][:], in1=BD[j][:, half * 512:(half + 1) * 512], op=ALU.add),
                             reads=[PS(db_), ("BD", j)], writes=[eok])
                    dma("sp", eo_d[e * CAP:(e + 1) * CAP, :].rearrange("(p b) d -> p b d", b=NBLK)[:, blk, :], eo[:], reads=[eok], writes=[("eo", e, blk)])
                    eo_keys.append(("eo", e, blk))
            p.barrier()

        with ExitStack() as es6:
            vec6 = sbuf(es6, "vec6", [128, 2, D], F32)
            dma("sp", vec6[:], vec_d[:, 4:6, :], writes=["vec6"])
            Hh = SRot(es6, "Hh", 5, [128, D], F32)
            G4 = SRot(es6, "G4", 20, [128, D], F32)
            AC = SRot(es6, "AC", 3, [128, D], F32)
            JT = SRot(es6, "JT", 3, [128, D], F32)
            OT = SRot(es6, "OT", 3, [128, D], F32)
            SM = SRot(es6, "SM2", 4, [128, 16], F32)
            def fetch(i):
                tsl = slice(i * 128, (i + 1) * 128)
                hh, hhk = Hh.next()
                dma("sp", hh[:], h32_d[tsl, :], reads=h32_keys, writes=[hhk])
                gs = []
                for k in range(4):
                    g_, gk_ = G4.next()
                    p.op("pool", lambda k=k, g_=g_: nc.gpsimd.indirect_dma_start(
                        out=g_[:], out_offset=None, in_=eo_d[:, :],
                        in_offset=bass.IndirectOffsetOnAxis(ap=pos_i[:, i * 4 + k:i * 4 + k + 1], axis=0)),
                        reads=[("pos", i)] + eo_keys, writes=[gk_], dma=True)
                    gs.append((g_, gk_))
                return hh, hhk, gs

            def combine(i, hh, hhk, gs):
                tsl = slice(i * 128, (i + 1) * 128)
                ac, ack = AC.next()
                p.op("act", lambda: nc.scalar.activation(out=ac[:], in_=gs[0][0][:], func=AF.Copy, scale=gates[:, i, 0:1]), reads=[gs[0][1], ("gates", i)], writes=[ack])
                yield
                for k in range(1, 4):
                    p.op("dve", lambda k=k: nc.vector.scalar_tensor_tensor(out=ac[:], in0=gs[k][0][:], scalar=gates[:, i, k:k + 1], in1=ac[:], op0=ALU.mult, op1=ALU.add),
                         reads=[gs[k][1], ("gates", i), ack], writes=[ack])
                    yield
                p.op("dve", lambda: nc.vector.scalar_tensor_tensor(out=ac[:], in0=hh[:], scalar=ALPHA, in1=ac[:], op0=ALU.mult, op1=ALU.add), reads=[hhk, ack], writes=[ack])
                yield
                if "dbg_pre2" in dram:
                    dma("sp", dram["dbg_pre2"][tsl, :], ac[:], reads=[ack])
                    yield
                jt, jtk = JT.next()
                ot, otk = OT.next()
                yield from layer_norm(ac, ack, ot, otk, vec6[:, 0, :], vec6[:, 1, :], "vec6", jt, jtk, SM)
                dma("sp", out_d[tsl, :], ot[:], reads=[otk], writes=[("out", i)])
                yield

            if stage >= 6:
                ft = {0: fetch(0), 1: fetch(1), 2: fetch(2)}

                def tileC(i):
                    if i + 3 < NT:
                        ft[i + 3] = fetch(i + 3)
                    yield from combine(i, *ft.pop(i))
                pipeline([lambda i=i: tileC(i) for i in range(NT)], stagger=8, depth=2)
        p.finish("sp")
    p.dram = dram
    return nc, p


def host_consts(flag):
    c = np.zeros((128, NCONST), np.float32)
    i = np.arange(128)
    c[:, C_IDENT:C_IDENT + 128] = np.eye(128, dtype=np.float32)
    c[:, C_TRI:C_TRI + 128] = (i[:, None] <= i[None, :])
    c[:, C_MPREV:C_MPREV + 128] = (i[:, None] >= i[None, :])
    c[:, C_MPREVC:C_MPREVC + 128] = flag * (i[:, None] >= i[None, :])
    c[:, C_LSTRICT:C_LSTRICT + 128] = (i[:, None] < i[None, :])
    c[:, C_ONES:C_ONES + 128] = 1.0
    c[:, C_IOTA:C_IOTA + 32] = np.arange(32)[None, :]
    c[:, C_TOKID:C_TOKID + 16] = np.arange(16)[None, :] * 128 + i[:, None]
    c[64, C_SEL:C_SEL + 64] = 1.0
    return c


def make_in_maps(inputs, cores=range(8)):
    f = lambda a: np.ascontiguousarray(np.asarray(a, dtype=np.float32))
    x = np.asarray(inputs["x"], dtype=np.float32)
    shared = {
        "w_in": f(inputs["w_in"][0]),
        "sgu_wT": f(np.transpose(inputs["sgu_w"][0], (2, 0, 1))),
        "sgu_bT": f(inputs["sgu_b"][0].T),
        "mix_gb64": f(inputs["mix_norm_g"][0, 512:].reshape(8, 64).T),
        "mix_gb": f(inputs["mix_norm_g"][0, 512:].reshape(4, 128).T),
        "w_out": f(inputs["w_out"][0]),
        "w_router": f(inputs["w_router"][0]),
        "b_router_bc": f(np.broadcast_to(inputs["b_router"][0][None, :], (128, NE))),
        "w_gate": f(inputs["w_gate"][0]),
        "w_up": f(inputs["w_up"][0]),
        "w_down": f(inputs["w_down"][0]),
        "b_gateT": f(np.transpose(inputs["b_gate"][0].reshape(NE, 8, 128), (2, 0, 1))),
        "b_upT": f(np.transpose(inputs["b_up"][0].reshape(NE, 8, 128), (2, 0, 1))),
        "b_down": f(inputs["b_down"][0]),
    }
    rows = np.zeros((6, D), np.float32)
    rows[0, :512] = inputs["sgu_ln_g"][0]
    rows[0, 512:] = inputs["sgu_ln_b"][0]
    rows[1, :512] = inputs["mix_norm_g"][0, :512]
    rows[2] = inputs["ln1_g"][0]
    rows[3] = inputs["ln1_b"][0]
    rows[4] = inputs["ln2_g"][0]
    rows[5] = inputs["ln2_b"][0]
    shared["vecs"] = f(np.broadcast_to(rows[None], (128, 6, D)))
    maps = []
    for c in cores:
        b, half = c // 2, c % 2
        own = x[b, half * NTOK:(half + 1) * NTOK]
        xT = np.zeros((D, NEXT), np.float32)
        if half == 1:
            xT[:, :NTOK] = x[b, :NTOK].T
        xT[:, NTOK:] = own.T
        m = dict(shared)
        m["xT"] = xT
        m["x"] = f(own)
        m["consts"] = host_consts(float(half))
        maps.append(m)
    return maps


_NC_CACHE = {}


def kernel(**inputs):
    if "nc" not in _NC_CACHE:
        _NC_CACHE["nc"] = build()[0]
    nc = _NC_CACHE["nc"]
    maps = make_in_maps(inputs)
    res = run_bass_kernel_spmd(nc, maps, core_ids=list(range(8)))
    out = np.zeros((4, 4096, D), np.float32)
    for c in range(8):
        b, half = c // 2, c % 2
        out[b, half * NTOK:(half + 1) * NTOK] = res.results[c]["out"]
    return out
```

```python
import numpy as np
from contextlib import ExitStack
import concourse.bass as bass
import concourse.mybir as mybir
from concourse.bass_utils import run_bass_kernel_spmd

F32 = mybir.dt.float32
BF16 = mybir.dt.bfloat16
I32 = mybir.dt.int32
U32 = mybir.dt.uint32
AF = mybir.ActivationFunctionType
ALU = mybir.AluOpType
AX = mybir.AxisListType

D = 1024
NTOK = 2048
NEXT = 4096
NT = NTOK // 128
NE = 32
TOPK = 4
CAP = 512
NBLK = CAP // 128
ALPHA = 2.0 ** 0.25
EPS = 1e-5
PATTERNS = (1, 4, 16)

C_IDENT = 0
C_TRI = 128
C_MPREV = 256
C_MPREVC = 384
C_LSTRICT = 512
C_ONES = 640
C_IOTA = 768
C_TOKID = 800
C_SEL = 816
NCONST = 880


class Prog:
    COMPUTE = ("pe", "act", "dve", "pool")

    def __init__(self, nc, n_dma_sems=20, same_engine_sync=True):
        self.nc = nc
        self.es = ExitStack()
        self.eng = {"pe": nc.tensor, "act": nc.scalar, "dve": nc.vector, "pool": nc.gpsimd, "sp": nc.sync}
        self.sem = {}
        self.cnt = {}
        for e in self.COMPUTE:
            self.sem[e] = self.es.enter_context(nc.semaphore("sem_" + e))
            self.cnt[e] = 0
        self.known = {e: {} for e in self.eng}
        self.last_w = {}
        self.readers = {}
        self.same_engine_sync = same_engine_sync
        self.dma_pool = {}
        self.dma_idx = {}
        for q in ("sp", "act", "pool"):
            self.dma_pool[q] = [[self.es.enter_context(nc.semaphore(f"dsem_{q}_{i}")), 0] for i in range(n_dma_sems)]
            self.dma_idx[q] = 0
        self.n_inst = {e: 0 for e in self.eng}

    def sb(self, name, shape, dt):
        return self.es.enter_context(self.nc.sbuf_tensor("s_" + name, shape, dt))

    def ps(self, name, shape, dt):
        return self.es.enter_context(self.nc.psum_tensor(name, shape, dt))

    def _wait(self, e, tok):
        sem, val, src = tok
        if src == e and (e == "pe" or not self.same_engine_sync):
            return
        k = self.known[e]
        sid = id(sem)
        if k.get(sid, 0) >= val:
            return
        self.eng[e].wait_ge(sem, val)
        k[sid] = val

    def op(self, e, fn, reads=(), writes=(), dma=False):
        deps = {}

        def add(t):
            key = id(t[0])
            if key not in deps or deps[key][1] < t[1]:
                deps[key] = t
        for kk in reads:
            t = self.last_w.get(kk)
            if t is not None:
                add(t)
        for kk in writes:
            t = self.last_w.get(kk)
            if t is not None:
                add(t)
            for t in self.readers.get(kk, ()):
                add(t)
        for t in deps.values():
            self._wait(e, t)
        if dma:
            pool = self.dma_pool[e]
            i = self.dma_idx[e]
            self.dma_idx[e] = (i + 1) % len(pool)
            slot = pool[i]
            if slot[1] > 0:
                self._wait(e, (slot[0], slot[1], "dma"))
            inst = fn()
            slot[1] += 16
            inst.then_inc(slot[0], 16)
            tok = (slot[0], slot[1], "dma")
        else:
            inst = fn()
            self.cnt[e] += 1
            inst.then_inc(self.sem[e], 1)
            tok = (self.sem[e], self.cnt[e], e)
        self.n_inst[e] += 1
        for kk in writes:
            self.last_w[kk] = tok
            self.readers[kk] = []
        for kk in reads:
            if kk in writes:
                continue
            self.readers.setdefault(kk, []).append(tok)
        return tok

    def barrier(self):
        for e in self.eng:
            self.finish(e)

    def finish(self, e="sp"):
        for pool in self.dma_pool.values():
            for sem, val in pool:
                if val > 0:
                    self._wait(e, (sem, val, "dma"))
        for c in self.COMPUTE:
            if self.cnt[c] > 0:
                self._wait(e, (self.sem[c], self.cnt[c], c))


class Rot:
    def __init__(self, p, name, n, shape, dt):
        self.tiles = [p.sb(f"{name}{i}", shape, dt) for i in range(n)]
        self.name = name
        self.n = n
        self.i = 0

    def next(self):
        j = self.i % self.n
        self.i += 1
        return self.tiles[j], (self.name, j)


def v_tiles():
    out = {}
    out[1] = [(n, 0) for n in range(15, 32)]
    out[4] = [(n, r) for n in range(3, 8) for r in range(4)]
    out[16] = [(n, r) for n in range(0, 2) for r in range(16)]
    return out


def build(stage=99, dbg=()):
    nc = bass.Bass("TRN2", target_bir_lowering=False)
    dram = {}

    def din(name, shape, dt=F32):
        dram[name] = nc.dram_tensor(name, list(shape), dt, kind="ExternalInput").ap()
        return dram[name]

    def dout(name, shape, dt=F32):
        dram[name] = nc.dram_tensor(name, list(shape), dt, kind="ExternalOutput").ap()
        return dram[name]

    xT_d = din("xT", [D, NEXT])
    x_d = din("x", [NTOK, D])
    win_d = din("w_in", [D, 2560])
    consts_d = din("consts", [128, NCONST])
    sguw_d = din("sgu_wT", [128, 8, 128])
    sgub_d = din("sgu_bT", [128, 8])
    vec_d = din("vecs", [128, 6, D])
    din("mix_gb64", [64, 8])
    gb_d = din("mix_gb", [128, 4])
    wout_d = din("w_out", [D, D])
    wr_d = din("w_router", [D, NE])
    br_d = din("b_router_bc", [128, NE])
    if stage >= 5:
        wg_d = din("w_gate", [NE, D, D])
        wu_d = din("w_up", [NE, D, D])
        wd_d = din("w_down", [NE, D, D])
    bg_d = din("b_gateT", [128, NE, 8])
    bu_d = din("b_upT", [128, NE, 8])
    bd_d = din("b_down", [NE, D])
    out_d = dout("out", [NTOK, D])
    for name, shape in dbg:
        dout(name, shape)

    p = Prog(nc)
    with p.es:
        E = p.eng
        psb = [p.ps(f"psb{i}", [128, 512], F32) for i in range(8)]

        def PS(b):
            return ("ps", b)

        def weave(*gens):
            its = [g for g in gens if g is not None]
            while its:
                for g in list(its):
                    try:
                        next(g)
                    except StopIteration:
                        its.remove(g)

        def run(g):
            for _ in g:
                pass

        def pipeline2(ents, stagger, depth):
            pending = list(ents)
            active = []
            since = stagger
            tiles_done = 0
            while pending or active:
                n_tiles = sum(1 for k, _ in active if k == "tile")
                for ent in list(pending):
                    kind, fact, need = ent
                    if kind == "tile":
                        if n_tiles < depth and since >= stagger:
                            active.append((kind, fact()))
                            pending.remove(ent)
                            since = 0
                            n_tiles += 1
                        break
                    if tiles_done >= need:
                        active.append((kind, fact()))
                        pending.remove(ent)
                for item in list(active):
                    try:
                        next(item[1])
                    except StopIteration:
                        active.remove(item)
                        if item[0] == "tile":
                            tiles_done += 1
                since += 1

        def pipeline(facts, stagger, depth):
            active = []
            idx = 0
            since = stagger
            while idx < len(facts) or active:
                if idx < len(facts) and len(active) < depth and since >= stagger:
                    active.append(facts[idx]())
                    idx += 1
                    since = 0
                for g in list(active):
                    try:
                        next(g)
                    except StopIteration:
                        active.remove(g)
                since += 1

        def sbuf_raw(es, name, shape, dt):
            return es.enter_context(nc.sbuf_tensor("s_" + name, shape, dt))

        def sbuf(es, name, shape, dt):
            return es.enter_context(nc.sbuf_tensor("s_" + name, shape, dt))

        class SRot:
            def __init__(self, es, name, n, shape, dt):
                self.tiles = [sbuf(es, f"{name}{i}", shape, dt) for i in range(n)]
                self.name, self.n, self.i = name, n, 0

            def next(self):
                j = self.i % self.n
                self.i += 1
                return self.tiles[j], (self.name, j)

        def dma(q, out, in_, reads=(), writes=()):
            return p.op(q, lambda: E[q].dma_start(out=out, in_=in_), reads=reads, writes=writes, dma=True)

        consts = p.sb("consts", [128, NCONST], F32)
        consts_bf = p.sb("consts_bf", [128, NCONST], BF16)
        dma("sp", consts[:], consts_d, writes=["consts"])
        dma("pool", consts_bf[:], consts_d, writes=["consts_bf"])
        sgub = p.sb("sgub", [128, 8], F32)
        dma("sp", sgub[:], sgub_d, writes=["sgub"])
        gb = p.sb("gb", [128, 4], F32)
        dma("sp", gb[:], gb_d, writes=["gb"])
        wcT = p.sb("wcT", [128, 8, 128], BF16)
        eps_t = p.sb("eps_t", [128, 1], F32)
        p.op("dve", lambda: nc.vector.memset(eps_t[:], EPS), writes=["eps"])
        ident_bf = consts_bf[:, C_IDENT:C_IDENT + 128]
        ident_f = consts[:, C_IDENT:C_IDENT + 128]

        ssq_a = p.sb("ssq_a", [128, NT], F32)
        ssq_b = p.sb("ssq_b", [128, NT, 8], F32)
        p.op("dve", lambda: nc.vector.memset(ssq_a[:], 0.0), writes=["ssq_a"])
        SLOTS = NE * CAP
        hbf_d = nc.dram_tensor("hbf_scr", [NTOK + 1, D], BF16).ap()
        h32_d = nc.dram_tensor("h32_scr", [NTOK, D], F32).ap()
        stok_d = nc.dram_tensor("stok_scr", [SLOTS + 128, 1], I32).ap()
        eo_d = nc.dram_tensor("eo_scr", [SLOTS + 1, D], F32).ap()
        gates = p.sb("gates", [128, NT, 4], F32)
        pos_i = p.sb("pos_i", [128, NT * 4], I32)
        Mall = p.sb("Mall", [128, NT, NE], BF16)
        tokid_i = p.sb("tokid_i", [128, NT], I32)
        p.op("dve", lambda: nc.vector.tensor_copy(out=tokid_i[:], in_=consts[:, C_TOKID:C_TOKID + NT]), reads=["consts"], writes=["tokid_i"])
        fill = p.sb("fill", [128, (SLOTS + 128) // 128], I32)
        p.op("dve", lambda: nc.vector.memset(fill[:], NTOK), writes=["fill"])
        dma("sp", stok_d.rearrange("(p f) o -> p (f o)", p=128), fill[:], reads=["fill"], writes=["stok_init"])
        esAB = ExitStack()
        aT = sbuf_raw(esAB, "aT", [128, 4, NTOK], BF16)
        bT = sbuf_raw(esAB, "bT", [128, 4, NTOK], BF16)
        vt = v_tiles()
        vidx = {}
        nv = 0
        for d_ in PATTERNS:
            for nr in vt[d_]:
                vidx[(d_,) + nr] = nv
                nv += 1

        with ExitStack() as esQ:
            qT = sbuf(esQ, "qT", [128, 4, NTOK], BF16)
            kT = sbuf(esQ, "kT", [128, 4, NEXT], BF16)
            vT = sbuf(esQ, "vT", [128, 4, NEXT], BF16)
            with ExitStack() as esX:
                xTo = sbuf(esX, "xT_own", [128, 8, NTOK], BF16)
                xT_v = xT_d.rearrange("(k p) t -> p k t", p=128)
                with ExitStack() as esB:
                    xTc = sbuf(esB, "xT_ctx", [128, 8, NTOK], BF16)
                    wkv = sbuf(esB, "win_kv", [128, 8, 1024], BF16)
                    sguw = sbuf(esB, "sguw", [128, 8, 128], F32)
                    dma("sp", sguw[:], sguw_d, writes=["sguw"])
                    tri_bc = consts[:, C_TRI:C_TRI + 128].unsqueeze(1).to_broadcast([128, 8, 128])
                    p.op("dve", lambda: nc.vector.tensor_tensor(out=wcT[:], in0=sguw[:], in1=tri_bc, op=ALU.mult),
                         reads=["sguw", "consts"], writes=["wcT"])
                    for k in range(0, 8, 2):
                        dma("pool", wkv[:, k:k + 2, :], win_d[k * 128:(k + 2) * 128, 1536:2560].rearrange("(k p) c -> p k c", p=128), writes=[("wkv", k), ("wkv", k + 1)])
                    for tcl in range(4):
                        dma("pool", xTc[:, :, tcl * 512:(tcl + 1) * 512], xT_v[:, :, tcl * 512:(tcl + 1) * 512], writes=[("xTc", tcl)])
                    for tcl in range(4):
                        dma("pool", xTo[:, :, tcl * 512:(tcl + 1) * 512], xT_v[:, :, NTOK + tcl * 512:NTOK + (tcl + 1) * 512], writes=[("xTo", tcl)])
                    ev = 0
                    for tc in range(8 if stage >= 2 else 0):
                        for (dst, dname, col0) in ((kT, "kT", 0), (vT, "vT", 512)):
                            for hp in range(4):
                                b = ev % 6
                                src, srck = (xTo, ("xTo", tc - 4)) if tc >= 4 else (xTc, ("xTc", tc))
                                tcl = tc % 4
                                for k in range(8):
                                    p.op("pe", lambda k=k, b=b, src=src, tcl=tcl: nc.tensor.matmul(
                                        psb[b][:], lhsT=wkv[:, k, col0 + hp * 128:col0 + (hp + 1) * 128],
                                        rhs=src[:, k, tcl * 512:(tcl + 1) * 512], start=(k == 0), stop=(k == 7)),
                                        reads=[srck, ("wkv", k)], writes=[PS(b)])
                                if ev % 2 == 0:
                                    p.op("act", lambda b=b: nc.scalar.copy(out=dst[:, hp, tc * 512:(tc + 1) * 512], in_=psb[b][:]), reads=[PS(b)], writes=[(dname, hp)])
                                else:
                                    p.op("dve", lambda b=b: nc.vector.tensor_copy(out=dst[:, hp, tc * 512:(tc + 1) * 512], in_=psb[b][:]), reads=[PS(b)], writes=[(dname, hp)])
                                ev += 1
                    p.barrier()
                with ExitStack() as esA:
                    win = sbuf(esA, "win_uvq", [128, 8, 1536], BF16)
                    for k in range(8):
                        dma("pool", win[:, k, :], win_d[k * 128:(k + 1) * 128, 0:1536], writes=[("win", k)])
                    wk = [("win", k) for k in range(8)]
                    vecA = sbuf(esA, "vecA", [128, 3, 512], F32)
                    dma("sp", vecA[:, 0:2, :], vec_d[:, 0, :].rearrange("p (a c) -> p a c", a=2), writes=["vecA"])
                    dma("sp", vecA[:, 2, :], vec_d[:, 1, 0:512], writes=["vecA"])
                    U = SRot(esA, "U", 2, [128, 512], F32)
                    V = SRot(esA, "V", 2, [128, 512], F32)
                    W1 = SRot(esA, "W1", 2, [128, 512], F32)
                    W2 = SRot(esA, "W2", 2, [128, 512], F32)
                    W3 = SRot(esA, "W3", 1, [128, 512], F32)
                    VN = SRot(esA, "VN", 2, [128, 512], BF16)
                    AB = SRot(esA, "AB", 2, [128, 512], BF16)
                    st = SRot(esA, "st", 2, [128, 8, 8], F32)

                    def gelu(src_ap, src_key, dst, dst_key):
                        p.op("act", lambda: nc.scalar.activation(out=dst[:], in_=src_ap, func=AF.Gelu_apprx_tanh), reads=[src_key], writes=[dst_key])

                    def emit_uv(i):
                        t0 = i * 128
                        b0 = 2 * (i % 2)
                        for half in range(2):
                            for k in range(8):
                                p.op("pe", lambda half=half, k=k: nc.tensor.matmul(
                                    psb[b0 + half][:], lhsT=xTo[:, k, t0:t0 + 128], rhs=win[:, k, half * 512:(half + 1) * 512],
                                    start=(k == 0), stop=(k == 7)), reads=[("xTo", i // 4), wk[k]], writes=[PS(b0 + half)])

                    hnd = {}

                    def S1(i):
                        b0 = 2 * (i % 2)
                        u, uk = U.next()
                        v, vk = V.next()
                        gelu(psb[b0][:], PS(b0), u, uk)
                        gelu(psb[b0 + 1][:], PS(b0 + 1), v, vk)
                        s, sk = st.next()
                        w1, w1k = W1.next()
                        v3 = v[:].rearrange("p (g c) -> p g c", g=8)
                        w13 = w1[:].rearrange("p (g c) -> p g c", g=8)
                        p.op("dve", lambda: nc.vector.tensor_reduce(out=s[:, 0, :], in_=v3, axis=AX.X, op=ALU.add), reads=[vk], writes=[sk])
                        yield
                        p.op("act", lambda: nc.scalar.activation(out=w1[:], in_=v[:], func=AF.Square), reads=[vk], writes=[w1k])
                        yield
                        p.op("dve", lambda: nc.vector.tensor_reduce(out=s[:, 1, :], in_=w13, axis=AX.X, op=ALU.add), reads=[w1k], writes=[sk])
                        yield
                        p.op("dve", lambda: nc.vector.tensor_single_scalar(out=s[:, 2, :], in_=s[:, 0, :], scalar=1.0 / 64, op=ALU.mult), reads=[sk], writes=[sk])
                        yield
                        p.op("dve", lambda: nc.vector.tensor_tensor(out=s[:, 3, :], in0=s[:, 2, :], in1=s[:, 2, :], op=ALU.mult), reads=[sk], writes=[sk])
                        yield
                        p.op("dve", lambda: nc.vector.scalar_tensor_tensor(out=s[:, 4, :], in0=s[:, 1, :], scalar=1.0 / 64, in1=s[:, 3, :], op0=ALU.mult, op1=ALU.subtract), reads=[sk], writes=[sk])
                        yield
                        p.op("act", lambda: nc.scalar.activation(out=s[:, 6, :], in_=s[:, 4, :], func=AF.Sqrt, bias=eps_t[:, 0:1], scale=1.0), reads=[sk, "eps"], writes=[sk])
                        yield
                        p.op("dve", lambda: nc.vector.reciprocal(out=s[:, 5, :], in_=s[:, 6, :]), reads=[sk], writes=[sk])
                        yield
                        mean_bc = s[:, 2, :].unsqueeze(2).to_broadcast([128, 8, 64])
                        rstd_bc = s[:, 5, :].unsqueeze(2).to_broadcast([128, 8, 64])
                        p.op("dve", lambda: nc.vector.tensor_tensor(out=w13, in0=v3, in1=mean_bc, op=ALU.subtract), reads=[vk, sk], writes=[w1k])
                        yield
                        p.op("dve", lambda: nc.vector.tensor_tensor(out=w13, in0=w13, in1=rstd_bc, op=ALU.mult), reads=[w1k, sk], writes=[w1k])
                        yield
                        vn, vnk = VN.next()
                        p.op("dve", lambda: nc.vector.tensor_tensor(out=w1[:], in0=w1[:], in1=vecA[:, 0, :], op=ALU.mult), reads=[w1k, "vecA"], writes=[w1k])
                        yield
                        p.op("dve", lambda: nc.vector.tensor_tensor(out=vn[:], in0=w1[:], in1=vecA[:, 1, :], op=ALU.add), reads=[w1k, "vecA"], writes=[vnk])
                        yield
                        hnd[i] = (u, uk, vn, vnk)

                    def S2(i):
                        u, uk, vn, vnk = hnd.pop(i)
                        for g in range(8):
                            p.op("pe", lambda g=g: nc.tensor.matmul(psb[4][:, g * 64:(g + 1) * 64], lhsT=wcT[:, g, :], rhs=vn[:, g * 64:(g + 1) * 64],
                                                                     start=True, stop=True), reads=["wcT", vnk], writes=[PS(4)])
                            yield
                        w2, w2k = W2.next()
                        w23 = w2[:].rearrange("p (g c) -> p g c", g=8)
                        bs_bc = sgub[:, :].unsqueeze(2).to_broadcast([128, 8, 64])
                        p.op("dve", lambda: nc.vector.tensor_tensor(out=w23, in0=psb[4][:].rearrange("p (g c) -> p g c", g=8), in1=bs_bc, op=ALU.add),
                             reads=[PS(4), "sgub"], writes=[w2k])
                        yield
                        p.op("dve", lambda: nc.vector.tensor_tensor(out=w2[:], in0=w2[:], in1=u[:], op=ALU.mult), reads=[w2k, uk], writes=[w2k])
                        yield
                        w1b, w1bk = W3.next()
                        p.op("act", lambda: nc.scalar.activation(out=w1b[:], in_=w2[:], func=AF.Square, accum_out=ssq_a[:, i:i + 1]), reads=[w2k], writes=[w1bk, "ssq_a"])
                        yield
                        ab, abk = AB.next()
                        p.op("dve", lambda: nc.vector.tensor_tensor(out=ab[:], in0=w2[:], in1=vecA[:, 2, :], op=ALU.mult), reads=[w2k, "vecA"], writes=[abk])
                        yield
                        ptr = psb[5][:].bitcast(BF16)
                        for c in range(4):
                            p.op("pe", lambda c=c: nc.tensor.transpose(ptr[:, c * 128:(c + 1) * 128], ab[:, c * 128:(c + 1) * 128], ident_bf),
                                 reads=[abk, "consts_bf"], writes=[PS(5)])
                            yield
                        p.op("act", lambda: nc.scalar.copy(out=aT[:, :, i * 128:(i + 1) * 128], in_=ptr[:, 0:512].rearrange("p (c t) -> p c t", c=4)),
                             reads=[PS(5)], writes=["aT"])
                        yield
                        if "dbg_a" in dram:
                            dma("sp", dram["dbg_a"][i * 128:(i + 1) * 128, :], w2[:], reads=[w2k])
                            yield

                    qgroups = [(hp, tc) for hp in range(4) for tc in range(4)]

                    def qproj(ev):
                        hp, tc = qgroups[ev]
                        b = 6 + (ev % 2)
                        for k in range(8):
                            p.op("pe", lambda k=k, b=b: nc.tensor.matmul(
                                psb[b][:], lhsT=win[:, k, 1024 + hp * 128:1024 + (hp + 1) * 128],
                                rhs=xTo[:, k, tc * 512:(tc + 1) * 512], start=(k == 0), stop=(k == 7)),
                                reads=[("xTo", tc), wk[k]], writes=[PS(b)])
                        yield
                        p.op("dve", lambda b=b: nc.vector.tensor_copy(out=qT[:, hp, tc * 512:(tc + 1) * 512], in_=psb[b][:]), reads=[PS(b)], writes=[("qT", hp)])
                        yield

                    if stage >= 1:
                        emit_uv(0)
                        emit_uv(1)
                        run(S1(0))
                        for i in range(NT):
                            if i + 2 < NT:
                                emit_uv(i + 2)
                            weave(S1(i + 1) if i + 1 < NT else None, S2(i), qproj(i) if stage >= 2 else None)
                    if "dbg_ssq" in dram:
                        dma("sp", dram["dbg_ssq"], ssq_a[:], reads=["ssq_a"])
                    p.barrier()
            with ExitStack() as esD:
                for nm, t_, rk in (("dbg_aT", aT, ["aT"]), ("dbg_qT", qT, [("qT", h_) for h_ in range(4)])):
                    if nm in dram:
                        tmpf = sbuf(esD, nm, [128, 4, NTOK], F32)
                        p.op("dve", lambda: nc.vector.tensor_copy(out=tmpf[:], in_=t_[:]), reads=rk, writes=[nm])
                        dma("sp", dram[nm], tmpf[:], reads=[nm])
                p.barrier()
            with ExitStack() as esT:
                vaugp = [sbuf(esT, f"vaugp{i}", [128, nv, 2, 66], BF16) for i in range(2)]
                for i_ in range(2):
                    p.op("pool", lambda i_=i_: nc.gpsimd.memset(vaugp[i_][:, :, :, 64:66], 1.0), writes=[("vones", i_)])
                accT = [sbuf(esT, f"accT{i}", [65, NTOK], F32) for i in range(2)]
                Eb = SRot(esT, "Eb", 3, [128, 512], BF16)
                Em = SRot(esT, "Em", 3, [128, 512], BF16)
                RD = SRot(esT, "RD", 2, [64, 512], F32)
                BQ = SRot(esT, "BQ", 2, [64, 512], F32)
                BS = SRot(esT, "BS", 2, [64, 512], BF16)
                masks = sbuf(esT, "masks", [128, 3, 512], BF16)
                mprev = consts_bf[:, C_MPREV:C_MPREV + 128]
                mprevc = consts_bf[:, C_MPREVC:C_MPREVC + 128]
                mcur = consts_bf[:, C_TRI:C_TRI + 128]
                for mi, pat in enumerate(((mprev, mcur, mprev, mcur), (mprevc, mcur, mprev, mcur), (mprevc, mcur, mprevc, mcur))):
                    for j, src in enumerate(pat):
                        p.op("dve", lambda mi=mi, j=j, src=src: nc.vector.tensor_copy(out=masks[:, mi, j * 128:(j + 1) * 128], in_=src),
                             reads=["consts_bf"], writes=["masks"])
                sel_f = consts[0:65, C_SEL:C_SEL + 64]
                ones_f = consts_bf[0:64, C_ONES:C_ONES + 1]
                gb64 = sbuf(esT, "gb64", [64, 8], F32)
                dma("sp", gb64[:], dram["mix_gb64"], writes=["gb64"])
                allv = [(d_, n, r) for d_ in PATTERNS for (n, r) in vt[d_]]
                tgc = [0]

                def build_v(hp):
                    vb = vaugp[hp % 2]
                    vbk = ("vaugp", hp % 2)
                    for g0 in range(0, nv, 8):
                        grp = allv[g0:g0 + 8]
                        tg = tgc[0]
                        tgc[0] += 1
                        b = 6 + (tg % 2)
                        ptr = psb[b][:].bitcast(BF16)
                        for j, (d_, n, r) in enumerate(grp):
                            span = 128 * d_
                            s0 = span * n + r
                            p.op("pe", lambda j=j, s0=s0, span=span, d_=d_, ptr=ptr: nc.tensor.transpose(
                                ptr[:, j * 128:(j + 1) * 128], vT[:, hp, s0:s0 + span - d_ + 1:d_], ident_bf),
                                reads=[("vT", hp), "consts_bf"], writes=[PS(b)])
                            yield
                        ng = len(grp)
                        src = ptr[:, 0:ng * 128].rearrange("p (t h c) -> p t h c", t=ng, h=2)
                        if tg % 2 == 0:
                            p.op("act", lambda src=src, g0=g0, ng=ng: nc.scalar.copy(out=vb[:, g0:g0 + ng, :, 0:64], in_=src), reads=[PS(b)], writes=[vbk])
                            yield
                        else:
                            p.op("dve", lambda src=src, g0=g0, ng=ng: nc.vector.tensor_copy(out=vb[:, g0:g0 + ng, :, 0:64], in_=src), reads=[PS(b)], writes=[vbk])
                            yield

                def unit_list(h):
                    us = []
                    for d_ in PATTERNS:
                        if d_ == 1:
                            blocks = [(n, 0) for n in range(16, 32)]
                        elif d_ == 4:
                            blocks = [(n, r) for n in range(4, 8) for r in range(4)]
                        else:
                            blocks = [(1, r) for r in range(16)]
                        for ui in range(0, 16, 2):
                            us.append((h, d_, blocks[ui:ui + 2]))
                    return us

                def emit_qk(u, su):
                    h, d_, pair = u
                    hp, hh = h // 2, h % 2
                    po = 64 * hh
                    span = 128 * d_
                    sb_ = su % 2
                    for j, (n, r) in enumerate(pair):
                        q0 = span * n + r - NTOK
                        qs = qT[po:po + 64, hp, q0:q0 + span - d_ + 1:d_]
                        kp0 = span * (n - 1) + r
                        kc0 = span * n + r
                        p.op("pe", lambda j=j, qs=qs, kp0=kp0: nc.tensor.matmul(
                            psb[sb_][:, j * 256:j * 256 + 128], lhsT=kT[po:po + 64, hp, kp0:kp0 + span - d_ + 1:d_], rhs=qs, start=True, stop=True),
                            reads=[("kT", hp), ("qT", hp)], writes=[PS(sb_)])
                        p.op("pe", lambda j=j, qs=qs, kc0=kc0: nc.tensor.matmul(
                            psb[sb_][:, j * 256 + 128:j * 256 + 256], lhsT=kT[po:po + 64, hp, kc0:kc0 + span - d_ + 1:d_], rhs=qs, start=True, stop=True),
                            reads=[("kT", hp), ("qT", hp)], writes=[PS(sb_)])

                def emit_mid(u, su):
                    h, d_, pair = u
                    span = 128 * d_
                    sb_ = su % 2
                    ctx = tuple((span * (n - 1) + r) < NTOK for (n, r) in pair)
                    mi = {(False, False): 0, (True, False): 1, (True, True): 2}[ctx]
                    eb, ebk = Eb.next()
                    em, emk = Em.next()
                    p.op("act", lambda: nc.scalar.activation(out=eb[:], in_=psb[sb_][:], func=AF.Exp, scale=0.125), reads=[PS(sb_)], writes=[ebk])
                    p.op("dve", lambda: nc.vector.tensor_tensor(out=em[:], in0=eb[:], in1=masks[:, mi, :], op=ALU.mult), reads=[ebk, "masks"], writes=[emk])
                    return em, emk

                def emit_pv(u, su, em, emk):
                    h, d_, pair = u
                    hp, hh = h // 2, h % 2
                    vb = vaugp[hp % 2]
                    vbk = ("vaugp", hp % 2)
                    ob_ = 2 + su % 2
                    for j, (n, r) in enumerate(pair):
                        vp = vidx[(d_, n - 1, r)]
                        vc = vidx[(d_, n, r)]
                        p.op("pe", lambda j=j, vp=vp: nc.tensor.matmul(
                            psb[ob_][0:65, j * 128:(j + 1) * 128], lhsT=vb[:, vp, hh, 0:65], rhs=em[:, j * 256:j * 256 + 128], start=True, stop=False),
                            reads=[vbk, ("vones", hp % 2), emk], writes=[PS(ob_)])
                        p.op("pe", lambda j=j, vc=vc: nc.tensor.matmul(
                            psb[ob_][0:65, j * 128:(j + 1) * 128], lhsT=vb[:, vc, hh, 0:65], rhs=em[:, j * 256 + 128:j * 256 + 256], start=False, stop=True),
                            reads=[vbk, ("vones", hp % 2), emk], writes=[PS(ob_)])

                def emit_acc(u, su):
                    h, d_, pair = u
                    acc = accT[h % 2]
                    acck = ("accT", h % 2)
                    span = 128 * d_
                    ob_ = 2 + su % 2
                    n0, r0 = pair[0]
                    src = psb[ob_][0:65, 0:256].rearrange("p (b j) -> p b j", b=2)
                    if d_ == 1:
                        q0 = span * n0 - NTOK
                        dest = acc[0:65, q0:q0 + 256].rearrange("p (b j) -> p b j", b=2)
                        p.op("act", lambda: nc.scalar.copy(out=dest, in_=src), reads=[PS(ob_)], writes=[acck])
                    else:
                        s0 = span * n0 - NTOK
                        dest = acc[0:65, s0:s0 + span].rearrange("p (j d) -> p d j", d=d_)[:, r0:r0 + 2, :]
                        p.op("dve", lambda: nc.vector.tensor_tensor(out=dest, in0=dest, in1=src, op=ALU.add), reads=[PS(ob_), acck], writes=[acck])

                def finalize(h):
                    hp, hh = h // 2, h % 2
                    po = 64 * hh
                    acc = accT[h % 2]
                    acck = ("accT", h % 2)
                    for c in range(4):
                        p.op("pe", lambda c=c: nc.tensor.matmul(psb[4][0:64, :], lhsT=sel_f, rhs=acc[0:65, c * 512:(c + 1) * 512], start=True, stop=True),
                             reads=["consts", acck], writes=[PS(4)])
                        yield
                        rd, rdk = RD.next()
                        bq_, bqk = BQ.next()
                        bs_, bsk = BS.next()
                        p.op("dve", lambda rd=rd: nc.vector.reciprocal(out=rd[:], in_=psb[4][0:64, :]), reads=[PS(4)], writes=[rdk])
                        yield
                        p.op("dve", lambda c=c, rd=rd, bq_=bq_: nc.vector.tensor_tensor(out=bq_[:], in0=acc[0:64, c * 512:(c + 1) * 512], in1=rd[:], op=ALU.mult),
                             reads=[acck, rdk], writes=[bqk])
                        yield
                        p.op("act", lambda bq_=bq_, bs_=bs_: nc.scalar.activation(out=bs_[:], in_=bq_[:], func=AF.Square), reads=[bqk], writes=[bsk])
                        yield
                        for t in range(4):
                            ti = c * 4 + t
                            col = ti * 8 + h
                            p.op("pe", lambda t=t, col=col, bs_=bs_: nc.tensor.matmul(psb[5][:, col:col + 1], lhsT=bs_[0:64, t * 128:(t + 1) * 128], rhs=ones_f, start=True, stop=True),
                                 reads=[bsk, "consts_bf"], writes=[PS(5)])
                            yield
                        p.op("act", lambda c=c, bq_=bq_: nc.scalar.activation(out=bT[po:po + 64, hp, c * 512:(c + 1) * 512], in_=bq_[:], func=AF.Copy, scale=gb64[:, h:h + 1]),
                             reads=[bqk, "gb64"], writes=["bT"])
                        yield
                        if "dbg_b" in dram:
                            dma("sp", dram["dbg_b"][h * 64:(h + 1) * 64, c * 512:(c + 1) * 512], bq_[:], reads=[bqk])
                            yield

                if stage >= 3:
                    units = []
                    for h in range(8):
                        units += unit_list(h)
                    run(build_v(0))
                    emit_qk(units[0], 0)
                    pend = None
                    bg = []

                    def step_bg(n):
                        for _ in range(n):
                            if not bg:
                                return
                            try:
                                next(bg[0])
                            except StopIteration:
                                bg.pop(0)

                    def drain_bg():
                        while bg:
                            step_bg(1)
                    for ui, u in enumerate(units):
                        if ui + 1 < len(units):
                            emit_qk(units[ui + 1], ui + 1)
                        em, emk = emit_mid(u, ui)
                        if pend is not None:
                            emit_acc(*pend)
                            pend = None
                        emit_pv(u, ui, em, emk)
                        pend = (u, ui)
                        step_bg(4)
                        h = u[0]
                        last_of_head = (ui + 1 == len(units)) or units[ui + 1][0] != h
                        if last_of_head:
                            emit_acc(*pend)
                            pend = None
                            drain_bg()
                            bg.append(finalize(h))
                            if h % 2 == 0 and h // 2 + 1 < 4:
                                bg.append(build_v(h // 2 + 1))
                    drain_bg()
                if stage >= 3:
                    p.op("dve", lambda: nc.vector.tensor_copy(out=ssq_b[:].rearrange("p t h -> p (t h)"), in_=psb[5][:, 0:128]), reads=[PS(5)], writes=["ssq_b"])
                    if "dbg_ssqb" in dram:
                        dma("sp", dram["dbg_ssqb"], ssq_b[:].rearrange("p t h -> p (t h)"), reads=["ssq_b"])
                p.barrier()
        scat_keys = []
        hbf_keys = ["hbf_z"]
        h32_keys = []
        with ExitStack() as es3:
            zrow = sbuf(es3, "zrow", [1, D], F32)
            zrow_bf = sbuf(es3, "zrow_bf", [1, D], BF16)
            p.op("dve", lambda: nc.vector.memset(zrow[:], 0.0), writes=["zrow"])
            p.op("dve", lambda: nc.vector.memset(zrow_bf[:], 0.0), writes=["zrow_bf"])
            dma("sp", hbf_d[NTOK:NTOK + 1, :], zrow_bf[:], reads=["zrow_bf"], writes=["hbf_z"])
            dma("sp", eo_d[SLOTS:SLOTS + 1, :], zrow[:], reads=["zrow"], writes=["eo_z"])
            wout = sbuf(es3, "wout", [128, 8, D], BF16)
            for k in range(8):
                dma("pool", wout[:, k, :], wout_d[k * 128:(k + 1) * 128, :], writes=[("wout", k)])
            vec3 = sbuf(es3, "vec3", [128, 2, D], F32)
            dma("sp", vec3[:], vec_d[:, 2:4, :], writes=["vec3"])
            wr = sbuf(es3, "wr", [128, 8, NE], F32)
            dma("sp", wr[:], wr_d.rearrange("(k p) e -> p k e", p=128), writes=["wr"])
            brb = sbuf(es3, "brb", [128, NE], F32)
            dma("sp", brb[:], br_d, writes=["brb"])
            rs = sbuf(es3, "rs", [128, 4, NT], F32)
            p.op("dve", lambda: nc.vector.tensor_reduce(out=rs[:, 2, :], in_=ssq_b[:], axis=AX.X, op=ALU.add), reads=["ssq_b"], writes=["rs"])
            p.op("act", lambda: nc.scalar.activation(out=rs[:, 3, :], in_=ssq_a[:], func=AF.Ln, bias=eps_t[:, 0:1], scale=1.0 / 512), reads=["ssq_a", "eps"], writes=["rs"])
            p.op("act", lambda: nc.scalar.activation(out=rs[:, 0, :], in_=rs[:, 3, :], func=AF.Exp, scale=-0.5), reads=["rs"], writes=["rs"])
            p.op("act", lambda: nc.scalar.activation(out=rs[:, 3, :], in_=rs[:, 2, :], func=AF.Ln, bias=eps_t[:, 0:1], scale=1.0 / 512), reads=["rs", "eps"], writes=["rs"])
            p.op("act", lambda: nc.scalar.activation(out=rs[:, 1, :], in_=rs[:, 3, :], func=AF.Exp, scale=-0.5), reads=["rs"], writes=["rs"])
            X = SRot(es3, "X", 6, [128, D], F32)
            T1 = SRot(es3, "T1", 3, [128, D], F32)
            T2 = SRot(es3, "T2", 3, [128, D], F32)
            HB = SRot(es3, "HB", 3, [128, D], BF16)
            HT = SRot(es3, "HT", 2, [128, 8, 128], F32)
            SM = SRot(es3, "SM", 4, [128, 16], F32)
            LG = SRot(es3, "LG", 2, [128, NE], F32)
            RK = SRot(es3, "RK", 2, [128, 6, NE], F32)
            OH = SRot(es3, "OH", 2, [128, 4, NE], F32)
            MX = SRot(es3, "MX", 2, [128, 24], F32)

            def layer_norm(src, srck, dst, dstk, g_ap, b_ap, gk, jt, jtk, SMr):
                sm, smk = SMr.next()
                p.op("dve", lambda: nc.vector.memset(sm[:], 0.0), writes=[smk])
                yield
                p.op("act", lambda: nc.scalar.activation(out=jt[:], in_=src[:], func=AF.Identity, accum_out=sm[:, 0:1]), reads=[srck, smk], writes=[jtk, smk])
                yield
                p.op("act", lambda: nc.scalar.activation(out=jt[:], in_=src[:], func=AF.Square, accum_out=sm[:, 1:2]), reads=[srck, smk], writes=[jtk, smk])
                yield
                p.op("dve", lambda: nc.vector.tensor_single_scalar(out=sm[:, 2:3], in_=sm[:, 0:1], scalar=1.0 / D, op=ALU.mult), reads=[smk], writes=[smk])
                yield
                p.op("dve", lambda: nc.vector.tensor_tensor(out=sm[:, 3:4], in0=sm[:, 2:3], in1=sm[:, 2:3], op=ALU.mult), reads=[smk], writes=[smk])
                yield
                p.op("dve", lambda: nc.vector.scalar_tensor_tensor(out=sm[:, 4:5], in0=sm[:, 1:2], scalar=1.0 / D, in1=sm[:, 3:4], op0=ALU.mult, op1=ALU.subtract), reads=[smk], writes=[smk])
                yield
                p.op("act", lambda: nc.scalar.activation(out=sm[:, 5:6], in_=sm[:, 4:5], func=AF.Ln, bias=eps_t[:, 0:1], scale=1.0), reads=[smk, "eps"], writes=[smk])
                yield
                p.op("act", lambda: nc.scalar.activation(out=sm[:, 6:7], in_=sm[:, 5:6], func=AF.Exp, scale=-0.5), reads=[smk], writes=[smk])
                yield
                p.op("dve", lambda: nc.vector.scalar_tensor_tensor(out=sm[:, 7:8], in0=sm[:, 2:3], scalar=-1.0, in1=sm[:, 6:7], op0=ALU.mult, op1=ALU.mult), reads=[smk], writes=[smk])
                yield
                p.op("act", lambda: nc.scalar.activation(out=jt[:], in_=src[:], func=AF.Identity, scale=sm[:, 6:7], bias=sm[:, 7:8]), reads=[srck, smk], writes=[jtk])
                yield
                p.op("dve", lambda: nc.vector.tensor_tensor(out=jt[:], in0=jt[:], in1=g_ap, op=ALU.mult), reads=[jtk, gk], writes=[jtk])
                yield
                p.op("dve", lambda: nc.vector.tensor_tensor(out=dst[:], in0=jt[:], in1=b_ap, op=ALU.add), reads=[jtk, gk], writes=[dstk])
                yield

            hh_ = {}

            xx_ = {}

            def mmA(i):
                tsl = slice(i * 128, (i + 1) * 128)
                x_, xk_ = X.next()
                xx_[i] = (x_, xk_)
                dma("sp", x_[:], x_d[tsl, :], writes=[xk_])
                for half in range(2):
                    for c in range(4):
                        p.op("pe", lambda half=half, c=c: nc.tensor.matmul(psb[half][:], lhsT=aT[:, c, tsl], rhs=wout[:, c, half * 512:(half + 1) * 512],
                                                                       start=(c == 0), stop=(c == 3)), reads=["aT", ("wout", c)], writes=[PS(half)])
                    for c in range(4):
                        p.op("pe", lambda half=half, c=c: nc.tensor.matmul(psb[2 + half][:], lhsT=bT[:, c, tsl], rhs=wout[:, 4 + c, half * 512:(half + 1) * 512],
                                                                       start=(c == 0), stop=(c == 3)), reads=["bT", ("wout", 4 + c)], writes=[PS(2 + half)])

            def stageA(i):
                tsl = slice(i * 128, (i + 1) * 128)
                x_, xk_ = xx_.pop(i)
                t1, t1k = T1.next()
                t2, t2k = T2.next()
                p.op("act", lambda: nc.scalar.mul(out=x_[:], in_=x_[:], mul=ALPHA), reads=[xk_], writes=[xk_])
                for half in range(2):
                    hs = slice(half * 512, (half + 1) * 512)
                    p.op("dve", lambda half=half, hs=hs: nc.vector.scalar_tensor_tensor(out=t1[:, hs], in0=psb[half][:], scalar=rs[:, 0, i:i + 1], in1=x_[:, hs], op0=ALU.mult, op1=ALU.add),
                         reads=[PS(half), "rs", xk_], writes=[t1k])
                    p.op("dve", lambda half=half, hs=hs: nc.vector.scalar_tensor_tensor(out=t1[:, hs], in0=psb[2 + half][:], scalar=rs[:, 1, i:i + 1], in1=t1[:, hs], op0=ALU.mult, op1=ALU.add),
                         reads=[PS(2 + half), "rs", t1k], writes=[t1k])
                if i + 1 < NT:
                    mmA(i + 1)
                yield
                h_, hk_ = X.next()
                yield from layer_norm(t1, t1k, h_, hk_, vec3[:, 0, :], vec3[:, 1, :], "vec3", t2, t2k, SM)
                dma("sp", h32_d[tsl, :], h_[:], reads=[hk_], writes=[("h32", i)])
                yield
                h32_keys.append(("h32", i))
                hb, hbk = HB.next()
                p.op("act", lambda: nc.scalar.copy(out=hb[:], in_=h_[:]), reads=[hk_], writes=[hbk])
                yield
                dma("sp", hbf_d[tsl, :], hb[:], reads=[hbk], writes=[("hbf", i)])
                yield
                hbf_keys.append(("hbf", i))
                if "dbg_h" in dram:
                    dma("sp", dram["dbg_h"][tsl, :], h_[:], reads=[hk_])
                    yield
                hh_[i] = (h_, hk_)

            LGall = sbuf(es3, "LGall", [128, NT, NE], F32)
            MX8 = sbuf(es3, "MX8", [128, NT, 8], F32)

            def stageB(i):
                h_, hk_ = hh_.pop(i)
                tsl = slice(i * 128, (i + 1) * 128)
                ht, htk = HT.next()
                for k in range(8):
                    b = 4 + k // 4
                    p.op("pe", lambda k=k, b=b: nc.tensor.transpose(psb[b][:, (k % 4) * 128:(k % 4 + 1) * 128], h_[:, k * 128:(k + 1) * 128], ident_f),
                         reads=[hk_, "consts"], writes=[PS(b)])
                p.op("act", lambda: nc.scalar.copy(out=ht[:, 0:4, :], in_=psb[4][:].rearrange("p (k t) -> p k t", k=4)), reads=[PS(4)], writes=[htk])
                p.op("dve", lambda: nc.vector.tensor_copy(out=ht[:, 4:8, :], in_=psb[5][:].rearrange("p (k t) -> p k t", k=4)), reads=[PS(5)], writes=[htk])
                for k in range(8):
                    p.op("pe", lambda k=k: nc.tensor.matmul(psb[6][:, 0:NE], lhsT=ht[:, k, :], rhs=wr[:, k, :], start=(k == 0), stop=(k == 7)),
                         reads=[htk, "wr"], writes=[PS(6)])
                p.op("dve", lambda: nc.vector.tensor_tensor(out=LGall[:, i, :], in0=psb[6][:, 0:NE], in1=brb[:], op=ALU.add), reads=[PS(6), "brb"], writes=[("LG", i)])
                p.op("dve", lambda: nc.vector.max(out=MX8[:, i, :], in_=LGall[:, i, :]), reads=[("LG", i)], writes=[("MX8", i)])

                yield
                if "dbg_logits" in dram:
                    dma("sp", dram["dbg_logits"][tsl, :], LGall[:, i, :], reads=[("LG", i)])
                    yield

            RT = sbuf(es3, "RT", [128, 3, NT, NE], F32)
            OHa = sbuf(es3, "OHa", [128, NT, 4, NE], F32)
            OHb = sbuf(es3, "OHb", [128, NT, 4, NE], F32)
            SC = sbuf(es3, "SC", [128, 9, NT, 4], F32)
            RB = 4

            def routing(bi):
                t0, t1 = bi * RB, (bi + 1) * RB
                ts_ = slice(t0, t1)
                nt = t1 - t0
                LGk = [("LG", i) for i in range(t0, t1)]
                MXk = [("MX8", i) for i in range(t0, t1)]
                K = lambda nm: (nm, bi)
                p.op("dve", lambda: nc.vector.tensor_tensor(out=RT[:, 0, ts_], in0=LGall[:, ts_], in1=MX8[:, ts_, 3:4].to_broadcast([128, nt, NE]), op=ALU.is_ge), reads=LGk + MXk, writes=[K("RT0")])
                yield
                p.op("act", lambda: nc.scalar.copy(out=Mall[:, ts_], in_=RT[:, 0, ts_]), reads=[K("RT0")], writes=[("Mall", bi)])
                yield
                for i in range(t0, t1):
                    for i2 in range(i + 1):
                        lhs = consts_bf[:, C_LSTRICT:C_LSTRICT + 128] if i2 == i else consts_bf[:, C_ONES:C_ONES + 128]
                        p.op("pe", lambda i=i, i2=i2, lhs=lhs: nc.tensor.matmul(psb[7][:, i * NE:(i + 1) * NE], lhsT=lhs, rhs=Mall[:, i2, :], start=(i2 == 0), stop=(i2 == i)),
                             reads=[("Mall", i2 // RB), "consts_bf"], writes=[PS(7)])
                    yield
                p.op("dve", lambda: nc.vector.tensor_tensor(out=SC[:, 0, ts_], in0=MX8[:, ts_, 0:4], in1=MX8[:, ts_, 0:1].to_broadcast([128, nt, 4]), op=ALU.subtract), reads=MXk, writes=[K("SC0")])
                yield
                p.op("act", lambda: nc.scalar.activation(out=SC[:, 1, ts_], in_=SC[:, 0, ts_], func=AF.Exp), reads=[K("SC0")], writes=[K("SC1")])
                yield
                p.op("dve", lambda: nc.vector.tensor_reduce(out=SC[:, 2, ts_, 0], in_=SC[:, 1, ts_], axis=AX.X, op=ALU.add), reads=[K("SC1")], writes=[K("SC2")])
                yield
                p.op("dve", lambda: nc.vector.reciprocal(out=SC[:, 2, ts_, 1], in_=SC[:, 2, ts_, 0]), reads=[K("SC2")], writes=[K("SC2b")])
                yield
                p.op("dve", lambda: nc.vector.tensor_tensor(out=SC[:, 3, ts_], in0=SC[:, 1, ts_], in1=SC[:, 2, ts_, 1:2].to_broadcast([128, nt, 4]), op=ALU.mult), reads=[K("SC1"), K("SC2b")], writes=[K("SC3")])
                yield
                lg_bc = LGall[:, ts_].unsqueeze(2).to_broadcast([128, nt, 4, NE])
                mx_bc = MX8[:, ts_, 0:4].unsqueeze(3).to_broadcast([128, nt, 4, NE])
                p.op("dve", lambda: nc.vector.tensor_tensor(out=OHa[:, ts_], in0=lg_bc, in1=mx_bc, op=ALU.is_equal), reads=LGk + MXk, writes=[K("OHa")])
                yield
                p.op("dve", lambda: nc.vector.tensor_copy(out=RT[:, 1, ts_], in_=psb[7][:, t0 * NE:t1 * NE].rearrange("p (t e) -> p t e", t=nt)), reads=[PS(7)], writes=[K("RT1")])
                yield
                iota_bc = consts[:, C_IOTA:C_IOTA + NE].unsqueeze(1).to_broadcast([128, nt, NE])
                p.op("dve", lambda: nc.vector.scalar_tensor_tensor(out=RT[:, 2, ts_], in0=iota_bc, scalar=float(CAP), in1=RT[:, 1, ts_], op0=ALU.mult, op1=ALU.add), reads=["consts", K("RT1")], writes=[K("RT2")])
                yield
                p.op("dve", lambda: nc.vector.tensor_tensor(out=OHb[:, ts_], in0=OHa[:, ts_], in1=RT[:, 1, ts_].unsqueeze(2).to_broadcast([128, nt, 4, NE]), op=ALU.mult), reads=[K("OHa"), K("RT1")], writes=[K("OHb")])
                yield
                p.op("dve", lambda: nc.vector.tensor_reduce(out=SC[:, 4, ts_], in_=OHb[:, ts_], axis=AX.X, op=ALU.add), reads=[K("OHb")], writes=[K("SC4")])
                yield
                p.op("dve", lambda: nc.vector.tensor_tensor(out=OHb[:, ts_], in0=OHa[:, ts_], in1=RT[:, 2, ts_].unsqueeze(2).to_broadcast([128, nt, 4, NE]), op=ALU.mult), reads=[K("OHa"), K("RT2"), K("SC4")], writes=[K("OHb")])
                yield
                p.op("dve", lambda: nc.vector.tensor_reduce(out=SC[:, 5, ts_], in_=OHb[:, ts_], axis=AX.X, op=ALU.add), reads=[K("OHb")], writes=[K("SC5")])
                yield
                p.op("dve", lambda: nc.vector.tensor_scalar(out=SC[:, 6, ts_], in0=SC[:, 4, ts_], scalar1=float(CAP), scalar2=None, op0=ALU.is_lt), reads=[K("SC4")], writes=[K("SC6")])
                yield
                p.op("dve", lambda: nc.vector.scalar_tensor_tensor(out=SC[:, 7, ts_], in0=SC[:, 5, ts_], scalar=-float(SLOTS), in1=SC[:, 6, ts_], op0=ALU.add, op1=ALU.mult), reads=[K("SC5"), K("SC6")], writes=[K("SC7")])
                yield
                p.op("dve", lambda: nc.vector.tensor_scalar(out=SC[:, 8, ts_], in0=SC[:, 7, ts_], scalar1=float(SLOTS), scalar2=None, op0=ALU.add), reads=[K("SC7")], writes=[K("SC8")])
                yield
                p.op("dve", lambda: nc.vector.tensor_copy(out=pos_i[:, t0 * 4:t1 * 4].rearrange("p (t k) -> p t k", k=4), in_=SC[:, 8, ts_]), reads=[K("SC8")], writes=[("pos", i) for i in range(t0, t1)])
                yield
                p.op("dve", lambda: nc.vector.tensor_tensor(out=gates[:, ts_], in0=SC[:, 3, ts_], in1=SC[:, 6, ts_], op=ALU.mult), reads=[K("SC3"), K("SC6")], writes=[("gates", i) for i in range(t0, t1)])
                yield
                if "dbg_pos" in dram:
                    for i in range(t0, t1):
                        dma("sp", dram["dbg_pos"][i * 128:(i + 1) * 128, :], SC[:, 8, i, :], reads=[K("SC8")])
                        dma("sp", dram["dbg_gates"][i * 128:(i + 1) * 128, :], gates[:, i, :], reads=[("gates", i)])
                for i in range(t0, t1):
                    for k in range(4):
                        p.op("pool", lambda i=i, k=k: nc.gpsimd.indirect_dma_start(
                            out=stok_d[:, :], out_offset=bass.IndirectOffsetOnAxis(ap=pos_i[:, i * 4 + k:i * 4 + k + 1], axis=0),
                            in_=tokid_i[:, i:i + 1], in_offset=None),
                            reads=[("pos", i), "tokid_i", "stok_init"], writes=[("scat", i, k)], dma=True)
                        scat_keys.append(("scat", i, k))
                        yield

            if stage >= 4:
                def tileAB(i):
                    yield from stageA(i)
                    yield from stageB(i)
                ents = []
                for i in range(NT):
                    ents.append(("tile", lambda i=i: tileAB(i), 0))
                    if (i + 1) % RB == 0:
                        ents.append(("bg", lambda bi=i // RB: routing(bi), i + 1))
                mmA(0)
                pipeline2(ents, stagger=8, depth=3)
            p.barrier()
        esAB.close()
        eo_keys = ["eo_z"]
        with ExitStack() as es5:
            bgT = sbuf(es5, "bgT", [128, NE, 8], F32)
            buT = sbuf(es5, "buT", [128, NE, 8], F32)
            dma("sp", bgT[:], bg_d, writes=["bgT"])
            dma("sp", buT[:], bu_d, writes=["buT"])
            bu1 = sbuf(es5, "bu1", [128, NE, 8], F32)
            p.op("dve", lambda: nc.vector.tensor_scalar(out=bu1[:], in0=buT[:], scalar1=1.0, scalar2=None, op0=ALU.add), reads=["buT"], writes=["bu1"])
            WG = [sbuf(es5, f"WG{i}", [128, 8, D], BF16) for i in range(2)]
            WU = [sbuf(es5, f"WU{i}", [128, 8, D], BF16) for i in range(2)]
            WD = [sbuf(es5, f"WD{i}", [128, 8, D], BF16) for i in range(2)]
            BD = [sbuf(es5, f"BD{i}", [128, D], F32) for i in range(2)]
            XB = [sbuf(es5, f"XB{i}", [128, NBLK, D], BF16) for i in range(2)]
            STI = [sbuf(es5, f"STI{i}", [128, NBLK], I32) for i in range(2)]
            XT = [sbuf(es5, f"XT{i}", [128, 8, CAP], BF16) for i in range(2)]
            ACT_T = sbuf(es5, "ACTT", [128, 8, CAP], BF16)
            GC = SRot(es5, "GC", 2, [128, CAP], F32)
            SG = SRot(es5, "SG", 2, [128, CAP], F32)
            UC = SRot(es5, "UC", 2, [128, CAP], F32)
            EO = SRot(es5, "EO", 2, [128, D], F32)

            def load_w(e):
                j = e % 2
                for (W_, wd_, nm) in ((WG, wg_d, "WG"), (WU, wu_d, "WU"), (WD, wd_d, "WD")):
                    for hk in range(2):
                        dma("pool", W_[j][:, hk * 4:(hk + 1) * 4, :], wd_[e, hk * 512:(hk + 1) * 512, :].rearrange("(k p) f -> p k f", p=128), writes=[(nm, j, hk)])
                dma("sp", BD[j][:], bd_d[e:e + 1, :].to_broadcast([128, D]), writes=[("BD", j)])

            def load_x(e):
                j = e % 2
                dma("sp", STI[j][:], stok_d[e * CAP:(e + 1) * CAP, :].rearrange("(p b) o -> p (b o)", p=128), reads=scat_keys + ["stok_init"], writes=[("STI", j)])
                for blk in range(NBLK):
                    p.op("pool", lambda blk=blk: nc.gpsimd.indirect_dma_start(
                        out=XB[j][:, blk, :], out_offset=None, in_=hbf_d[:, :],
                        in_offset=bass.IndirectOffsetOnAxis(ap=STI[j][:, blk:blk + 1], axis=0)),
                        reads=[("STI", j)] + hbf_keys, writes=[("XB", j, blk)], dma=True)

            nexp = NE if stage >= 5 else 0
            if nexp:
                load_x(0)
                load_w(0)
            tr = 0
            for e in range(nexp):
                j = e % 2
                if e + 1 < nexp:
                    load_x(e + 1)
                    load_w(e + 1)
                for blk in range(NBLK):
                    b = 6 + tr % 2
                    tr += 1
                    ptr = psb[b][:].bitcast(BF16)
                    for k in range(8):
                        p.op("pe", lambda blk=blk, k=k, ptr=ptr: nc.tensor.transpose(ptr[:, k * 128:(k + 1) * 128], XB[j][:, blk, k * 128:(k + 1) * 128], ident_bf),
                             reads=[("XB", j, blk), "consts_bf"], writes=[PS(b)])
                    src = ptr[:, :].rearrange("p (k t) -> p k t", k=8)
                    if blk % 2 == 0:
                        p.op("act", lambda blk=blk, src=src: nc.scalar.copy(out=XT[j][:, :, blk * 128:(blk + 1) * 128], in_=src), reads=[PS(b)], writes=[("XT", j)])
                    else:
                        p.op("dve", lambda blk=blk, src=src: nc.vector.tensor_copy(out=XT[j][:, :, blk * 128:(blk + 1) * 128], in_=src), reads=[PS(b)], writes=[("XT", j)])
                for f in range(8):
                    gb_ = f % 2
                    ub_ = 2 + f % 2
                    for k in range(8):
                        p.op("pe", lambda f=f, k=k: nc.tensor.matmul(psb[gb_][:, 0:CAP], lhsT=WG[j][:, k, f * 128:(f + 1) * 128], rhs=XT[j][:, k, :], start=(k == 0), stop=(k == 7)),
                             reads=[("WG", j, k // 4), ("XT", j)], writes=[PS(gb_)])
                    for k in range(8):
                        p.op("pe", lambda f=f, k=k: nc.tensor.matmul(psb[ub_][:, 0:CAP], lhsT=WU[j][:, k, f * 128:(f + 1) * 128], rhs=XT[j][:, k, :], start=(k == 0), stop=(k == 7)),
                             reads=[("WU", j, k // 4), ("XT", j)], writes=[PS(ub_)])
                    gc, gck = GC.next()
                    sg, sgk = SG.next()
                    uc, uck = UC.next()
                    p.op("dve", lambda f=f, gc=gc: nc.vector.tensor_scalar(out=gc[:], in0=psb[gb_][:, 0:CAP], scalar1=bgT[:, e, f:f + 1], scalar2=7.0, op0=ALU.add, op1=ALU.min),
                         reads=[PS(gb_), "bgT"], writes=[gck])
                    p.op("act", lambda gc=gc, sg=sg: nc.scalar.activation(out=sg[:], in_=gc[:], func=AF.Gelu_apprx_sigmoid), reads=[gck], writes=[sgk])
                    p.op("dve", lambda f=f, uc=uc: nc.vector.tensor_scalar(out=uc[:], in0=psb[ub_][:, 0:CAP], scalar1=bu1[:, e, f:f + 1], scalar2=-6.0, op0=ALU.add, op1=ALU.max),
                         reads=[PS(ub_), "bu1"], writes=[uck])
                    p.op("dve", lambda f=f, uc=uc, sg=sg: nc.vector.scalar_tensor_tensor(out=ACT_T[:, f, :], in0=uc[:], scalar=8.0, in1=sg[:], op0=ALU.min, op1=ALU.mult),
                         reads=[uck, sgk], writes=[("ACTT", f)])
                for blk in range(NBLK):
                    eo, eok = EO.next()
                    for half in range(2):
                        db_ = 4 + half
                        for f in range(8):
                            p.op("pe", lambda blk=blk, half=half, f=f: nc.tensor.matmul(psb[db_][:], lhsT=ACT_T[:, f, blk * 128:(blk + 1) * 128], rhs=WD[j][:, f, half * 512:(half + 1) * 512],
                                                                                     start=(f == 0), stop=(f == 7)),
                                 reads=[("ACTT", f), ("WD", j, f // 4)], writes=[PS(db_)])
                        p.op("dve", lambda half=half, eo=eo: nc.vector.tensor_tensor(out=eo[:, half * 512:(half + 1) * 512], in0=psb[db_][:], in1=BD[j][:, half * 512:(half + 1) * 512], op=ALU.add),
                             reads=[PS(db_), ("BD", j)], writes=[eok])
                    dma("sp", eo_d[e * CAP:(e + 1) * CAP, :].rearrange("(p b) d -> p b d", b=NBLK)[:, blk, :], eo[:], reads=[eok], writes=[("eo", e, blk)])
                    eo_keys.append(("eo", e, blk))
            p.barrier()

        with ExitStack() as es6:
            vec6 = sbuf(es6, "vec6", [128, 2, D], F32)
            dma("sp", vec6[:], vec_d[:, 4:6, :], writes=["vec6"])
            Hh = SRot(es6, "Hh", 5, [128, D], F32)
            G4 = SRot(es6, "G4", 20, [128, D], F32)
            AC = SRot(es6, "AC", 3, [128, D], F32)
            JT = SRot(es6, "JT", 3, [128, D], F32)
            OT = SRot(es6, "OT", 3, [128, D], F32)
            SM = SRot(es6, "SM2", 4, [128, 16], F32)
            def fetch(i):
                tsl = slice(i * 128, (i + 1) * 128)
                hh, hhk = Hh.next()
                dma("sp", hh[:], h32_d[tsl, :], reads=h32_keys, writes=[hhk])
                gs = []
                for k in range(4):
                    g_, gk_ = G4.next()
                    p.op("pool", lambda k=k, g_=g_: nc.gpsimd.indirect_dma_start(
                        out=g_[:], out_offset=None, in_=eo_d[:, :],
                        in_offset=bass.IndirectOffsetOnAxis(ap=pos_i[:, i * 4 + k:i * 4 + k + 1], axis=0)),
                        reads=[("pos", i)] + eo_keys, writes=[gk_], dma=True)
                    gs.append((g_, gk_))
                return hh, hhk, gs

            def combine(i, hh, hhk, gs):
                tsl = slice(i * 128, (i + 1) * 128)
                ac, ack = AC.next()
                p.op("act", lambda: nc.scalar.activation(out=ac[:], in_=gs[0][0][:], func=AF.Copy, scale=gates[:, i, 0:1]), reads=[gs[0][1], ("gates", i)], writes=[ack])
                yield
                for k in range(1, 4):
                    p.op("dve", lambda k=k: nc.vector.scalar_tensor_tensor(out=ac[:], in0=gs[k][0][:], scalar=gates[:, i, k:k + 1], in1=ac[:], op0=ALU.mult, op1=ALU.add),
                         reads=[gs[k][1], ("gates", i), ack], writes=[ack])
                    yield
                p.op("dve", lambda: nc.vector.scalar_tensor_tensor(out=ac[:], in0=hh[:], scalar=ALPHA, in1=ac[:], op0=ALU.mult, op1=ALU.add), reads=[hhk, ack], writes=[ack])
                yield
                if "dbg_pre2" in dram:
                    dma("sp", dram["dbg_pre2"][tsl, :], ac[:], reads=[ack])
                    yield
                jt, jtk = JT.next()
                ot, otk = OT.next()
                yield from layer_norm(ac, ack, ot, otk, vec6[:, 0, :], vec6[:, 1, :], "vec6", jt, jtk, SM)
                dma("sp", out_d[tsl, :], ot[:], reads=[otk], writes=[("out", i)])
                yield

            if stage >= 6:
                ft = {0: fetch(0), 1: fetch(1), 2: fetch(2)}

                def tileC(i):
                    if i + 3 < NT:
                        ft[i + 3] = fetch(i + 3)
                    yield from combine(i, *ft.pop(i))
                pipeline([lambda i=i: tileC(i) for i in range(NT)], stagger=8, depth=2)
        p.finish("sp")
    p.dram = dram
    return nc, p


def host_consts(flag):
    c = np.zeros((128, NCONST), np.float32)
    i = np.arange(128)
    c[:, C_IDENT:C_IDENT + 128] = np.eye(128, dtype=np.float32)
    c[:, C_TRI:C_TRI + 128] = (i[:, None] <= i[None, :])
    c[:, C_MPREV:C_MPREV + 128] = (i[:, None] >= i[None, :])
    c[:, C_MPREVC:C_MPREVC + 128] = flag * (i[:, None] >= i[None, :])
    c[:, C_LSTRICT:C_LSTRICT + 128] = (i[:, None] < i[None, :])
    c[:, C_ONES:C_ONES + 128] = 1.0
    c[:, C_IOTA:C_IOTA + 32] = np.arange(32)[None, :]
    c[:, C_TOKID:C_TOKID + 16] = np.arange(16)[None, :] * 128 + i[:, None]
    c[64, C_SEL:C_SEL + 64] = 1.0
    return c


def make_in_maps(inputs, cores=range(8)):
    f = lambda a: np.ascontiguousarray(np.asarray(a, dtype=np.float32))
    x = np.asarray(inputs["x"], dtype=np.float32)
    shared = {
        "w_in": f(inputs["w_in"][0]),
        "sgu_wT": f(np.transpose(inputs["sgu_w"][0], (2, 0, 1))),
        "sgu_bT": f(inputs["sgu_b"][0].T),
        "mix_gb64": f(inputs["mix_norm_g"][0, 512:].reshape(8, 64).T),
        "mix_gb": f(inputs["mix_norm_g"][0, 512:].reshape(4, 128).T),
        "w_out": f(inputs["w_out"][0]),
        "w_router": f(inputs["w_router"][0]),
        "b_router_bc": f(np.broadcast_to(inputs["b_router"][0][None, :], (128, NE))),
        "w_gate": f(inputs["w_gate"][0]),
        "w_up": f(inputs["w_up"][0]),
        "w_down": f(inputs["w_down"][0]),
        "b_gateT": f(np.transpose(inputs["b_gate"][0].reshape(NE, 8, 128), (2, 0, 1))),
        "b_upT": f(np.transpose(inputs["b_up"][0].reshape(NE, 8, 128), (2, 0, 1))),
        "b_down": f(inputs["b_down"][0]),
    }
    rows = np.zeros((6, D), np.float32)
    rows[0, :512] = inputs["sgu_ln_g"][0]
    rows[0, 512:] = inputs["sgu_ln_b"][0]
    rows[1, :512] = inputs["mix_norm_g"][0, :512]
    rows[2] = inputs["ln1_g"][0]
    rows[3] = inputs["ln1_b"][0]
    rows[4] = inputs["ln2_g"][0]
    rows[5] = inputs["ln2_b"][0]
    shared["vecs"] = f(np.broadcast_to(rows[None], (128, 6, D)))
    maps = []
    for c in cores:
        b, half = c // 2, c % 2
        own = x[b, half * NTOK:(half + 1) * NTOK]
        xT = np.zeros((D, NEXT), np.float32)
        if half == 1:
            xT[:, :NTOK] = x[b, :NTOK].T
        xT[:, NTOK:] = own.T
        m = dict(shared)
        m["xT"] = xT
        m["x"] = f(own)
        m["consts"] = host_consts(float(half))
        maps.append(m)
    return maps


_NC_CACHE = {}


def kernel(**inputs):
    if "nc" not in _NC_CACHE:
        _NC_CACHE["nc"] = build()[0]
    nc = _NC_CACHE["nc"]
    maps = make_in_maps(inputs)
    res = run_bass_kernel_spmd(nc, maps, core_ids=list(range(8)))
    out = np.zeros((4, 4096, D), np.float32)
    for c in range(8):
        b, half = c // 2, c % 2
        out[b, half * NTOK:(half + 1) * NTOK] = res.results[c]["out"]
    return out
```

```python
import numpy as np
from contextlib import ExitStack
import concourse.bass as bass
import concourse.mybir as mybir
from concourse.bass_utils import run_bass_kernel_spmd

F32 = mybir.dt.float32
BF16 = mybir.dt.bfloat16
I32 = mybir.dt.int32
U32 = mybir.dt.uint32
AF = mybir.ActivationFunctionType
ALU = mybir.AluOpType
AX = mybir.AxisListType

D = 1024
NTOK = 2048
NEXT = 4096
NT = NTOK // 128
NE = 32
TOPK = 4
CAP = 512
NBLK = CAP // 128
ALPHA = 2.0 ** 0.25
EPS = 1e-5
PATTERNS = (1, 4, 16)

C_IDENT = 0
C_TRI = 128
C_MPREV = 256
C_MPREVC = 384
C_LSTRICT = 512
C_ONES = 640
C_IOTA = 768
C_TOKID = 800
C_SEL = 816
NCONST = 880


class Prog:
    COMPUTE = ("pe", "act", "dve", "pool")

    def __init__(self, nc, n_dma_sems=20, same_engine_sync=True):
        self.nc = nc
        self.es = ExitStack()
        self.eng = {"pe": nc.tensor, "act": nc.scalar, "dve": nc.vector, "pool": nc.gpsimd, "sp": nc.sync}
        self.sem = {}
        self.cnt = {}
        for e in self.COMPUTE:
            self.sem[e] = self.es.enter_context(nc.semaphore("sem_" + e))
            self.cnt[e] = 0
        self.known = {e: {} for e in self.eng}
        self.last_w = {}
        self.readers = {}
        self.same_engine_sync = same_engine_sync
        self.dma_pool = {}
        self.dma_idx = {}
        for q in ("sp", "act", "pool"):
            self.dma_pool[q] = [[self.es.enter_context(nc.semaphore(f"dsem_{q}_{i}")), 0] for i in range(n_dma_sems)]
            self.dma_idx[q] = 0
        self.n_inst = {e: 0 for e in self.eng}

    def sb(self, name, shape, dt):
        return self.es.enter_context(self.nc.sbuf_tensor("s_" + name, shape, dt))

    def ps(self, name, shape, dt):
        return self.es.enter_context(self.nc.psum_tensor(name, shape, dt))

    def _wait(self, e, tok):
        sem, val, src = tok
        if src == e and (e == "pe" or not self.same_engine_sync):
            return
        k = self.known[e]
        sid = id(sem)
        if k.get(sid, 0) >= val:
            return
        self.eng[e].wait_ge(sem, val)
        k[sid] = val

    def op(self, e, fn, reads=(), writes=(), dma=False):
        deps = {}

        def add(t):
            key = id(t[0])
            if key not in deps or deps[key][1] < t[1]:
                deps[key] = t
        for kk in reads:
            t = self.last_w.get(kk)
            if t is not None:
                add(t)
        for kk in writes:
            t = self.last_w.get(kk)
            if t is not None:
                add(t)
            for t in self.readers.get(kk, ()):
                add(t)
        for t in deps.values():
            self._wait(e, t)
        if dma:
            pool = self.dma_pool[e]
            i = self.dma_idx[e]
            self.dma_idx[e] = (i + 1) % len(pool)
            slot = pool[i]
            if slot[1] > 0:
                self._wait(e, (slot[0], slot[1], "dma"))
            inst = fn()
            slot[1] += 16
            inst.then_inc(slot[0], 16)
            tok = (slot[0], slot[1], "dma")
        else:
            inst = fn()
            self.cnt[e] += 1
            inst.then_inc(self.sem[e], 1)
            tok = (self.sem[e], self.cnt[e], e)
        self.n_inst[e] += 1
        for kk in writes:
            self.last_w[kk] = tok
            self.readers[kk] = []
        for kk in reads:
            if kk in writes:
                continue
            self.readers.setdefault(kk, []).append(tok)
        return tok

    def barrier(self):
        for e in self.eng:
            self.finish(e)

    def finish(self, e="sp"):
        for pool in self.dma_pool.values():
            for sem, val in pool:
                if val > 0:
                    self._wait(e, (sem, val, "dma"))
        for c in self.COMPUTE:
            if self.cnt[c] > 0:
                self._wait(e, (self.sem[c], self.cnt[c], c))


class Rot:
    def __init__(self, p, name, n, shape, dt):
        self.tiles = [p.sb(f"{name}{i}", shape, dt) for i in range(n)]
        self.name = name
        self.n = n
        self.i = 0

    def next(self):
        j = self.i % self.n
        self.i += 1
        return self.tiles[j], (self.name, j)


def v_tiles():
    out = {}
    out[1] = [(n, 0) for n in range(15, 32)]
    out[4] = [(n, r) for n in range(3, 8) for r in range(4)]
    out[16] = [(n, r) for n in range(0, 2) for r in range(16)]
    return out


def build(stage=99, dbg=()):
    nc = bass.Bass("TRN2", target_bir_lowering=False)
    dram = {}

    def din(name, shape, dt=F32):
        dram[name] = nc.dram_tensor(name, list(shape), dt, kind="ExternalInput").ap()
        return dram[name]

    def dout(name, shape, dt=F32):
        dram[name] = nc.dram_tensor(name, list(shape), dt, kind="ExternalOutput").ap()
        return dram[name]

    xT_d = din("xT", [D, NEXT])
    x_d = din("x", [NTOK, D])
    win_d = din("w_in", [D, 2560])
    consts_d = din("consts", [128, NCONST])
    sguw_d = din("sgu_wT", [128, 8, 128])
    sgub_d = din("sgu_bT", [128, 8])
    vec_d = din("vecs", [128, 6, D])
    din("mix_gb64", [64, 8])
    gb_d = din("mix_gb", [128, 4])
    wout_d = din("w_out", [D, D])
    wr_d = din("w_router", [D, NE])
    br_d = din("b_router_bc", [128, NE])
    if stage >= 5:
        wg_d = din("w_gate", [NE, D, D])
        wu_d = din("w_up", [NE, D, D])
        wd_d = din("w_down", [NE, D, D])
    bg_d = din("b_gateT", [128, NE, 8])
    bu_d = din("b_upT", [128, NE, 8])
    bd_d = din("b_down", [NE, D])
    out_d = dout("out", [NTOK, D])
    for name, shape in dbg:
        dout(name, shape)

    p = Prog(nc)
    with p.es:
        E = p.eng
        psb = [p.ps(f"psb{i}", [128, 512], F32) for i in range(8)]

        def PS(b):
            return ("ps", b)

        def weave(*gens):
            its = [g for g in gens if g is not None]
            while its:
                for g in list(its):
                    try:
                        next(g)
                    except StopIteration:
                        its.remove(g)

        def run(g):
            for _ in g:
                pass

        def pipeline2(ents, stagger, depth):
            pending = list(ents)
            active = []
            since = stagger
            tiles_done = 0
            while pending or active:
                n_tiles = sum(1 for k, _ in active if k == "tile")
                for ent in list(pending):
                    kind, fact, need = ent
                    if kind == "tile":
                        if n_tiles < depth and since >= stagger:
                            active.append((kind, fact()))
                            pending.remove(ent)
                            since = 0
                            n_tiles += 1
                        break
                    if tiles_done >= need:
                        active.append((kind, fact()))
                        pending.remove(ent)
                for item in list(active):
                    try:
                        next(item[1])
                    except StopIteration:
                        active.remove(item)
                        if item[0] == "tile":
                            tiles_done += 1
                since += 1

        def pipeline(facts, stagger, depth):
            active = []
            idx = 0
            since = stagger
            while idx < len(facts) or active:
                if idx < len(facts) and len(active) < depth and since >= stagger:
                    active.append(facts[idx]())
                    idx += 1
                    since = 0
                for g in list(active):
                    try:
                        next(g)
                    except StopIteration:
                        active.remove(g)
                since += 1

        def sbuf_raw(es, name, shape, dt):
            return es.enter_context(nc.sbuf_tensor("s_" + name, shape, dt))

        def sbuf(es, name, shape, dt):
            return es.enter_context(nc.sbuf_tensor("s_" + name, shape, dt))

        class SRot:
            def __init__(self, es, name, n, shape, dt):
                self.tiles = [sbuf(es, f"{name}{i}", shape, dt) for i in range(n)]
                self.name, self.n, self.i = name, n, 0

            def next(self):
                j = self.i % self.n
                self.i += 1
                return self.tiles[j], (self.name, j)

        def dma(q, out, in_, reads=(), writes=()):
            return p.op(q, lambda: E[q].dma_start(out=out, in_=in_), reads=reads, writes=writes, dma=True)

        consts = p.sb("consts", [128, NCONST], F32)
        consts_bf = p.sb("consts_bf", [128, NCONST], BF16)
        dma("sp", consts[:], consts_d, writes=["consts"])
        dma("pool", consts_bf[:], consts_d, writes=["consts_bf"])
        sgub = p.sb("sgub", [128, 8], F32)
        dma("sp", sgub[:], sgub_d, writes=["sgub"])
        gb = p.sb("gb", [128, 4], F32)
        dma("sp", gb[:], gb_d, writes=["gb"])
        wcT = p.sb("wcT", [128, 8, 128], BF16)
        eps_t = p.sb("eps_t", [128, 1], F32)
        p.op("dve", lambda: nc.vector.memset(eps_t[:], EPS), writes=["eps"])
        ident_bf = consts_bf[:, C_IDENT:C_IDENT + 128]
        ident_f = consts[:, C_IDENT:C_IDENT + 128]

        ssq_a = p.sb("ssq_a", [128, NT], F32)
        ssq_b = p.sb("ssq_b", [128, NT, 8], F32)
        p.op("dve", lambda: nc.vector.memset(ssq_a[:], 0.0), writes=["ssq_a"])
        SLOTS = NE * CAP
        hbf_d = nc.dram_tensor("hbf_scr", [NTOK + 1, D], BF16).ap()
        h32_d = nc.dram_tensor("h32_scr", [NTOK, D], F32).ap()
        stok_d = nc.dram_tensor("stok_scr", [SLOTS + 128, 1], I32).ap()
        eo_d = nc.dram_tensor("eo_scr", [SLOTS + 1, D], F32).ap()
        gates = p.sb("gates", [128, NT, 4], F32)
        pos_i = p.sb("pos_i", [128, NT * 4], I32)
        Mall = p.sb("Mall", [128, NT, NE], BF16)
        tokid_i = p.sb("tokid_i", [128, NT], I32)
        p.op("dve", lambda: nc.vector.tensor_copy(out=tokid_i[:], in_=consts[:, C_TOKID:C_TOKID + NT]), reads=["consts"], writes=["tokid_i"])
        fill = p.sb("fill", [128, (SLOTS + 128) // 128], I32)
        p.op("dve", lambda: nc.vector.memset(fill[:], NTOK), writes=["fill"])
        dma("sp", stok_d.rearrange("(p f) o -> p (f o)", p=128), fill[:], reads=["fill"], writes=["stok_init"])
        esAB = ExitStack()
        aT = sbuf_raw(esAB, "aT", [128, 4, NTOK], BF16)
        bT = sbuf_raw(esAB, "bT", [128, 4, NTOK], BF16)
        vt = v_tiles()
        vidx = {}
        nv = 0
        for d_ in PATTERNS:
            for nr in vt[d_]:
                vidx[(d_,) + nr] = nv
                nv += 1

        with ExitStack() as esQ:
            qT = sbuf(esQ, "qT", [128, 4, NTOK], BF16)
            kT = sbuf(esQ, "kT", [128, 4, NEXT], BF16)
            vT = sbuf(esQ, "vT", [128, 4, NEXT], BF16)
            with ExitStack() as esX:
                xTo = sbuf(esX, "xT_own", [128, 8, NTOK], BF16)
                xT_v = xT_d.rearrange("(k p) t -> p k t", p=128)
                with ExitStack() as esB:
                    xTc = sbuf(esB, "xT_ctx", [128, 8, NTOK], BF16)
                    wkv = sbuf(esB, "win_kv", [128, 8, 1024], BF16)
                    sguw = sbuf(esB, "sguw", [128, 8, 128], F32)
                    dma("sp", sguw[:], sguw_d, writes=["sguw"])
                    tri_bc = consts[:, C_TRI:C_TRI + 128].unsqueeze(1).to_broadcast([128, 8, 128])
                    p.op("dve", lambda: nc.vector.tensor_tensor(out=wcT[:], in0=sguw[:], in1=tri_bc, op=ALU.mult),
                         reads=["sguw", "consts"], writes=["wcT"])
                    for k in range(0, 8, 2):
                        dma("pool", wkv[:, k:k + 2, :], win_d[k * 128:(k + 2) * 128, 1536:2560].rearrange("(k p) c -> p k c", p=128), writes=[("wkv", k), ("wkv", k + 1)])
                    for tcl in range(4):
                        dma("pool", xTc[:, :, tcl * 512:(tcl + 1) * 512], xT_v[:, :, tcl * 512:(tcl + 1) * 512], writes=[("xTc", tcl)])
                    for tcl in range(4):
                        dma("pool", xTo[:, :, tcl * 512:(tcl + 1) * 512], xT_v[:, :, NTOK + tcl * 512:NTOK + (tcl + 1) * 512], writes=[("xTo", tcl)])
                    ev = 0
                    for tc in range(8 if stage >= 2 else 0):
                        for (dst, dname, col0) in ((kT, "kT", 0), (vT, "vT", 512)):
                            for hp in range(4):
                                b = ev % 6
                                src, srck = (xTo, ("xTo", tc - 4)) if tc >= 4 else (xTc, ("xTc", tc))
                                tcl = tc % 4
                                for k in range(8):
                                    p.op("pe", lambda k=k, b=b, src=src, tcl=tcl: nc.tensor.matmul(
                                        psb[b][:], lhsT=wkv[:, k, col0 + hp * 128:col0 + (hp + 1) * 128],
                                        rhs=src[:, k, tcl * 512:(tcl + 1) * 512], start=(k == 0), stop=(k == 7)),
                                        reads=[srck, ("wkv", k)], writes=[PS(b)])
                                if ev % 2 == 0:
                                    p.op("act", lambda b=b: nc.scalar.copy(out=dst[:, hp, tc * 512:(tc + 1) * 512], in_=psb[b][:]), reads=[PS(b)], writes=[(dname, hp)])
                                else:
                                    p.op("dve", lambda b=b: nc.vector.tensor_copy(out=dst[:, hp, tc * 512:(tc + 1) * 512], in_=psb[b][:]), reads=[PS(b)], writes=[(dname, hp)])
                                ev += 1
                    p.barrier()
                with ExitStack() as esA:
                    win = sbuf(esA, "win_uvq", [128, 8, 1536], BF16)
                    for k in range(8):
                        dma("pool", win[:, k, :], win_d[k * 128:(k + 1) * 128, 0:1536], writes=[("win", k)])
                    wk = [("win", k) for k in range(8)]
                    vecA = sbuf(esA, "vecA", [128, 3, 512], F32)
                    dma("sp", vecA[:, 0:2, :], vec_d[:, 0, :].rearrange("p (a c) -> p a c", a=2), writes=["vecA"])
                    dma("sp", vecA[:, 2, :], vec_d[:, 1, 0:512], writes=["vecA"])
                    U = SRot(esA, "U", 2, [128, 512], F32)
                    V = SRot(esA, "V", 2, [128, 512], F32)
                    W1 = SRot(esA, "W1", 2, [128, 512], F32)
                    W2 = SRot(esA, "W2", 2, [128, 512], F32)
                    W3 = SRot(esA, "W3", 1, [128, 512], F32)
                    VN = SRot(esA, "VN", 2, [128, 512], BF16)
                    AB = SRot(esA, "AB", 2, [128, 512], BF16)
                    st = SRot(esA, "st", 2, [128, 8, 8], F32)

                    def gelu(src_ap, src_key, dst, dst_key):
                        p.op("act", lambda: nc.scalar.activation(out=dst[:], in_=src_ap, func=AF.Gelu_apprx_tanh), reads=[src_key], writes=[dst_key])

                    def emit_uv(i):
                        t0 = i * 128
                        b0 = 2 * (i % 2)
                        for half in range(2):
                            for k in range(8):
                                p.op("pe", lambda half=half, k=k: nc.tensor.matmul(
                                    psb[b0 + half][:], lhsT=xTo[:, k, t0:t0 + 128], rhs=win[:, k, half * 512:(half + 1) * 512],
                                    start=(k == 0), stop=(k == 7)), reads=[("xTo", i // 4), wk[k]], writes=[PS(b0 + half)])

                    hnd = {}

                    def S1(i):
                        b0 = 2 * (i % 2)
                        u, uk = U.next()
                        v, vk = V.next()
                        gelu(psb[b0][:], PS(b0), u, uk)
                        gelu(psb[b0 + 1][:], PS(b0 + 1), v, vk)
                        s, sk = st.next()
                        w1, w1k = W1.next()
                        v3 = v[:].rearrange("p (g c) -> p g c", g=8)
                        w13 = w1[:].rearrange("p (g c) -> p g c", g=8)
                        p.op("dve", lambda: nc.vector.tensor_reduce(out=s[:, 0, :], in_=v3, axis=AX.X, op=ALU.add), reads=[vk], writes=[sk])
                        yield
                        p.op("act", lambda: nc.scalar.activation(out=w1[:], in_=v[:], func=AF.Square), reads=[vk], writes=[w1k])
                        yield
                        p.op("dve", lambda: nc.vector.tensor_reduce(out=s[:, 1, :], in_=w13, axis=AX.X, op=ALU.add), reads=[w1k], writes=[sk])
                        yield
                        p.op("dve", lambda: nc.vector.tensor_single_scalar(out=s[:, 2, :], in_=s[:, 0, :], scalar=1.0 / 64, op=ALU.mult), reads=[sk], writes=[sk])
                        yield
                        p.op("dve", lambda: nc.vector.tensor_tensor(out=s[:, 3, :], in0=s[:, 2, :], in1=s[:, 2, :], op=ALU.mult), reads=[sk], writes=[sk])
                        yield
                        p.op("dve", lambda: nc.vector.scalar_tensor_tensor(out=s[:, 4, :], in0=s[:, 1, :], scalar=1.0 / 64, in1=s[:, 3, :], op0=ALU.mult, op1=ALU.subtract), reads=[sk], writes=[sk])
                        yield
                        p.op("act", lambda: nc.scalar.activation(out=s[:, 6, :], in_=s[:, 4, :], func=AF.Sqrt, bias=eps_t[:, 0:1], scale=1.0), reads=[sk, "eps"], writes=[sk])
                        yield
                        p.op("dve", lambda: nc.vector.reciprocal(out=s[:, 5, :], in_=s[:, 6, :]), reads=[sk], writes=[sk])
                        yield
                        mean_bc = s[:, 2, :].unsqueeze(2).to_broadcast([128, 8, 64])
                        rstd_bc = s[:, 5, :].unsqueeze(2).to_broadcast([128, 8, 64])
                        p.op("dve", lambda: nc.vector.tensor_tensor(out=w13, in0=v3, in1=mean_bc, op=ALU.subtract), reads=[vk, sk], writes=[w1k])
                        yield
                        p.op("dve", lambda: nc.vector.tensor_tensor(out=w13, in0=w13, in1=rstd_bc, op=ALU.mult), reads=[w1k, sk], writes=[w1k])
                        yield
                        vn, vnk = VN.next()
                        p.op("dve", lambda: nc.vector.tensor_tensor(out=w1[:], in0=w1[:], in1=vecA[:, 0, :], op=ALU.mult), reads=[w1k, "vecA"], writes=[w1k])
                        yield
                        p.op("dve", lambda: nc.vector.tensor_tensor(out=vn[:], in0=w1[:], in1=vecA[:, 1, :], op=ALU.add), reads=[w1k, "vecA"], writes=[vnk])
                        yield
                        hnd[i] = (u, uk, vn, vnk)

                    def S2(i):
                        u, uk, vn, vnk = hnd.pop(i)
                        for g in range(8):
                            p.op("pe", lambda g=g: nc.tensor.matmul(psb[4][:, g * 64:(g + 1) * 64], lhsT=wcT[:, g, :], rhs=vn[:, g * 64:(g + 1) * 64],
                                                                     start=True, stop=True), reads=["wcT", vnk], writes=[PS(4)])
                            yield
                        w2, w2k = W2.next()
                        w23 = w2[:].rearrange("p (g c) -> p g c", g=8)
                        bs_bc = sgub[:, :].unsqueeze(2).to_broadcast([128, 8, 64])
                        p.op("dve", lambda: nc.vector.tensor_tensor(out=w23, in0=psb[4][:].rearrange("p (g c) -> p g c", g=8), in1=bs_bc, op=ALU.add),
                             reads=[PS(4), "sgub"], writes=[w2k])
                        yield
                        p.op("dve", lambda: nc.vector.tensor_tensor(out=w2[:], in0=w2[:], in1=u[:], op=ALU.mult), reads=[w2k, uk], writes=[w2k])
                        yield
                        w1b, w1bk = W3.next()
                        p.op("act", lambda: nc.scalar.activation(out=w1b[:], in_=w2[:], func=AF.Square, accum_out=ssq_a[:, i:i + 1]), reads=[w2k], writes=[w1bk, "ssq_a"])
                        yield
                        ab, abk = AB.next()
                        p.op("dve", lambda: nc.vector.tensor_tensor(out=ab[:], in0=w2[:], in1=vecA[:, 2, :], op=ALU.mult), reads=[w2k, "vecA"], writes=[abk])
                        yield
                        ptr = psb[5][:].bitcast(BF16)
                        for c in range(4):
                            p.op("pe", lambda c=c: nc.tensor.transpose(ptr[:, c * 128:(c + 1) * 128], ab[:, c * 128:(c + 1) * 128], ident_bf),
                                 reads=[abk, "consts_bf"], writes=[PS(5)])
                            yield
                        p.op("act", lambda: nc.scalar.copy(out=aT[:, :, i * 128:(i + 1) * 128], in_=ptr[:, 0:512].rearrange("p (c t) -> p c t", c=4)),
                             reads=[PS(5)], writes=["aT"])
                        yield
                        if "dbg_a" in dram:
                            dma("sp", dram["dbg_a"][i * 128:(i + 1) * 128, :], w2[:], reads=[w2k])
                            yield

                    qgroups = [(hp, tc) for hp in range(4) for tc in range(4)]

                    def qproj(ev):
                        hp, tc = qgroups[ev]
                        b = 6 + (ev % 2)
                        for k in range(8):
                            p.op("pe", lambda k=k, b=b: nc.tensor.matmul(
                                psb[b][:], lhsT=win[:, k, 1024 + hp * 128:1024 + (hp + 1) * 128],
                                rhs=xTo[:, k, tc * 512:(tc + 1) * 512], start=(k == 0), stop=(k == 7)),
                                reads=[("xTo", tc), wk[k]], writes=[PS(b)])
                        yield
                        p.op("dve", lambda b=b: nc.vector.tensor_copy(out=qT[:, hp, tc * 512:(tc + 1) * 512], in_=psb[b][:]), reads=[PS(b)], writes=[("qT", hp)])
                        yield

                    if stage >= 1:
                        emit_uv(0)
                        emit_uv(1)
                        run(S1(0))
                        for i in range(NT):
                            if i + 2 < NT:
                                emit_uv(i + 2)
                            weave(S1(i + 1) if i + 1 < NT else None, S2(i), qproj(i) if stage >= 2 else None)
                    if "dbg_ssq" in dram:
                        dma("sp", dram["dbg_ssq"], ssq_a[:], reads=["ssq_a"])
                    p.barrier()
            with ExitStack() as esD:
                for nm, t_, rk in (("dbg_aT", aT, ["aT"]), ("dbg_qT", qT, [("qT", h_) for h_ in range(4)])):
                    if nm in dram:
                        tmpf = sbuf(esD, nm, [128, 4, NTOK], F32)
                        p.op("dve", lambda: nc.vector.tensor_copy(out=tmpf[:], in_=t_[:]), reads=rk, writes=[nm])
                        dma("sp", dram[nm], tmpf[:], reads=[nm])
                p.barrier()
            with ExitStack() as esT:
                vaugp = [sbuf(esT, f"vaugp{i}", [128, nv, 2, 66], BF16) for i in range(2)]
                for i_ in range(2):
                    p.op("pool", lambda i_=i_: nc.gpsimd.memset(vaugp[i_][:, :, :, 64:66], 1.0), writes=[("vones", i_)])
                accT = [sbuf(esT, f"accT{i}", [65, NTOK], F32) for i in range(2)]
                Eb = SRot(esT, "Eb", 3, [128, 512], BF16)
                Em = SRot(esT, "Em", 3, [128, 512], BF16)
                RD = SRot(esT, "RD", 2, [64, 512], F32)
                BQ = SRot(esT, "BQ", 2, [64, 512], F32)
                BS = SRot(esT, "BS", 2, [64, 512], BF16)
                masks = sbuf(esT, "masks", [128, 3, 512], BF16)
                mprev = consts_bf[:, C_MPREV:C_MPREV + 128]
                mprevc = consts_bf[:, C_MPREVC:C_MPREVC + 128]
                mcur = consts_bf[:, C_TRI:C_TRI + 128]
                for mi, pat in enumerate(((mprev, mcur, mprev, mcur), (mprevc, mcur, mprev, mcur), (mprevc, mcur, mprevc, mcur))):
                    for j, src in enumerate(pat):
                        p.op("dve", lambda mi=mi, j=j, src=src: nc.vector.tensor_copy(out=masks[:, mi, j * 128:(j + 1) * 128], in_=src),
                             reads=["consts_bf"], writes=["masks"])
                sel_f = consts[0:65, C_SEL:C_SEL + 64]
                ones_f = consts_bf[0:64, C_ONES:C_ONES + 1]
                gb64 = sbuf(esT, "gb64", [64, 8], F32)
                dma("sp", gb64[:], dram["mix_gb64"], writes=["gb64"])
                allv = [(d_, n, r) for d_ in PATTERNS for (n, r) in vt[d_]]
                tgc = [0]

                def build_v(hp):
                    vb = vaugp[hp % 2]
                    vbk = ("vaugp", hp % 2)
                    for g0 in range(0, nv, 8):
                        grp = allv[g0:g0 + 8]
                        tg = tgc[0]
                        tgc[0] += 1
                        b = 6 + (tg % 2)
                        ptr = psb[b][:].bitcast(BF16)
                        for j, (d_, n, r) in enumerate(grp):
                            span = 128 * d_
                            s0 = span * n + r
                            p.op("pe", lambda j=j, s0=s0, span=span, d_=d_, ptr=ptr: nc.tensor.transpose(
                                ptr[:, j * 128:(j + 1) * 128], vT[:, hp, s0:s0 + span - d_ + 1:d_], ident_bf),
                                reads=[("vT", hp), "consts_bf"], writes=[PS(b)])
                            yield
                        ng = len(grp)
                        src = ptr[:, 0:ng * 128].rearrange("p (t h c) -> p t h c", t=ng, h=2)
                        if tg % 2 == 0:
                            p.op("act", lambda src=src, g0=g0, ng=ng: nc.scalar.copy(out=vb[:, g0:g0 + ng, :, 0:64], in_=src), reads=[PS(b)], writes=[vbk])
                            yield
                        else:
                            p.op("dve", lambda src=src, g0=g0, ng=ng: nc.vector.tensor_copy(out=vb[:, g0:g0 + ng, :, 0:64], in_=src), reads=[PS(b)], writes=[vbk])
                            yield

                def unit_list(h):
                    us = []
                    for d_ in PATTERNS:
                        if d_ == 1:
                            blocks = [(n, 0) for n in range(16, 32)]
                        elif d_ == 4:
                            blocks = [(n, r) for n in range(4, 8) for r in range(4)]
                        else:
                            blocks = [(1, r) for r in range(16)]
                        for ui in range(0, 16, 2):
                            us.append((h, d_, blocks[ui:ui + 2]))
                    return us

                def emit_qk(u, su):
                    h, d_, pair = u
                    hp, hh = h // 2, h % 2
                    po = 64 * hh
                    span = 128 * d_
                    sb_ = su % 2
                    for j, (n, r) in enumerate(pair):
                        q0 = span * n + r - NTOK
                        qs = qT[po:po + 64, hp, q0:q0 + span - d_ + 1:d_]
                        kp0 = span * (n - 1) + r
                        kc0 = span * n + r
                        p.op("pe", lambda j=j, qs=qs, kp0=kp0: nc.tensor.matmul(
                            psb[sb_][:, j * 256:j * 256 + 128], lhsT=kT[po:po + 64, hp, kp0:kp0 + span - d_ + 1:d_], rhs=qs, start=True, stop=True),
                            reads=[("kT", hp), ("qT", hp)], writes=[PS(sb_)])
                        p.op("pe", lambda j=j, qs=qs, kc0=kc0: nc.tensor.matmul(
                            psb[sb_][:, j * 256 + 128:j * 256 + 256], lhsT=kT[po:po + 64, hp, kc0:kc0 + span - d_ + 1:d_], rhs=qs, start=True, stop=True),
                            reads=[("kT", hp), ("qT", hp)], writes=[PS(sb_)])

                def emit_mid(u, su):
                    h, d_, pair = u
                    span = 128 * d_
                    sb_ = su % 2
                    ctx = tuple((span * (n - 1) + r) < NTOK for (n, r) in pair)
                    mi = {(False, False): 0, (True, False): 1, (True, True): 2}[ctx]
                    eb, ebk = Eb.next()
                    em, emk = Em.next()
                    p.op("act", lambda: nc.scalar.activation(out=eb[:], in_=psb[sb_][:], func=AF.Exp, scale=0.125), reads=[PS(sb_)], writes=[ebk])
                    p.op("dve", lambda: nc.vector.tensor_tensor(out=em[:], in0=eb[:], in1=masks[:, mi, :], op=ALU.mult), reads=[ebk, "masks"], writes=[emk])
                    return em, emk

                def emit_pv(u, su, em, emk):
                    h, d_, pair = u
                    hp, hh = h // 2, h % 2
                    vb = vaugp[hp % 2]
                    vbk = ("vaugp", hp % 2)
                    ob_ = 2 + su % 2
                    for j, (n, r) in enumerate(pair):
                        vp = vidx[(d_, n - 1, r)]
                        vc = vidx[(d_, n, r)]
                        p.op("pe", lambda j=j, vp=vp: nc.tensor.matmul(
                            psb[ob_][0:65, j * 128:(j + 1) * 128], lhsT=vb[:, vp, hh, 0:65], rhs=em[:, j * 256:j * 256 + 128], start=True, stop=False),
                            reads=[vbk, ("vones", hp % 2), emk], writes=[PS(ob_)])
                        p.op("pe", lambda j=j, vc=vc: nc.tensor.matmul(
                            psb[ob_][0:65, j * 128:(j + 1) * 128], lhsT=vb[:, vc, hh, 0:65], rhs=em[:, j * 256 + 128:j * 256 + 256], start=False, stop=True),
                            reads=[vbk, ("vones", hp % 2), emk], writes=[PS(ob_)])

                def emit_acc(u, su):
                    h, d_, pair = u
                    acc = accT[h % 2]
                    acck = ("accT", h % 2)
                    span = 128 * d_
                    ob_ = 2 + su % 2
                    n0, r0 = pair[0]
                    src = psb[ob_][0:65, 0:256].rearrange("p (b j) -> p b j", b=2)
                    if d_ == 1:
                        q0 = span * n0 - NTOK
                        dest = acc[0:65, q0:q0 + 256].rearrange("p (b j) -> p b j", b=2)
                        p.op("act", lambda: nc.scalar.copy(out=dest, in_=src), reads=[PS(ob_)], writes=[acck])
                    else:
                        s0 = span * n0 - NTOK
                        dest = acc[0:65, s0:s0 + span].rearrange("p (j d) -> p d j", d=d_)[:, r0:r0 + 2, :]
                        p.op("dve", lambda: nc.vector.tensor_tensor(out=dest, in0=dest, in1=src, op=ALU.add), reads=[PS(ob_), acck], writes=[acck])

                def finalize(h):
                    hp, hh = h // 2, h % 2
                    po = 64 * hh
                    acc = accT[h % 2]
                    acck = ("accT", h % 2)
                    for c in range(4):
                        p.op("pe", lambda c=c: nc.tensor.matmul(psb[4][0:64, :], lhsT=sel_f, rhs=acc[0:65, c * 512:(c + 1) * 512], start=True, stop=True),
                             reads=["consts", acck], writes=[PS(4)])
                        yield
                        rd, rdk = RD.next()
                        bq_, bqk = BQ.next()
                        bs_, bsk = BS.next()
                        p.op("dve", lambda rd=rd: nc.vector.reciprocal(out=rd[:], in_=psb[4][0:64, :]), reads=[PS(4)], writes=[rdk])
                        yield
                        p.op("dve", lambda c=c, rd=rd, bq_=bq_: nc.vector.tensor_tensor(out=bq_[:], in0=acc[0:64, c * 512:(c + 1) * 512], in1=rd[:], op=ALU.mult),
                             reads=[acck, rdk], writes=[bqk])
                        yield
                        p.op("act", lambda bq_=bq_, bs_=bs_: nc.scalar.activation(out=bs_[:], in_=bq_[:], func=AF.Square), reads=[bqk], writes=[bsk])
                        yield
                        for t in range(4):
                            ti = c * 4 + t
                            col = ti * 8 + h
                            p.op("pe", lambda t=t, col=col, bs_=bs_: nc.tensor.matmul(psb[5][:, col:col + 1], lhsT=bs_[0:64, t * 128:(t + 1) * 128], rhs=ones_f, start=True, stop=True),
                                 reads=[bsk, "consts_bf"], writes=[PS(5)])
                            yield
                        p.op("act", lambda c=c, bq_=bq_: nc.scalar.activation(out=bT[po:po + 64, hp, c * 512:(c + 1) * 512], in_=bq_[:], func=AF.Copy, scale=gb64[:, h:h + 1]),
                             reads=[bqk, "gb64"], writes=["bT"])
                        yield
                        if "dbg_b" in dram:
                            dma("sp", dram["dbg_b"][h * 64:(h + 1) * 64, c * 512:(c + 1) * 512], bq_[:], reads=[bqk])
                            yield

                if stage >= 3:
                    units = []
                    for h in range(8):
                        units += unit_list(h)
                    run(build_v(0))
                    emit_qk(units[0], 0)
                    pend = None
                    bg = []

                    def step_bg(n):
                        for _ in range(n):
                            if not bg:
                                return
                            try:
                                next(bg[0])
                            except StopIteration:
                                bg.pop(0)

                    def drain_bg():
                        while bg:
                            step_bg(1)
                    for ui, u in enumerate(units):
                        if ui + 1 < len(units):
                            emit_qk(units[ui + 1], ui + 1)
                        em, emk = emit_mid(u, ui)
                        if pend is not None:
                            emit_acc(*pend)
                            pend = None
                        emit_pv(u, ui, em, emk)
                        pend = (u, ui)
                        step_bg(4)
                        h = u[0]
                        last_of_head = (ui + 1 == len(units)) or units[ui + 1][0] != h
                        if last_of_head:
                            emit_acc(*pend)
                            pend = None
                            drain_bg()
                            bg.append(finalize(h))
                            if h % 2 == 0 and h // 2 + 1 < 4:
                                bg.append(build_v(h // 2 + 1))
                    drain_bg()
                if stage >= 3:
                    p.op("dve", lambda: nc.vector.tensor_copy(out=ssq_b[:].rearrange("p t h -> p (t h)"), in_=psb[5][:, 0:128]), reads=[PS(5)], writes=["ssq_b"])
                    if "dbg_ssqb" in dram:
                        dma("sp", dram["dbg_ssqb"], ssq_b[:].rearrange("p t h -> p (t h)"), reads=["ssq_b"])
                p.barrier()
        scat_keys = []
        hbf_keys = ["hbf_z"]
        h32_keys = []
        with ExitStack() as es3:
            zrow = sbuf(es3, "zrow", [1, D], F32)
            zrow_bf = sbuf(es3, "zrow_bf", [1, D], BF16)
            p.op("dve", lambda: nc.vector.memset(zrow[:], 0.0), writes=["zrow"])
            p.op("dve", lambda: nc.vector.memset(zrow_bf[:], 0.0), writes=["zrow_bf"])
            dma("sp", hbf_d[NTOK:NTOK + 1, :], zrow_bf[:], reads=["zrow_bf"], writes=["hbf_z"])
            dma("sp", eo_d[SLOTS:SLOTS + 1, :], zrow[:], reads=["zrow"], writes=["eo_z"])
            wout = sbuf(es3, "wout", [128, 8, D], BF16)
            for k in range(8):
                dma("pool", wout[:, k, :], wout_d[k * 128:(k + 1) * 128, :], writes=[("wout", k)])
            vec3 = sbuf(es3, "vec3", [128, 2, D], F32)
            dma("sp", vec3[:], vec_d[:, 2:4, :], writes=["vec3"])
            wr = sbuf(es3, "wr", [128, 8, NE], F32)
            dma("sp", wr[:], wr_d.rearrange("(k p) e -> p k e", p=128), writes=["wr"])
            brb = sbuf(es3, "brb", [128, NE], F32)
            dma("sp", brb[:], br_d, writes=["brb"])
            rs = sbuf(es3, "rs", [128, 4, NT], F32)
            p.op("dve", lambda: nc.vector.tensor_reduce(out=rs[:, 2, :], in_=ssq_b[:], axis=AX.X, op=ALU.add), reads=["ssq_b"], writes=["rs"])
            p.op("act", lambda: nc.scalar.activation(out=rs[:, 3, :], in_=ssq_a[:], func=AF.Ln, bias=eps_t[:, 0:1], scale=1.0 / 512), reads=["ssq_a", "eps"], writes=["rs"])
            p.op("act", lambda: nc.scalar.activation(out=rs[:, 0, :], in_=rs[:, 3, :], func=AF.Exp, scale=-0.5), reads=["rs"], writes=["rs"])
            p.op("act", lambda: nc.scalar.activation(out=rs[:, 3, :], in_=rs[:, 2, :], func=AF.Ln, bias=eps_t[:, 0:1], scale=1.0 / 512), reads=["rs", "eps"], writes=["rs"])
            p.op("act", lambda: nc.scalar.activation(out=rs[:, 1, :], in_=rs[:, 3, :], func=AF.Exp, scale=-0.5), reads=["rs"], writes=["rs"])
            X = SRot(es3, "X", 8, [128, D], F32)
            T1 = SRot(es3, "T1", 4, [128, D], F32)
            T2 = SRot(es3, "T2", 4, [128, D], F32)
            HB = SRot(es3, "HB", 4, [128, D], BF16)
            HT = SRot(es3, "HT", 2, [128, 8, 128], F32)
            SM = SRot(es3, "SM", 6, [128, 16], F32)
            LG = SRot(es3, "LG", 2, [128, NE], F32)
            RK = SRot(es3, "RK", 2, [128, 6, NE], F32)
            OH = SRot(es3, "OH", 2, [128, 4, NE], F32)
            MX = SRot(es3, "MX", 2, [128, 24], F32)

            def layer_norm(src, srck, dst, dstk, g_ap, b_ap, gk, jt, jtk, SMr):
                sm, smk = SMr.next()
                p.op("dve", lambda: nc.vector.memset(sm[:], 0.0), writes=[smk])
                yield
                p.op("act", lambda: nc.scalar.activation(out=jt[:], in_=src[:], func=AF.Identity, accum_out=sm[:, 0:1]), reads=[srck, smk], writes=[jtk, smk])
                yield
                p.op("act", lambda: nc.scalar.activation(out=jt[:], in_=src[:], func=AF.Square, accum_out=sm[:, 1:2]), reads=[srck, smk], writes=[jtk, smk])
                yield
                p.op("dve", lambda: nc.vector.tensor_single_scalar(out=sm[:, 2:3], in_=sm[:, 0:1], scalar=1.0 / D, op=ALU.mult), reads=[smk], writes=[smk])
                yield
                p.op("dve", lambda: nc.vector.tensor_tensor(out=sm[:, 3:4], in0=sm[:, 2:3], in1=sm[:, 2:3], op=ALU.mult), reads=[smk], writes=[smk])
                yield
                p.op("dve", lambda: nc.vector.scalar_tensor_tensor(out=sm[:, 4:5], in0=sm[:, 1:2], scalar=1.0 / D, in1=sm[:, 3:4], op0=ALU.mult, op1=ALU.subtract), reads=[smk], writes=[smk])
                yield
                p.op("act", lambda: nc.scalar.activation(out=sm[:, 5:6], in_=sm[:, 4:5], func=AF.Ln, bias=eps_t[:, 0:1], scale=1.0), reads=[smk, "eps"], writes=[smk])
                yield
                p.op("act", lambda: nc.scalar.activation(out=sm[:, 6:7], in_=sm[:, 5:6], func=AF.Exp, scale=-0.5), reads=[smk], writes=[smk])
                yield
                p.op("dve", lambda: nc.vector.scalar_tensor_tensor(out=sm[:, 7:8], in0=sm[:, 2:3], scalar=-1.0, in1=sm[:, 6:7], op0=ALU.mult, op1=ALU.mult), reads=[smk], writes=[smk])
                yield
                p.op("act", lambda: nc.scalar.activation(out=jt[:], in_=src[:], func=AF.Identity, scale=sm[:, 6:7], bias=sm[:, 7:8]), reads=[srck, smk], writes=[jtk])
                yield
                p.op("dve", lambda: nc.vector.tensor_tensor(out=jt[:], in0=jt[:], in1=g_ap, op=ALU.mult), reads=[jtk, gk], writes=[jtk])
                yield
                p.op("dve", lambda: nc.vector.tensor_tensor(out=dst[:], in0=jt[:], in1=b_ap, op=ALU.add), reads=[jtk, gk], writes=[dstk])
                yield

            hh_ = {}

            xx_ = {}

            def mmA(i):
                tsl = slice(i * 128, (i + 1) * 128)
                x_, xk_ = X.next()
                xx_[i] = (x_, xk_)
                dma("sp", x_[:], x_d[tsl, :], writes=[xk_])
                for half in range(2):
                    for c in range(4):
                        p.op("pe", lambda half=half, c=c: nc.tensor.matmul(psb[half][:], lhsT=aT[:, c, tsl], rhs=wout[:, c, half * 512:(half + 1) * 512],
                                                                       start=(c == 0), stop=(c == 3)), reads=["aT", ("wout", c)], writes=[PS(half)])
                    for c in range(4):
                        p.op("pe", lambda half=half, c=c: nc.tensor.matmul(psb[2 + half][:], lhsT=bT[:, c, tsl], rhs=wout[:, 4 + c, half * 512:(half + 1) * 512],
                                                                       start=(c == 0), stop=(c == 3)), reads=["bT", ("wout", 4 + c)], writes=[PS(2 + half)])

            def stageA(i):
                tsl = slice(i * 128, (i + 1) * 128)
                x_, xk_ = xx_.pop(i)
                t1, t1k = T1.next()
                t2, t2k = T2.next()
                p.op("act", lambda: nc.scalar.mul(out=x_[:], in_=x_[:], mul=ALPHA), reads=[xk_], writes=[xk_])
                for half in range(2):
                    hs = slice(half * 512, (half + 1) * 512)
                    p.op("dve", lambda half=half, hs=hs: nc.vector.scalar_tensor_tensor(out=t1[:, hs], in0=psb[half][:], scalar=rs[:, 0, i:i + 1], in1=x_[:, hs], op0=ALU.mult, op1=ALU.add),
                         reads=[PS(half), "rs", xk_], writes=[t1k])
                    p.op("dve", lambda half=half, hs=hs: nc.vector.scalar_tensor_tensor(out=t1[:, hs], in0=psb[2 + half][:], scalar=rs[:, 1, i:i + 1], in1=t1[:, hs], op0=ALU.mult, op1=ALU.add),
                         reads=[PS(2 + half), "rs", t1k], writes=[t1k])
                if i + 1 < NT:
                    mmA(i + 1)
                yield
                h_, hk_ = X.next()
                yield from layer_norm(t1, t1k, h_, hk_, vec3[:, 0, :], vec3[:, 1, :], "vec3", t2, t2k, SM)
                dma("sp", h32_d[tsl, :], h_[:], reads=[hk_], writes=[("h32", i)])
                yield
                h32_keys.append(("h32", i))
                hb, hbk = HB.next()
                p.op("act", lambda: nc.scalar.copy(out=hb[:], in_=h_[:]), reads=[hk_], writes=[hbk])
                yield
                dma("sp", hbf_d[tsl, :], hb[:], reads=[hbk], writes=[("hbf", i)])
                yield
                hbf_keys.append(("hbf", i))
                if "dbg_h" in dram:
                    dma("sp", dram["dbg_h"][tsl, :], h_[:], reads=[hk_])
                    yield
                hh_[i] = (h_, hk_)

            LGall = sbuf(es3, "LGall", [128, NT, NE], F32)
            MX8 = sbuf(es3, "MX8", [128, NT, 8], F32)

            def stageB(i):
                h_, hk_ = hh_.pop(i)
                tsl = slice(i * 128, (i + 1) * 128)
                ht, htk = HT.next()
                for k in range(8):
                    b = 4 + k // 4
                    p.op("pe", lambda k=k, b=b: nc.tensor.transpose(psb[b][:, (k % 4) * 128:(k % 4 + 1) * 128], h_[:, k * 128:(k + 1) * 128], ident_f),
                         reads=[hk_, "consts"], writes=[PS(b)])
                p.op("act", lambda: nc.scalar.copy(out=ht[:, 0:4, :], in_=psb[4][:].rearrange("p (k t) -> p k t", k=4)), reads=[PS(4)], writes=[htk])
                p.op("dve", lambda: nc.vector.tensor_copy(out=ht[:, 4:8, :], in_=psb[5][:].rearrange("p (k t) -> p k t", k=4)), reads=[PS(5)], writes=[htk])
                for k in range(8):
                    p.op("pe", lambda k=k: nc.tensor.matmul(psb[6][:, 0:NE], lhsT=ht[:, k, :], rhs=wr[:, k, :], start=(k == 0), stop=(k == 7)),
                         reads=[htk, "wr"], writes=[PS(6)])
                p.op("dve", lambda: nc.vector.tensor_tensor(out=LGall[:, i, :], in0=psb[6][:, 0:NE], in1=brb[:], op=ALU.add), reads=[PS(6), "brb"], writes=[("LG", i)])
                p.op("dve", lambda: nc.vector.max(out=MX8[:, i, :], in_=LGall[:, i, :]), reads=[("LG", i)], writes=[("MX8", i)])

                yield
                if "dbg_logits" in dram:
                    dma("sp", dram["dbg_logits"][tsl, :], LGall[:, i, :], reads=[("LG", i)])
                    yield

            RT = sbuf(es3, "RT", [128, 3, NT, NE], F32)
            OHa = sbuf(es3, "OHa", [128, NT, 4, NE], F32)
            OHb = sbuf(es3, "OHb", [128, NT, 4, NE], F32)
            SC = sbuf(es3, "SC", [128, 9, NT, 4], F32)
            RB = 4

            def routing(bi):
                t0, t1 = bi * RB, (bi + 1) * RB
                ts_ = slice(t0, t1)
                nt = t1 - t0
                LGk = [("LG", i) for i in range(t0, t1)]
                MXk = [("MX8", i) for i in range(t0, t1)]
                K = lambda nm: (nm, bi)
                p.op("dve", lambda: nc.vector.tensor_tensor(out=RT[:, 0, ts_], in0=LGall[:, ts_], in1=MX8[:, ts_, 3:4].to_broadcast([128, nt, NE]), op=ALU.is_ge), reads=LGk + MXk, writes=[K("RT0")])
                yield
                p.op("act", lambda: nc.scalar.copy(out=Mall[:, ts_], in_=RT[:, 0, ts_]), reads=[K("RT0")], writes=[("Mall", bi)])
                yield
                for i in range(t0, t1):
                    for i2 in range(i + 1):
                        lhs = consts_bf[:, C_LSTRICT:C_LSTRICT + 128] if i2 == i else consts_bf[:, C_ONES:C_ONES + 128]
                        p.op("pe", lambda i=i, i2=i2, lhs=lhs: nc.tensor.matmul(psb[7][:, i * NE:(i + 1) * NE], lhsT=lhs, rhs=Mall[:, i2, :], start=(i2 == 0), stop=(i2 == i)),
                             reads=[("Mall", i2 // RB), "consts_bf"], writes=[PS(7)])
                    yield
                p.op("dve", lambda: nc.vector.tensor_tensor(out=SC[:, 0, ts_], in0=MX8[:, ts_, 0:4], in1=MX8[:, ts_, 0:1].to_broadcast([128, nt, 4]), op=ALU.subtract), reads=MXk, writes=[K("SC0")])
                yield
                p.op("act", lambda: nc.scalar.activation(out=SC[:, 1, ts_], in_=SC[:, 0, ts_], func=AF.Exp), reads=[K("SC0")], writes=[K("SC1")])
                yield
                p.op("dve", lambda: nc.vector.tensor_reduce(out=SC[:, 2, ts_, 0], in_=SC[:, 1, ts_], axis=AX.X, op=ALU.add), reads=[K("SC1")], writes=[K("SC2")])
                yield
                p.op("dve", lambda: nc.vector.reciprocal(out=SC[:, 2, ts_, 1], in_=SC[:, 2, ts_, 0]), reads=[K("SC2")], writes=[K("SC2b")])
                yield
                p.op("dve", lambda: nc.vector.tensor_tensor(out=SC[:, 3, ts_], in0=SC[:, 1, ts_], in1=SC[:, 2, ts_, 1:2].to_broadcast([128, nt, 4]), op=ALU.mult), reads=[K("SC1"), K("SC2b")], writes=[K("SC3")])
                yield
                lg_bc = LGall[:, ts_].unsqueeze(2).to_broadcast([128, nt, 4, NE])
                mx_bc = MX8[:, ts_, 0:4].unsqueeze(3).to_broadcast([128, nt, 4, NE])
                p.op("dve", lambda: nc.vector.tensor_tensor(out=OHa[:, ts_], in0=lg_bc, in1=mx_bc, op=ALU.is_equal), reads=LGk + MXk, writes=[K("OHa")])
                yield
                p.op("dve", lambda: nc.vector.tensor_copy(out=RT[:, 1, ts_], in_=psb[7][:, t0 * NE:t1 * NE].rearrange("p (t e) -> p t e", t=nt)), reads=[PS(7)], writes=[K("RT1")])
                yield
                iota_bc = consts[:, C_IOTA:C_IOTA + NE].unsqueeze(1).to_broadcast([128, nt, NE])
                p.op("dve", lambda: nc.vector.scalar_tensor_tensor(out=RT[:, 2, ts_], in0=iota_bc, scalar=float(CAP), in1=RT[:, 1, ts_], op0=ALU.mult, op1=ALU.add), reads=["consts", K("RT1")], writes=[K("RT2")])
                yield
                p.op("dve", lambda: nc.vector.tensor_tensor(out=OHb[:, ts_], in0=OHa[:, ts_], in1=RT[:, 1, ts_].unsqueeze(2).to_broadcast([128, nt, 4, NE]), op=ALU.mult), reads=[K("OHa"), K("RT1")], writes=[K("OHb")])
                yield
                p.op("dve", lambda: nc.vector.tensor_reduce(out=SC[:, 4, ts_], in_=OHb[:, ts_], axis=AX.X, op=ALU.add), reads=[K("OHb")], writes=[K("SC4")])
                yield
                p.op("dve", lambda: nc.vector.tensor_tensor(out=OHb[:, ts_], in0=OHa[:, ts_], in1=RT[:, 2, ts_].unsqueeze(2).to_broadcast([128, nt, 4, NE]), op=ALU.mult), reads=[K("OHa"), K("RT2"), K("SC4")], writes=[K("OHb")])
                yield
                p.op("dve", lambda: nc.vector.tensor_reduce(out=SC[:, 5, ts_], in_=OHb[:, ts_], axis=AX.X, op=ALU.add), reads=[K("OHb")], writes=[K("SC5")])
                yield
                p.op("dve", lambda: nc.vector.tensor_scalar(out=SC[:, 6, ts_], in0=SC[:, 4, ts_], scalar1=float(CAP), scalar2=None, op0=ALU.is_lt), reads=[K("SC4")], writes=[K("SC6")])
                yield
                p.op("dve", lambda: nc.vector.scalar_tensor_tensor(out=SC[:, 7, ts_], in0=SC[:, 5, ts_], scalar=-float(SLOTS), in1=SC[:, 6, ts_], op0=ALU.add, op1=ALU.mult), reads=[K("SC5"), K("SC6")], writes=[K("SC7")])
                yield
                p.op("dve", lambda: nc.vector.tensor_scalar(out=SC[:, 8, ts_], in0=SC[:, 7, ts_], scalar1=float(SLOTS), scalar2=None, op0=ALU.add), reads=[K("SC7")], writes=[K("SC8")])
                yield
                p.op("dve", lambda: nc.vector.tensor_copy(out=pos_i[:, t0 * 4:t1 * 4].rearrange("p (t k) -> p t k", k=4), in_=SC[:, 8, ts_]), reads=[K("SC8")], writes=[("pos", i) for i in range(t0, t1)])
                yield
                p.op("dve", lambda: nc.vector.tensor_tensor(out=gates[:, ts_], in0=SC[:, 3, ts_], in1=SC[:, 6, ts_], op=ALU.mult), reads=[K("SC3"), K("SC6")], writes=[("gates", i) for i in range(t0, t1)])
                yield
                if "dbg_pos" in dram:
                    for i in range(t0, t1):
                        dma("sp", dram["dbg_pos"][i * 128:(i + 1) * 128, :], SC[:, 8, i, :], reads=[K("SC8")])
                        dma("sp", dram["dbg_gates"][i * 128:(i + 1) * 128, :], gates[:, i, :], reads=[("gates", i)])
                for i in range(t0, t1):
                    for k in range(4):
                        p.op("pool", lambda i=i, k=k: nc.gpsimd.indirect_dma_start(
                            out=stok_d[:, :], out_offset=bass.IndirectOffsetOnAxis(ap=pos_i[:, i * 4 + k:i * 4 + k + 1], axis=0),
                            in_=tokid_i[:, i:i + 1], in_offset=None),
                            reads=[("pos", i), "tokid_i", "stok_init"], writes=[("scat", i, k)], dma=True)
                        scat_keys.append(("scat", i, k))
                        yield

            if stage >= 4:
                def tileAB(i):
                    yield from stageA(i)
                    yield from stageB(i)
                ents = []
                for i in range(NT):
                    ents.append(("tile", lambda i=i: tileAB(i), 0))
                    if (i + 1) % RB == 0:
                        ents.append(("bg", lambda bi=i // RB: routing(bi), i + 1))
                mmA(0)
                pipeline2(ents, stagger=6, depth=4)
            p.barrier()
        esAB.close()
        eo_keys = ["eo_z"]
        with ExitStack() as es5:
            bgT = sbuf(es5, "bgT", [128, NE, 8], F32)
            buT = sbuf(es5, "buT", [128, NE, 8], F32)
            dma("sp", bgT[:], bg_d, writes=["bgT"])
            dma("sp", buT[:], bu_d, writes=["buT"])
            bu1 = sbuf(es5, "bu1", [128, NE, 8], F32)
            p.op("dve", lambda: nc.vector.tensor_scalar(out=bu1[:], in0=buT[:], scalar1=1.0, scalar2=None, op0=ALU.add), reads=["buT"], writes=["bu1"])
            WG = [sbuf(es5, f"WG{i}", [128, 8, D], BF16) for i in range(2)]
            WU = [sbuf(es5, f"WU{i}", [128, 8, D], BF16) for i in range(2)]
            WD = [sbuf(es5, f"WD{i}", [128, 8, D], BF16) for i in range(2)]
            BD = [sbuf(es5, f"BD{i}", [128, D], F32) for i in range(2)]
            XB = [sbuf(es5, f"XB{i}", [128, NBLK, D], BF16) for i in range(2)]
            STI = [sbuf(es5, f"STI{i}", [128, NBLK], I32) for i in range(2)]
            XT = [sbuf(es5, f"XT{i}", [128, 8, CAP], BF16) for i in range(2)]
            ACT_T = sbuf(es5, "ACTT", [128, 8, CAP], BF16)
            GC = SRot(es5, "GC", 2, [128, CAP], F32)
            SG = SRot(es5, "SG", 2, [128, CAP], F32)
            UC = SRot(es5, "UC", 2, [128, CAP], F32)
            EO = SRot(es5, "EO", 2, [128, D], F32)

            def load_w(e):
                j = e % 2
                for (W_, wd_, nm) in ((WG, wg_d, "WG"), (WU, wu_d, "WU"), (WD, wd_d, "WD")):
                    for hk in range(2):
                        dma("pool", W_[j][:, hk * 4:(hk + 1) * 4, :], wd_[e, hk * 512:(hk + 1) * 512, :].rearrange("(k p) f -> p k f", p=128), writes=[(nm, j, hk)])
                dma("sp", BD[j][:], bd_d[e:e + 1, :].to_broadcast([128, D]), writes=[("BD", j)])

            def load_x(e):
                j = e % 2
                dma("sp", STI[j][:], stok_d[e * CAP:(e + 1) * CAP, :].rearrange("(p b) o -> p (b o)", p=128), reads=scat_keys + ["stok_init"], writes=[("STI", j)])
                for blk in range(NBLK):
                    p.op("pool", lambda blk=blk: nc.gpsimd.indirect_dma_start(
                        out=XB[j][:, blk, :], out_offset=None, in_=hbf_d[:, :],
                        in_offset=bass.IndirectOffsetOnAxis(ap=STI[j][:, blk:blk + 1], axis=0)),
                        reads=[("STI", j)] + hbf_keys, writes=[("XB", j, blk)], dma=True)

            nexp = NE if stage >= 5 else 0
            if nexp:
                load_x(0)
                load_w(0)
            tr = 0
            for e in range(nexp):
                j = e % 2
                if e + 1 < nexp:
                    load_x(e + 1)
                    load_w(e + 1)
                for blk in range(NBLK):
                    b = 6 + tr % 2
                    tr += 1
                    ptr = psb[b][:].bitcast(BF16)
                    for k in range(8):
                        p.op("pe", lambda blk=blk, k=k, ptr=ptr: nc.tensor.transpose(ptr[:, k * 128:(k + 1) * 128], XB[j][:, blk, k * 128:(k + 1) * 128], ident_bf),
                             reads=[("XB", j, blk), "consts_bf"], writes=[PS(b)])
                    src = ptr[:, :].rearrange("p (k t) -> p k t", k=8)
                    if blk % 2 == 0:
                        p.op("act", lambda blk=blk, src=src: nc.scalar.copy(out=XT[j][:, :, blk * 128:(blk + 1) * 128], in_=src), reads=[PS(b)], writes=[("XT", j)])
                    else:
                        p.op("dve", lambda blk=blk, src=src: nc.vector.tensor_copy(out=XT[j][:, :, blk * 128:(blk + 1) * 128], in_=src), reads=[PS(b)], writes=[("XT", j)])
                for f in range(8):
                    gb_ = f % 2
                    ub_ = 2 + f % 2
                    for k in range(8):
                        p.op("pe", lambda f=f, k=k: nc.tensor.matmul(psb[gb_][:, 0:CAP], lhsT=WG[j][:, k, f * 128:(f + 1) * 128], rhs=XT[j][:, k, :], start=(k == 0), stop=(k == 7)),
                             reads=[("WG", j, k // 4), ("XT", j)], writes=[PS(gb_)])
                    for k in range(8):
                        p.op("pe", lambda f=f, k=k: nc.tensor.matmul(psb[ub_][:, 0:CAP], lhsT=WU[j][:, k, f * 128:(f + 1) * 128], rhs=XT[j][:, k, :], start=(k == 0), stop=(k == 7)),
                             reads=[("WU", j, k // 4), ("XT", j)], writes=[PS(ub_)])
                    gc, gck = GC.next()
                    sg, sgk = SG.next()
                    uc, uck = UC.next()
                    p.op("dve", lambda f=f, gc=gc: nc.vector.tensor_scalar(out=gc[:], in0=psb[gb_][:, 0:CAP], scalar1=bgT[:, e, f:f + 1], scalar2=7.0, op0=ALU.add, op1=ALU.min),
                         reads=[PS(gb_), "bgT"], writes=[gck])
                    p.op("act", lambda gc=gc, sg=sg: nc.scalar.activation(out=sg[:], in_=gc[:], func=AF.Gelu_apprx_sigmoid), reads=[gck], writes=[sgk])
                    p.op("dve", lambda f=f, uc=uc: nc.vector.tensor_scalar(out=uc[:], in0=psb[ub_][:, 0:CAP], scalar1=bu1[:, e, f:f + 1], scalar2=-6.0, op0=ALU.add, op1=ALU.max),
                         reads=[PS(ub_), "bu1"], writes=[uck])
                    p.op("dve", lambda f=f, uc=uc, sg=sg: nc.vector.scalar_tensor_tensor(out=ACT_T[:, f, :], in0=uc[:], scalar=8.0, in1=sg[:], op0=ALU.min, op1=ALU.mult),
                         reads=[uck, sgk], writes=[("ACTT", f)])
                for blk in range(NBLK):
                    eo, eok = EO.next()
                    for half in range(2):
                        db_ = 4 + half
                        for f in range(8):
                            p.op("pe", lambda blk=blk, half=half, f=f: nc.tensor.matmul(psb[db_][:], lhsT=ACT_T[:, f, blk * 128:(blk + 1) * 128], rhs=WD[j][:, f, half * 512:(half + 1) * 512],
                                                                                     start=(f == 0), stop=(f == 7)),
                                 reads=[("ACTT", f), ("WD", j, f // 4)], writes=[PS(db_)])
                        p.op("dve", lambda half=half, eo=eo: nc.vector.tensor_tensor(out=eo[:, half * 512:(half + 1) * 512], in0=psb[db_][:], in1=BD[j][:, half * 512:(half + 1) * 512], op=ALU.add),
                             reads=[PS(db_), ("BD", j)], writes=[eok])
                    dma("sp", eo_d[e * CAP:(e + 1) * CAP, :].rearrange("(p b) d -> p b d", b=NBLK)[:, blk, :], eo[:], reads=[eok], writes=[("eo", e, blk)])
                    eo_keys.append(("eo", e, blk))
            p.barrier()

        with ExitStack() as es6:
            vec6 = sbuf(es6, "vec6", [128, 2, D], F32)
            dma("sp", vec6[:], vec_d[:, 4:6, :], writes=["vec6"])
            Hh = SRot(es6, "Hh", 5, [128, D], F32)
            G4 = SRot(es6, "G4", 20, [128, D], F32)
            AC = SRot(es6, "AC", 3, [128, D], F32)
            JT = SRot(es6, "JT", 3, [128, D], F32)
            OT = SRot(es6, "OT", 3, [128, D], F32)
            SM = SRot(es6, "SM2", 4, [128, 16], F32)
            def fetch(i):
                tsl = slice(i * 128, (i + 1) * 128)
                hh, hhk = Hh.next()
                dma("sp", hh[:], h32_d[tsl, :], reads=h32_keys, writes=[hhk])
                gs = []
                for k in range(4):
                    g_, gk_ = G4.next()
                    p.op("pool", lambda k=k, g_=g_: nc.gpsimd.indirect_dma_start(
                        out=g_[:], out_offset=None, in_=eo_d[:, :],
                        in_offset=bass.IndirectOffsetOnAxis(ap=pos_i[:, i * 4 + k:i * 4 + k + 1], axis=0)),
                        reads=[("pos", i)] + eo_keys, writes=[gk_], dma=True)
                    gs.append((g_, gk_))
                return hh, hhk, gs

            def combine(i, hh, hhk, gs):
                tsl = slice(i * 128, (i + 1) * 128)
                ac, ack = AC.next()
                p.op("act", lambda: nc.scalar.activation(out=ac[:], in_=gs[0][0][:], func=AF.Copy, scale=gates[:, i, 0:1]), reads=[gs[0][1], ("gates", i)], writes=[ack])
                yield
                for k in range(1, 4):
                    p.op("dve", lambda k=k: nc.vector.scalar_tensor_tensor(out=ac[:], in0=gs[k][0][:], scalar=gates[:, i, k:k + 1], in1=ac[:], op0=ALU.mult, op1=ALU.add),
                         reads=[gs[k][1], ("gates", i), ack], writes=[ack])
                    yield
                p.op("dve", lambda: nc.vector.scalar_tensor_tensor(out=ac[:], in0=hh[:], scalar=ALPHA, in1=ac[:], op0=ALU.mult, op1=ALU.add), reads=[hhk, ack], writes=[ack])
                yield
                if "dbg_pre2" in dram:
                    dma("sp", dram["dbg_pre2"][tsl, :], ac[:], reads=[ack])
                    yield
                jt, jtk = JT.next()
                ot, otk = OT.next()
                yield from layer_norm(ac, ack, ot, otk, vec6[:, 0, :], vec6[:, 1, :], "vec6", jt, jtk, SM)
                dma("sp", out_d[tsl, :], ot[:], reads=[otk], writes=[("out", i)])
                yield

            if stage >= 6:
                ft = {0: fetch(0), 1: fetch(1), 2: fetch(2)}

                def tileC(i):
                    if i + 3 < NT:
                        ft[i + 3] = fetch(i + 3)
                    yield from combine(i, *ft.pop(i))
                pipeline([lambda i=i: tileC(i) for i in range(NT)], stagger=8, depth=2)
        p.finish("sp")
    p.dram = dram
    return nc, p


def host_consts(flag):
    c = np.zeros((128, NCONST), np.float32)
    i = np.arange(128)
    c[:, C_IDENT:C_IDENT + 128] = np.eye(128, dtype=np.float32)
    c[:, C_TRI:C_TRI + 128] = (i[:, None] <= i[None, :])
    c[:, C_MPREV:C_MPREV + 128] = (i[:, None] >= i[None, :])
    c[:, C_MPREVC:C_MPREVC + 128] = flag * (i[:, None] >= i[None, :])
    c[:, C_LSTRICT:C_LSTRICT + 128] = (i[:, None] < i[None, :])
    c[:, C_ONES:C_ONES + 128] = 1.0
    c[:, C_IOTA:C_IOTA + 32] = np.arange(32)[None, :]
    c[:, C_TOKID:C_TOKID + 16] = np.arange(16)[None, :] * 128 + i[:, None]
    c[64, C_SEL:C_SEL + 64] = 1.0
    return c


def make_in_maps(inputs, cores=range(8)):
    f = lambda a: np.ascontiguousarray(np.asarray(a, dtype=np.float32))
    x = np.asarray(inputs["x"], dtype=np.float32)
    shared = {
        "w_in": f(inputs["w_in"][0]),
        "sgu_wT": f(np.transpose(inputs["sgu_w"][0], (2, 0, 1))),
        "sgu_bT": f(inputs["sgu_b"][0].T),
        "mix_gb64": f(inputs["mix_norm_g"][0, 512:].reshape(8, 64).T),
        "mix_gb": f(inputs["mix_norm_g"][0, 512:].reshape(4, 128).T),
        "w_out": f(inputs["w_out"][0]),
        "w_router": f(inputs["w_router"][0]),
        "b_router_bc": f(np.broadcast_to(inputs["b_router"][0][None, :], (128, NE))),
        "w_gate": f(inputs["w_gate"][0]),
        "w_up": f(inputs["w_up"][0]),
        "w_down": f(inputs["w_down"][0]),
        "b_gateT": f(np.transpose(inputs["b_gate"][0].reshape(NE, 8, 128), (2, 0, 1))),
        "b_upT": f(np.transpose(inputs["b_up"][0].reshape(NE, 8, 128), (2, 0, 1))),
        "b_down": f(inputs["b_down"][0]),
    }
    rows = np.zeros((6, D), np.float32)
    rows[0, :512] = inputs["sgu_ln_g"][0]
    rows[0, 512:] = inputs["sgu_ln_b"][0]
    rows[1, :512] = inputs["mix_norm_g"][0, :512]
    rows[2] = inputs["ln1_g"][0]
    rows[3] = inputs["ln1_b"][0]
    rows[4] = inputs["ln2_g"][0]
    rows[5] = inputs["ln2_b"][0]
    shared["vecs"] = f(np.broadcast_to(rows[None], (128, 6, D)))
    maps = []
    for c in cores:
        b, half = c // 2, c % 2
        own = x[b, half * NTOK:(half + 1) * NTOK]
        xT = np.zeros((D, NEXT), np.float32)
        if half == 1:
            xT[:, :NTOK] = x[b, :NTOK].T
        xT[:, NTOK:] = own.T
        m = dict(shared)
        m["xT"] = xT
        m["x"] = f(own)
        m["consts"] = host_consts(float(half))
        maps.append(m)
    return maps


_NC_CACHE = {}


def kernel(**inputs):
    if "nc" not in _NC_CACHE:
        _NC_CACHE["nc"] = build()[0]
    nc = _NC_CACHE["nc"]
    maps = make_in_maps(inputs)
    res = run_bass_kernel_spmd(nc, maps, core_ids=list(range(8)))
    out = np.zeros((4, 4096, D), np.float32)
    for c in range(8):
        b, half = c // 2, c % 2
        out[b, half * NTOK:(half + 1) * NTOK] = res.results[c]["out"]
    return out
```
